# Optimizing a Trainium2 kernel written in Bass

```python
import math
import jax
import jax.numpy as jnp
from jax import lax
import numpy as np

D_MODEL = 2048
BATCH = 2
SEQ = 4096
DEPTH = 2

N_A_LAYERS = DEPTH // 2
N_B_LAYERS = DEPTH - N_A_LAYERS

GDN_HEAD_DIM = 128
GDN_QK_HEADS = D_MODEL // GDN_HEAD_DIM
GDN_V_HEADS = 2 * GDN_QK_HEADS
GDN_QK_WIDTH = GDN_QK_HEADS * GDN_HEAD_DIM
GDN_V_WIDTH = GDN_V_HEADS * GDN_HEAD_DIM
GDN_CONV_CH = 2 * GDN_QK_WIDTH + GDN_V_WIDTH
GDN_IN_WIDTH = GDN_CONV_CH + GDN_V_WIDTH + 2 * GDN_V_HEADS
GDN_CONV = 4
GDN_CHUNK = 64

NSA_HEAD_DIM = 128
NSA_HEADS = D_MODEL // NSA_HEAD_DIM
NSA_KV_GROUPS = 4
NSA_HEADS_PER_GROUP = NSA_HEADS // NSA_KV_GROUPS
NSA_Q_WIDTH = NSA_HEADS * NSA_HEAD_DIM
NSA_KV_WIDTH = 6 * NSA_KV_GROUPS * NSA_HEAD_DIM
CMP_BLOCK = 32
CMP_STRIDE = 16
CMP_HIDDEN = 512
SLC_BLOCK = 64
SLC_TOPK = 16
SLC_LOCAL = 2
WINDOW = 512
WIN_Q_BLOCK = 128
SLC_Q_BLOCK = 64
ROPE_THETA = 10000.0

N_EXPERTS = 32
N_GROUPS = 8
EXPERTS_PER_GROUP = N_EXPERTS // N_GROUPS
TOP_K = 2
D_EXPERT = D_MODEL // 4

DEEPNORM_ALPHA = (2 * DEPTH) ** 0.25
DEEPNORM_BETA = (8 * DEPTH) ** -0.25
LN_EPS = 1e-5
RMS_EPS = 1e-6
NEG_INF = -1e30
FORCE_SCORE = 1e6

kernel_name = 'yoco_gdn_nsa_group_moe_deepnorm'


def layer_norm(x, g, b):
    xf = x.astype(jnp.float32)
    mu = jnp.mean(xf, axis=-1, keepdims=True)
    var = jnp.mean(jnp.square(xf - mu), axis=-1, keepdims=True)
    return (xf - mu) * lax.rsqrt(var + LN_EPS) * g.astype(jnp.float32) + b.astype(jnp.float32)


def l2_normalize(a):
    return a * lax.rsqrt(jnp.sum(a * a, axis=-1, keepdims=True) + RMS_EPS)


def rope(x, pos):
    half = x.shape[-1] // 2
    inv_freq = ROPE_THETA ** (-jnp.arange(half, dtype=jnp.float32) / half)
    ang = pos.astype(jnp.float32)[:, None] * inv_freq[None, :]
    cos = jnp.cos(ang)[None, :, None, :]
    sin = jnp.sin(ang)[None, :, None, :]
    xf = x.astype(jnp.float32)
    x1, x2 = xf[..., :half], xf[..., half:]
    return jnp.concatenate([x1 * cos - x2 * sin, x2 * cos + x1 * sin], axis=-1).astype(x.dtype)


def causal_depthwise_conv(x, w):
    k_width, ch = w.shape
    return lax.conv_general_dilated(
        x, w[:, None, :].astype(x.dtype), window_strides=(1,), padding=[(k_width - 1, 0)],
        dimension_numbers=('NWC', 'WIO', 'NWC'), feature_group_count=ch)


def chunked_gated_delta_rule(q, k, v, g, beta):
    B, T, H, Dk = q.shape
    Dv = v.shape[-1]
    C = GDN_CHUNK
    N = T // C

    def to_chunks(a):
        return jnp.moveaxis(a.reshape((B, N, C, H) + a.shape[3:]), 3, 1)

    qc, kc, vc = to_chunks(q), to_chunks(k), to_chunks(v)
    bc = to_chunks(beta)
    gc = jnp.cumsum(to_chunks(g), axis=-1)
    idx = jnp.arange(C)
    causal = idx[:, None] >= idx[None, :]
    strict = idx[:, None] > idx[None, :]
    diff = gc[..., :, None] - gc[..., None, :]
    decay = jnp.where(causal, jnp.exp(jnp.where(causal, diff, 0.0)), 0.0)
    kb = kc * bc[..., None]
    m = jnp.where(strict, jnp.einsum('bhnid,bhnjd->bhnij', kb, kc) * decay, 0.0)
    lhs = m + jnp.eye(C, dtype=m.dtype)
    rhs = jnp.concatenate([vc * bc[..., None], kb * jnp.exp(gc)[..., None]], axis=-1)
    sol = lax.linalg.triangular_solve(lhs, rhs, left_side=True, lower=True)
    u, w = sol[..., :Dv], sol[..., Dv:]
    a_intra = jnp.einsum('bhnid,bhnjd->bhnij', qc, kc) * decay

    def step(state, inp):
        q_i, k_i, u_i, w_i, g_i, a_i = inp
        v_new = u_i - jnp.einsum('bhck,bhkv->bhcv', w_i, state)
        o_i = (jnp.einsum('bhck,bhkv->bhcv', q_i * jnp.exp(g_i)[..., None], state)
               + jnp.einsum('bhij,bhjv->bhiv', a_i, v_new))
        g_last = g_i[..., -1:]
        state = (state * jnp.exp(g_last)[..., None]
                 + jnp.einsum('bhck,bhcv->bhkv', k_i * jnp.exp(g_last - g_i)[..., None], v_new))
        return state, o_i

    xs = tuple(jnp.moveaxis(a, 2, 0) for a in (qc, kc, u, w, gc, a_intra))
    state0 = jnp.zeros((B, H, Dk, Dv), jnp.float32)
    _, o = lax.scan(step, state0, xs)
    o = jnp.moveaxis(o, 0, 2)
    return jnp.moveaxis(o, 1, 3).reshape(B, T, H, Dv)


def gdn_mixer(x, w_in, conv_w, a_log, dt_bias, norm_w, w_out):
    B, T, _ = x.shape
    Hk, Hv, Dh = GDN_QK_HEADS, GDN_V_HEADS, GDN_HEAD_DIM
    proj = x @ w_in
    qkv, z, a_raw, b_raw = jnp.split(
        proj, [GDN_CONV_CH, GDN_CONV_CH + GDN_V_WIDTH, GDN_CONV_CH + GDN_V_WIDTH + Hv], axis=-1)
    qkv = jax.nn.silu(causal_depthwise_conv(qkv, conv_w)).astype(jnp.float32)
    q, k, v = jnp.split(qkv, [GDN_QK_WIDTH, 2 * GDN_QK_WIDTH], axis=-1)
    rep = Hv // Hk
    q = jnp.repeat(l2_normalize(q.reshape(B, T, Hk, Dh)), rep, axis=2) * Dh ** -0.5
    k = jnp.repeat(l2_normalize(k.reshape(B, T, Hk, Dh)), rep, axis=2)
    v = v.reshape(B, T, Hv, Dh)
    g = -jnp.exp(a_log.astype(jnp.float32)) * jax.nn.softplus(a_raw.astype(jnp.float32) + dt_bias.astype(jnp.float32))
    beta = jax.nn.sigmoid(b_raw.astype(jnp.float32))
    o = chunked_gated_delta_rule(q, k, v, g, beta)
    o = (o * lax.rsqrt(jnp.mean(o * o, axis=-1, keepdims=True) + RMS_EPS) * norm_w.astype(jnp.float32)
         * jax.nn.silu(z.astype(jnp.float32).reshape(B, T, Hv, Dh)))
    return (o.reshape(B, T, GDN_V_WIDTH).astype(x.dtype) @ w_out).astype(x.dtype)


def compress_blocks(a, pe, w1, w2):
    B, T, G, D = a.shape
    n_cmp = (T - CMP_BLOCK) // CMP_STRIDE + 1
    idx = np.arange(n_cmp)[:, None] * CMP_STRIDE + np.arange(CMP_BLOCK)[None, :]
    blocks = a[:, idx] + pe[None, None, :, None, :]
    flat = jnp.moveaxis(blocks, 3, 2).reshape(B, n_cmp, G, CMP_BLOCK * D)
    return jax.nn.silu(flat @ w1) @ w2


def nsa_shared_kv(h, kv_w, cmp_pe, cmp_w1, cmp_w2):
    B, T, _ = h.shape
    kv = (h @ kv_w).reshape(B, T, 6, NSA_KV_GROUPS, NSA_HEAD_DIM)
    pos = jnp.arange(T)
    k_cmp = compress_blocks(kv[:, :, 0], cmp_pe[0], cmp_w1[0], cmp_w2[0])
    v_cmp = compress_blocks(kv[:, :, 1], cmp_pe[1], cmp_w1[1], cmp_w2[1])
    k_slc = rope(kv[:, :, 2], pos)
    v_slc = kv[:, :, 3]
    k_win = rope(kv[:, :, 4], pos)
    v_win = kv[:, :, 5]
    return (k_cmp, v_cmp, k_slc, v_slc, k_win, v_win)


def compression_overlap(n_cmp, n_slc):
    c0 = np.arange(n_cmp) * CMP_STRIDE
    s0 = np.arange(n_slc) * SLC_BLOCK
    ov = (np.minimum(c0[:, None] + CMP_BLOCK, s0[None, :] + SLC_BLOCK)
          - np.maximum(c0[:, None], s0[None, :]))
    return jnp.asarray(np.clip(ov, 0, None) / CMP_BLOCK, dtype=jnp.float32)


def nsa_compressed(q, k_cmp, v_cmp):
    B, T, G, R, D = q.shape
    n_cmp = k_cmp.shape[1]
    s = jnp.einsum('btgrd,bcgd->bgrtc', q, k_cmp).astype(jnp.float32) * D ** -0.5
    block_end = jnp.arange(n_cmp) * CMP_STRIDE + CMP_BLOCK - 1
    visible = block_end[None, :] <= jnp.arange(T)[:, None]
    p = jax.nn.softmax(jnp.where(visible, s, NEG_INF), axis=-1) * visible
    o = jnp.einsum('bgrtc,bcgd->btgrd', p, v_cmp)
    p_slc = jnp.einsum('bgtc,cs->bgts', p.sum(axis=2), compression_overlap(n_cmp, T // SLC_BLOCK))
    return o, p_slc


def nsa_selected(q, k, v, p_slc):
    B, T, G, R, D = q.shape
    L = SLC_BLOCK
    n_slc = T // L
    n_sel = min(SLC_TOPK, n_slc)
    cur = (jnp.arange(T) // L)[:, None]
    blk = jnp.arange(n_slc)[None, :]
    causal_blk = blk <= cur
    forced = (blk == 0) | (causal_blk & (blk > cur - SLC_LOCAL))
    score = jnp.where(causal_blk, jnp.where(forced, FORCE_SCORE, p_slc), -1.0)
    top_score, top_idx = lax.top_k(score, n_sel)
    top_valid = top_score >= 0.0
    kb = jnp.moveaxis(k.reshape(B, n_slc, L, G, D), 3, 1)
    vb = jnp.moveaxis(v.reshape(B, n_slc, L, G, D), 3, 1)
    nq = T // SLC_Q_BLOCK
    b_ix = jnp.arange(B)[:, None, None, None]
    g_ix = jnp.arange(G)[None, :, None, None]
    scale = D ** -0.5

    def block(inp):
        q_c, idx_c, valid_c, t0 = inp
        k_sel = kb[b_ix, g_ix, idx_c]
        v_sel = vb[b_ix, g_ix, idx_c]
        s = jnp.einsum('bqgrd,bgqnld->bgrqnl', q_c, k_sel).astype(jnp.float32) * scale
        t_c = t0 + jnp.arange(SLC_Q_BLOCK)
        key_pos = idx_c[..., None] * L + jnp.arange(L)
        ok = (key_pos <= t_c[None, None, :, None, None]) & valid_c[..., None]
        p = jax.nn.softmax(jnp.where(ok[:, :, None], s, NEG_INF), axis=(-2, -1))
        return jnp.einsum('bgrqnl,bgqnld->bqgrd', p, v_sel)

    q_blocks = jnp.moveaxis(q.reshape(B, nq, SLC_Q_BLOCK, G, R, D), 1, 0)
    idx_blocks = jnp.moveaxis(top_idx.reshape(B, G, nq, SLC_Q_BLOCK, n_sel), 2, 0)
    valid_blocks = jnp.moveaxis(top_valid.reshape(B, G, nq, SLC_Q_BLOCK, n_sel), 2, 0)
    starts = jnp.arange(nq) * SLC_Q_BLOCK
    o = lax.map(block, (q_blocks, idx_blocks, valid_blocks, starts))
    return jnp.moveaxis(o, 0, 1).reshape(B, T, G, R, D)


def nsa_window(q, k, v):
    B, T, G, R, D = q.shape
    QB = WIN_Q_BLOCK
    nb = T // QB
    n_prev = WINDOW // QB
    kw = (n_prev + 1) * QB

    def band(a):
        ap = jnp.pad(a, ((0, 0), (WINDOW, 0), (0, 0), (0, 0))).reshape(B, nb + n_prev, QB, G, D)
        return jnp.concatenate([ap[:, i:i + nb] for i in range(n_prev + 1)], axis=2)

    kband, vband = band(k), band(v)
    qb = q.reshape(B, nb, QB, G, R, D)
    s = jnp.einsum('bnqgrd,bnkgd->bngrqk', qb, kband).astype(jnp.float32) * D ** -0.5
    q_pos = (jnp.arange(nb)[:, None] * QB + jnp.arange(QB)[None, :])[:, :, None]
    k_pos = (jnp.arange(nb)[:, None] * QB - WINDOW + jnp.arange(kw)[None, :])[:, None, :]
    ok = (k_pos <= q_pos) & (k_pos > q_pos - WINDOW) & (k_pos >= 0)
    p = jax.nn.softmax(jnp.where(ok[None, :, None, None], s, NEG_INF), axis=-1)
    o = jnp.einsum('bngrqk,bnkgd->bnqgrd', p, vband)
    return o.reshape(B, T, G, R, D)


def nsa_mixer(x, w_q, w_out, k_cmp, v_cmp, k_slc, v_slc, k_win, v_win):
    B, T, _ = x.shape
    G, R, D = NSA_KV_GROUPS, NSA_HEADS_PER_GROUP, NSA_HEAD_DIM
    proj = x @ w_q
    q = proj[..., :NSA_Q_WIDTH].reshape(B, T, NSA_HEADS, D)
    gates = jax.nn.sigmoid(proj[..., NSA_Q_WIDTH:].astype(jnp.float32)).reshape(B, T, 3, G, R, 1)
    q_rot = rope(q, jnp.arange(T)).reshape(B, T, G, R, D)
    o_cmp, p_slc = nsa_compressed(q.reshape(B, T, G, R, D), k_cmp, v_cmp)
    o_slc = nsa_selected(q_rot, k_slc, v_slc, p_slc)
    o_win = nsa_window(q_rot, k_win, v_win)
    o = gates[:, :, 0] * o_cmp + gates[:, :, 1] * o_slc + gates[:, :, 2] * o_win
    return (o.reshape(B, T, NSA_Q_WIDTH).astype(x.dtype) @ w_out).astype(x.dtype)


def moe_ffn(x, router_w, router_bias, w_gate, w_up, w_down):
    B, T, D = x.shape
    h = x.reshape(B * T, D)
    aff = jax.nn.sigmoid((h @ router_w).astype(jnp.float32))
    biased = (aff + router_bias.astype(jnp.float32)).reshape(-1, N_GROUPS, EXPERTS_PER_GROUP)
    group_score = lax.top_k(biased, TOP_K)[0].sum(axis=-1)
    best_group = jnp.argmax(group_score, axis=-1)
    in_group = jnp.arange(N_GROUPS)[None, :] == best_group[:, None]
    cand = jnp.where(in_group[:, :, None], biased, NEG_INF).reshape(-1, N_EXPERTS)
    _, top_idx = lax.top_k(cand, TOP_K)
    top_aff = jnp.take_along_axis(aff, top_idx, axis=-1)
    top_w = top_aff / jnp.sum(top_aff, axis=-1, keepdims=True)
    gate = jnp.einsum('nk,nke->ne', top_w, jax.nn.one_hot(top_idx, N_EXPERTS, dtype=jnp.float32))
    hid = jax.nn.silu(jnp.einsum('nd,edf->nef', h, w_gate)) * jnp.einsum('nd,edf->nef', h, w_up)
    y = jnp.einsum('nef,efd->nd', hid * gate[:, :, None], w_down)
    return y.reshape(B, T, D).astype(x.dtype)


def setup_inputs(seed: int = 0) -> dict:
    key = jax.random.key(seed)
    ks = jax.random.split(key, 20)
    f32 = jnp.float32

    def dense(k, shape, fan_in, gain=1.0):
        return jax.random.normal(k, shape, f32) * (gain * fan_in ** -0.5)

    la, lb = N_A_LAYERS, N_B_LAYERS
    dt = jnp.exp(jax.random.uniform(ks[4], (la, GDN_V_HEADS), f32, math.log(1e-3), math.log(1e-1)))
    return {
        'x': jax.random.normal(ks[0], (BATCH, SEQ, D_MODEL), f32),
        'a_w_in': dense(ks[1], (la, D_MODEL, GDN_IN_WIDTH), D_MODEL),
        'a_conv_w': dense(ks[2], (la, GDN_CONV, GDN_CONV_CH), GDN_CONV),
        'a_a_log': jnp.log(jax.random.uniform(ks[3], (la, GDN_V_HEADS), f32, 1.0, 16.0)),
        'a_dt_bias': dt + jnp.log(-jnp.expm1(-dt)),
        'a_norm_w': 1.0 + 0.01 * jax.random.normal(ks[5], (la, GDN_HEAD_DIM), f32),
        'a_w_out': dense(ks[6], (la, GDN_V_WIDTH, D_MODEL), GDN_V_WIDTH, DEEPNORM_BETA),
        'kv_w': dense(ks[7], (D_MODEL, NSA_KV_WIDTH), D_MODEL),
        'cmp_pe': 0.1 * jax.random.normal(ks[8], (2, CMP_BLOCK, NSA_HEAD_DIM), f32),
        'cmp_w1': dense(ks[9], (2, CMP_BLOCK * NSA_HEAD_DIM, CMP_HIDDEN), CMP_BLOCK * NSA_HEAD_DIM),
        'cmp_w2': dense(ks[10], (2, CMP_HIDDEN, NSA_HEAD_DIM), CMP_HIDDEN),
        'b_w_q': dense(ks[11], (lb, D_MODEL, NSA_Q_WIDTH + 3 * NSA_HEADS), D_MODEL),
        'b_w_out': dense(ks[12], (lb, NSA_Q_WIDTH, D_MODEL), NSA_Q_WIDTH, DEEPNORM_BETA),
        'router_w': dense(ks[13], (D_MODEL, N_EXPERTS), D_MODEL),
        'router_bias': 0.01 * jax.random.normal(ks[14], (N_EXPERTS,), f32),
        'moe_w_gate': dense(ks[15], (DEPTH, N_EXPERTS, D_MODEL, D_EXPERT), D_MODEL),
        'moe_w_up': dense(ks[16], (DEPTH, N_EXPERTS, D_MODEL, D_EXPERT), D_MODEL),
        'moe_w_down': dense(ks[17], (DEPTH, N_EXPERTS, D_EXPERT, D_MODEL), D_EXPERT, DEEPNORM_BETA),
        'ln_g': 1.0 + 0.01 * jax.random.normal(ks[18], (DEPTH, 2, D_MODEL), f32),
        'ln_b': 0.01 * jax.random.normal(ks[19], (DEPTH, 2, D_MODEL), f32),
    }


def reference(x, a_w_in, a_conv_w, a_a_log, a_dt_bias, a_norm_w, a_w_out, kv_w, cmp_pe, cmp_w1, cmp_w2,
              b_w_q, b_w_out, router_w, router_bias, moe_w_gate, moe_w_up, moe_w_down, ln_g, ln_b):
    shared_kv = None
    for layer in range(DEPTH):
        if layer < N_A_LAYERS:
            mix = gdn_mixer(x, a_w_in[layer], a_conv_w[layer], a_a_log[layer], a_dt_bias[layer],
                            a_norm_w[layer], a_w_out[layer])
        else:
            if shared_kv is None:
                shared_kv = nsa_shared_kv(x, kv_w, cmp_pe, cmp_w1, cmp_w2)
            j = layer - N_A_LAYERS
            mix = nsa_mixer(x, b_w_q[j], b_w_out[j], *shared_kv)
        x = layer_norm(DEEPNORM_ALPHA * x + mix, ln_g[layer, 0], ln_b[layer, 0]).astype(x.dtype)
        ffn = moe_ffn(x, router_w, router_bias, moe_w_gate[layer], moe_w_up[layer], moe_w_down[layer])
        x = layer_norm(DEEPNORM_ALPHA * x + ffn, ln_g[layer, 1], ln_b[layer, 1]).astype(x.dtype)
    return x
```

```python
import contextlib
import numpy as np
import ml_dtypes
import concourse.bass as bass
import concourse.mybir as mybir
from concourse.bass_utils import run_bass_kernel_spmd

F32 = mybir.dt.float32
BF16 = mybir.dt.bfloat16
AF = mybir.ActivationFunctionType
ALU = mybir.AluOpType
AX = mybir.AxisListType
NPBF = ml_dtypes.bfloat16

NCORES = 8
D = 2048
ALPHA = 4.0 ** 0.25
LN_EPS = 1e-5
RMS_EPS = 1e-6


class Sched:
    LIMIT = 30000
    NDMA = 16

    def __init__(self, nc, es):
        self.nc, self.es = nc, es
        self.eng = {'pe': nc.tensor, 'act': nc.scalar, 'dve': nc.vector, 'pool': nc.gpsimd, 'sp': nc.sync}
        self.sems = []
        self.cur = {}
        self.known = {e: {} for e in self.eng}
        self.lastw = {}
        self.readers = {}
        self.pe_sids = set()
        for e in ('pe', 'act', 'dve', 'pool'):
            self.cur[e] = [self._newsem(e), 0]
        self.pe_sids.add(self.cur['pe'][0])
        self.dma_slots = [[self._newsem('dma%d' % i), 0] for i in range(self.NDMA)]
        self.dma_rr = 0

    def _newsem(self, name):
        s = self.es.enter_context(self.nc.semaphore('%s_%d' % (name, len(self.sems))))
        self.sems.append(s)
        return len(self.sems) - 1

    def _wait(self, e, deps):
        for sid, val in deps.items():
            if self.known[e].get(sid, 0) < val:
                self.eng[e].wait_ge(self.sems[sid], val)
                self.known[e][sid] = val

    def _deps(self, e, r, w):
        d = {}

        def add(sid, val):
            if d.get(sid, 0) < val:
                d[sid] = val
        for k in r:
            ev = self.lastw.get(k)
            if ev is not None:
                add(*ev)
        for k in w:
            ev = self.lastw.get(k)
            if ev is not None:
                add(*ev)
            for sid, val in self.readers.get(k, {}).items():
                add(sid, val)
        if e == 'pe':
            for sid in list(d):
                if sid in self.pe_sids:
                    del d[sid]
        return d

    def _record(self, ev, r, w):
        sid, val = ev
        for k in r:
            rd = self.readers.setdefault(k, {})
            if rd.get(sid, 0) < val:
                rd[sid] = val
        for k in w:
            self.lastw[k] = ev
            self.readers[k] = {}

    def op(self, e, fn, r=(), w=()):
        w = list(w) + [k for k in r if isinstance(k, tuple) and k[0] == 'bk']
        self._wait(e, self._deps(e, r, w))
        ins = fn(self.eng[e])
        c = self.cur[e]
        if c[1] >= self.LIMIT:
            c[0] = self._newsem(e)
            c[1] = 0
            if e == 'pe':
                self.pe_sids.add(c[0])
        c[1] += 1
        ins.then_inc(self.sems[c[0]], 1)
        self._record((c[0], c[1]), r, w)

    def dma(self, q, out, in_, r=(), w=(), **kw):
        slot = self.dma_slots[self.dma_rr]
        self.dma_rr = (self.dma_rr + 1) % self.NDMA
        d = self._deps(q, r, w)
        if slot[1] > 0:
            d[slot[0]] = max(d.get(slot[0], 0), 16 * slot[1])
        self._wait(q, d)
        ins = self.eng[q].dma_start(out=out, in_=in_, **kw)
        slot[1] += 1
        ins.then_inc(self.sems[slot[0]], 16)
        self._record((slot[0], 16 * slot[1]), r, w)

    def _all_events(self):
        d = {slot[0]: 16 * slot[1] for slot in self.dma_slots if slot[1] > 0}
        for e, c in self.cur.items():
            if c[1] > 0:
                d[c[0]] = c[1]
        return d

    def sync_all(self):
        d = self._all_events()
        for e in self.eng:
            self._wait(e, d)

    def finish(self):
        self._wait('sp', self._all_events())


def _mk():
    return bass.Bass("TRN2", target_bir_lowering=False)


def _run(nc, in_maps):
    res = run_bass_kernel_spmd(nc, in_maps, core_ids=list(range(NCORES)))
    return res.results


def _layer_norm_tile(S, es, nc, h, tt, gbc, bbc, tmp):
    hk = ('h', tt)
    st, mv, rs = tmp['st'], tmp['mv'], tmp['rs']
    for c in range(4):
        S.op('dve', lambda e, c=c: e.bn_stats(out=st[:, c, :], in_=h[:, tt, c * 512:(c + 1) * 512]),
             r=[hk], w=['ln_st'])
    S.op('dve', lambda e: e.bn_aggr(out=mv[:, :], in_=st[:, :, :]), r=['ln_st'], w=['ln_mv'])
    S.op('act', lambda e: e.activation(out=rs[:, :], in_=mv[:, 1:2], func=AF.Sqrt, bias=tmp['eps'][:, 0:1]),
         r=['ln_mv', 'ln_eps'], w=['ln_rs'])
    S.op('dve', lambda e: e.reciprocal(out=rs[:, :], in_=rs[:, :]), r=['ln_rs'], w=['ln_rs'])
    S.op('dve', lambda e: e.tensor_scalar(out=h[:, tt, :], in0=h[:, tt, :], scalar1=mv[:, 0:1], scalar2=rs[:, 0:1],
                                          op0=ALU.subtract, op1=ALU.mult), r=[hk, 'ln_mv', 'ln_rs'], w=[hk])
    S.op('pool', lambda e: e.tensor_tensor(out=h[:, tt, :], in0=h[:, tt, :], in1=gbc[:, :], op=ALU.mult),
         r=[hk, 'gbc'], w=[hk])
    S.op('pool', lambda e: e.tensor_tensor(out=h[:, tt, :], in0=h[:, tt, :], in1=bbc[:, :], op=ALU.add),
         r=[hk, 'bbc'], w=[hk])


def build_post1(KIN):
    nc = _mk()
    KC = KIN // 128
    NT = 8
    aT = nc.dram_tensor("aT", [KIN, 1024], BF16, kind="ExternalInput").ap()
    xres = nc.dram_tensor("xres", [1024, D], F32, kind="ExternalInput").ap()
    w_out = nc.dram_tensor("w_out", [KIN, D], F32, kind="ExternalInput").ap()
    ln_gb = nc.dram_tensor("ln_gb", [2, D], F32, kind="ExternalInput").ap()
    router_w = nc.dram_tensor("router_w", [D, 32], F32, kind="ExternalInput").ap()
    router_b = nc.dram_tensor("router_b", [1, 32], F32, kind="ExternalInput").ap()
    ident_in = nc.dram_tensor("ident", [128, 128], F32, kind="ExternalInput").ap()
    x1 = nc.dram_tensor("x1", [1024, D], F32, kind="ExternalOutput").ap()
    x1T = nc.dram_tensor("x1T", [D, 1024], BF16, kind="ExternalOutput").ap()
    gates = nc.dram_tensor("gates", [1024, 32], F32, kind="ExternalOutput").ap()
    DC = 256
    with contextlib.ExitStack() as es:
        S = Sched(nc, es)
        sb = lambda name, shape, dt: es.enter_context(nc.sbuf_tensor(name, shape, dt))
        h = sb("h", [128, NT, D], F32)
        banks = [es.enter_context(nc.psum_tensor("bk%d" % i, [128, 512], F32)) for i in range(8)]
        with contextlib.ExitStack() as es1:
            a_sb = es1.enter_context(nc.sbuf_tensor("a_sb", [128, KC, 1024], BF16))
            wbuf = [es1.enter_context(nc.sbuf_tensor("wb%d" % i, [128, KC, DC], BF16)) for i in range(2)]
            half = KC // 2
            S.dma('sp', a_sb[:, 0:half, :], aT[0:half * 128, :].rearrange("(kc p) t -> p kc t", p=128), w=['a_sb'])
            S.dma('sp', a_sb[:, half:KC, :], aT[half * 128:KIN, :].rearrange("(kc p) t -> p kc t", p=128), w=['a_sb'])
            for tt in range(NT):
                S.dma('sp', h[:, tt, :], xres[tt * 128:(tt + 1) * 128, :], w=[('h', tt)])
            nb = 0
            for dc in range(D // DC):
                wb = wbuf[dc % 2]
                wk = ('wb', dc % 2)
                S.dma('pool', wb[:, :, :], w_out[:, dc * DC:(dc + 1) * DC].rearrange("(kc p) f -> p kc f", p=128), w=[wk])
                for tt in range(NT):
                    bk = banks[nb % 8]
                    bkk = ('bk', nb % 8)
                    nb += 1
                    for kc in range(KC):
                        S.op('pe', lambda e, kc=kc, bk=bk, tt=tt, wb=wb: e.matmul(
                            bk[:, 0:DC], a_sb[:, kc, tt * 128:(tt + 1) * 128], wb[:, kc, :],
                            start=(kc == 0), stop=(kc == KC - 1)), r=['a_sb', wk], w=[bkk])
                    S.op('dve', lambda e, bk=bk, tt=tt, dc=dc: e.scalar_tensor_tensor(
                        out=h[:, tt, dc * DC:(dc + 1) * DC], in0=h[:, tt, dc * DC:(dc + 1) * DC], scalar=ALPHA,
                        in1=bk[:, 0:DC], op0=ALU.mult, op1=ALU.add), r=[bkk, ('h', tt)], w=[('h', tt)])
            S.sync_all()
        gbc = sb("gbc", [128, D], F32)
        bbc = sb("bbc", [128, D], F32)
        rw = sb("rw", [128, 16, 32], F32)
        rb = sb("rb", [128, 32], F32)
        ident = sb("ident_sb", [128, 128], F32)
        tmp = dict(st=sb("ln_st", [128, 4, 6], F32), mv=sb("ln_mv", [128, 2], F32), rs=sb("ln_rs", [128, 1], F32),
                   eps=sb("ln_eps", [128, 1], F32))
        S.op('dve', lambda e: e.memset(tmp['eps'][:, :], LN_EPS), w=['ln_eps'])
        xT32 = sb("xT32", [128, 16, 128], F32)
        xTb = sb("xTb", [128, 16, 128], BF16)
        S.dma('sp', gbc[:, :], ln_gb[0:1, :].partition_broadcast(128), w=['gbc'])
        S.dma('sp', bbc[:, :], ln_gb[1:2, :].partition_broadcast(128), w=['bbc'])
        S.dma('sp', rw[:, :, :], router_w.rearrange("(kc p) e -> p kc e", p=128), w=['rw'])
        S.dma('sp', rb[:, :], router_b[0:1, :].partition_broadcast(128), w=['rb'])
        S.dma('sp', ident[:, :], ident_in[:, :], w=['ident'])
        r_aff = sb("r_aff", [128, 32], F32)
        r_bia = sb("r_bia", [128, 32], F32)
        r_ps = [sb("r_ps%d" % i, [128, 8], F32) for i in range(6)]
        r_gs = sb("r_gs", [128, 8], F32)
        r_gm = sb("r_gm", [128, 1], F32)
        r_gmask = sb("r_gmask", [128, 8], F32)
        r_m1 = sb("r_m1", [128, 8], F32)
        r_eq = sb("r_eq", [128, 32], F32)
        r_tmp = sb("r_tmp", [128, 32], F32)
        r_m2 = sb("r_m2", [128, 8], F32)
        r_sel = sb("r_sel", [128, 32], F32)
        r_den = sb("r_den", [128, 1], F32)
        r_gate = sb("r_gate", [128, 32], F32)
        RT = ['rtmp']
        for tt in range(NT):
            _layer_norm_tile(S, es, nc, h, tt, gbc, bbc, tmp)
            S.dma('sp', x1[tt * 128:(tt + 1) * 128, :], h[:, tt, :], r=[('h', tt)])
            for q4 in range(4):
                bk = banks[q4]
                bkk = ('bk', q4)
                for j in range(4):
                    kc = q4 * 4 + j
                    S.op('pe', lambda e, bk=bk, j=j, kc=kc, tt=tt: e.transpose(
                        bk[:, j * 128:(j + 1) * 128], h[:, tt, kc * 128:(kc + 1) * 128], ident[:, :]),
                        r=[('h', tt), 'ident'], w=[bkk])
                S.op('act', lambda e, bk=bk, q4=q4: e.copy(
                    out=xT32[:, q4 * 4:(q4 + 1) * 4, :], in_=bk[:, :].rearrange("p (j t) -> p j t", j=4)),
                    r=[bkk], w=['xT32'])
                S.op('dve', lambda e, bk=bk, q4=q4: e.tensor_copy(
                    out=xTb[:, q4 * 4:(q4 + 1) * 4, :], in_=bk[:, :].rearrange("p (j t) -> p j t", j=4)),
                    r=[bkk], w=['xTb'])
            S.dma('sp', x1T[:, tt * 128:(tt + 1) * 128].rearrange("(kc p) t -> p kc t", p=128), xTb[:, :, :], r=['xTb'])
            bk = banks[4]
            bkk = ('bk', 4)
            for kc in range(16):
                S.op('pe', lambda e, kc=kc, bk=bk: e.matmul(bk[:, 0:32], xT32[:, kc, :], rw[:, kc, :],
                                                           start=(kc == 0), stop=(kc == 15)),
                     r=['xT32', 'rw'], w=[bkk])
            S.op('act', lambda e, bk=bk: e.activation(out=r_aff[:, :], in_=bk[:, 0:32], func=AF.Sigmoid),
                 r=[bkk], w=['r_aff'])
            S.op('dve', lambda e: e.tensor_tensor(out=r_bia[:, :], in0=r_aff[:, :], in1=rb[:, :], op=ALU.add),
                 r=['r_aff', 'rb'], w=RT)
            b3 = r_bia[:, :].rearrange("p (g i) -> p g i", i=4)
            pairs = [(0, 1), (0, 2), (0, 3), (1, 2), (1, 3), (2, 3)]
            for pi, (i0, i1) in enumerate(pairs):
                S.op('dve', lambda e, pi=pi, i0=i0, i1=i1: e.tensor_tensor(
                    out=r_ps[pi][:, :], in0=b3[:, :, i0], in1=b3[:, :, i1], op=ALU.add), r=RT, w=RT)
            S.op('dve', lambda e: e.tensor_tensor(out=r_gs[:, :], in0=r_ps[0][:, :], in1=r_ps[1][:, :], op=ALU.max), r=RT, w=RT)
            for pi in range(2, 6):
                S.op('dve', lambda e, pi=pi: e.tensor_tensor(out=r_gs[:, :], in0=r_gs[:, :], in1=r_ps[pi][:, :], op=ALU.max), r=RT, w=RT)
            S.op('dve', lambda e: e.tensor_reduce(out=r_gm[:, :], in_=r_gs[:, :], axis=AX.X, op=ALU.max), r=RT, w=RT)
            S.op('dve', lambda e: e.tensor_scalar(out=r_gmask[:, :], in0=r_gs[:, :], scalar1=r_gm[:, 0:1], scalar2=None,
                                                  op0=ALU.is_ge), r=RT, w=RT)
            S.op('dve', lambda e: e.tensor_reduce(out=r_m1[:, :], in_=b3, axis=AX.X, op=ALU.max), r=RT, w=RT)
            S.op('dve', lambda e: e.tensor_tensor(out=r_eq[:, :].rearrange("p (g i) -> p g i", i=4), in0=b3,
                                                  in1=r_m1[:, :].unsqueeze(2).to_broadcast([128, 8, 4]), op=ALU.is_equal), r=RT, w=RT)
            S.op('dve', lambda e: e.scalar_tensor_tensor(out=r_tmp[:, :], in0=r_eq[:, :], scalar=-1e30, in1=r_bia[:, :],
                                                         op0=ALU.mult, op1=ALU.add), r=RT, w=RT)
            S.op('dve', lambda e: e.tensor_reduce(out=r_m2[:, :], in_=r_tmp[:, :].rearrange("p (g i) -> p g i", i=4),
                                                  axis=AX.X, op=ALU.max), r=RT, w=RT)
            S.op('dve', lambda e: e.tensor_tensor(out=r_sel[:, :].rearrange("p (g i) -> p g i", i=4), in0=b3,
                                                  in1=r_m2[:, :].unsqueeze(2).to_broadcast([128, 8, 4]), op=ALU.is_ge), r=RT, w=RT)
            S.op('dve', lambda e: e.tensor_tensor(out=r_sel[:, :].rearrange("p (g i) -> p g i", i=4),
                                                  in0=r_sel[:, :].rearrange("p (g i) -> p g i", i=4),
                                                  in1=r_gmask[:, :].unsqueeze(2).to_broadcast([128, 8, 4]), op=ALU.mult), r=RT, w=RT)
            S.op('dve', lambda e: e.tensor_tensor(out=r_sel[:, :], in0=r_sel[:, :], in1=r_aff[:, :], op=ALU.mult),
                 r=RT + ['r_aff'], w=RT)
            S.op('dve', lambda e: e.tensor_reduce(out=r_den[:, :], in_=r_sel[:, :], axis=AX.X, op=ALU.add), r=RT, w=RT)
            S.op('dve', lambda e: e.reciprocal(out=r_den[:, :], in_=r_den[:, :]), r=RT, w=RT)
            S.op('dve', lambda e: e.tensor_scalar(out=r_gate[:, :], in0=r_sel[:, :], scalar1=r_den[:, 0:1], scalar2=None,
                                                  op0=ALU.mult), r=RT, w=['r_gate'])
            S.dma('sp', gates[tt * 128:(tt + 1) * 128, :], r_gate[:, :], r=['r_gate'])
        S.finish()
    return nc


def build_moe(NTOK=8192):
    nc = _mk()
    NB = NTOK // 512
    xT = nc.dram_tensor("xT", [D, NTOK], BF16, kind="ExternalInput").ap()
    gates_c = nc.dram_tensor("gates_c", [NTOK, 4], F32, kind="ExternalInput").ap()
    wg = nc.dram_tensor("wg", [4, D, 512], F32, kind="ExternalInput").ap()
    wu = nc.dram_tensor("wu", [4, D, 512], F32, kind="ExternalInput").ap()
    wd = nc.dram_tensor("wd", [4, 512, D], F32, kind="ExternalInput").ap()
    y = nc.dram_tensor("y", [NTOK, D], F32, kind="ExternalOutput").ap()
    with contextlib.ExitStack() as es:
        S = Sched(nc, es)
        sb = lambda name, shape, dt: es.enter_context(nc.sbuf_tensor(name, shape, dt))
        banks = [es.enter_context(nc.psum_tensor("bk%d" % i, [128, 512], F32)) for i in range(8)]
        wg_sb = [sb("wg%d" % i, [128, 16, 512], BF16) for i in range(2)]
        wu_sb = [sb("wu%d" % i, [128, 16, 512], BF16) for i in range(2)]
        wd_sb = [sb("wd%d" % i, [128, 4, D], BF16) for i in range(2)]
        x_sb = [sb("x%d" % i, [128, 16, 512], BF16) for i in range(2)]
        hid = [sb("hid%d" % i, [128, 4, 512], BF16) for i in range(2)]
        sg = [sb("sg%d" % i, [128, 512], F32) for i in range(2)]
        ost = [sb("ost%d" % i, [128, D], F32) for i in range(3)]
        g_sb = sb("g_sb", [128, NTOK // 128, 4], F32)
        S.dma('sp', g_sb[:, :, :], gates_c.rearrange("(tt p) e -> p tt e", p=128), w=['g_sb'])

        def load_w(e):
            b = e % 2
            S.dma('pool', wg_sb[b][:, :, :], wg[e].rearrange("(kc p) f -> p kc f", p=128), w=[('wg', b)])
            S.dma('pool', wu_sb[b][:, :, :], wu[e].rearrange("(kc p) f -> p kc f", p=128), w=[('wu', b)])
            S.dma('pool', wd_sb[b][:, :, :], wd[e].rearrange("(fc p) d -> p fc d", p=128), w=[('wd', b)])

        def load_x(i):
            tb = i % NB
            b = i % 2
            S.dma('sp', x_sb[b][:, :, :], xT[:, tb * 512:(tb + 1) * 512].rearrange("(kc p) t -> p kc t", p=128),
                  w=[('x', b)])

        load_w(0)
        load_x(0)
        it = 0
        ngu = 0
        nd = 0
        nos = 0
        for e in range(4):
            if e + 1 < 4:
                load_w(e + 1)
            wb = e % 2
            for tb in range(NB):
                if it + 1 < 4 * NB:
                    load_x(it + 1)
                xb = it % 2
                hb = it % 2
                for fc in range(4):
                    gb, ub = (0, 1) if ngu % 2 == 0 else (2, 3)
                    ngu += 1
                    for kc in range(16):
                        S.op('pe', lambda en, kc=kc, fc=fc, gb=gb: en.matmul(
                            banks[gb][:, :], wg_sb[wb][:, kc, fc * 128:(fc + 1) * 128], x_sb[xb][:, kc, :],
                            start=(kc == 0), stop=(kc == 15)), r=[('wg', wb), ('x', xb)], w=[('bk', gb)])
                    for kc in range(16):
                        S.op('pe', lambda en, kc=kc, fc=fc, ub=ub: en.matmul(
                            banks[ub][:, :], wu_sb[wb][:, kc, fc * 128:(fc + 1) * 128], x_sb[xb][:, kc, :],
                            start=(kc == 0), stop=(kc == 15)), r=[('wu', wb), ('x', xb)], w=[('bk', ub)])
                    sgi = ngu % 2
                    S.op('act', lambda en, gb=gb, sgi=sgi: en.activation(out=sg[sgi][:, :], in_=banks[gb][:, :], func=AF.Silu),
                         r=[('bk', gb)], w=[('sg', sgi)])
                    S.op('dve', lambda en, ub=ub, sgi=sgi, fc=fc: en.tensor_tensor(
                        out=hid[hb][:, fc, :], in0=sg[sgi][:, :], in1=banks[ub][:, :], op=ALU.mult),
                        r=[('sg', sgi), ('bk', ub)], w=[('hid', hb)])
                for t4 in range(4):
                    tt = tb * 4 + t4
                    oi = nos % 3
                    nos += 1
                    for dc in range(4):
                        db = 4 + nd % 4
                        nd += 1
                        for fc in range(4):
                            S.op('pe', lambda en, fc=fc, dc=dc, db=db, t4=t4: en.matmul(
                                banks[db][:, :], hid[hb][:, fc, t4 * 128:(t4 + 1) * 128], wd_sb[wb][:, fc, dc * 512:(dc + 1) * 512],
                                start=(fc == 0), stop=(fc == 3)), r=[('hid', hb), ('wd', wb)], w=[('bk', db)])
                        if dc % 2 == 0:
                            S.op('act', lambda en, db=db, dc=dc, oi=oi, tt=tt: en.activation(
                                out=ost[oi][:, dc * 512:(dc + 1) * 512], in_=banks[db][:, :], func=AF.Identity,
                                scale=g_sb[:, tt, e:e + 1]), r=[('bk', db), 'g_sb'], w=[('os', oi)])
                        else:
                            S.op('dve', lambda en, db=db, dc=dc, oi=oi, tt=tt: en.tensor_scalar(
                                out=ost[oi][:, dc * 512:(dc + 1) * 512], in0=banks[db][:, :], scalar1=g_sb[:, tt, e:e + 1],
                                scalar2=None, op0=ALU.mult), r=[('bk', db), 'g_sb'], w=[('os', oi)])
                    if e == 0:
                        S.dma('sp', y[tt * 128:(tt + 1) * 128, :], ost[oi][:, :], r=[('os', oi)], w=[('y', tt)])
                    else:
                        S.dma('pool', y[tt * 128:(tt + 1) * 128, :], ost[oi][:, :], r=[('os', oi)], w=[('y', tt)],
                              accum_op=ALU.add)
                it += 1
        S.finish()
    return nc


def build_post2(PROJ):
    nc = _mk()
    NT = 8
    yp = nc.dram_tensor("yp", [8, 1024, D], F32, kind="ExternalInput").ap()
    x1 = nc.dram_tensor("x1", [1024, D], F32, kind="ExternalInput").ap()
    ln_gb = nc.dram_tensor("ln_gb", [2, D], F32, kind="ExternalInput").ap()
    x2 = nc.dram_tensor("x2", [1024, D], F32, kind="ExternalOutput").ap()
    if PROJ:
        kv_w = nc.dram_tensor("kv_w", [D, 3072], F32, kind="ExternalInput").ap()
        w_q = nc.dram_tensor("w_q", [D, 2096], F32, kind="ExternalInput").ap()
        cs = nc.dram_tensor("cs", [1024, 128], F32, kind="ExternalInput").ap()
        ident_in = nc.dram_tensor("ident", [128, 128], F32, kind="ExternalInput").ap()
        proj = nc.dram_tensor("proj", [1024, 7168], BF16, kind="ExternalOutput").ap()
        qgate = nc.dram_tensor("qgate", [1024, 48], F32, kind="ExternalOutput").ap()
    with contextlib.ExitStack() as es:
        S = Sched(nc, es)
        sb = lambda name, shape, dt: es.enter_context(nc.sbuf_tensor(name, shape, dt))
        h = sb("h", [128, NT, D], F32)
        banks = [es.enter_context(nc.psum_tensor("bk%d" % i, [128, 512], F32)) for i in range(8)]
        gbc = sb("gbc", [128, D], F32)
        bbc = sb("bbc", [128, D], F32)
        tmp = dict(st=sb("ln_st", [128, 4, 6], F32), mv=sb("ln_mv", [128, 2], F32), rs=sb("ln_rs", [128, 1], F32),
                   eps=sb("ln_eps", [128, 1], F32))
        S.op('dve', lambda e: e.memset(tmp['eps'][:, :], LN_EPS), w=['ln_eps'])
        S.dma('sp', gbc[:, :], ln_gb[0:1, :].partition_broadcast(128), w=['gbc'])
        S.dma('sp', bbc[:, :], ln_gb[1:2, :].partition_broadcast(128), w=['bbc'])
        with contextlib.ExitStack() as es1:
            stg = [es1.enter_context(nc.sbuf_tensor("stg%d" % i, [128, D], F32)) for i in range(3)]
            ns = 0
            for tt in range(NT):
                S.dma('sp', h[:, tt, :], x1[tt * 128:(tt + 1) * 128, :], w=[('h', tt)])
                for c in range(8):
                    si = ns % 3
                    ns += 1
                    S.dma('sp', stg[si][:, :], yp[c, tt * 128:(tt + 1) * 128, :], w=[('stg', si)])
                    eng = 'dve' if c % 2 == 0 else 'pool'
                    if c == 0:
                        S.op(eng, lambda e, si=si, tt=tt: e.scalar_tensor_tensor(
                            out=h[:, tt, :], in0=h[:, tt, :], scalar=ALPHA, in1=stg[si][:, :], op0=ALU.mult, op1=ALU.add),
                            r=[('stg', si), ('h', tt)], w=[('h', tt)])
                    else:
                        S.op(eng, lambda e, si=si, tt=tt: e.tensor_tensor(
                            out=h[:, tt, :], in0=h[:, tt, :], in1=stg[si][:, :], op=ALU.add),
                            r=[('stg', si), ('h', tt)], w=[('h', tt)])
                _layer_norm_tile(S, es, nc, h, tt, gbc, bbc, tmp)
                S.dma('sp', x2[tt * 128:(tt + 1) * 128, :], h[:, tt, :], r=[('h', tt)])
            S.sync_all()
        if PROJ:
            ident = sb("ident_sb", [128, 128], F32)
            xT = sb("xT", [128, 16, 1024], BF16)
            cs_sb = sb("cs_sb", [128, NT, 128], F32)
            wbuf = [sb("wb%d" % i, [128, 16, 512], BF16) for i in range(2)]
            rsb = [sb("rsb%d" % i, [128, 512], F32) for i in range(2)]
            t1 = [sb("t1_%d" % i, [128, 256], F32) for i in range(2)]
            t2 = [sb("t2_%d" % i, [128, 256], F32) for i in range(2)]
            ob = [sb("ob%d" % i, [128, 512], BF16) for i in range(4)]
            gst = [sb("gst%d" % i, [128, 48], F32) for i in range(2)]
            S.dma('sp', ident[:, :], ident_in[:, :], w=['ident'])
            S.dma('sp', cs_sb[:, :, :], cs.rearrange("(tt p) c -> p tt c", p=128), w=['cs'])
            for tt in range(NT):
                for q4 in range(4):
                    bk = banks[q4]
                    bkk = ('bk', q4)
                    for j in range(4):
                        kc = q4 * 4 + j
                        S.op('pe', lambda e, bk=bk, j=j, kc=kc, tt=tt: e.transpose(
                            bk[:, j * 128:(j + 1) * 128], h[:, tt, kc * 128:(kc + 1) * 128], ident[:, :]),
                            r=[('h', tt), 'ident'], w=[bkk])
                    eng = 'act' if q4 % 2 == 0 else 'dve'
                    if eng == 'act':
                        S.op('act', lambda e, bk=bk, q4=q4, tt=tt: e.copy(
                            out=xT[:, q4 * 4:(q4 + 1) * 4, tt * 128:(tt + 1) * 128],
                            in_=bk[:, :].rearrange("p (j t) -> p j t", j=4)), r=[bkk], w=[('xT', tt)])
                    else:
                        S.op('dve', lambda e, bk=bk, q4=q4, tt=tt: e.tensor_copy(
                            out=xT[:, q4 * 4:(q4 + 1) * 4, tt * 128:(tt + 1) * 128],
                            in_=bk[:, :].rearrange("p (j t) -> p j t", j=4)), r=[bkk], w=[('xT', tt)])
            chunks = []
            for c in range(6):
                chunks.append((kv_w[:, c * 512:(c + 1) * 512], 512, 'rope' if c in (2, 4) else 'plain', c * 512))
            for c in range(4):
                chunks.append((w_q[:, c * 512:(c + 1) * 512], 512, 'q', c * 512))
            chunks.append((w_q[:, 2048:2096], 48, 'gate', 0))
            nb = 0
            no = 0
            nr = 0
            for ci, (wsrc, ncol, kind, c0) in enumerate(chunks):
                wb = wbuf[ci % 2]
                wk = ('wb', ci % 2)
                S.dma('pool', wb[:, :, 0:ncol], wsrc.rearrange("(kc p) f -> p kc f", p=128), w=[wk])
                for tt in range(NT):
                    bi = nb % 8
                    nb += 1
                    bk, bkk = banks[bi], ('bk', bi)
                    for kc in range(16):
                        S.op('pe', lambda e, kc=kc, bk=bk, tt=tt, wb=wb, ncol=ncol: e.matmul(
                            bk[:, 0:ncol], xT[:, kc, tt * 128:(tt + 1) * 128], wb[:, kc, 0:ncol],
                            start=(kc == 0), stop=(kc == 15)), r=[('xT', tt), wk], w=[bkk])
                    rows = slice(tt * 128, (tt + 1) * 128)
                    if kind == 'gate':
                        gi = tt % 2
                        S.op('act', lambda e, bk=bk, gi=gi: e.activation(out=gst[gi][:, :], in_=bk[:, 0:48], func=AF.Sigmoid),
                             r=[bkk], w=[('gst', gi)])
                        S.dma('sp', qgate[rows, :], gst[gi][:, :], r=[('gst', gi)])
                        continue
                    if kind in ('plain', 'q'):
                        oi = no % 4
                        no += 1
                        S.op('act', lambda e, bk=bk, oi=oi: e.copy(out=ob[oi][:, :], in_=bk[:, :]), r=[bkk], w=[('ob', oi)])
                        oc = c0 if kind == 'plain' else 3072 + c0
                        S.dma('sp', proj[rows, oc:oc + 512], ob[oi][:, :], r=[('ob', oi)])
                    if kind in ('rope', 'q'):
                        ri = nr % 2
                        nr += 1
                        oi = no % 4
                        no += 1
                        S.op('dve', lambda e, bk=bk, ri=ri: e.tensor_copy(out=rsb[ri][:, :], in_=bk[:, :]), r=[bkk], w=[('rsb', ri)])
                        rv = rsb[ri][:, :].rearrange("p (h d) -> p h d", h=4)
                        ov = ob[oi][:, :].rearrange("p (h d) -> p h d", h=4)
                        cosb = cs_sb[:, tt, 0:64].unsqueeze(1).to_broadcast([128, 4, 64])
                        sinb = cs_sb[:, tt, 64:128].unsqueeze(1).to_broadcast([128, 4, 64])
                        t1v = t1[ri][:, :].rearrange("p (h d) -> p h d", h=4)
                        t2v = t2[ri][:, :].rearrange("p (h d) -> p h d", h=4)
                        S.op('dve', lambda e, rv=rv, t1v=t1v, cosb=cosb: e.tensor_tensor(out=t1v, in0=rv[:, :, 0:64], in1=cosb, op=ALU.mult),
                             r=[('rsb', ri), 'cs'], w=[('t1', ri)])
                        S.op('dve', lambda e, rv=rv, t1v=t1v, sinb=sinb: e.scalar_tensor_tensor(
                            out=ov[:, :, 0:64], in0=rv[:, :, 64:128], scalar=-1.0, in1=sinb, op0=ALU.mult, op1=ALU.mult),
                            r=[('rsb', ri), 'cs'], w=[('ob', oi)])
                        S.op('dve', lambda e, ov=ov, t1v=t1v: e.tensor_tensor(out=ov[:, :, 0:64], in0=ov[:, :, 0:64], in1=t1v, op=ALU.add),
                             r=[('t1', ri), ('ob', oi)], w=[('ob', oi)])
                        S.op('pool', lambda e, rv=rv, t2v=t2v, cosb=cosb: e.tensor_tensor(out=t2v, in0=rv[:, :, 64:128], in1=cosb, op=ALU.mult),
                             r=[('rsb', ri), 'cs'], w=[('t2', ri)])
                        S.op('pool', lambda e, rv=rv, ov=ov, sinb=sinb: e.tensor_tensor(out=ov[:, :, 64:128], in0=rv[:, :, 0:64], in1=sinb, op=ALU.mult),
                             r=[('rsb', ri), 'cs', ('ob', oi)], w=[('ob', oi)])
                        S.op('pool', lambda e, ov=ov, t2v=t2v: e.tensor_tensor(out=ov[:, :, 64:128], in0=ov[:, :, 64:128], in1=t2v, op=ALU.add),
                             r=[('t2', ri), ('ob', oi)], w=[('ob', oi)])
                        oc = c0 if kind == 'rope' else 5120 + c0
                        S.dma('sp', proj[rows, oc:oc + 512], ob[oi][:, :], r=[('ob', oi)])
        S.finish()
    return nc


def build_gdn_a():
    nc = _mk()
    T = 4096
    HT = 2048
    xT = nc.dram_tensor("xT", [D, T], F32, kind="ExternalInput").ap()
    w_sl = nc.dram_tensor("w_sl", [D, 3088], F32, kind="ExternalInput").ap()
    conv_w = nc.dram_tensor("conv_w", [2048, 4], F32, kind="ExternalInput").ap()
    qkvT = nc.dram_tensor("qkvT", [2048, T], BF16, kind="ExternalOutput").ap()
    zT = nc.dram_tensor("zT", [1024, T], BF16, kind="ExternalOutput").ap()
    abT = nc.dram_tensor("abT", [16, T], F32, kind="ExternalOutput").ap()
    with contextlib.ExitStack() as es:
        S = Sched(nc, es)
        sb = lambda name, shape, dt: es.enter_context(nc.sbuf_tensor(name, shape, dt))
        banks = [es.enter_context(nc.psum_tensor("bk%d" % i, [128, 512], F32)) for i in range(8)]
        x_sb = sb("x_sb", [128, 16, HT], BF16)
        wt = [sb("wt%d" % i, [128, 16, 128], BF16) for i in range(2)]
        P = [sb("P%d" % i, [128, HT + 3], F32) for i in range(2)]
        acc = [sb("acc%d" % i, [128, HT], F32) for i in range(2)]
        ost = [sb("ost%d" % i, [128, HT], BF16) for i in range(2)]
        abst = sb("abst", [16, HT], F32)
        carry = sb("carry", [128, 16, 3], F32)
        cw = sb("cw", [128, 16, 4], F32)
        S.dma('sp', cw[:, :, :], conv_w.rearrange("(ct p) j -> p ct j", p=128), w=['cw'])
        nb = 0
        it = 0
        for hf in range(2):
            for g4 in range(4):
                S.dma('pool', x_sb[:, g4 * 4:(g4 + 1) * 4, :],
                      xT[g4 * 512:(g4 + 1) * 512, hf * HT:(hf + 1) * HT].rearrange("(kc p) t -> p kc t", p=128),
                      w=[('x', g4)])
            xk = [('x', g4) for g4 in range(4)]
            for ct in range(25):
                nch = 128 if ct < 24 else 16
                wi = it % 2
                pi = it % 2
                it += 1
                S.dma('pool', wt[wi][:, :, 0:nch], w_sl[:, ct * 128:ct * 128 + nch].rearrange("(kc p) f -> p kc f", p=128),
                      w=[('wt', wi)])
                if ct < 16:
                    if hf == 0:
                        S.op('dve', lambda e, pi=pi: e.memset(P[pi][:, 0:3], 0.0), w=[('P', pi)])
                    else:
                        S.op('dve', lambda e, pi=pi, ct=ct: e.tensor_copy(out=P[pi][:, 0:3], in_=carry[:, ct, :]),
                             r=['carry'], w=[('P', pi)])
                for tb in range(4):
                    bi = nb % 8
                    nb += 1
                    bk, bkk = banks[bi], ('bk', bi)
                    for kc in range(16):
                        S.op('pe', lambda e, kc=kc, bk=bk, tb=tb, wi=wi, nch=nch: e.matmul(
                            bk[0:nch, :], wt[wi][:, kc, 0:nch], x_sb[:, kc, tb * 512:(tb + 1) * 512],
                            start=(kc == 0), stop=(kc == 15)), r=[('wt', wi)] + xk, w=[bkk])
                    cols = slice(tb * 512, (tb + 1) * 512)
                    if ct < 16:
                        S.op('act', lambda e, bk=bk, tb=tb, pi=pi: e.copy(out=P[pi][:, 3 + tb * 512:3 + (tb + 1) * 512], in_=bk[:, :]),
                             r=[bkk], w=[('P', pi)])
                    elif ct < 24:
                        S.op('act', lambda e, bk=bk, cols=cols, pi=pi: e.copy(out=ost[pi][:, cols], in_=bk[:, :]),
                             r=[bkk], w=[('ost', pi)])
                    else:
                        S.op('act', lambda e, bk=bk, cols=cols: e.copy(out=abst[:, cols], in_=bk[0:16, :]),
                             r=[bkk], w=['abst'])
                tsl = slice(hf * HT, (hf + 1) * HT)
                if ct < 16:
                    S.op('dve', lambda e, pi=pi, ct=ct: e.tensor_copy(out=carry[:, ct, :], in_=P[pi][:, HT:HT + 3]),
                         r=[('P', pi)], w=['carry'])
                    S.op('dve', lambda e, pi=pi, ct=ct: e.tensor_scalar(
                        out=acc[pi][:, :], in0=P[pi][:, 0:HT], scalar1=cw[:, ct, 0:1], scalar2=None, op0=ALU.mult),
                        r=[('P', pi), 'cw'], w=[('acc', pi)])
                    for j in range(1, 4):
                        S.op('dve', lambda e, pi=pi, ct=ct, j=j: e.scalar_tensor_tensor(
                            out=acc[pi][:, :], in0=P[pi][:, j:j + HT], scalar=cw[:, ct, j:j + 1], in1=acc[pi][:, :],
                            op0=ALU.mult, op1=ALU.add), r=[('P', pi), 'cw', ('acc', pi)], w=[('acc', pi)])
                    S.op('act', lambda e, pi=pi: e.activation(out=ost[pi][:, :], in_=acc[pi][:, :], func=AF.Silu),
                         r=[('acc', pi)], w=[('ost', pi)])
                    S.dma('sp', qkvT[ct * 128:(ct + 1) * 128, tsl], ost[pi][:, :], r=[('ost', pi)])
                elif ct < 24:
                    S.dma('sp', zT[(ct - 16) * 128:(ct - 15) * 128, tsl], ost[pi][:, :], r=[('ost', pi)])
                else:
                    S.dma('sp', abT[:, tsl], abst[:, :], r=['abst'])
        S.finish()
    return nc


def gdn_a_inputs(x, a_w_in, a_conv_w):
    ims = []
    for c in range(NCORES):
        b, hg = c // 4, c % 4
        w = a_w_in
        w_sl = np.concatenate([w[:, hg * 512:(hg + 1) * 512], w[:, 2048 + hg * 512:2048 + (hg + 1) * 512],
                               w[:, 4096 + hg * 1024:4096 + (hg + 1) * 1024], w[:, 8192 + hg * 1024:8192 + (hg + 1) * 1024],
                               w[:, 12288 + hg * 8:12288 + (hg + 1) * 8], w[:, 12320 + hg * 8:12320 + (hg + 1) * 8]], axis=1)
        cwf = a_conv_w
        cw = np.concatenate([cwf[:, hg * 512:(hg + 1) * 512], cwf[:, 2048 + hg * 512:2048 + (hg + 1) * 512],
                             cwf[:, 4096 + hg * 1024:4096 + (hg + 1) * 1024]], axis=1).T
        ims.append({"xT": np.ascontiguousarray(x[b].T), "w_sl": np.ascontiguousarray(w_sl),
                    "conv_w": np.ascontiguousarray(cw)})
    return ims


def build_gdn_b(NCH=32):
    nc = _mk()
    T = 4096
    qk = nc.dram_tensor("qk", [T, 8, 128], BF16, kind="ExternalInput").ap()
    v_in = nc.dram_tensor("v", [T, 8, 128], BF16, kind="ExternalInput").ap()
    z_in = nc.dram_tensor("z", [T, 8, 128], BF16, kind="ExternalInput").ap()
    ab = nc.dram_tensor("ab", [T, 16], F32, kind="ExternalInput").ap()
    hp = nc.dram_tensor("hp", [2, 8], F32, kind="ExternalInput").ap()
    nw = nc.dram_tensor("nw", [1, 128], F32, kind="ExternalInput").ap()
    cmask = nc.dram_tensor("cmask", [4, 128, 128], F32, kind="ExternalInput").ap()
    identb_in = nc.dram_tensor("identb", [128, 128], BF16, kind="ExternalInput").ap()
    og = nc.dram_tensor("og", [T, 1024], BF16, kind="ExternalOutput").ap()
    with contextlib.ExitStack() as es:
        S = Sched(nc, es)
        sb = lambda name, shape, dt: es.enter_context(nc.sbuf_tensor(name, shape, dt))
        banks = [es.enter_context(nc.psum_tensor("bk%d" % i, [128, 512], F32)) for i in range(7)]
        tpb = es.enter_context(nc.psum_tensor("tpb", [128, 1024], BF16))
        TPK = ('bk', 7)
        nbk = [0]

        def newbank():
            i = nbk[0] % 7
            nbk[0] += 1
            return banks[i], ('bk', i)

        ML = sb("ML", [128, 128], F32)
        MU = sb("MU", [128, 128], F32)
        MUI = sb("MUI", [128, 128], F32)
        IDF = sb("IDF", [128, 128], F32)
        ONES = sb("ONES", [128, 128], F32)
        IDB = sb("IDB", [128, 128], BF16)
        hpb = sb("hpb", [128, 16], F32)
        nwb = sb("nwb", [128, 128], F32)
        negexpA = sb("negexpA", [128, 8], F32)
        epsr = sb("epsr", [128, 1], F32)
        one1 = sb("one1", [128, 1], F32)
        for i, t in enumerate((ML, MU, MUI, IDF)):
            S.dma('sp', t[:, :], cmask[i], w=[t.name])
        S.dma('sp', IDB[:, :], identb_in[:, :], w=['IDB'])
        S.dma('sp', hpb[:, 0:8], hp[0:1, :].partition_broadcast(128), w=['hpb'])
        S.dma('sp', hpb[:, 8:16], hp[1:2, :].partition_broadcast(128), w=['hpb'])
        S.dma('sp', nwb[:, :], nw[0:1, :].partition_broadcast(128), w=['nwb'])
        S.op('dve', lambda e: e.memset(ONES[:, :], 1.0), w=['ONES'])
        S.op('dve', lambda e: e.memset(epsr[:, :], RMS_EPS), w=['epsr'])
        S.op('dve', lambda e: e.memset(one1[:, :], 1.0), w=['one1'])
        S.op('act', lambda e: e.activation(out=negexpA[:, :], in_=hpb[:, 0:8], func=AF.Exp), r=['hpb'], w=['negexpA'])
        S.op('dve', lambda e: e.tensor_scalar(out=negexpA[:, :], in0=negexpA[:, :], scalar1=-1.0, scalar2=None, op0=ALU.mult),
             r=['negexpA'], w=['negexpA'])

        qk_sb = [sb("qk%d" % i, [128, 8, 128], BF16) for i in range(2)]
        v_sb = [sb("v%d" % i, [128, 8, 128], BF16) for i in range(2)]
        z_sb = [sb("z%d" % i, [128, 8, 128], BF16) for i in range(2)]
        ab_sb = [sb("ab%d" % i, [128, 16], F32) for i in range(2)]
        sq = sb("sq", [128, 8, 128], F32)
        ssq = sb("ssq", [128, 8], F32)
        rn = sb("rn", [128, 8], F32)
        qkn = sb("qkn", [128, 8, 128], BF16)
        qkT = sb("qkT", [128, 8, 128], BF16)
        qg = sb("qg", [128, 8, 128], BF16)
        qgT = sb("qgT", [128, 8, 128], BF16)
        kd = sb("kd", [128, 8, 128], BF16)
        sz = sb("sz", [128, 8, 128], F32)
        nwz = sb("nwz", [128, 8, 128], F32)
        xa = sb("xa", [128, 8], F32)
        ax = sb("ax", [128, 8], F32)
        ex = sb("ex", [128, 8], F32)
        gg = sb("gg", [128, 8], F32)
        beta = sb("beta", [128, 8], F32)
        gcs = sb("gcs", [128, 16], F32)
        eg = sb("eg", [128, 8], F32)
        negeg = sb("negeg", [128, 8], F32)
        dl = sb("dl", [128, 8], F32)
        egl = sb("egl", [128, 8], F32)
        eglast = sb("eglast", [128, 8], F32)
        KKm = [sb("KKm%d" % i, [128, 128], F32) for i in range(4)]
        KQm = [sb("KQm%d" % i, [128, 128], F32) for i in range(4)]
        Gm = [sb("Gm%d" % i, [128, 128], F32) for i in range(8)]
        Ee = [sb("Ee%d" % i, [128, 128], F32) for i in range(8)]
        Wa = [sb("Wa%d" % i, [128, 128], F32) for i in range(8)]
        WTa = [sb("WTa%d" % i, [128, 128], F32) for i in range(8)]
        Wb = [sb("Wb%d" % i, [128, 128], F32) for i in range(8)]
        WTb = [sb("WTb%d" % i, [128, 128], F32) for i in range(8)]
        Qm = [sb("Qm%d" % i, [128, 128], F32) for i in range(8)]
        AT = [sb("AT%d" % i, [128, 128], BF16) for i in range(8)]
        PTb = [sb("PTb%d" % i, [128, 128], BF16) for i in range(8)]
        Xv = [sb("Xv%d" % i, [128, 128], BF16) for i in range(8)]
        vnew = [sb("vnew%d" % i, [128, 128], BF16) for i in range(8)]
        Sf = [sb("Sf%d" % i, [128, 128], F32) for i in range(8)]
        Sb = [sb("Sb%d" % i, [128, 128], BF16) for i in range(8)]
        o_sb = sb("o_sb", [128, 8, 128], F32)
        osq = sb("osq", [128, 8, 128], F32)
        ossq = sb("ossq", [128, 8], F32)
        orstd = sb("orstd", [128, 8], F32)
        ogst = [sb("ogst%d" % i, [128, 8, 128], BF16) for i in range(2)]
        for hv in range(8):
            S.op('dve', lambda e, hv=hv: e.memset(Sf[hv][:, :], 0.0), w=[Sf[hv].name])
            S.op('pool', lambda e, hv=hv: e.memset(Sb[hv][:, :], 0.0), w=[Sb[hv].name])

        def load(n):
            p = n % 2
            rows = slice(n * 128, (n + 1) * 128)
            S.dma('sp', qk_sb[p][:, :, :], qk[rows], w=[qk_sb[p].name])
            S.dma('sp', v_sb[p][:, :, :], v_in[rows], w=[v_sb[p].name])
            S.dma('sp', z_sb[p][:, :, :], z_in[rows], w=[z_sb[p].name])
            S.dma('sp', ab_sb[p][:, :], ab[rows, :], w=[ab_sb[p].name])

        def bc3(ap2, n):
            return ap2.unsqueeze(2).to_broadcast([128, n, 128])

        load(0)
        for n in range(NCH):
            p = n % 2
            if n + 1 < NCH:
                load(n + 1)
            QK, V, Z, AB = qk_sb[p], v_sb[p], z_sb[p], ab_sb[p]
            S.op('dve', lambda e: e.tensor_tensor(out=sq[:, :, :], in0=QK[:, :, :], in1=QK[:, :, :], op=ALU.mult), r=[QK.name], w=['sq'])
            S.op('dve', lambda e: e.tensor_reduce(out=ssq[:, :], in_=sq[:, :, :], axis=AX.X, op=ALU.add), r=['sq'], w=['ssq'])
            S.op('act', lambda e: e.activation(out=rn[:, :], in_=ssq[:, :], func=AF.Sqrt, bias=epsr[:, 0:1]), r=['ssq', 'epsr'], w=['rn'])
            S.op('dve', lambda e: e.reciprocal(out=rn[:, :], in_=rn[:, :]), r=['rn'], w=['rn'])
            S.op('dve', lambda e: e.tensor_scalar(out=rn[:, 0:4], in0=rn[:, 0:4], scalar1=128.0 ** -0.5, scalar2=None, op0=ALU.mult), r=['rn'], w=['rn'])
            S.op('dve', lambda e: e.tensor_tensor(out=qkn[:, :, :], in0=QK[:, :, :], in1=bc3(rn[:, :], 8), op=ALU.mult), r=[QK.name, 'rn'], w=['qkn'])
            S.op('dve', lambda e: e.tensor_tensor(out=xa[:, :], in0=AB[:, 0:8], in1=hpb[:, 8:16], op=ALU.add), r=[AB.name, 'hpb'], w=['xa'])
            S.op('dve', lambda e: e.tensor_scalar(out=ax[:, :], in0=xa[:, :], scalar1=-1.0, scalar2=None, op0=ALU.mult), r=['xa'], w=['ax'])
            S.op('dve', lambda e: e.tensor_tensor(out=ax[:, :], in0=ax[:, :], in1=xa[:, :], op=ALU.min), r=['xa', 'ax'], w=['ax'])
            S.op('act', lambda e: e.activation(out=ex[:, :], in_=ax[:, :], func=AF.Exp), r=['ax'], w=['ex'])
            S.op('act', lambda e: e.activation(out=ex[:, :], in_=ex[:, :], func=AF.Ln, bias=one1[:, 0:1]), r=['ex', 'one1'], w=['ex'])
            S.op('dve', lambda e: e.scalar_tensor_tensor(out=gg[:, :], in0=xa[:, :], scalar=0.0, in1=ex[:, :], op0=ALU.max, op1=ALU.add), r=['xa', 'ex'], w=['gg'])
            S.op('dve', lambda e: e.tensor_tensor(out=gg[:, :], in0=gg[:, :], in1=negexpA[:, :], op=ALU.mult), r=['gg', 'negexpA'], w=['gg'])
            S.op('act', lambda e: e.activation(out=beta[:, :], in_=AB[:, 8:16], func=AF.Sigmoid), r=[AB.name], w=['beta'])
            bk, bkk = newbank()
            S.op('pe', lambda e, bk=bk: e.matmul(bk[:, 0:8], MUI[:, :], gg[:, :], start=True, stop=True), r=['MUI', 'gg'], w=[bkk])
            S.op('pe', lambda e, bk=bk: e.matmul(bk[:, 8:16], ONES[:, :], gg[:, :], start=True, stop=True), r=['ONES', 'gg'], w=[bkk])
            S.op('act', lambda e, bk=bk: e.copy(out=gcs[:, :], in_=bk[:, 0:16]), r=[bkk], w=['gcs'])
            S.op('act', lambda e: e.activation(out=eg[:, :], in_=gcs[:, 0:8], func=AF.Exp), r=['gcs'], w=['eg'])
            S.op('dve', lambda e: e.tensor_scalar(out=negeg[:, :], in0=eg[:, :], scalar1=-1.0, scalar2=None, op0=ALU.mult), r=['eg'], w=['negeg'])
            S.op('dve', lambda e: e.tensor_tensor(out=dl[:, :], in0=gcs[:, 8:16], in1=gcs[:, 0:8], op=ALU.subtract), r=['gcs'], w=['dl'])
            S.op('act', lambda e: e.activation(out=egl[:, :], in_=dl[:, :], func=AF.Exp), r=['dl'], w=['egl'])
            S.op('act', lambda e: e.activation(out=eglast[:, :], in_=gcs[:, 8:16], func=AF.Exp), r=['gcs'], w=['eglast'])
            for hk in range(4):
                S.op('dve', lambda e, hk=hk: e.tensor_tensor(
                    out=qg[:, 2 * hk:2 * hk + 2, :], in0=qkn[:, hk:hk + 1, :].to_broadcast([128, 2, 128]),
                    in1=bc3(eg[:, 2 * hk:2 * hk + 2], 2), op=ALU.mult), r=['qkn', 'eg'], w=['qg'])
                S.op('pool', lambda e, hk=hk: e.tensor_tensor(
                    out=kd[:, 2 * hk:2 * hk + 2, :], in0=qkn[:, 4 + hk:5 + hk, :].to_broadcast([128, 2, 128]),
                    in1=bc3(egl[:, 2 * hk:2 * hk + 2], 2), op=ALU.mult), r=['qkn', 'egl'], w=['kd'])
            for j in range(8):
                S.op('pe', lambda e, j=j: e.transpose(tpb[:, j * 128:(j + 1) * 128], qkn[:, j, :], IDB[:, :]), r=['qkn', 'IDB'], w=[TPK])
            S.op('act', lambda e: e.copy(out=qkT[:, :, :], in_=tpb[:, :].rearrange("p (j t) -> p j t", j=8)), r=[TPK], w=['qkT'])
            for j in range(8):
                S.op('pe', lambda e, j=j: e.transpose(tpb[:, j * 128:(j + 1) * 128], qg[:, j, :], IDB[:, :]), r=['qg', 'IDB'], w=[TPK])
            S.op('dve', lambda e: e.tensor_copy(out=qgT[:, :, :], in_=tpb[:, :].rearrange("p (j t) -> p j t", j=8)), r=[TPK], w=['qgT'])
            S.op('act', lambda e: e.activation(out=sz[:, :, :], in_=Z[:, :, :], func=AF.Silu), r=[Z.name], w=['sz'])
            S.op('pool', lambda e: e.tensor_tensor(out=nwz[:, :, :], in0=sz[:, :, :], in1=nwb[:, :].unsqueeze(1).to_broadcast([128, 8, 128]), op=ALU.mult),
                 r=['sz', 'nwb'], w=['nwz'])
            for hk in range(4):
                bk, bkk = newbank()
                S.op('pe', lambda e, bk=bk, hk=hk: e.matmul(bk[:, 0:128], qkT[:, 4 + hk, :], qkT[:, 4 + hk, :], start=True, stop=True), r=['qkT'], w=[bkk])
                S.op('pe', lambda e, bk=bk, hk=hk: e.matmul(bk[:, 128:256], qkT[:, 4 + hk, :], qkT[:, hk, :], start=True, stop=True), r=['qkT'], w=[bkk])
                S.op('dve', lambda e, bk=bk, hk=hk: e.tensor_tensor(out=KKm[hk][:, :], in0=bk[:, 0:128], in1=MU[:, :], op=ALU.mult), r=[bkk, 'MU'], w=[KKm[hk].name])
                S.op('dve', lambda e, bk=bk, hk=hk: e.tensor_tensor(out=KQm[hk][:, :], in0=bk[:, 128:256], in1=MUI[:, :], op=ALU.mult), r=[bkk, 'MUI'], w=[KQm[hk].name])
            for hv in range(8):
                hk = hv // 2
                S.op('pool', lambda e, hv=hv: e.tensor_scalar(out=Gm[hv][:, :], in0=ML[:, :], scalar1=gg[:, hv:hv + 1], scalar2=None, op0=ALU.mult),
                     r=['ML', 'gg'], w=[Gm[hv].name])
                bk, bkk = newbank()
                S.op('pe', lambda e, bk=bk, hv=hv: e.matmul(bk[:, 0:128], Gm[hv][:, :], MUI[:, :], start=True, stop=True), r=[Gm[hv].name, 'MUI'], w=[bkk])
                S.op('act', lambda e, bk=bk, hv=hv: e.activation(out=Ee[hv][:, :], in_=bk[:, 0:128], func=AF.Exp), r=[bkk], w=[Ee[hv].name])
                S.op('dve', lambda e, hv=hv, hk=hk: e.scalar_tensor_tensor(out=Wa[hv][:, :], in0=KKm[hk][:, :], scalar=beta[:, hv:hv + 1], in1=Ee[hv][:, :],
                                                                         op0=ALU.mult, op1=ALU.mult), r=[KKm[hk].name, 'beta', Ee[hv].name], w=[Wa[hv].name])
                S.op('pool', lambda e, hv=hv, hk=hk: e.tensor_tensor(out=AT[hv][:, :], in0=KQm[hk][:, :], in1=Ee[hv][:, :], op=ALU.mult),
                     r=[KQm[hk].name, Ee[hv].name], w=[AT[hv].name])
            for grp in range(2):
                hvs = range(grp * 4, grp * 4 + 4)
                cur = {}
                for hv in hvs:
                    bk, bkk = newbank()
                    S.op('pe', lambda e, bk=bk, hv=hv: e.transpose(bk[:, 0:128], Wa[hv][:, :], IDF[:, :]), r=[Wa[hv].name, 'IDF'], w=[bkk])
                    S.op('act', lambda e, bk=bk, hv=hv: e.copy(out=WTa[hv][:, :], in_=bk[:, 0:128]), r=[bkk], w=[WTa[hv].name])
                    S.op('dve', lambda e, hv=hv: e.tensor_tensor(out=Qm[hv][:, :], in0=IDF[:, :], in1=Wa[hv][:, :], op=ALU.subtract),
                         r=['IDF', Wa[hv].name], w=[Qm[hv].name])
                    cur[hv] = (Wa[hv], WTa[hv], Wb[hv], WTb[hv])
                for k in range(1, 7):
                    for hv in hvs:
                        W, WT, Wn, WTn = cur[hv]
                        bk, bkk = newbank()
                        S.op('pe', lambda e, bk=bk, W=W, WT=WT: e.matmul(bk[:, 0:128], W[:, :], WT[:, :], start=True, stop=True), r=[W.name, WT.name], w=[bkk])
                        S.op('act', lambda e, bk=bk, WTn=WTn: e.copy(out=WTn[:, :], in_=bk[:, 0:128]), r=[bkk], w=[WTn.name])
                        if k < 6:
                            bk2, bkk2 = newbank()
                            S.op('pe', lambda e, bk2=bk2, W=W, WT=WT: e.matmul(bk2[:, 0:128], WT[:, :], W[:, :], start=True, stop=True), r=[W.name, WT.name], w=[bkk2])
                            S.op('dve', lambda e, bk2=bk2, Wn=Wn: e.tensor_copy(out=Wn[:, :], in_=bk2[:, 0:128]), r=[bkk2], w=[Wn.name])
                    for hv in hvs:
                        W, WT, Wn, WTn = cur[hv]
                        bk, bkk = newbank()
                        S.op('pe', lambda e, bk=bk, WTn=WTn, hv=hv: e.matmul(bk[:, 0:128], WTn[:, :], Qm[hv][:, :], start=True, stop=True),
                             r=[WTn.name, Qm[hv].name], w=[bkk])
                        S.op('dve', lambda e, bk=bk, hv=hv: e.tensor_tensor(out=Qm[hv][:, :], in0=Qm[hv][:, :], in1=bk[:, 0:128], op=ALU.add),
                             r=[bkk, Qm[hv].name], w=[Qm[hv].name])
                        cur[hv] = (Wn, WTn, W, WT)
                for hv in hvs:
                    S.op('act', lambda e, hv=hv: e.copy(out=PTb[hv][:, :], in_=Qm[hv][:, :]), r=[Qm[hv].name], w=[PTb[hv].name])
            for grp in range(2):
                hvs = range(grp * 4, grp * 4 + 4)
                bR = {}
                for hv in hvs:
                    hk = hv // 2
                    bR[hv] = newbank()
                    bk, bkk = bR[hv]
                    S.op('pe', lambda e, bk=bk, hv=hv, hk=hk: e.matmul(bk[:, 0:128], qkT[:, 4 + hk, :], Sb[hv][:, :], start=True, stop=True),
                         r=['qkT', Sb[hv].name], w=[bkk])
                for hv in hvs:
                    bk, bkk = bR[hv]
                    S.op('dve', lambda e, bk=bk, hv=hv: e.scalar_tensor_tensor(out=Xv[hv][:, :], in0=bk[:, 0:128], scalar=negeg[:, hv:hv + 1], in1=V[:, hv, :],
                                                                         op0=ALU.mult, op1=ALU.add), r=[bkk, 'negeg', V.name], w=[Xv[hv].name])
                for hv in hvs:
                    bR[hv] = newbank()
                    bk, bkk = bR[hv]
                    S.op('pe', lambda e, bk=bk, hv=hv: e.matmul(bk[:, 0:128], PTb[hv][:, :], Xv[hv][:, :], start=True, stop=True),
                         r=[PTb[hv].name, Xv[hv].name], w=[bkk])
                for hv in hvs:
                    bk, bkk = bR[hv]
                    S.op('act', lambda e, bk=bk, hv=hv: e.activation(out=vnew[hv][:, :], in_=bk[:, 0:128], func=AF.Identity, scale=beta[:, hv:hv + 1]),
                         r=[bkk, 'beta'], w=[vnew[hv].name])
                for hv in hvs:
                    bk, bkk = newbank()
                    S.op('pe', lambda e, bk=bk, hv=hv: e.matmul(bk[:, 0:128], qgT[:, hv, :], Sb[hv][:, :], start=True, stop=False),
                         r=['qgT', Sb[hv].name], w=[bkk])
                    S.op('pe', lambda e, bk=bk, hv=hv: e.matmul(bk[:, 0:128], AT[hv][:, :], vnew[hv][:, :], start=False, stop=True),
                         r=[AT[hv].name, vnew[hv].name], w=[bkk])
                    S.op('pe', lambda e, bk=bk, hv=hv: e.matmul(bk[:, 128:256], kd[:, hv, :], vnew[hv][:, :], start=True, stop=True),
                         r=['kd', vnew[hv].name], w=[bkk])
                    S.op('act', lambda e, bk=bk, hv=hv: e.copy(out=o_sb[:, hv, :], in_=bk[:, 0:128]), r=[bkk], w=['o_sb'])
                    S.op('dve', lambda e, bk=bk, hv=hv: e.scalar_tensor_tensor(out=Sf[hv][:, :], in0=Sf[hv][:, :], scalar=eglast[:, hv:hv + 1], in1=bk[:, 128:256],
                                                                         op0=ALU.mult, op1=ALU.add), r=[bkk, 'eglast', Sf[hv].name], w=[Sf[hv].name])
                    S.op('act', lambda e, hv=hv: e.copy(out=Sb[hv][:, :], in_=Sf[hv][:, :]), r=[Sf[hv].name], w=[Sb[hv].name])
            S.op('dve', lambda e: e.tensor_tensor(out=osq[:, :, :], in0=o_sb[:, :, :], in1=o_sb[:, :, :], op=ALU.mult), r=['o_sb'], w=['osq'])
            S.op('dve', lambda e: e.tensor_reduce(out=ossq[:, :], in_=osq[:, :, :], axis=AX.X, op=ALU.add), r=['osq'], w=['ossq'])
            S.op('act', lambda e: e.activation(out=orstd[:, :], in_=ossq[:, :], func=AF.Sqrt, bias=epsr[:, 0:1], scale=1.0 / 128.0), r=['ossq', 'epsr'], w=['orstd'])
            S.op('dve', lambda e: e.reciprocal(out=orstd[:, :], in_=orstd[:, :]), r=['orstd'], w=['orstd'])
            S.op('dve', lambda e: e.tensor_tensor(out=osq[:, :, :], in0=o_sb[:, :, :], in1=bc3(orstd[:, :], 8), op=ALU.mult), r=['o_sb', 'orstd'], w=['osq'])
            S.op('pool', lambda e, p=p: e.tensor_tensor(out=ogst[p][:, :, :], in0=osq[:, :, :], in1=nwz[:, :, :], op=ALU.mult), r=['osq', 'nwz'], w=[ogst[p].name])
            S.dma('sp', og[n * 128:(n + 1) * 128, :], ogst[p][:, :, :].rearrange("p h d -> p (h d)"), r=[ogst[p].name])
        S.finish()
    return nc


def gdn_b_consts():
    i = np.arange(128)
    ML = (i[:, None] > i[None, :]).astype(np.float32)
    MU = (i[None, :] > i[:, None]).astype(np.float32)
    MUI = (i[None, :] >= i[:, None]).astype(np.float32)
    return np.stack([ML, MU, MUI, np.eye(128, dtype=np.float32)]), np.eye(128, dtype=np.float32).astype(NPBF)


def gdn_b_inputs(qkvT_all, zT_all, abT_all, a_log, dt_bias, norm_w):
    cm, idb = gdn_b_consts()
    ims = []
    for c in range(NCORES):
        hg = c % 4
        qkvT, zT, abT = qkvT_all[c], zT_all[c], abT_all[c]
        qk = np.ascontiguousarray(qkvT[0:1024].T).reshape(4096, 8, 128)
        v = np.ascontiguousarray(qkvT[1024:2048].T).reshape(4096, 8, 128)
        z = np.ascontiguousarray(zT.T).reshape(4096, 8, 128)
        ab = np.ascontiguousarray(abT.T)
        hp = np.stack([a_log[hg * 8:(hg + 1) * 8], dt_bias[hg * 8:(hg + 1) * 8]]).astype(np.float32)
        ims.append({"qk": qk, "v": v, "z": z, "ab": ab, "hp": hp, "nw": norm_w.reshape(1, 128).astype(np.float32),
                    "cmask": cm, "identb": idb})
    return ims


SCL = 128.0 ** -0.5
NEGB = -30000.0


def build_nsa(NQT=32):
    nc = _mk()
    T = 4096
    qn_d = nc.dram_tensor("qn", [128, 32 * 512], BF16, kind="ExternalInput").ap()
    qr_d = nc.dram_tensor("qr", [128, 32 * 512], BF16, kind="ExternalInput").ap()
    kcT_d = nc.dram_tensor("kcT", [128, T], BF16, kind="ExternalInput").ap()
    vcT_d = nc.dram_tensor("vcT", [128, T], BF16, kind="ExternalInput").ap()
    kslT_d = nc.dram_tensor("kslT", [128, T], BF16, kind="ExternalInput").ap()
    kwT_d = nc.dram_tensor("kwT", [128, T], BF16, kind="ExternalInput").ap()
    vsl_d = nc.dram_tensor("vsl", [T, 128], BF16, kind="ExternalInput").ap()
    vw_d = nc.dram_tensor("vw", [T, 128], BF16, kind="ExternalInput").ap()
    gates_d = nc.dram_tensor("gates", [T, 12], F32, kind="ExternalInput").ap()
    w1_d = nc.dram_tensor("w1", [2, 4096, 512], F32, kind="ExternalInput").ap()
    w2_d = nc.dram_tensor("w2", [2, 512, 128], F32, kind="ExternalInput").ap()
    peT_d = nc.dram_tensor("peT", [2, 128, 32], F32, kind="ExternalInput").ap()
    ov_d = nc.dram_tensor("ov", [2, 128, 65], F32, kind="ExternalInput").ap()
    maskc_d = nc.dram_tensor("maskc", [128, 32 * 2 * 128], BF16, kind="ExternalInput").ap()
    sm_d = nc.dram_tensor("sm", [2, 128, 32 * 64], F32, kind="ExternalInput").ap()
    ind_d = nc.dram_tensor("ind", [64, 32 * 128], BF16, kind="ExternalInput").ap()
    cbias_d = nc.dram_tensor("cbias", [128, 2 * 512], BF16, kind="ExternalInput").ap()
    identb_d = nc.dram_tensor("identb", [128, 128], BF16, kind="ExternalInput").ap()
    identf_d = nc.dram_tensor("identf", [128, 128], F32, kind="ExternalInput").ap()
    o_d = nc.dram_tensor("o", [T, 512], BF16, kind="ExternalOutput").ap()
    with contextlib.ExitStack() as es:
        S = Sched(nc, es)
        sb = lambda name, shape, dt: es.enter_context(nc.sbuf_tensor(name + "_s", shape, dt))
        banks = [es.enter_context(nc.psum_tensor("bk%d" % i, [128, 512], F32)) for i in range(4)]
        accs = [es.enter_context(nc.psum_tensor("acc%d" % i, [128, 4, 256], F32)) for i in range(2)]
        ACK = [('bk', 'A'), ('bk', 'B')]
        nbk = [0]

        def newbank(n=3):
            i = nbk[0] % n
            nbk[0] += 1
            return banks[i], ('bk', i)

        kcmpT = sb("kcmpT", [128, 256], BF16)
        Rext = sb("Rext", [128, 2, 193], BF16)
        IDB = sb("IDB", [128, 128], BF16)
        IDF = sb("IDF", [128, 128], F32)
        S.dma('sp', IDB[:, :], identb_d[:, :], w=['IDB'])
        S.dma('sp', IDF[:, :], identf_d[:, :], w=['IDF'])
        S.op('dve', lambda e: e.memset(kcmpT[:, :], 0.0), w=['kcmpT'])
        for cc in range(2):
            S.dma('pool', Rext[:, cc, 128:193], ov_d[cc], w=['Rext'])
        with contextlib.ExitStack() as es0:
            sb0 = lambda name, shape, dt: es0.enter_context(nc.sbuf_tensor(name + "_s", shape, dt))
            srcT = [sb0("kcT", [128, T], BF16), sb0("vcT", [128, T], BF16)]
            S.dma('sp', srcT[0][:, :], kcT_d[:, :], w=['srcT0'])
            S.dma('sp', srcT[1][:, :], vcT_d[:, :], w=['srcT1'])
            w1b = [sb0("w1b%d" % i, [128, 32, 128], BF16) for i in range(2)]
            w2b = [sb0("w2b%d" % i, [128, 4, 128], BF16) for i in range(2)]
            peb = [sb0("peb%d" % i, [128, 32], BF16) for i in range(2)]
            hid = [sb0("hid%d" % i, [128, 4, 256], BF16) for i in range(2)]
            biasv = sb0("biasv", [128, 8], F32)
            nw1 = 0
            for kv in range(2):
                S.dma('pool', w2b[kv][:, :, :], w2_d[kv].rearrange("(hc p) d -> p hc d", p=128), w=['w2b%d' % kv])
                S.dma('pool', peb[kv][:, :], peT_d[kv], w=['peb%d' % kv])
                S.op('dve', lambda e, kv=kv: e.memset(hid[kv][:, :, :], 0.0), w=['hid%d' % kv])
                for hc in range(4):
                    wi = nw1 % 2
                    nw1 += 1
                    for half in range(2):
                        S.dma('pool', w1b[wi][:, half * 16:(half + 1) * 16, :],
                              w1_d[kv, half * 2048:(half + 1) * 2048, hc * 128:(hc + 1) * 128].rearrange("(l d) f -> d l f", d=128),
                              w=[('w1b', wi)])
                    bk, bkk = newbank()
                    for l in range(32):
                        S.op('pe', lambda e, bk=bk, l=l, wi=wi, kv=kv: e.matmul(bk[:, 0:1], w1b[wi][:, l, :], peb[kv][:, l:l + 1],
                                                                              start=(l == 0), stop=(l == 31)), r=[('w1b', wi), 'peb%d' % kv], w=[bkk])
                    S.op('act', lambda e, bk=bk, kv=kv, hc=hc: e.copy(out=biasv[:, kv * 4 + hc:kv * 4 + hc + 1], in_=bk[:, 0:1]), r=[bkk], w=['biasv'])
                    bk, bkk = newbank()
                    for l in range(32):
                        S.op('pe', lambda e, bk=bk, l=l, wi=wi, kv=kv: e.matmul(bk[:, 0:255], w1b[wi][:, l, :], srcT[kv][:, l:l + 16 * 254 + 1:16],
                                                                              start=(l == 0), stop=(l == 31)), r=[('w1b', wi), 'srcT%d' % kv], w=[bkk])
                    S.op('act', lambda e, bk=bk, kv=kv, hc=hc: e.activation(out=hid[kv][:, hc, 0:255], in_=bk[:, 0:255], func=AF.Silu,
                                                                          bias=biasv[:, kv * 4 + hc:kv * 4 + hc + 1]), r=[bkk, 'biasv'], w=['hid%d' % kv])
            bk, bkk = newbank()
            for hc in range(4):
                S.op('pe', lambda e, bk=bk, hc=hc: e.matmul(bk[:, 0:255], w2b[0][:, hc, :], hid[0][:, hc, 0:255], start=(hc == 0), stop=(hc == 3)),
                     r=['w2b0', 'hid0'], w=[bkk])
            S.op('act', lambda e, bk=bk: e.copy(out=kcmpT[:, 0:255], in_=bk[:, 0:255]), r=[bkk], w=['kcmpT'])
            for cc in range(2):
                bk, bkk = newbank()
                for hc in range(4):
                    S.op('pe', lambda e, bk=bk, hc=hc, cc=cc: e.matmul(bk[:, 0:128], hid[1][:, hc, cc * 128:(cc + 1) * 128], w2b[1][:, hc, :],
                                                                     start=(hc == 0), stop=(hc == 3)), r=['w2b1', 'hid1'], w=[bkk])
                S.op('act', lambda e, bk=bk, cc=cc: e.copy(out=Rext[:, cc, 0:128], in_=bk[:, 0:128]), r=[bkk], w=['Rext'])
            S.sync_all()
        qn = sb("qn", [128, 32, 512], BF16)
        qr = sb("qr", [128, 32, 512], BF16)
        kslT = sb("kslT", [128, T], BF16)
        kwT = sb("kwT", [128, T], BF16)
        vsl = sb("vsl", [128, 32, 129], BF16)
        vw = sb("vw", [128, 32, 129], BF16)
        gts = sb("gts", [128, 32, 12], F32)
        maskc = sb("maskc", [128, 32, 2, 128], BF16)
        sm1 = sb("sm1", [128, 32, 64], F32)
        sm2 = sb("sm2", [128, 32, 64], F32)
        ind = sb("ind", [64, 32, 128], BF16)
        cbias = sb("cbias", [128, 2, 512], BF16)
        for half in range(2):
            hs = slice(half * 16, (half + 1) * 16)
            S.dma('sp', qn[:, hs, :], qn_d[:, half * 8192:(half + 1) * 8192].rearrange("p (a b) -> p a b", b=512), w=['qn'])
            S.dma('sp', qr[:, hs, :], qr_d[:, half * 8192:(half + 1) * 8192].rearrange("p (a b) -> p a b", b=512), w=['qr'])
        S.dma('sp', kslT[:, :], kslT_d[:, :], w=['kslT'])
        S.dma('sp', kwT[:, :], kwT_d[:, :], w=['kwT'])
        S.op('dve', lambda e: e.memset(vsl[:, :, 128:129], 1.0), w=['vsl'])
        S.op('dve', lambda e: e.memset(vw[:, :, 128:129], 1.0), w=['vw'])
        S.dma('sp', vsl[:, :, 0:128], vsl_d.rearrange("(kt p) d -> p kt d", p=128), w=['vsl'])
        S.dma('sp', vw[:, :, 0:128], vw_d.rearrange("(kt p) d -> p kt d", p=128), w=['vw'])
        S.dma('sp', gts[:, :, :], gates_d.rearrange("(qt p) g -> p qt g", p=128), w=['gts'])
        S.dma('sp', maskc[:, :, :, :], maskc_d.rearrange("p (a b c) -> p a b c", a=32, b=2), w=['maskc'])
        S.dma('sp', sm1[:, :, :], sm_d[0].rearrange("p (a b) -> p a b", b=64), w=['sm1'])
        S.dma('sp', sm2[:, :, :], sm_d[1].rearrange("p (a b) -> p a b", b=64), w=['sm2'])
        S.dma('sp', ind[:, :, :], ind_d.rearrange("p (a b) -> p a b", b=128), w=['ind'])
        S.dma('sp', cbias[:, :, :], cbias_d.rearrange("p (a b) -> p a b", b=512), w=['cbias'])
        Ec = [sb("Ec%d" % i, [128, 4, 128], BF16) for i in range(2)]
        Es = [sb("Es%d" % i, [128, 512], BF16) for i in range(3)]
        den = sb("den", [128, 4], F32)
        rec = sb("rec", [128, 4], F32)
        rg = sb("rg", [128, 4], F32)
        pn = sb("pn", [128, 4, 64], F32)
        pslc = sb("pslc", [128, 64], F32)
        score = sb("score", [128, 64], F32)
        work = sb("work", [128, 64], F32)
        m8a = sb("m8a", [128, 8], F32)
        m8b = sb("m8b", [128, 8], F32)
        thr = sb("thr", [128, 1], F32)
        selm = sb("selm", [128, 64], F32)
        selmT = sb("selmT", [64, 128], BF16)
        oacc = sb("oacc", [128, 4, 128], F32)
        otmp = sb("otmp", [128, 4, 128], F32)
        ob = [sb("ob%d" % i, [128, 4, 128], BF16) for i in range(2)]
        nes = [0]

        def bc(ap2):
            return ap2.unsqueeze(2).to_broadcast([128, 4, 128])

        def attend(qt, kts, kT, kTk, vext, vk, acc, ack, use_sel):
            for idx, kt in enumerate(kts):
                bk, bkk = newbank()
                diag = (kt == qt)
                far = (not use_sel) and (kt == qt - 4)
                more = use_sel or diag or far
                S.op('pe', lambda e, bk=bk, kt=kt: e.matmul(bk[:, :], kT[:, kt * 128:(kt + 1) * 128], qr[:, qt, :], start=True, stop=not more),
                     r=[kTk, 'qr'], w=[bkk])
                if use_sel:
                    for h in range(4):
                        S.op('pe', lambda e, bk=bk, kt=kt, h=h: e.matmul(bk[:, h * 128:(h + 1) * 128], ind[:, kt, :], selmT[:, :], start=False,
                                                                       stop=(h == 3 and not diag)), r=['ind', 'selmT'], w=[bkk])
                if diag:
                    S.op('pe', lambda e, bk=bk: e.matmul(bk[:, :], IDB[:, :], cbias[:, 0, :], start=False, stop=not far), r=['IDB', 'cbias'], w=[bkk])
                if far:
                    S.op('pe', lambda e, bk=bk: e.matmul(bk[:, :], IDB[:, :], cbias[:, 1, :], start=False, stop=True), r=['IDB', 'cbias'], w=[bkk])
                ei = nes[0] % 3
                nes[0] += 1
                S.op('act', lambda e, bk=bk, ei=ei: e.activation(out=Es[ei][:, :], in_=bk[:, :], func=AF.Exp, scale=SCL), r=[bkk], w=[('Es', ei)])
                for h in range(4):
                    S.op('pe', lambda e, h=h, ei=ei, kt=kt, idx=idx: e.matmul(acc[:, h, 0:129], Es[ei][:, h * 128:(h + 1) * 128], vext[:, kt, :],
                                                                            start=(idx == 0 and h % 2 == 0), stop=(idx == len(kts) - 1)),
                         r=[('Es', ei), vk], w=[ack])

        def finish_branch(acc, ack, gcol, first):
            S.op('dve', lambda e: e.reciprocal(out=rec[:, :], in_=acc[:, :, 128]), r=[ack], w=['rec'])
            S.op('dve', lambda e: e.tensor_tensor(out=rg[:, :], in0=rec[:, :], in1=gcol, op=ALU.mult), r=['rec', 'gts'], w=['rg'])
            if first:
                S.op('dve', lambda e: e.tensor_tensor(out=oacc[:, :, :], in0=acc[:, :, 0:128], in1=bc(rg[:, :]), op=ALU.mult), r=[ack, 'rg'], w=['oacc'])
            else:
                S.op('dve', lambda e: e.tensor_tensor(out=otmp[:, :, :], in0=acc[:, :, 0:128], in1=bc(rg[:, :]), op=ALU.mult), r=[ack, 'rg'], w=['otmp'])
                S.op('pool', lambda e: e.tensor_tensor(out=oacc[:, :, :], in0=oacc[:, :, :], in1=otmp[:, :, :], op=ALU.add), r=['otmp', 'oacc'], w=['oacc'])

        for qt in range(NQT):
            ccs = [0] if qt < 16 else [0, 1]
            A, AK = accs[0], ACK[0]
            for cc in ccs:
                bk, bkk = newbank()
                S.op('pe', lambda e, bk=bk, cc=cc: e.matmul(bk[:, :], kcmpT[:, cc * 128:(cc + 1) * 128], qn[:, qt, :], start=True, stop=True),
                     r=['kcmpT', 'qn'], w=[bkk])
                S.op('act', lambda e, bk=bk, cc=cc: e.activation(out=Ec[cc][:, :, :], in_=bk[:, :].rearrange("p (h q) -> p h q", h=4), func=AF.Exp, scale=SCL),
                     r=[bkk], w=[('Ec', cc)])
                S.op('dve', lambda e, cc=cc: e.tensor_tensor(out=Ec[cc][:, :, :], in0=Ec[cc][:, :, :],
                                                            in1=maskc[:, qt, cc, :].unsqueeze(1).to_broadcast([128, 4, 128]), op=ALU.mult),
                     r=[('Ec', cc), 'maskc'], w=[('Ec', cc)])
            for ci, cc in enumerate(ccs):
                for h in range(4):
                    S.op('pe', lambda e, h=h, cc=cc, ci=ci: e.matmul(A[:, h, 0:193], Ec[cc][:, h, :], Rext[:, cc, :],
                                                                   start=(ci == 0 and h % 2 == 0), stop=(ci == len(ccs) - 1)),
                         r=[('Ec', cc), 'Rext'], w=[AK])
            S.op('dve', lambda e: e.tensor_scalar(out=den[:, :], in0=A[:, :, 192], scalar1=1e-30, scalar2=None, op0=ALU.max), r=[AK], w=['den'])
            S.op('dve', lambda e: e.reciprocal(out=rec[:, :], in_=den[:, :]), r=['den'], w=['rec'])
            S.op('dve', lambda e: e.tensor_tensor(out=pn[:, :, :], in0=A[:, :, 128:192], in1=rec[:, :].unsqueeze(2).to_broadcast([128, 4, 64]), op=ALU.mult),
                 r=[AK, 'rec'], w=['pn'])
            S.op('dve', lambda e: e.tensor_reduce(out=pslc[:, :], in_=pn[:, :, :].rearrange("p h s -> p s h"), axis=AX.X, op=ALU.add), r=['pn'], w=['pslc'])
            S.op('dve', lambda e: e.tensor_tensor(out=rg[:, :], in0=rec[:, :], in1=gts[:, qt, 0:4], op=ALU.mult), r=['rec', 'gts'], w=['rg'])
            S.op('dve', lambda e: e.tensor_tensor(out=oacc[:, :, :], in0=A[:, :, 0:128], in1=bc(rg[:, :]), op=ALU.mult), r=[AK, 'rg'], w=['oacc'])
            S.op('dve', lambda e: e.tensor_tensor(out=score[:, :], in0=pslc[:, :], in1=sm1[:, qt, :], op=ALU.mult), r=['pslc', 'sm1'], w=['score'])
            S.op('dve', lambda e: e.tensor_tensor(out=score[:, :], in0=score[:, :], in1=sm2[:, qt, :], op=ALU.add), r=['score', 'sm2'], w=['score'])
            S.op('dve', lambda e: e.max(out=m8a[:, :], in_=score[:, :]), r=['score'], w=['m8a'])
            S.op('dve', lambda e: e.match_replace(out=work[:, :], in_to_replace=m8a[:, :], in_values=score[:, :], imm_value=-2.0), r=['score', 'm8a'], w=['work'])
            S.op('dve', lambda e: e.max(out=m8b[:, :], in_=work[:, :]), r=['work'], w=['m8b'])
            S.op('dve', lambda e: e.tensor_scalar(out=thr[:, :], in0=m8b[:, 7:8], scalar1=0.0, scalar2=None, op0=ALU.max), r=['m8b'], w=['thr'])
            S.op('dve', lambda e: e.tensor_scalar(out=selm[:, :], in0=score[:, :], scalar1=thr[:, 0:1], scalar2=None, op0=ALU.is_ge), r=['score', 'thr'], w=['selm'])
            S.op('dve', lambda e: e.tensor_scalar(out=selm[:, :], in0=selm[:, :], scalar1=-NEGB, scalar2=NEGB, op0=ALU.mult, op1=ALU.add), r=['selm'], w=['selm'])
            bk, bkk = banks[3], ('bk', 3)
            S.op('pe', lambda e, bk=bk: e.transpose(bk[0:64, 0:128], selm[:, :], IDF[:, :]), r=['selm', 'IDF'], w=[bkk])
            S.op('act', lambda e, bk=bk: e.copy(out=selmT[:, :], in_=bk[0:64, 0:128]), r=[bkk], w=['selmT'])
            attend(qt, list(range(0, qt + 1)), kslT, 'kslT', vsl, 'vsl', accs[1], ACK[1], True)
            finish_branch(accs[1], ACK[1], gts[:, qt, 4:8], False)
            attend(qt, list(range(max(0, qt - 4), qt + 1)), kwT, 'kwT', vw, 'vw', accs[0], ACK[0], False)
            finish_branch(accs[0], ACK[0], gts[:, qt, 8:12], False)
            oi = qt % 2
            S.op('act', lambda e, oi=oi: e.copy(out=ob[oi][:, :, :], in_=oacc[:, :, :]), r=['oacc'], w=[('ob', oi)])
            S.dma('sp', o_d[qt * 128:(qt + 1) * 128, :], ob[oi][:, :, :].rearrange("p h d -> p (h d)"), r=[('ob', oi)])
        S.finish()
    return nc


def nsa_consts():
    p = np.arange(128)
    c_all = np.arange(256)
    maskc = np.zeros((128, 32, 2, 128), np.float32)
    for qt in range(32):
        t = qt * 128 + p
        for cc in range(2):
            c = cc * 128 + p
            maskc[:, qt, cc, :] = ((16 * c[:, None] + 31 <= t[None, :]) & (c[:, None] < 255))
    blk = np.arange(64)
    sm = np.zeros((2, 128, 32, 64), np.float32)
    for qt in range(32):
        t = qt * 128 + p
        cur = (t // 64)[:, None]
        causal = blk[None, :] <= cur
        forced = (blk[None, :] == 0) | (causal & (blk[None, :] > cur - 2))
        sm[0, :, qt, :] = (causal & ~forced)
        sm[1, :, qt, :] = np.where(forced, 1e6, np.where(causal, 0.0, -1.0))
    ind = np.zeros((64, 32, 128), np.float32)
    for kt in range(32):
        ind[2 * kt + p // 64, kt, p] = 1.0
    cb = np.zeros((128, 2, 4, 128), np.float32)
    cb[:, 0] = np.where(p[:, None] > p[None, :], NEGB, 0.0)[:, None, :]
    cb[:, 1] = np.where(p[:, None] <= p[None, :], NEGB, 0.0)[:, None, :]
    c0 = np.arange(255) * 16
    s0 = np.arange(64) * 64
    ovm = np.clip(np.minimum(c0[:, None] + 32, s0[None, :] + 64) - np.maximum(c0[:, None], s0[None, :]), 0, None) / 32.0
    ov = np.zeros((256, 65), np.float32)
    ov[:255, :64] = ovm
    ov[:255, 64] = 1.0
    return dict(maskc=maskc.reshape(128, -1).astype(NPBF), sm=sm.reshape(2, 128, -1), ind=ind.reshape(64, -1).astype(NPBF),
                cbias=cb.reshape(128, -1).astype(NPBF), ov=ov.reshape(2, 128, 65),
                identb=np.eye(128, dtype=np.float32).astype(NPBF), identf=np.eye(128, dtype=np.float32))


def nsa_inputs(proj, qgate, cmp_w1, cmp_w2, cmp_pe):
    cst = nsa_consts()
    ims = []
    pj = proj.reshape(2, 4096, 7168)
    qg = qgate.reshape(2, 4096, 3, 16)
    peT = np.ascontiguousarray(np.transpose(cmp_pe, (0, 2, 1))).astype(np.float32)
    for c in range(NCORES):
        b, g = c // 4, c % 4
        P = pj[b]

        def sec(s):
            return P[:, s * 512 + g * 128: s * 512 + (g + 1) * 128]

        def qlay(off):
            q = P[:, off + g * 512: off + (g + 1) * 512].reshape(32, 128, 4, 128)
            return np.ascontiguousarray(np.transpose(q, (3, 0, 2, 1))).reshape(128, 32 * 512)
        im = dict(qn=qlay(3072), qr=qlay(5120),
                  kcT=np.ascontiguousarray(sec(0).T), vcT=np.ascontiguousarray(sec(1).T),
                  kslT=np.ascontiguousarray(sec(2).T), kwT=np.ascontiguousarray(sec(4).T),
                  vsl=np.ascontiguousarray(sec(3)), vw=np.ascontiguousarray(sec(5)),
                  gates=np.ascontiguousarray(qg[b][:, :, g * 4:(g + 1) * 4].reshape(4096, 12)),
                  w1=cmp_w1, w2=cmp_w2, peT=peT)
        im.update(cst)
        ims.append(im)
    return ims


def _rope_table():
    pos = np.arange(4096, dtype=np.float32)
    inv = (np.float32(10000.0) ** (-np.arange(64, dtype=np.float32) / np.float32(64))).astype(np.float32)
    ang = (pos[:, None] * inv[None, :]).astype(np.float32)
    return np.concatenate([np.cos(ang), np.sin(ang)], axis=1).astype(np.float32)


def _post1(aT_list, xres, w_out, g, b, router_w, router_bias):
    KIN = w_out.shape[0]
    nc = build_post1(KIN)
    ln_gb = np.stack([g, b]).astype(np.float32)
    ident = np.eye(128, dtype=np.float32)
    rb = router_bias.reshape(1, 32).astype(np.float32)
    ims = [{"aT": aT_list[c], "xres": np.ascontiguousarray(xres[c * 1024:(c + 1) * 1024]), "w_out": w_out, "ln_gb": ln_gb,
            "router_w": router_w, "router_b": rb, "ident": ident} for c in range(NCORES)]
    res = _run(nc, ims)
    x1 = np.concatenate([r["x1"] for r in res], axis=0)
    x1T = np.concatenate([r["x1T"] for r in res], axis=1)
    gates = np.concatenate([r["gates"] for r in res], axis=0)
    return x1, x1T, gates


def _moe(x1T, gates, wg, wu, wd):
    nc = build_moe(8192)
    x1T = np.ascontiguousarray(x1T)
    ims = [{"xT": x1T, "gates_c": np.ascontiguousarray(gates[:, 4 * c:4 * c + 4]), "wg": wg[4 * c:4 * c + 4],
            "wu": wu[4 * c:4 * c + 4], "wd": wd[4 * c:4 * c + 4]} for c in range(NCORES)]
    res = _run(nc, ims)
    return [r["y"] for r in res]


def _post2(ys, x1, g, b, proj_w=None):
    PROJ = proj_w is not None
    nc = build_post2(PROJ)
    ln_gb = np.stack([g, b]).astype(np.float32)
    ims = []
    if PROJ:
        cs = _rope_table()
        ident = np.eye(128, dtype=np.float32)
    for c in range(NCORES):
        rows = slice(c * 1024, (c + 1) * 1024)
        im = {"yp": np.stack([y[rows] for y in ys]), "x1": np.ascontiguousarray(x1[rows]), "ln_gb": ln_gb}
        if PROJ:
            p0 = (c * 1024) % 4096
            im.update({"kv_w": proj_w[0], "w_q": proj_w[1], "cs": np.ascontiguousarray(cs[p0:p0 + 1024]), "ident": ident})
        ims.append(im)
    res = _run(nc, ims)
    x2 = np.concatenate([r["x2"] for r in res], axis=0)
    if PROJ:
        return x2, np.concatenate([r["proj"] for r in res], axis=0), np.concatenate([r["qgate"] for r in res], axis=0)
    return x2


def kernel(x, a_w_in, a_conv_w, a_a_log, a_dt_bias, a_norm_w, a_w_out, kv_w, cmp_pe, cmp_w1, cmp_w2,
           b_w_q, b_w_out, router_w, router_bias, moe_w_gate, moe_w_up, moe_w_down, ln_g, ln_b):
    f = lambda a: np.asarray(a, dtype=np.float32)
    x, a_w_in, a_conv_w, a_a_log, a_dt_bias, a_norm_w, a_w_out = map(f, (x, a_w_in, a_conv_w, a_a_log, a_dt_bias, a_norm_w, a_w_out))
    kv_w, cmp_pe, cmp_w1, cmp_w2, b_w_q, b_w_out, router_w, router_bias = map(f, (kv_w, cmp_pe, cmp_w1, cmp_w2, b_w_q, b_w_out, router_w, router_bias))
    moe_w_gate, moe_w_up, moe_w_down, ln_g, ln_b = map(f, (moe_w_gate, moe_w_up, moe_w_down, ln_g, ln_b))
    xf = x.reshape(8192, D)
    res = _run(build_gdn_a(), gdn_a_inputs(x, a_w_in[0], a_conv_w[0]))
    res = _run(build_gdn_b(32), gdn_b_inputs([r["qkvT"] for r in res], [r["zT"] for r in res], [r["abT"] for r in res],
                                            a_a_log[0], a_dt_bias[0], a_norm_w[0]))
    og = np.zeros((2, 4096, 4096), dtype=NPBF)
    for c in range(NCORES):
        og[c // 4, :, (c % 4) * 1024:(c % 4 + 1) * 1024] = res[c]["og"]
    ogf = og.reshape(8192, 4096)
    aT = [np.ascontiguousarray(ogf[c * 1024:(c + 1) * 1024].T) for c in range(NCORES)]
    x1, x1T, gates = _post1(aT, xf, a_w_out[0], ln_g[0, 0], ln_b[0, 0], router_w, router_bias)
    ys = _moe(x1T, gates, moe_w_gate[0], moe_w_up[0], moe_w_down[0])
    x2, proj, qgate = _post2(ys, x1, ln_g[0, 1], ln_b[0, 1], (kv_w, b_w_q[0]))
    del ys
    res = _run(build_nsa(32), nsa_inputs(proj, qgate, cmp_w1, cmp_w2, cmp_pe))
    o = np.zeros((2, 4096, 2048), dtype=NPBF)
    for c in range(NCORES):
        o[c // 4, :, (c % 4) * 512:(c % 4 + 1) * 512] = res[c]["o"]
    of = o.reshape(8192, 2048)
    aT = [np.ascontiguousarray(of[c * 1024:(c + 1) * 1024].T) for c in range(NCORES)]
    x3, x3T, gates = _post1(aT, x2, b_w_out[0], ln_g[1, 0], ln_b[1, 0], router_w, router_bias)
    ys = _moe(x3T, gates, moe_w_gate[1], moe_w_up[1], moe_w_down[1])
    x4 = _post2(ys, x3, ln_g[1, 1], ln_b[1, 1], None)
    return x4.reshape(2, 4096, D).astype(np.float32)
```

```python
import contextlib
import os
import numpy as np
import ml_dtypes
import concourse.bass as bass
import concourse.mybir as mybir
from concourse.bass_utils import run_bass_kernel_spmd

F32 = mybir.dt.float32
BF16 = mybir.dt.bfloat16
AF = mybir.ActivationFunctionType
ALU = mybir.AluOpType
AX = mybir.AxisListType
NPBF = ml_dtypes.bfloat16

NCORES = 8
D = 2048
ALPHA = 4.0 ** 0.25
LN_EPS = 1e-5
RMS_EPS = 1e-6


class Sched:
    LIMIT = 30000
    NDMA = 16

    def __init__(self, nc, es):
        self.nc, self.es = nc, es
        self.eng = {'pe': nc.tensor, 'act': nc.scalar, 'dve': nc.vector, 'pool': nc.gpsimd, 'sp': nc.sync}
        self.sems = []
        self.cur = {}
        self.known = {e: {} for e in self.eng}
        self.lastw = {}
        self.readers = {}
        self.pe_sids = set()
        for e in ('pe', 'act', 'dve', 'pool'):
            self.cur[e] = [self._newsem(e), 0]
        self.pe_sids.add(self.cur['pe'][0])
        self.dma_slots = [[self._newsem('dma%d' % i), 0] for i in range(self.NDMA)]
        self.dma_rr = 0
        self.rec = None

    def record(self):
        self.rec = []

    def stop(self):
        l, self.rec = self.rec, None
        return l

    def play(self, items):
        for kind, a, kw in items:
            (self.op if kind == 'op' else self.dma)(*a, **kw)

    def play_interleaved(self, la, lb):
        na, nb_ = len(la), len(lb)
        ia = ib = 0
        while ia < na or ib < nb_:
            if ib >= nb_ or (ia < na and ia * nb_ <= ib * na):
                self.play([la[ia]])
                ia += 1
            else:
                self.play([lb[ib]])
                ib += 1

    def _newsem(self, name):
        s = self.es.enter_context(self.nc.semaphore('%s_%d' % (name, len(self.sems))))
        self.sems.append(s)
        return len(self.sems) - 1

    def _wait(self, e, deps):
        for sid, val in deps.items():
            if self.known[e].get(sid, 0) < val:
                self.eng[e].wait_ge(self.sems[sid], val)
                self.known[e][sid] = val

    def _deps(self, e, r, w):
        d = {}

        def add(sid, val):
            if d.get(sid, 0) < val:
                d[sid] = val
        for k in r:
            ev = self.lastw.get(k)
            if ev is not None:
                add(*ev)
        for k in w:
            ev = self.lastw.get(k)
            if ev is not None:
                add(*ev)
            for sid, val in self.readers.get(k, {}).items():
                add(sid, val)
        if e == 'pe':
            for sid in list(d):
                if sid in self.pe_sids:
                    del d[sid]
        return d

    def _record(self, ev, r, w):
        sid, val = ev
        for k in r:
            rd = self.readers.setdefault(k, {})
            if rd.get(sid, 0) < val:
                rd[sid] = val
        for k in w:
            self.lastw[k] = ev
            self.readers[k] = {}

    def op(self, e, fn, r=(), w=()):
        if self.rec is not None:
            self.rec.append(('op', (e, fn), dict(r=r, w=w)))
            return
        w = list(w) + [k for k in r if isinstance(k, tuple) and k[0] == 'bk']
        self._wait(e, self._deps(e, r, w))
        ins = fn(self.eng[e])
        c = self.cur[e]
        if c[1] >= self.LIMIT:
            c[0] = self._newsem(e)
            c[1] = 0
            if e == 'pe':
                self.pe_sids.add(c[0])
        c[1] += 1
        ins.then_inc(self.sems[c[0]], 1)
        self._record((c[0], c[1]), r, w)

    def dma(self, q, out, in_, r=(), w=(), **kw):
        if self.rec is not None:
            self.rec.append(('dma', (q, out, in_), dict(r=r, w=w, **kw)))
            return
        slot = self.dma_slots[self.dma_rr]
        self.dma_rr = (self.dma_rr + 1) % self.NDMA
        d = self._deps(q, r, w)
        if slot[1] > 0:
            d[slot[0]] = max(d.get(slot[0], 0), 16 * slot[1])
        self._wait(q, d)
        ins = self.eng[q].dma_start(out=out, in_=in_, **kw)
        slot[1] += 1
        ins.then_inc(self.sems[slot[0]], 16)
        self._record((slot[0], 16 * slot[1]), r, w)

    def _all_events(self):
        d = {slot[0]: 16 * slot[1] for slot in self.dma_slots if slot[1] > 0}
        for e, c in self.cur.items():
            if c[1] > 0:
                d[c[0]] = c[1]
        return d

    def sync_all(self):
        d = self._all_events()
        for e in self.eng:
            self._wait(e, d)

    def finish(self):
        self._wait('sp', self._all_events())


def _mk():
    return bass.Bass("TRN2", target_bir_lowering=False)


def _run(nc, in_maps):
    if os.environ.get("MK_TRACE"):
        res = run_bass_kernel_spmd(nc, in_maps, core_ids=list(range(NCORES)), trace=True)
        print("MK_TRACE exec_time_ns", res.exec_time_ns, flush=True)
        return res.results
    res = run_bass_kernel_spmd(nc, in_maps, core_ids=list(range(NCORES)))
    return res.results


def _layer_norm_tile(S, es, nc, h, tt, gbc, bbc, tmp):
    hk = ('h', tt)
    st, mv, rs = tmp['st'], tmp['mv'], tmp['rs']
    for c in range(4):
        S.op('dve', lambda e, c=c: e.bn_stats(out=st[:, c, :], in_=h[:, tt, c * 512:(c + 1) * 512]),
             r=[hk], w=['ln_st'])
    S.op('dve', lambda e: e.bn_aggr(out=mv[:, :], in_=st[:, :, :]), r=['ln_st'], w=['ln_mv'])
    S.op('act', lambda e: e.activation(out=rs[:, :], in_=mv[:, 1:2], func=AF.Sqrt, bias=tmp['eps'][:, 0:1]),
         r=['ln_mv', 'ln_eps'], w=['ln_rs'])
    S.op('dve', lambda e: e.reciprocal(out=rs[:, :], in_=rs[:, :]), r=['ln_rs'], w=['ln_rs'])
    S.op('dve', lambda e: e.tensor_scalar(out=h[:, tt, :], in0=h[:, tt, :], scalar1=mv[:, 0:1], scalar2=rs[:, 0:1],
                                          op0=ALU.subtract, op1=ALU.mult), r=[hk, 'ln_mv', 'ln_rs'], w=[hk])
    S.op('pool', lambda e: e.tensor_tensor(out=h[:, tt, :], in0=h[:, tt, :], in1=gbc[:, :], op=ALU.mult),
         r=[hk, 'gbc'], w=[hk])
    S.op('pool', lambda e: e.tensor_tensor(out=h[:, tt, :], in0=h[:, tt, :], in1=bbc[:, :], op=ALU.add),
         r=[hk, 'bbc'], w=[hk])


def build_post1(KIN):
    nc = _mk()
    KC = KIN // 128
    NT = 8
    aT = nc.dram_tensor("aT", [KIN, 1024], BF16, kind="ExternalInput").ap()
    xres = nc.dram_tensor("xres", [1024, D], F32, kind="ExternalInput").ap()
    w_out = nc.dram_tensor("w_out", [KIN, D], F32, kind="ExternalInput").ap()
    ln_gb = nc.dram_tensor("ln_gb", [2, D], F32, kind="ExternalInput").ap()
    router_w = nc.dram_tensor("router_w", [D, 32], F32, kind="ExternalInput").ap()
    router_b = nc.dram_tensor("router_b", [1, 32], F32, kind="ExternalInput").ap()
    ident_in = nc.dram_tensor("ident", [128, 128], F32, kind="ExternalInput").ap()
    x1 = nc.dram_tensor("x1", [1024, D], F32, kind="ExternalOutput").ap()
    x1T = nc.dram_tensor("x1T", [D, 1024], BF16, kind="ExternalOutput").ap()
    gates = nc.dram_tensor("gates", [1024, 32], F32, kind="ExternalOutput").ap()
    DC = 256
    with contextlib.ExitStack() as es:
        S = Sched(nc, es)
        sb = lambda name, shape, dt: es.enter_context(nc.sbuf_tensor(name, shape, dt))
        h = sb("h", [128, NT, D], F32)
        banks = [es.enter_context(nc.psum_tensor("bk%d" % i, [128, 512], F32)) for i in range(8)]
        with contextlib.ExitStack() as es1:
            a_sb = es1.enter_context(nc.sbuf_tensor("a_sb", [128, KC, 1024], BF16))
            wbuf = [es1.enter_context(nc.sbuf_tensor("wb%d" % i, [128, KC, DC], BF16)) for i in range(2)]
            half = KC // 2
            S.dma('sp', a_sb[:, 0:half, :], aT[0:half * 128, :].rearrange("(kc p) t -> p kc t", p=128), w=['a_sb'])
            S.dma('sp', a_sb[:, half:KC, :], aT[half * 128:KIN, :].rearrange("(kc p) t -> p kc t", p=128), w=['a_sb'])
            for tt in range(NT):
                S.dma('sp', h[:, tt, :], xres[tt * 128:(tt + 1) * 128, :], w=[('h', tt)])
            nb = 0
            for dc in range(D // DC):
                wb = wbuf[dc % 2]
                wk = ('wb', dc % 2)
                S.dma('pool', wb[:, :, :], w_out[:, dc * DC:(dc + 1) * DC].rearrange("(kc p) f -> p kc f", p=128), w=[wk])
                for tt in range(NT):
                    bk = banks[nb % 8]
                    bkk = ('bk', nb % 8)
                    nb += 1
                    for kc in range(KC):
                        S.op('pe', lambda e, kc=kc, bk=bk, tt=tt, wb=wb: e.matmul(
                            bk[:, 0:DC], a_sb[:, kc, tt * 128:(tt + 1) * 128], wb[:, kc, :],
                            start=(kc == 0), stop=(kc == KC - 1)), r=['a_sb', wk], w=[bkk])
                    S.op('dve', lambda e, bk=bk, tt=tt, dc=dc: e.scalar_tensor_tensor(
                        out=h[:, tt, dc * DC:(dc + 1) * DC], in0=h[:, tt, dc * DC:(dc + 1) * DC], scalar=ALPHA,
                        in1=bk[:, 0:DC], op0=ALU.mult, op1=ALU.add), r=[bkk, ('h', tt)], w=[('h', tt)])
            S.sync_all()
        gbc = sb("gbc", [128, D], F32)
        bbc = sb("bbc", [128, D], F32)
        rw = sb("rw", [128, 16, 32], F32)
        rb = sb("rb", [128, 32], F32)
        ident = sb("ident_sb", [128, 128], F32)
        tmp = dict(st=sb("ln_st", [128, 4, 6], F32), mv=sb("ln_mv", [128, 2], F32), rs=sb("ln_rs", [128, 1], F32),
                   eps=sb("ln_eps", [128, 1], F32))
        S.op('dve', lambda e: e.memset(tmp['eps'][:, :], LN_EPS), w=['ln_eps'])
        xT32 = sb("xT32", [128, 16, 128], F32)
        xTb = sb("xTb", [128, 16, 128], BF16)
        S.dma('sp', gbc[:, :], ln_gb[0:1, :].partition_broadcast(128), w=['gbc'])
        S.dma('sp', bbc[:, :], ln_gb[1:2, :].partition_broadcast(128), w=['bbc'])
        S.dma('sp', rw[:, :, :], router_w.rearrange("(kc p) e -> p kc e", p=128), w=['rw'])
        S.dma('sp', rb[:, :], router_b[0:1, :].partition_broadcast(128), w=['rb'])
        S.dma('sp', ident[:, :], ident_in[:, :], w=['ident'])
        r_aff = sb("r_aff", [128, 32], F32)
        r_bia = sb("r_bia", [128, 32], F32)
        r_ps = [sb("r_ps%d" % i, [128, 8], F32) for i in range(6)]
        r_gs = sb("r_gs", [128, 8], F32)
        r_gm = sb("r_gm", [128, 1], F32)
        r_gmask = sb("r_gmask", [128, 8], F32)
        r_m1 = sb("r_m1", [128, 8], F32)
        r_eq = sb("r_eq", [128, 32], F32)
        r_tmp = sb("r_tmp", [128, 32], F32)
        r_m2 = sb("r_m2", [128, 8], F32)
        r_sel = sb("r_sel", [128, 32], F32)
        r_den = sb("r_den", [128, 1], F32)
        r_gate = sb("r_gate", [128, 32], F32)
        RT = ['rtmp']
        for tt in range(NT):
            _layer_norm_tile(S, es, nc, h, tt, gbc, bbc, tmp)
            S.dma('sp', x1[tt * 128:(tt + 1) * 128, :], h[:, tt, :], r=[('h', tt)])
            for q4 in range(4):
                bk = banks[q4]
                bkk = ('bk', q4)
                for j in range(4):
                    kc = q4 * 4 + j
                    S.op('pe', lambda e, bk=bk, j=j, kc=kc, tt=tt: e.transpose(
                        bk[:, j * 128:(j + 1) * 128], h[:, tt, kc * 128:(kc + 1) * 128], ident[:, :]),
                        r=[('h', tt), 'ident'], w=[bkk])
                S.op('act', lambda e, bk=bk, q4=q4: e.copy(
                    out=xT32[:, q4 * 4:(q4 + 1) * 4, :], in_=bk[:, :].rearrange("p (j t) -> p j t", j=4)),
                    r=[bkk], w=['xT32'])
                S.op('dve', lambda e, bk=bk, q4=q4: e.tensor_copy(
                    out=xTb[:, q4 * 4:(q4 + 1) * 4, :], in_=bk[:, :].rearrange("p (j t) -> p j t", j=4)),
                    r=[bkk], w=['xTb'])
            S.dma('sp', x1T[:, tt * 128:(tt + 1) * 128].rearrange("(kc p) t -> p kc t", p=128), xTb[:, :, :], r=['xTb'])
            bk = banks[4]
            bkk = ('bk', 4)
            for kc in range(16):
                S.op('pe', lambda e, kc=kc, bk=bk: e.matmul(bk[:, 0:32], xT32[:, kc, :], rw[:, kc, :],
                                                           start=(kc == 0), stop=(kc == 15)),
                     r=['xT32', 'rw'], w=[bkk])
            S.op('act', lambda e, bk=bk: e.activation(out=r_aff[:, :], in_=bk[:, 0:32], func=AF.Sigmoid),
                 r=[bkk], w=['r_aff'])
            S.op('dve', lambda e: e.tensor_tensor(out=r_bia[:, :], in0=r_aff[:, :], in1=rb[:, :], op=ALU.add),
                 r=['r_aff', 'rb'], w=RT)
            b3 = r_bia[:, :].rearrange("p (g i) -> p g i", i=4)
            pairs = [(0, 1), (0, 2), (0, 3), (1, 2), (1, 3), (2, 3)]
            for pi, (i0, i1) in enumerate(pairs):
                S.op('dve', lambda e, pi=pi, i0=i0, i1=i1: e.tensor_tensor(
                    out=r_ps[pi][:, :], in0=b3[:, :, i0], in1=b3[:, :, i1], op=ALU.add), r=RT, w=RT)
            S.op('dve', lambda e: e.tensor_tensor(out=r_gs[:, :], in0=r_ps[0][:, :], in1=r_ps[1][:, :], op=ALU.max), r=RT, w=RT)
            for pi in range(2, 6):
                S.op('dve', lambda e, pi=pi: e.tensor_tensor(out=r_gs[:, :], in0=r_gs[:, :], in1=r_ps[pi][:, :], op=ALU.max), r=RT, w=RT)
            S.op('dve', lambda e: e.tensor_reduce(out=r_gm[:, :], in_=r_gs[:, :], axis=AX.X, op=ALU.max), r=RT, w=RT)
            S.op('dve', lambda e: e.tensor_scalar(out=r_gmask[:, :], in0=r_gs[:, :], scalar1=r_gm[:, 0:1], scalar2=None,
                                                  op0=ALU.is_ge), r=RT, w=RT)
            S.op('dve', lambda e: e.tensor_reduce(out=r_m1[:, :], in_=b3, axis=AX.X, op=ALU.max), r=RT, w=RT)
            S.op('dve', lambda e: e.tensor_tensor(out=r_eq[:, :].rearrange("p (g i) -> p g i", i=4), in0=b3,
                                                  in1=r_m1[:, :].unsqueeze(2).to_broadcast([128, 8, 4]), op=ALU.is_equal), r=RT, w=RT)
            S.op('dve', lambda e: e.scalar_tensor_tensor(out=r_tmp[:, :], in0=r_eq[:, :], scalar=-1e30, in1=r_bia[:, :],
                                                         op0=ALU.mult, op1=ALU.add), r=RT, w=RT)
            S.op('dve', lambda e: e.tensor_reduce(out=r_m2[:, :], in_=r_tmp[:, :].rearrange("p (g i) -> p g i", i=4),
                                                  axis=AX.X, op=ALU.max), r=RT, w=RT)
            S.op('dve', lambda e: e.tensor_tensor(out=r_sel[:, :].rearrange("p (g i) -> p g i", i=4), in0=b3,
                                                  in1=r_m2[:, :].unsqueeze(2).to_broadcast([128, 8, 4]), op=ALU.is_ge), r=RT, w=RT)
            S.op('dve', lambda e: e.tensor_tensor(out=r_sel[:, :].rearrange("p (g i) -> p g i", i=4),
                                                  in0=r_sel[:, :].rearrange("p (g i) -> p g i", i=4),
                                                  in1=r_gmask[:, :].unsqueeze(2).to_broadcast([128, 8, 4]), op=ALU.mult), r=RT, w=RT)
            S.op('dve', lambda e: e.tensor_tensor(out=r_sel[:, :], in0=r_sel[:, :], in1=r_aff[:, :], op=ALU.mult),
                 r=RT + ['r_aff'], w=RT)
            S.op('dve', lambda e: e.tensor_reduce(out=r_den[:, :], in_=r_sel[:, :], axis=AX.X, op=ALU.add), r=RT, w=RT)
            S.op('dve', lambda e: e.reciprocal(out=r_den[:, :], in_=r_den[:, :]), r=RT, w=RT)
            S.op('dve', lambda e: e.tensor_scalar(out=r_gate[:, :], in0=r_sel[:, :], scalar1=r_den[:, 0:1], scalar2=None,
                                                  op0=ALU.mult), r=RT, w=['r_gate'])
            S.dma('sp', gates[tt * 128:(tt + 1) * 128, :], r_gate[:, :], r=['r_gate'])
        S.finish()
    return nc


def build_moe(NTOK=8192):
    nc = _mk()
    NB = NTOK // 512
    xT = nc.dram_tensor("xT", [D, NTOK], BF16, kind="ExternalInput").ap()
    gates_c = nc.dram_tensor("gates_c", [NTOK, 4], F32, kind="ExternalInput").ap()
    wg = nc.dram_tensor("wg", [4, D, 512], F32, kind="ExternalInput").ap()
    wu = nc.dram_tensor("wu", [4, D, 512], F32, kind="ExternalInput").ap()
    wd = nc.dram_tensor("wd", [4, 512, D], F32, kind="ExternalInput").ap()
    y = nc.dram_tensor("y", [NTOK, D], F32, kind="ExternalOutput").ap()
    with contextlib.ExitStack() as es:
        S = Sched(nc, es)
        sb = lambda name, shape, dt: es.enter_context(nc.sbuf_tensor(name, shape, dt))
        banks = [es.enter_context(nc.psum_tensor("bk%d" % i, [128, 512], F32)) for i in range(8)]
        wg_sb = [sb("wg%d" % i, [128, 16, 512], BF16) for i in range(2)]
        wu_sb = [sb("wu%d" % i, [128, 16, 512], BF16) for i in range(2)]
        wd_sb = [sb("wd%d" % i, [128, 4, D], BF16) for i in range(2)]
        x_sb = [sb("x%d" % i, [128, 16, 512], BF16) for i in range(2)]
        hid = [sb("hid%d" % i, [128, 4, 512], BF16) for i in range(2)]
        sg = [sb("sg%d" % i, [128, 512], F32) for i in range(2)]
        ost = [sb("ost%d" % i, [128, D], F32) for i in range(3)]
        g_sb = sb("g_sb", [128, NTOK // 128, 4], F32)
        S.dma('sp', g_sb[:, :, :], gates_c.rearrange("(tt p) e -> p tt e", p=128), w=['g_sb'])

        def load_w(e):
            b = e % 2
            S.dma('pool', wg_sb[b][:, :, :], wg[e].rearrange("(kc p) f -> p kc f", p=128), w=[('wg', b)])
            S.dma('pool', wu_sb[b][:, :, :], wu[e].rearrange("(kc p) f -> p kc f", p=128), w=[('wu', b)])
            S.dma('pool', wd_sb[b][:, :, :], wd[e].rearrange("(fc p) d -> p fc d", p=128), w=[('wd', b)])

        def load_x(i):
            tb = i % NB
            b = i % 2
            S.dma('sp', x_sb[b][:, :, :], xT[:, tb * 512:(tb + 1) * 512].rearrange("(kc p) t -> p kc t", p=128),
                  w=[('x', b)])

        load_w(0)
        load_x(0)
        it = 0
        ngu = 0
        nd = 0
        nos = 0
        for e in range(4):
            if e + 1 < 4:
                load_w(e + 1)
            wb = e % 2
            for tb in range(NB):
                if it + 1 < 4 * NB:
                    load_x(it + 1)
                xb = it % 2
                hb = it % 2
                for fc in range(4):
                    gb, ub = (0, 1) if ngu % 2 == 0 else (2, 3)
                    ngu += 1
                    for kc in range(16):
                        S.op('pe', lambda en, kc=kc, fc=fc, gb=gb: en.matmul(
                            banks[gb][:, :], wg_sb[wb][:, kc, fc * 128:(fc + 1) * 128], x_sb[xb][:, kc, :],
                            start=(kc == 0), stop=(kc == 15)), r=[('wg', wb), ('x', xb)], w=[('bk', gb)])
                    for kc in range(16):
                        S.op('pe', lambda en, kc=kc, fc=fc, ub=ub: en.matmul(
                            banks[ub][:, :], wu_sb[wb][:, kc, fc * 128:(fc + 1) * 128], x_sb[xb][:, kc, :],
                            start=(kc == 0), stop=(kc == 15)), r=[('wu', wb), ('x', xb)], w=[('bk', ub)])
                    sgi = ngu % 2
                    S.op('act', lambda en, gb=gb, sgi=sgi: en.activation(out=sg[sgi][:, :], in_=banks[gb][:, :], func=AF.Silu),
                         r=[('bk', gb)], w=[('sg', sgi)])
                    S.op('dve', lambda en, ub=ub, sgi=sgi, fc=fc: en.tensor_tensor(
                        out=hid[hb][:, fc, :], in0=sg[sgi][:, :], in1=banks[ub][:, :], op=ALU.mult),
                        r=[('sg', sgi), ('bk', ub)], w=[('hid', hb)])
                for t4 in range(4):
                    tt = tb * 4 + t4
                    oi = nos % 3
                    nos += 1
                    for dc in range(4):
                        db = 4 + nd % 4
                        nd += 1
                        for fc in range(4):
                            S.op('pe', lambda en, fc=fc, dc=dc, db=db, t4=t4: en.matmul(
                                banks[db][:, :], hid[hb][:, fc, t4 * 128:(t4 + 1) * 128], wd_sb[wb][:, fc, dc * 512:(dc + 1) * 512],
                                start=(fc == 0), stop=(fc == 3)), r=[('hid', hb), ('wd', wb)], w=[('bk', db)])
                        if dc % 2 == 0:
                            S.op('act', lambda en, db=db, dc=dc, oi=oi, tt=tt: en.activation(
                                out=ost[oi][:, dc * 512:(dc + 1) * 512], in_=banks[db][:, :], func=AF.Identity,
                                scale=g_sb[:, tt, e:e + 1]), r=[('bk', db), 'g_sb'], w=[('os', oi)])
                        else:
                            S.op('dve', lambda en, db=db, dc=dc, oi=oi, tt=tt: en.tensor_scalar(
                                out=ost[oi][:, dc * 512:(dc + 1) * 512], in0=banks[db][:, :], scalar1=g_sb[:, tt, e:e + 1],
                                scalar2=None, op0=ALU.mult), r=[('bk', db), 'g_sb'], w=[('os', oi)])
                    if e == 0:
                        S.dma('sp', y[tt * 128:(tt + 1) * 128, :], ost[oi][:, :], r=[('os', oi)], w=[('y', tt)])
                    else:
                        S.dma('pool', y[tt * 128:(tt + 1) * 128, :], ost[oi][:, :], r=[('os', oi)], w=[('y', tt)],
                              accum_op=ALU.add)
                it += 1
        S.finish()
    return nc


def build_post2(PROJ):
    nc = _mk()
    NT = 8
    yp = nc.dram_tensor("yp", [8, 1024, D], F32, kind="ExternalInput").ap()
    x1 = nc.dram_tensor("x1", [1024, D], F32, kind="ExternalInput").ap()
    ln_gb = nc.dram_tensor("ln_gb", [2, D], F32, kind="ExternalInput").ap()
    x2 = nc.dram_tensor("x2", [1024, D], F32, kind="ExternalOutput").ap()
    if PROJ:
        kv_w = nc.dram_tensor("kv_w", [D, 3072], F32, kind="ExternalInput").ap()
        w_q = nc.dram_tensor("w_q", [D, 2096], F32, kind="ExternalInput").ap()
        cs = nc.dram_tensor("cs", [1024, 128], F32, kind="ExternalInput").ap()
        ident_in = nc.dram_tensor("ident", [128, 128], F32, kind="ExternalInput").ap()
        proj = nc.dram_tensor("proj", [1024, 7168], BF16, kind="ExternalOutput").ap()
        qgate = nc.dram_tensor("qgate", [1024, 48], F32, kind="ExternalOutput").ap()
    with contextlib.ExitStack() as es:
        S = Sched(nc, es)
        sb = lambda name, shape, dt: es.enter_context(nc.sbuf_tensor(name, shape, dt))
        h = sb("h", [128, NT, D], F32)
        banks = [es.enter_context(nc.psum_tensor("bk%d" % i, [128, 512], F32)) for i in range(8)]
        gbc = sb("gbc", [128, D], F32)
        bbc = sb("bbc", [128, D], F32)
        tmp = dict(st=sb("ln_st", [128, 4, 6], F32), mv=sb("ln_mv", [128, 2], F32), rs=sb("ln_rs", [128, 1], F32),
                   eps=sb("ln_eps", [128, 1], F32))
        S.op('dve', lambda e: e.memset(tmp['eps'][:, :], LN_EPS), w=['ln_eps'])
        S.dma('sp', gbc[:, :], ln_gb[0:1, :].partition_broadcast(128), w=['gbc'])
        S.dma('sp', bbc[:, :], ln_gb[1:2, :].partition_broadcast(128), w=['bbc'])
        with contextlib.ExitStack() as es1:
            stg = [es1.enter_context(nc.sbuf_tensor("stg%d" % i, [128, D], F32)) for i in range(3)]
            ns = 0
            for tt in range(NT):
                S.dma('sp', h[:, tt, :], x1[tt * 128:(tt + 1) * 128, :], w=[('h', tt)])
                for c in range(8):
                    si = ns % 3
                    ns += 1
                    S.dma('sp', stg[si][:, :], yp[c, tt * 128:(tt + 1) * 128, :], w=[('stg', si)])
                    eng = 'dve' if c % 2 == 0 else 'pool'
                    if c == 0:
                        S.op(eng, lambda e, si=si, tt=tt: e.scalar_tensor_tensor(
                            out=h[:, tt, :], in0=h[:, tt, :], scalar=ALPHA, in1=stg[si][:, :], op0=ALU.mult, op1=ALU.add),
                            r=[('stg', si), ('h', tt)], w=[('h', tt)])
                    else:
                        S.op(eng, lambda e, si=si, tt=tt: e.tensor_tensor(
                            out=h[:, tt, :], in0=h[:, tt, :], in1=stg[si][:, :], op=ALU.add),
                            r=[('stg', si), ('h', tt)], w=[('h', tt)])
                _layer_norm_tile(S, es, nc, h, tt, gbc, bbc, tmp)
                S.dma('sp', x2[tt * 128:(tt + 1) * 128, :], h[:, tt, :], r=[('h', tt)])
            S.sync_all()
        if PROJ:
            ident = sb("ident_sb", [128, 128], F32)
            xT = sb("xT", [128, 16, 1024], BF16)
            cs_sb = sb("cs_sb", [128, NT, 128], F32)
            wbuf = [sb("wb%d" % i, [128, 16, 512], BF16) for i in range(2)]
            rsb = [sb("rsb%d" % i, [128, 512], F32) for i in range(2)]
            t1 = [sb("t1_%d" % i, [128, 256], F32) for i in range(2)]
            t2 = [sb("t2_%d" % i, [128, 256], F32) for i in range(2)]
            ob = [sb("ob%d" % i, [128, 512], BF16) for i in range(4)]
            gst = [sb("gst%d" % i, [128, 48], F32) for i in range(2)]
            S.dma('sp', ident[:, :], ident_in[:, :], w=['ident'])
            S.dma('sp', cs_sb[:, :, :], cs.rearrange("(tt p) c -> p tt c", p=128), w=['cs'])
            for tt in range(NT):
                for q4 in range(4):
                    bk = banks[q4]
                    bkk = ('bk', q4)
                    for j in range(4):
                        kc = q4 * 4 + j
                        S.op('pe', lambda e, bk=bk, j=j, kc=kc, tt=tt: e.transpose(
                            bk[:, j * 128:(j + 1) * 128], h[:, tt, kc * 128:(kc + 1) * 128], ident[:, :]),
                            r=[('h', tt), 'ident'], w=[bkk])
                    eng = 'act' if q4 % 2 == 0 else 'dve'
                    if eng == 'act':
                        S.op('act', lambda e, bk=bk, q4=q4, tt=tt: e.copy(
                            out=xT[:, q4 * 4:(q4 + 1) * 4, tt * 128:(tt + 1) * 128],
                            in_=bk[:, :].rearrange("p (j t) -> p j t", j=4)), r=[bkk], w=[('xT', tt)])
                    else:
                        S.op('dve', lambda e, bk=bk, q4=q4, tt=tt: e.tensor_copy(
                            out=xT[:, q4 * 4:(q4 + 1) * 4, tt * 128:(tt + 1) * 128],
                            in_=bk[:, :].rearrange("p (j t) -> p j t", j=4)), r=[bkk], w=[('xT', tt)])
            chunks = []
            for c in range(6):
                chunks.append((kv_w[:, c * 512:(c + 1) * 512], 512, 'rope' if c in (2, 4) else 'plain', c * 512))
            for c in range(4):
                chunks.append((w_q[:, c * 512:(c + 1) * 512], 512, 'q', c * 512))
            chunks.append((w_q[:, 2048:2096], 48, 'gate', 0))
            nb = 0
            no = 0
            nr = 0
            for ci, (wsrc, ncol, kind, c0) in enumerate(chunks):
                wb = wbuf[ci % 2]
                wk = ('wb', ci % 2)
                S.dma('pool', wb[:, :, 0:ncol], wsrc.rearrange("(kc p) f -> p kc f", p=128), w=[wk])
                for tt in range(NT):
                    bi = nb % 8
                    nb += 1
                    bk, bkk = banks[bi], ('bk', bi)
                    for kc in range(16):
                        S.op('pe', lambda e, kc=kc, bk=bk, tt=tt, wb=wb, ncol=ncol: e.matmul(
                            bk[:, 0:ncol], xT[:, kc, tt * 128:(tt + 1) * 128], wb[:, kc, 0:ncol],
                            start=(kc == 0), stop=(kc == 15)), r=[('xT', tt), wk], w=[bkk])
                    rows = slice(tt * 128, (tt + 1) * 128)
                    if kind == 'gate':
                        gi = tt % 2
                        S.op('act', lambda e, bk=bk, gi=gi: e.activation(out=gst[gi][:, :], in_=bk[:, 0:48], func=AF.Sigmoid),
                             r=[bkk], w=[('gst', gi)])
                        S.dma('sp', qgate[rows, :], gst[gi][:, :], r=[('gst', gi)])
                        continue
                    if kind in ('plain', 'q'):
                        oi = no % 4
                        no += 1
                        S.op('act', lambda e, bk=bk, oi=oi: e.copy(out=ob[oi][:, :], in_=bk[:, :]), r=[bkk], w=[('ob', oi)])
                        oc = c0 if kind == 'plain' else 3072 + c0
                        S.dma('sp', proj[rows, oc:oc + 512], ob[oi][:, :], r=[('ob', oi)])
                    if kind in ('rope', 'q'):
                        ri = nr % 2
                        nr += 1
                        oi = no % 4
                        no += 1
                        S.op('dve', lambda e, bk=bk, ri=ri: e.tensor_copy(out=rsb[ri][:, :], in_=bk[:, :]), r=[bkk], w=[('rsb', ri)])
                        rv = rsb[ri][:, :].rearrange("p (h d) -> p h d", h=4)
                        ov = ob[oi][:, :].rearrange("p (h d) -> p h d", h=4)
                        cosb = cs_sb[:, tt, 0:64].unsqueeze(1).to_broadcast([128, 4, 64])
                        sinb = cs_sb[:, tt, 64:128].unsqueeze(1).to_broadcast([128, 4, 64])
                        t1v = t1[ri][:, :].rearrange("p (h d) -> p h d", h=4)
                        t2v = t2[ri][:, :].rearrange("p (h d) -> p h d", h=4)
                        S.op('dve', lambda e, rv=rv, t1v=t1v, cosb=cosb: e.tensor_tensor(out=t1v, in0=rv[:, :, 0:64], in1=cosb, op=ALU.mult),
                             r=[('rsb', ri), 'cs'], w=[('t1', ri)])
                        S.op('dve', lambda e, rv=rv, t1v=t1v, sinb=sinb: e.scalar_tensor_tensor(
                            out=ov[:, :, 0:64], in0=rv[:, :, 64:128], scalar=-1.0, in1=sinb, op0=ALU.mult, op1=ALU.mult),
                            r=[('rsb', ri), 'cs'], w=[('ob', oi)])
                        S.op('dve', lambda e, ov=ov, t1v=t1v: e.tensor_tensor(out=ov[:, :, 0:64], in0=ov[:, :, 0:64], in1=t1v, op=ALU.add),
                             r=[('t1', ri), ('ob', oi)], w=[('ob', oi)])
                        S.op('pool', lambda e, rv=rv, t2v=t2v, cosb=cosb: e.tensor_tensor(out=t2v, in0=rv[:, :, 64:128], in1=cosb, op=ALU.mult),
                             r=[('rsb', ri), 'cs'], w=[('t2', ri)])
                        S.op('pool', lambda e, rv=rv, ov=ov, sinb=sinb: e.tensor_tensor(out=ov[:, :, 64:128], in0=rv[:, :, 0:64], in1=sinb, op=ALU.mult),
                             r=[('rsb', ri), 'cs', ('ob', oi)], w=[('ob', oi)])
                        S.op('pool', lambda e, ov=ov, t2v=t2v: e.tensor_tensor(out=ov[:, :, 64:128], in0=ov[:, :, 64:128], in1=t2v, op=ALU.add),
                             r=[('t2', ri), ('ob', oi)], w=[('ob', oi)])
                        oc = c0 if kind == 'rope' else 5120 + c0
                        S.dma('sp', proj[rows, oc:oc + 512], ob[oi][:, :], r=[('ob', oi)])
        S.finish()
    return nc


def build_gdn_a():
    nc = _mk()
    T = 4096
    HT = 2048
    xT = nc.dram_tensor("xT", [D, T], F32, kind="ExternalInput").ap()
    w_sl = nc.dram_tensor("w_sl", [D, 3088], F32, kind="ExternalInput").ap()
    conv_w = nc.dram_tensor("conv_w", [2048, 4], F32, kind="ExternalInput").ap()
    qkvT = nc.dram_tensor("qkvT", [2048, T], BF16, kind="ExternalOutput").ap()
    zT = nc.dram_tensor("zT", [1024, T], BF16, kind="ExternalOutput").ap()
    abT = nc.dram_tensor("abT", [16, T], F32, kind="ExternalOutput").ap()
    with contextlib.ExitStack() as es:
        S = Sched(nc, es)
        sb = lambda name, shape, dt: es.enter_context(nc.sbuf_tensor(name, shape, dt))
        banks = [es.enter_context(nc.psum_tensor("bk%d" % i, [128, 512], F32)) for i in range(8)]
        x_sb = sb("x_sb", [128, 16, HT], BF16)
        wt = [sb("wt%d" % i, [128, 16, 128], BF16) for i in range(2)]
        P = [sb("P%d" % i, [128, HT + 3], F32) for i in range(2)]
        acc = [sb("acc%d" % i, [128, HT], F32) for i in range(2)]
        ost = [sb("ost%d" % i, [128, HT], BF16) for i in range(2)]
        abst = sb("abst", [16, HT], F32)
        carry = sb("carry", [128, 16, 3], F32)
        cw = sb("cw", [128, 16, 4], F32)
        S.dma('sp', cw[:, :, :], conv_w.rearrange("(ct p) j -> p ct j", p=128), w=['cw'])
        nb = 0
        it = 0
        for hf in range(2):
            for g4 in range(4):
                S.dma('pool', x_sb[:, g4 * 4:(g4 + 1) * 4, :],
                      xT[g4 * 512:(g4 + 1) * 512, hf * HT:(hf + 1) * HT].rearrange("(kc p) t -> p kc t", p=128),
                      w=[('x', g4)])
            xk = [('x', g4) for g4 in range(4)]
            for ct in range(25):
                nch = 128 if ct < 24 else 16
                wi = it % 2
                pi = it % 2
                it += 1
                S.dma('pool', wt[wi][:, :, 0:nch], w_sl[:, ct * 128:ct * 128 + nch].rearrange("(kc p) f -> p kc f", p=128),
                      w=[('wt', wi)])
                if ct < 16:
                    if hf == 0:
                        S.op('dve', lambda e, pi=pi: e.memset(P[pi][:, 0:3], 0.0), w=[('P', pi)])
                    else:
                        S.op('dve', lambda e, pi=pi, ct=ct: e.tensor_copy(out=P[pi][:, 0:3], in_=carry[:, ct, :]),
                             r=['carry'], w=[('P', pi)])
                for tb in range(4):
                    bi = nb % 8
                    nb += 1
                    bk, bkk = banks[bi], ('bk', bi)
                    for kc in range(16):
                        S.op('pe', lambda e, kc=kc, bk=bk, tb=tb, wi=wi, nch=nch: e.matmul(
                            bk[0:nch, :], wt[wi][:, kc, 0:nch], x_sb[:, kc, tb * 512:(tb + 1) * 512],
                            start=(kc == 0), stop=(kc == 15)), r=[('wt', wi)] + xk, w=[bkk])
                    cols = slice(tb * 512, (tb + 1) * 512)
                    if ct < 16:
                        S.op('act', lambda e, bk=bk, tb=tb, pi=pi: e.copy(out=P[pi][:, 3 + tb * 512:3 + (tb + 1) * 512], in_=bk[:, :]),
                             r=[bkk], w=[('P', pi)])
                    elif ct < 24:
                        S.op('act', lambda e, bk=bk, cols=cols, pi=pi: e.copy(out=ost[pi][:, cols], in_=bk[:, :]),
                             r=[bkk], w=[('ost', pi)])
                    else:
                        S.op('act', lambda e, bk=bk, cols=cols: e.copy(out=abst[:, cols], in_=bk[0:16, :]),
                             r=[bkk], w=['abst'])
                tsl = slice(hf * HT, (hf + 1) * HT)
                if ct < 16:
                    S.op('dve', lambda e, pi=pi, ct=ct: e.tensor_copy(out=carry[:, ct, :], in_=P[pi][:, HT:HT + 3]),
                         r=[('P', pi)], w=['carry'])
                    S.op('dve', lambda e, pi=pi, ct=ct: e.tensor_scalar(
                        out=acc[pi][:, :], in0=P[pi][:, 0:HT], scalar1=cw[:, ct, 0:1], scalar2=None, op0=ALU.mult),
                        r=[('P', pi), 'cw'], w=[('acc', pi)])
                    for j in range(1, 4):
                        S.op('dve', lambda e, pi=pi, ct=ct, j=j: e.scalar_tensor_tensor(
                            out=acc[pi][:, :], in0=P[pi][:, j:j + HT], scalar=cw[:, ct, j:j + 1], in1=acc[pi][:, :],
                            op0=ALU.mult, op1=ALU.add), r=[('P', pi), 'cw', ('acc', pi)], w=[('acc', pi)])
                    S.op('act', lambda e, pi=pi: e.activation(out=ost[pi][:, :], in_=acc[pi][:, :], func=AF.Silu),
                         r=[('acc', pi)], w=[('ost', pi)])
                    S.dma('sp', qkvT[ct * 128:(ct + 1) * 128, tsl], ost[pi][:, :], r=[('ost', pi)])
                elif ct < 24:
                    S.dma('sp', zT[(ct - 16) * 128:(ct - 15) * 128, tsl], ost[pi][:, :], r=[('ost', pi)])
                else:
                    S.dma('sp', abT[:, tsl], abst[:, :], r=['abst'])
        S.finish()
    return nc


def gdn_a_inputs(x, a_w_in, a_conv_w):
    ims = []
    for c in range(NCORES):
        b, hg = c // 4, c % 4
        w = a_w_in
        w_sl = np.concatenate([w[:, hg * 512:(hg + 1) * 512], w[:, 2048 + hg * 512:2048 + (hg + 1) * 512],
                               w[:, 4096 + hg * 1024:4096 + (hg + 1) * 1024], w[:, 8192 + hg * 1024:8192 + (hg + 1) * 1024],
                               w[:, 12288 + hg * 8:12288 + (hg + 1) * 8], w[:, 12320 + hg * 8:12320 + (hg + 1) * 8]], axis=1)
        cwf = a_conv_w
        cw = np.concatenate([cwf[:, hg * 512:(hg + 1) * 512], cwf[:, 2048 + hg * 512:2048 + (hg + 1) * 512],
                             cwf[:, 4096 + hg * 1024:4096 + (hg + 1) * 1024]], axis=1).T
        ims.append({"xT": np.ascontiguousarray(x[b].T), "w_sl": np.ascontiguousarray(w_sl),
                    "conv_w": np.ascontiguousarray(cw)})
    return ims


def build_gdn_b(NCH=32):
    nc = _mk()
    T = 4096
    qk = nc.dram_tensor("qk", [T, 8, 128], BF16, kind="ExternalInput").ap()
    v_in = nc.dram_tensor("v", [T, 8, 128], BF16, kind="ExternalInput").ap()
    z_in = nc.dram_tensor("z", [T, 8, 128], BF16, kind="ExternalInput").ap()
    ab = nc.dram_tensor("ab", [T, 16], F32, kind="ExternalInput").ap()
    hp = nc.dram_tensor("hp", [2, 8], F32, kind="ExternalInput").ap()
    nw = nc.dram_tensor("nw", [1, 128], F32, kind="ExternalInput").ap()
    cmask = nc.dram_tensor("cmask", [4, 128, 128], F32, kind="ExternalInput").ap()
    identb_in = nc.dram_tensor("identb", [128, 128], BF16, kind="ExternalInput").ap()
    og = nc.dram_tensor("og", [T, 1024], BF16, kind="ExternalOutput").ap()
    with contextlib.ExitStack() as es:
        S = Sched(nc, es)
        sb = lambda name, shape, dt: es.enter_context(nc.sbuf_tensor(name, shape, dt))
        banks = [es.enter_context(nc.psum_tensor("bk%d" % i, [128, 512], F32)) for i in range(7)]
        tpb = es.enter_context(nc.psum_tensor("tpb", [128, 1024], BF16))
        TPK = ('bk', 7)
        nbk = [0]

        def newbank():
            i = nbk[0] % 7
            nbk[0] += 1
            return banks[i], ('bk', i)

        ML = sb("ML", [128, 128], F32)
        MU = sb("MU", [128, 128], F32)
        MUI = sb("MUI", [128, 128], F32)
        IDF = sb("IDF", [128, 128], F32)
        ONES = sb("ONES", [128, 128], F32)
        IDB = sb("IDB", [128, 128], BF16)
        hpb = sb("hpb", [128, 16], F32)
        nwb = sb("nwb", [128, 128], F32)
        negexpA = sb("negexpA", [128, 8], F32)
        epsr = sb("epsr", [128, 1], F32)
        one1 = sb("one1", [128, 1], F32)
        for i, t in enumerate((ML, MU, MUI, IDF)):
            S.dma('sp', t[:, :], cmask[i], w=[t.name])
        S.dma('sp', IDB[:, :], identb_in[:, :], w=['IDB'])
        S.dma('sp', hpb[:, 0:8], hp[0:1, :].partition_broadcast(128), w=['hpb'])
        S.dma('sp', hpb[:, 8:16], hp[1:2, :].partition_broadcast(128), w=['hpb'])
        S.dma('sp', nwb[:, :], nw[0:1, :].partition_broadcast(128), w=['nwb'])
        S.op('dve', lambda e: e.memset(ONES[:, :], 1.0), w=['ONES'])
        S.op('dve', lambda e: e.memset(epsr[:, :], RMS_EPS), w=['epsr'])
        S.op('dve', lambda e: e.memset(one1[:, :], 1.0), w=['one1'])
        S.op('act', lambda e: e.activation(out=negexpA[:, :], in_=hpb[:, 0:8], func=AF.Exp), r=['hpb'], w=['negexpA'])
        S.op('dve', lambda e: e.tensor_scalar(out=negexpA[:, :], in0=negexpA[:, :], scalar1=-1.0, scalar2=None, op0=ALU.mult),
             r=['negexpA'], w=['negexpA'])

        qk_sb = [sb("qk%d" % i, [128, 8, 128], BF16) for i in range(2)]
        v_sb = [sb("v%d" % i, [128, 8, 128], BF16) for i in range(2)]
        z_sb = [sb("z%d" % i, [128, 8, 128], BF16) for i in range(2)]
        ab_sb = [sb("ab%d" % i, [128, 16], F32) for i in range(2)]
        sq = sb("sq", [128, 8, 128], F32)
        ssq = sb("ssq", [128, 8], F32)
        rn = sb("rn", [128, 8], F32)
        qkn = sb("qkn", [128, 8, 128], BF16)
        qkT = sb("qkT", [128, 8, 128], BF16)
        qg = sb("qg", [128, 8, 128], BF16)
        qgT = sb("qgT", [128, 8, 128], BF16)
        kd = sb("kd", [128, 8, 128], BF16)
        sz = sb("sz", [128, 8, 128], F32)
        nwz = sb("nwz", [128, 8, 128], F32)
        xa = sb("xa", [128, 8], F32)
        ax = sb("ax", [128, 8], F32)
        ex = sb("ex", [128, 8], F32)
        gg = sb("gg", [128, 8], F32)
        beta = sb("beta", [128, 8], F32)
        gcs = sb("gcs", [128, 16], F32)
        eg = sb("eg", [128, 8], F32)
        negeg = sb("negeg", [128, 8], F32)
        dl = sb("dl", [128, 8], F32)
        egl = sb("egl", [128, 8], F32)
        eglast = sb("eglast", [128, 8], F32)
        KKm = [sb("KKm%d" % i, [128, 128], F32) for i in range(4)]
        KQm = [sb("KQm%d" % i, [128, 128], F32) for i in range(4)]
        Gm = [sb("Gm%d" % i, [128, 128], F32) for i in range(8)]
        Ee = [sb("Ee%d" % i, [128, 128], F32) for i in range(8)]
        Wa = [sb("Wa%d" % i, [128, 128], F32) for i in range(8)]
        WTa = [sb("WTa%d" % i, [128, 128], F32) for i in range(8)]
        Wb = [sb("Wb%d" % i, [128, 128], F32) for i in range(8)]
        WTb = [sb("WTb%d" % i, [128, 128], F32) for i in range(8)]
        Qm = [sb("Qm%d" % i, [128, 128], F32) for i in range(8)]
        AT = [sb("AT%d" % i, [128, 128], BF16) for i in range(8)]
        PTb = [sb("PTb%d" % i, [128, 128], BF16) for i in range(8)]
        Xv = [sb("Xv%d" % i, [128, 128], BF16) for i in range(8)]
        vnew = [sb("vnew%d" % i, [128, 128], BF16) for i in range(8)]
        Sf = [sb("Sf%d" % i, [128, 128], F32) for i in range(8)]
        Sb = [sb("Sb%d" % i, [128, 128], BF16) for i in range(8)]
        o_sb = sb("o_sb", [128, 8, 128], F32)
        osq = sb("osq", [128, 8, 128], F32)
        ossq = sb("ossq", [128, 8], F32)
        orstd = sb("orstd", [128, 8], F32)
        ogst = [sb("ogst%d" % i, [128, 8, 128], BF16) for i in range(2)]
        for hv in range(8):
            S.op('dve', lambda e, hv=hv: e.memset(Sf[hv][:, :], 0.0), w=[Sf[hv].name])
            S.op('pool', lambda e, hv=hv: e.memset(Sb[hv][:, :], 0.0), w=[Sb[hv].name])

        def load(n):
            p = n % 2
            rows = slice(n * 128, (n + 1) * 128)
            S.dma('sp', qk_sb[p][:, :, :], qk[rows], w=[qk_sb[p].name])
            S.dma('sp', v_sb[p][:, :, :], v_in[rows], w=[v_sb[p].name])
            S.dma('sp', z_sb[p][:, :, :], z_in[rows], w=[z_sb[p].name])
            S.dma('sp', ab_sb[p][:, :], ab[rows, :], w=[ab_sb[p].name])

        def bc3(ap2, n):
            return ap2.unsqueeze(2).to_broadcast([128, n, 128])

        load(0)
        for n in range(NCH):
            p = n % 2
            if n + 1 < NCH:
                load(n + 1)
            QK, V, Z, AB = qk_sb[p], v_sb[p], z_sb[p], ab_sb[p]
            S.op('dve', lambda e: e.tensor_tensor(out=sq[:, :, :], in0=QK[:, :, :], in1=QK[:, :, :], op=ALU.mult), r=[QK.name], w=['sq'])
            S.op('dve', lambda e: e.tensor_reduce(out=ssq[:, :], in_=sq[:, :, :], axis=AX.X, op=ALU.add), r=['sq'], w=['ssq'])
            S.op('act', lambda e: e.activation(out=rn[:, :], in_=ssq[:, :], func=AF.Sqrt, bias=epsr[:, 0:1]), r=['ssq', 'epsr'], w=['rn'])
            S.op('dve', lambda e: e.reciprocal(out=rn[:, :], in_=rn[:, :]), r=['rn'], w=['rn'])
            S.op('dve', lambda e: e.tensor_scalar(out=rn[:, 0:4], in0=rn[:, 0:4], scalar1=128.0 ** -0.5, scalar2=None, op0=ALU.mult), r=['rn'], w=['rn'])
            S.op('dve', lambda e: e.tensor_tensor(out=qkn[:, :, :], in0=QK[:, :, :], in1=bc3(rn[:, :], 8), op=ALU.mult), r=[QK.name, 'rn'], w=['qkn'])
            S.op('dve', lambda e: e.tensor_tensor(out=xa[:, :], in0=AB[:, 0:8], in1=hpb[:, 8:16], op=ALU.add), r=[AB.name, 'hpb'], w=['xa'])
            S.op('dve', lambda e: e.tensor_scalar(out=ax[:, :], in0=xa[:, :], scalar1=-1.0, scalar2=None, op0=ALU.mult), r=['xa'], w=['ax'])
            S.op('dve', lambda e: e.tensor_tensor(out=ax[:, :], in0=ax[:, :], in1=xa[:, :], op=ALU.min), r=['xa', 'ax'], w=['ax'])
            S.op('act', lambda e: e.activation(out=ex[:, :], in_=ax[:, :], func=AF.Exp), r=['ax'], w=['ex'])
            S.op('act', lambda e: e.activation(out=ex[:, :], in_=ex[:, :], func=AF.Ln, bias=one1[:, 0:1]), r=['ex', 'one1'], w=['ex'])
            S.op('dve', lambda e: e.scalar_tensor_tensor(out=gg[:, :], in0=xa[:, :], scalar=0.0, in1=ex[:, :], op0=ALU.max, op1=ALU.add), r=['xa', 'ex'], w=['gg'])
            S.op('dve', lambda e: e.tensor_tensor(out=gg[:, :], in0=gg[:, :], in1=negexpA[:, :], op=ALU.mult), r=['gg', 'negexpA'], w=['gg'])
            S.op('act', lambda e: e.activation(out=beta[:, :], in_=AB[:, 8:16], func=AF.Sigmoid), r=[AB.name], w=['beta'])
            bk, bkk = newbank()
            S.op('pe', lambda e, bk=bk: e.matmul(bk[:, 0:8], MUI[:, :], gg[:, :], start=True, stop=True), r=['MUI', 'gg'], w=[bkk])
            S.op('pe', lambda e, bk=bk: e.matmul(bk[:, 8:16], ONES[:, :], gg[:, :], start=True, stop=True), r=['ONES', 'gg'], w=[bkk])
            S.op('act', lambda e, bk=bk: e.copy(out=gcs[:, :], in_=bk[:, 0:16]), r=[bkk], w=['gcs'])
            S.op('act', lambda e: e.activation(out=eg[:, :], in_=gcs[:, 0:8], func=AF.Exp), r=['gcs'], w=['eg'])
            S.op('dve', lambda e: e.tensor_scalar(out=negeg[:, :], in0=eg[:, :], scalar1=-1.0, scalar2=None, op0=ALU.mult), r=['eg'], w=['negeg'])
            S.op('dve', lambda e: e.tensor_tensor(out=dl[:, :], in0=gcs[:, 8:16], in1=gcs[:, 0:8], op=ALU.subtract), r=['gcs'], w=['dl'])
            S.op('act', lambda e: e.activation(out=egl[:, :], in_=dl[:, :], func=AF.Exp), r=['dl'], w=['egl'])
            S.op('act', lambda e: e.activation(out=eglast[:, :], in_=gcs[:, 8:16], func=AF.Exp), r=['gcs'], w=['eglast'])
            for hk in range(4):
                S.op('dve', lambda e, hk=hk: e.tensor_tensor(
                    out=qg[:, 2 * hk:2 * hk + 2, :], in0=qkn[:, hk:hk + 1, :].to_broadcast([128, 2, 128]),
                    in1=bc3(eg[:, 2 * hk:2 * hk + 2], 2), op=ALU.mult), r=['qkn', 'eg'], w=['qg'])
                S.op('pool', lambda e, hk=hk: e.tensor_tensor(
                    out=kd[:, 2 * hk:2 * hk + 2, :], in0=qkn[:, 4 + hk:5 + hk, :].to_broadcast([128, 2, 128]),
                    in1=bc3(egl[:, 2 * hk:2 * hk + 2], 2), op=ALU.mult), r=['qkn', 'egl'], w=['kd'])
            for j in range(8):
                S.op('pe', lambda e, j=j: e.transpose(tpb[:, j * 128:(j + 1) * 128], qkn[:, j, :], IDB[:, :]), r=['qkn', 'IDB'], w=[TPK])
            S.op('act', lambda e: e.copy(out=qkT[:, :, :], in_=tpb[:, :].rearrange("p (j t) -> p j t", j=8)), r=[TPK], w=['qkT'])
            for j in range(8):
                S.op('pe', lambda e, j=j: e.transpose(tpb[:, j * 128:(j + 1) * 128], qg[:, j, :], IDB[:, :]), r=['qg', 'IDB'], w=[TPK])
            S.op('dve', lambda e: e.tensor_copy(out=qgT[:, :, :], in_=tpb[:, :].rearrange("p (j t) -> p j t", j=8)), r=[TPK], w=['qgT'])
            S.op('act', lambda e: e.activation(out=sz[:, :, :], in_=Z[:, :, :], func=AF.Silu), r=[Z.name], w=['sz'])
            S.op('pool', lambda e: e.tensor_tensor(out=nwz[:, :, :], in0=sz[:, :, :], in1=nwb[:, :].unsqueeze(1).to_broadcast([128, 8, 128]), op=ALU.mult),
                 r=['sz', 'nwb'], w=['nwz'])
            for hk in range(4):
                bk, bkk = newbank()
                S.op('pe', lambda e, bk=bk, hk=hk: e.matmul(bk[:, 0:128], qkT[:, 4 + hk, :], qkT[:, 4 + hk, :], start=True, stop=True), r=['qkT'], w=[bkk])
                S.op('pe', lambda e, bk=bk, hk=hk: e.matmul(bk[:, 128:256], qkT[:, 4 + hk, :], qkT[:, hk, :], start=True, stop=True), r=['qkT'], w=[bkk])
                S.op('dve', lambda e, bk=bk, hk=hk: e.tensor_tensor(out=KKm[hk][:, :], in0=bk[:, 0:128], in1=MU[:, :], op=ALU.mult), r=[bkk, 'MU'], w=[KKm[hk].name])
                S.op('dve', lambda e, bk=bk, hk=hk: e.tensor_tensor(out=KQm[hk][:, :], in0=bk[:, 128:256], in1=MUI[:, :], op=ALU.mult), r=[bkk, 'MUI'], w=[KQm[hk].name])
            for hv in range(8):
                hk = hv // 2
                S.op('pool', lambda e, hv=hv: e.tensor_scalar(out=Gm[hv][:, :], in0=ML[:, :], scalar1=gg[:, hv:hv + 1], scalar2=None, op0=ALU.mult),
                     r=['ML', 'gg'], w=[Gm[hv].name])
                bk, bkk = newbank()
                S.op('pe', lambda e, bk=bk, hv=hv: e.matmul(bk[:, 0:128], Gm[hv][:, :], MUI[:, :], start=True, stop=True), r=[Gm[hv].name, 'MUI'], w=[bkk])
                S.op('act', lambda e, bk=bk, hv=hv: e.activation(out=Ee[hv][:, :], in_=bk[:, 0:128], func=AF.Exp), r=[bkk], w=[Ee[hv].name])
                S.op('dve', lambda e, hv=hv, hk=hk: e.scalar_tensor_tensor(out=Wa[hv][:, :], in0=KKm[hk][:, :], scalar=beta[:, hv:hv + 1], in1=Ee[hv][:, :],
                                                                         op0=ALU.mult, op1=ALU.mult), r=[KKm[hk].name, 'beta', Ee[hv].name], w=[Wa[hv].name])
                S.op('pool', lambda e, hv=hv, hk=hk: e.tensor_tensor(out=AT[hv][:, :], in0=KQm[hk][:, :], in1=Ee[hv][:, :], op=ALU.mult),
                     r=[KQm[hk].name, Ee[hv].name], w=[AT[hv].name])
            for grp in range(2):
                hvs = range(grp * 4, grp * 4 + 4)
                cur = {}
                for hv in hvs:
                    bk, bkk = newbank()
                    S.op('pe', lambda e, bk=bk, hv=hv: e.transpose(bk[:, 0:128], Wa[hv][:, :], IDF[:, :]), r=[Wa[hv].name, 'IDF'], w=[bkk])
                    S.op('act', lambda e, bk=bk, hv=hv: e.copy(out=WTa[hv][:, :], in_=bk[:, 0:128]), r=[bkk], w=[WTa[hv].name])
                    S.op('dve', lambda e, hv=hv: e.tensor_tensor(out=Qm[hv][:, :], in0=IDF[:, :], in1=Wa[hv][:, :], op=ALU.subtract),
                         r=['IDF', Wa[hv].name], w=[Qm[hv].name])
                    cur[hv] = (Wa[hv], WTa[hv], Wb[hv], WTb[hv])
                for k in range(1, 7):
                    for hv in hvs:
                        W, WT, Wn, WTn = cur[hv]
                        bk, bkk = newbank()
                        S.op('pe', lambda e, bk=bk, W=W, WT=WT: e.matmul(bk[:, 0:128], W[:, :], WT[:, :], start=True, stop=True), r=[W.name, WT.name], w=[bkk])
                        S.op('act', lambda e, bk=bk, WTn=WTn: e.copy(out=WTn[:, :], in_=bk[:, 0:128]), r=[bkk], w=[WTn.name])
                        if k < 6:
                            bk2, bkk2 = newbank()
                            S.op('pe', lambda e, bk2=bk2, W=W, WT=WT: e.matmul(bk2[:, 0:128], WT[:, :], W[:, :], start=True, stop=True), r=[W.name, WT.name], w=[bkk2])
                            S.op('dve', lambda e, bk2=bk2, Wn=Wn: e.tensor_copy(out=Wn[:, :], in_=bk2[:, 0:128]), r=[bkk2], w=[Wn.name])
                    for hv in hvs:
                        W, WT, Wn, WTn = cur[hv]
                        bk, bkk = newbank()
                        S.op('pe', lambda e, bk=bk, WTn=WTn, hv=hv: e.matmul(bk[:, 0:128], WTn[:, :], Qm[hv][:, :], start=True, stop=True),
                             r=[WTn.name, Qm[hv].name], w=[bkk])
                        S.op('dve', lambda e, bk=bk, hv=hv: e.tensor_tensor(out=Qm[hv][:, :], in0=Qm[hv][:, :], in1=bk[:, 0:128], op=ALU.add),
                             r=[bkk, Qm[hv].name], w=[Qm[hv].name])
                        cur[hv] = (Wn, WTn, W, WT)
                for hv in hvs:
                    S.op('act', lambda e, hv=hv: e.copy(out=PTb[hv][:, :], in_=Qm[hv][:, :]), r=[Qm[hv].name], w=[PTb[hv].name])
            for grp in range(2):
                hvs = range(grp * 4, grp * 4 + 4)
                bR = {}
                for hv in hvs:
                    hk = hv // 2
                    bR[hv] = newbank()
                    bk, bkk = bR[hv]
                    S.op('pe', lambda e, bk=bk, hv=hv, hk=hk: e.matmul(bk[:, 0:128], qkT[:, 4 + hk, :], Sb[hv][:, :], start=True, stop=True),
                         r=['qkT', Sb[hv].name], w=[bkk])
                for hv in hvs:
                    bk, bkk = bR[hv]
                    S.op('dve', lambda e, bk=bk, hv=hv: e.scalar_tensor_tensor(out=Xv[hv][:, :], in0=bk[:, 0:128], scalar=negeg[:, hv:hv + 1], in1=V[:, hv, :],
                                                                         op0=ALU.mult, op1=ALU.add), r=[bkk, 'negeg', V.name], w=[Xv[hv].name])
                for hv in hvs:
                    bR[hv] = newbank()
                    bk, bkk = bR[hv]
                    S.op('pe', lambda e, bk=bk, hv=hv: e.matmul(bk[:, 0:128], PTb[hv][:, :], Xv[hv][:, :], start=True, stop=True),
                         r=[PTb[hv].name, Xv[hv].name], w=[bkk])
                for hv in hvs:
                    bk, bkk = bR[hv]
                    S.op('act', lambda e, bk=bk, hv=hv: e.activation(out=vnew[hv][:, :], in_=bk[:, 0:128], func=AF.Identity, scale=beta[:, hv:hv + 1]),
                         r=[bkk, 'beta'], w=[vnew[hv].name])
                for hv in hvs:
                    bk, bkk = newbank()
                    S.op('pe', lambda e, bk=bk, hv=hv: e.matmul(bk[:, 0:128], qgT[:, hv, :], Sb[hv][:, :], start=True, stop=False),
                         r=['qgT', Sb[hv].name], w=[bkk])
                    S.op('pe', lambda e, bk=bk, hv=hv: e.matmul(bk[:, 0:128], AT[hv][:, :], vnew[hv][:, :], start=False, stop=True),
                         r=[AT[hv].name, vnew[hv].name], w=[bkk])
                    S.op('pe', lambda e, bk=bk, hv=hv: e.matmul(bk[:, 128:256], kd[:, hv, :], vnew[hv][:, :], start=True, stop=True),
                         r=['kd', vnew[hv].name], w=[bkk])
                    S.op('act', lambda e, bk=bk, hv=hv: e.copy(out=o_sb[:, hv, :], in_=bk[:, 0:128]), r=[bkk], w=['o_sb'])
                    S.op('dve', lambda e, bk=bk, hv=hv: e.scalar_tensor_tensor(out=Sf[hv][:, :], in0=Sf[hv][:, :], scalar=eglast[:, hv:hv + 1], in1=bk[:, 128:256],
                                                                         op0=ALU.mult, op1=ALU.add), r=[bkk, 'eglast', Sf[hv].name], w=[Sf[hv].name])
                    S.op('act', lambda e, hv=hv: e.copy(out=Sb[hv][:, :], in_=Sf[hv][:, :]), r=[Sf[hv].name], w=[Sb[hv].name])
            S.op('dve', lambda e: e.tensor_tensor(out=osq[:, :, :], in0=o_sb[:, :, :], in1=o_sb[:, :, :], op=ALU.mult), r=['o_sb'], w=['osq'])
            S.op('dve', lambda e: e.tensor_reduce(out=ossq[:, :], in_=osq[:, :, :], axis=AX.X, op=ALU.add), r=['osq'], w=['ossq'])
            S.op('act', lambda e: e.activation(out=orstd[:, :], in_=ossq[:, :], func=AF.Sqrt, bias=epsr[:, 0:1], scale=1.0 / 128.0), r=['ossq', 'epsr'], w=['orstd'])
            S.op('dve', lambda e: e.reciprocal(out=orstd[:, :], in_=orstd[:, :]), r=['orstd'], w=['orstd'])
            S.op('dve', lambda e: e.tensor_tensor(out=osq[:, :, :], in0=o_sb[:, :, :], in1=bc3(orstd[:, :], 8), op=ALU.mult), r=['o_sb', 'orstd'], w=['osq'])
            S.op('pool', lambda e, p=p: e.tensor_tensor(out=ogst[p][:, :, :], in0=osq[:, :, :], in1=nwz[:, :, :], op=ALU.mult), r=['osq', 'nwz'], w=[ogst[p].name])
            S.dma('sp', og[n * 128:(n + 1) * 128, :], ogst[p][:, :, :].rearrange("p h d -> p (h d)"), r=[ogst[p].name])
        S.finish()
    return nc


def gdn_b_consts():
    i = np.arange(128)
    ML = (i[:, None] > i[None, :]).astype(np.float32)
    MU = (i[None, :] > i[:, None]).astype(np.float32)
    MUI = (i[None, :] >= i[:, None]).astype(np.float32)
    return np.stack([ML, MU, MUI, np.eye(128, dtype=np.float32)]), np.eye(128, dtype=np.float32).astype(NPBF)


def gdn_b_inputs(qkvT_all, zT_all, abT_all, a_log, dt_bias, norm_w):
    cm, idb = gdn_b_consts()
    ims = []
    for c in range(NCORES):
        hg = c % 4
        qkvT, zT, abT = qkvT_all[c], zT_all[c], abT_all[c]
        qk = np.ascontiguousarray(qkvT[0:1024].T).reshape(4096, 8, 128)
        v = np.ascontiguousarray(qkvT[1024:2048].T).reshape(4096, 8, 128)
        z = np.ascontiguousarray(zT.T).reshape(4096, 8, 128)
        ab = np.ascontiguousarray(abT.T)
        hp = np.stack([a_log[hg * 8:(hg + 1) * 8], dt_bias[hg * 8:(hg + 1) * 8]]).astype(np.float32)
        ims.append({"qk": qk, "v": v, "z": z, "ab": ab, "hp": hp, "nw": norm_w.reshape(1, 128).astype(np.float32),
                    "cmask": cm, "identb": idb})
    return ims


SCL = 128.0 ** -0.5
NEGB = -30000.0


def build_nsa(NQT=32):
    nc = _mk()
    T = 4096
    qn_d = nc.dram_tensor("qn", [128, 32 * 512], BF16, kind="ExternalInput").ap()
    qr_d = nc.dram_tensor("qr", [128, 32 * 512], BF16, kind="ExternalInput").ap()
    kcT_d = nc.dram_tensor("kcT", [128, T], BF16, kind="ExternalInput").ap()
    vcT_d = nc.dram_tensor("vcT", [128, T], BF16, kind="ExternalInput").ap()
    kslT_d = nc.dram_tensor("kslT", [128, T], BF16, kind="ExternalInput").ap()
    kwT_d = nc.dram_tensor("kwT", [128, T], BF16, kind="ExternalInput").ap()
    vsl_d = nc.dram_tensor("vsl", [T, 128], BF16, kind="ExternalInput").ap()
    vw_d = nc.dram_tensor("vw", [T, 128], BF16, kind="ExternalInput").ap()
    gates_d = nc.dram_tensor("gates", [T, 12], F32, kind="ExternalInput").ap()
    w1_d = nc.dram_tensor("w1", [2, 4096, 512], F32, kind="ExternalInput").ap()
    w2_d = nc.dram_tensor("w2", [2, 512, 128], F32, kind="ExternalInput").ap()
    peT_d = nc.dram_tensor("peT", [2, 128, 32], F32, kind="ExternalInput").ap()
    ov_d = nc.dram_tensor("ov", [2, 128, 65], F32, kind="ExternalInput").ap()
    maskc_d = nc.dram_tensor("maskc", [128, 32 * 2 * 128], BF16, kind="ExternalInput").ap()
    sm_d = nc.dram_tensor("sm", [2, 128, 32 * 64], F32, kind="ExternalInput").ap()
    ind_d = nc.dram_tensor("ind", [64, 32 * 128], BF16, kind="ExternalInput").ap()
    cbias_d = nc.dram_tensor("cbias", [128, 2 * 512], BF16, kind="ExternalInput").ap()
    identb_d = nc.dram_tensor("identb", [128, 128], BF16, kind="ExternalInput").ap()
    identf_d = nc.dram_tensor("identf", [128, 128], F32, kind="ExternalInput").ap()
    o_d = nc.dram_tensor("o", [T, 512], BF16, kind="ExternalOutput").ap()
    with contextlib.ExitStack() as es:
        S = Sched(nc, es)
        sb = lambda name, shape, dt: es.enter_context(nc.sbuf_tensor(name + "_s", shape, dt))
        banks = [es.enter_context(nc.psum_tensor("bk%d" % i, [128, 512], F32)) for i in range(4)]
        accs = [es.enter_context(nc.psum_tensor("acc%d" % i, [128, 4, 256], F32)) for i in range(2)]
        ACK = [('bk', 'A'), ('bk', 'B')]
        nbk = [0]

        def newbank(n=3):
            i = nbk[0] % n
            nbk[0] += 1
            return banks[i], ('bk', i)

        kcmpT = sb("kcmpT", [128, 256], BF16)
        Rext = sb("Rext", [128, 2, 193], BF16)
        IDB = sb("IDB", [128, 128], BF16)
        IDF = sb("IDF", [128, 128], F32)
        S.dma('sp', IDB[:, :], identb_d[:, :], w=['IDB'])
        S.dma('sp', IDF[:, :], identf_d[:, :], w=['IDF'])
        S.op('dve', lambda e: e.memset(kcmpT[:, :], 0.0), w=['kcmpT'])
        for cc in range(2):
            S.dma('pool', Rext[:, cc, 128:193], ov_d[cc], w=['Rext'])
        with contextlib.ExitStack() as es0:
            sb0 = lambda name, shape, dt: es0.enter_context(nc.sbuf_tensor(name + "_s", shape, dt))
            srcT = [sb0("kcT", [128, T], BF16), sb0("vcT", [128, T], BF16)]
            S.dma('sp', srcT[0][:, :], kcT_d[:, :], w=['srcT0'])
            S.dma('sp', srcT[1][:, :], vcT_d[:, :], w=['srcT1'])
            w1b = [sb0("w1b%d" % i, [128, 32, 128], BF16) for i in range(2)]
            w2b = [sb0("w2b%d" % i, [128, 4, 128], BF16) for i in range(2)]
            peb = [sb0("peb%d" % i, [128, 32], BF16) for i in range(2)]
            hid = [sb0("hid%d" % i, [128, 4, 256], BF16) for i in range(2)]
            biasv = sb0("biasv", [128, 8], F32)
            nw1 = 0
            for kv in range(2):
                S.dma('pool', w2b[kv][:, :, :], w2_d[kv].rearrange("(hc p) d -> p hc d", p=128), w=['w2b%d' % kv])
                S.dma('pool', peb[kv][:, :], peT_d[kv], w=['peb%d' % kv])
                S.op('dve', lambda e, kv=kv: e.memset(hid[kv][:, :, :], 0.0), w=['hid%d' % kv])
                for hc in range(4):
                    wi = nw1 % 2
                    nw1 += 1
                    for half in range(2):
                        S.dma('pool', w1b[wi][:, half * 16:(half + 1) * 16, :],
                              w1_d[kv, half * 2048:(half + 1) * 2048, hc * 128:(hc + 1) * 128].rearrange("(l d) f -> d l f", d=128),
                              w=[('w1b', wi)])
                    bk, bkk = newbank()
                    for l in range(32):
                        S.op('pe', lambda e, bk=bk, l=l, wi=wi, kv=kv: e.matmul(bk[:, 0:1], w1b[wi][:, l, :], peb[kv][:, l:l + 1],
                                                                              start=(l == 0), stop=(l == 31)), r=[('w1b', wi), 'peb%d' % kv], w=[bkk])
                    S.op('act', lambda e, bk=bk, kv=kv, hc=hc: e.copy(out=biasv[:, kv * 4 + hc:kv * 4 + hc + 1], in_=bk[:, 0:1]), r=[bkk], w=['biasv'])
                    bk, bkk = newbank()
                    for l in range(32):
                        S.op('pe', lambda e, bk=bk, l=l, wi=wi, kv=kv: e.matmul(bk[:, 0:255], w1b[wi][:, l, :], srcT[kv][:, l:l + 16 * 254 + 1:16],
                                                                              start=(l == 0), stop=(l == 31)), r=[('w1b', wi), 'srcT%d' % kv], w=[bkk])
                    S.op('act', lambda e, bk=bk, kv=kv, hc=hc: e.activation(out=hid[kv][:, hc, 0:255], in_=bk[:, 0:255], func=AF.Silu,
                                                                          bias=biasv[:, kv * 4 + hc:kv * 4 + hc + 1]), r=[bkk, 'biasv'], w=['hid%d' % kv])
            bk, bkk = newbank()
            for hc in range(4):
                S.op('pe', lambda e, bk=bk, hc=hc: e.matmul(bk[:, 0:255], w2b[0][:, hc, :], hid[0][:, hc, 0:255], start=(hc == 0), stop=(hc == 3)),
                     r=['w2b0', 'hid0'], w=[bkk])
            S.op('act', lambda e, bk=bk: e.copy(out=kcmpT[:, 0:255], in_=bk[:, 0:255]), r=[bkk], w=['kcmpT'])
            for cc in range(2):
                bk, bkk = newbank()
                for hc in range(4):
                    S.op('pe', lambda e, bk=bk, hc=hc, cc=cc: e.matmul(bk[:, 0:128], hid[1][:, hc, cc * 128:(cc + 1) * 128], w2b[1][:, hc, :],
                                                                     start=(hc == 0), stop=(hc == 3)), r=['w2b1', 'hid1'], w=[bkk])
                S.op('act', lambda e, bk=bk, cc=cc: e.copy(out=Rext[:, cc, 0:128], in_=bk[:, 0:128]), r=[bkk], w=['Rext'])
            S.sync_all()
        qn = sb("qn", [128, 32, 512], BF16)
        qr = sb("qr", [128, 32, 512], BF16)
        kslT = sb("kslT", [128, T], BF16)
        kwT = sb("kwT", [128, T], BF16)
        vsl = sb("vsl", [128, 32, 129], BF16)
        vw = sb("vw", [128, 32, 129], BF16)
        gts = sb("gts", [128, 32, 12], F32)
        maskc = sb("maskc", [128, 32, 2, 128], BF16)
        sm1 = sb("sm1", [128, 32, 64], F32)
        sm2 = sb("sm2", [128, 32, 64], F32)
        ind = sb("ind", [64, 32, 128], BF16)
        cbias = sb("cbias", [128, 2, 512], BF16)
        for half in range(2):
            hs = slice(half * 16, (half + 1) * 16)
            S.dma('sp', qn[:, hs, :], qn_d[:, half * 8192:(half + 1) * 8192].rearrange("p (a b) -> p a b", b=512), w=['qn'])
            S.dma('sp', qr[:, hs, :], qr_d[:, half * 8192:(half + 1) * 8192].rearrange("p (a b) -> p a b", b=512), w=['qr'])
        S.dma('sp', kslT[:, :], kslT_d[:, :], w=['kslT'])
        S.dma('sp', kwT[:, :], kwT_d[:, :], w=['kwT'])
        S.op('dve', lambda e: e.memset(vsl[:, :, 128:129], 1.0), w=['vsl'])
        S.op('dve', lambda e: e.memset(vw[:, :, 128:129], 1.0), w=['vw'])
        S.dma('sp', vsl[:, :, 0:128], vsl_d.rearrange("(kt p) d -> p kt d", p=128), w=['vsl'])
        S.dma('sp', vw[:, :, 0:128], vw_d.rearrange("(kt p) d -> p kt d", p=128), w=['vw'])
        S.dma('sp', gts[:, :, :], gates_d.rearrange("(qt p) g -> p qt g", p=128), w=['gts'])
        S.dma('sp', maskc[:, :, :, :], maskc_d.rearrange("p (a b c) -> p a b c", a=32, b=2), w=['maskc'])
        S.dma('sp', sm1[:, :, :], sm_d[0].rearrange("p (a b) -> p a b", b=64), w=['sm1'])
        S.dma('sp', sm2[:, :, :], sm_d[1].rearrange("p (a b) -> p a b", b=64), w=['sm2'])
        S.dma('sp', ind[:, :, :], ind_d.rearrange("p (a b) -> p a b", b=128), w=['ind'])
        S.dma('sp', cbias[:, :, :], cbias_d.rearrange("p (a b) -> p a b", b=512), w=['cbias'])
        Ec = [sb("Ec%d" % i, [128, 4, 128], BF16) for i in range(2)]
        Es = [sb("Es%d" % i, [128, 512], BF16) for i in range(3)]
        den = sb("den", [128, 4], F32)
        rec = sb("rec", [128, 4], F32)
        rg = sb("rg", [128, 4], F32)
        pn = sb("pn", [128, 4, 64], F32)
        pslc = sb("pslc", [128, 64], F32)
        score = sb("score", [128, 64], F32)
        work = sb("work", [128, 64], F32)
        m8a = sb("m8a", [128, 8], F32)
        m8b = sb("m8b", [128, 8], F32)
        thr = sb("thr", [128, 1], F32)
        selm = sb("selm", [128, 64], F32)
        selmT = sb("selmT", [64, 4, 128], BF16)
        oacc = sb("oacc", [128, 4, 128], F32)
        otmp = sb("otmp", [128, 4, 128], F32)
        ob = [sb("ob%d" % i, [128, 4, 128], BF16) for i in range(2)]
        nes = [0]

        def bc(ap2):
            return ap2.unsqueeze(2).to_broadcast([128, 4, 128])

        def attend(qt, kts, kT, kTk, vext, vk, acc, ack, use_sel):
            for idx, kt in enumerate(kts):
                bk, bkk = newbank()
                diag = (kt == qt)
                far = (not use_sel) and (kt == qt - 4)
                more = use_sel or diag or far
                S.op('pe', lambda e, bk=bk, kt=kt: e.matmul(bk[:, :], kT[:, kt * 128:(kt + 1) * 128], qr[:, qt, :], start=True, stop=not more),
                     r=[kTk, 'qr'], w=[bkk])
                if use_sel:
                    S.op('pe', lambda e, bk=bk, kt=kt: e.matmul(bk[:, :], ind[:, kt, :], selmT[:, :, :].rearrange("p h q -> p (h q)"), start=False,
                                                              stop=not diag), r=['ind', 'selmT'], w=[bkk])
                if diag:
                    S.op('pe', lambda e, bk=bk: e.matmul(bk[:, :], IDB[:, :], cbias[:, 0, :], start=False, stop=not far), r=['IDB', 'cbias'], w=[bkk])
                if far:
                    S.op('pe', lambda e, bk=bk: e.matmul(bk[:, :], IDB[:, :], cbias[:, 1, :], start=False, stop=True), r=['IDB', 'cbias'], w=[bkk])
                ei = nes[0] % 3
                nes[0] += 1
                S.op('act', lambda e, bk=bk, ei=ei: e.activation(out=Es[ei][:, :], in_=bk[:, :], func=AF.Exp, scale=SCL), r=[bkk], w=[('Es', ei)])
                for h in range(4):
                    S.op('pe', lambda e, h=h, ei=ei, kt=kt, idx=idx: e.matmul(acc[:, h, 0:129], Es[ei][:, h * 128:(h + 1) * 128], vext[:, kt, :],
                                                                            start=(idx == 0 and h % 2 == 0), stop=(idx == len(kts) - 1)),
                         r=[('Es', ei), vk], w=[ack])

        def finish_branch(acc, ack, gcol, first):
            S.op('dve', lambda e: e.reciprocal(out=rec[:, :], in_=acc[:, :, 128]), r=[ack], w=['rec'])
            S.op('dve', lambda e: e.tensor_tensor(out=rg[:, :], in0=rec[:, :], in1=gcol, op=ALU.mult), r=['rec', 'gts'], w=['rg'])
            if first:
                S.op('dve', lambda e: e.tensor_tensor(out=oacc[:, :, :], in0=acc[:, :, 0:128], in1=bc(rg[:, :]), op=ALU.mult), r=[ack, 'rg'], w=['oacc'])
            else:
                S.op('dve', lambda e: e.tensor_tensor(out=otmp[:, :, :], in0=acc[:, :, 0:128], in1=bc(rg[:, :]), op=ALU.mult), r=[ack, 'rg'], w=['otmp'])
                S.op('pool', lambda e: e.tensor_tensor(out=oacc[:, :, :], in0=oacc[:, :, :], in1=otmp[:, :, :], op=ALU.add), r=['otmp', 'oacc'], w=['oacc'])

        for qt in range(NQT):
            ccs = [0] if qt < 16 else [0, 1]
            A, AK = accs[0], ACK[0]
            for cc in ccs:
                bk, bkk = newbank()
                S.op('pe', lambda e, bk=bk, cc=cc: e.matmul(bk[:, :], kcmpT[:, cc * 128:(cc + 1) * 128], qn[:, qt, :], start=True, stop=True),
                     r=['kcmpT', 'qn'], w=[bkk])
                S.op('act', lambda e, bk=bk, cc=cc: e.activation(out=Ec[cc][:, :, :], in_=bk[:, :].rearrange("p (h q) -> p h q", h=4), func=AF.Exp, scale=SCL),
                     r=[bkk], w=[('Ec', cc)])
                S.op('dve', lambda e, cc=cc: e.tensor_tensor(out=Ec[cc][:, :, :], in0=Ec[cc][:, :, :],
                                                            in1=maskc[:, qt, cc, :].unsqueeze(1).to_broadcast([128, 4, 128]), op=ALU.mult),
                     r=[('Ec', cc), 'maskc'], w=[('Ec', cc)])
            for ci, cc in enumerate(ccs):
                for h in range(4):
                    S.op('pe', lambda e, h=h, cc=cc, ci=ci: e.matmul(A[:, h, 0:193], Ec[cc][:, h, :], Rext[:, cc, :],
                                                                   start=(ci == 0 and h % 2 == 0), stop=(ci == len(ccs) - 1)),
                         r=[('Ec', cc), 'Rext'], w=[AK])
            S.op('dve', lambda e: e.tensor_scalar(out=den[:, :], in0=A[:, :, 192], scalar1=1e-30, scalar2=None, op0=ALU.max), r=[AK], w=['den'])
            S.op('dve', lambda e: e.reciprocal(out=rec[:, :], in_=den[:, :]), r=['den'], w=['rec'])
            S.op('dve', lambda e: e.tensor_tensor(out=pn[:, :, :], in0=A[:, :, 128:192], in1=rec[:, :].unsqueeze(2).to_broadcast([128, 4, 64]), op=ALU.mult),
                 r=[AK, 'rec'], w=['pn'])
            S.op('dve', lambda e: e.tensor_reduce(out=pslc[:, :], in_=pn[:, :, :].rearrange("p h s -> p s h"), axis=AX.X, op=ALU.add), r=['pn'], w=['pslc'])
            S.op('dve', lambda e: e.tensor_tensor(out=rg[:, :], in0=rec[:, :], in1=gts[:, qt, 0:4], op=ALU.mult), r=['rec', 'gts'], w=['rg'])
            S.op('dve', lambda e: e.tensor_tensor(out=oacc[:, :, :], in0=A[:, :, 0:128], in1=bc(rg[:, :]), op=ALU.mult), r=[AK, 'rg'], w=['oacc'])
            S.op('dve', lambda e: e.tensor_tensor(out=score[:, :], in0=pslc[:, :], in1=sm1[:, qt, :], op=ALU.mult), r=['pslc', 'sm1'], w=['score'])
            S.op('dve', lambda e: e.tensor_tensor(out=score[:, :], in0=score[:, :], in1=sm2[:, qt, :], op=ALU.add), r=['score', 'sm2'], w=['score'])
            S.op('dve', lambda e: e.max(out=m8a[:, :], in_=score[:, :]), r=['score'], w=['m8a'])
            S.op('dve', lambda e: e.match_replace(out=work[:, :], in_to_replace=m8a[:, :], in_values=score[:, :], imm_value=-2.0), r=['score', 'm8a'], w=['work'])
            S.op('dve', lambda e: e.max(out=m8b[:, :], in_=work[:, :]), r=['work'], w=['m8b'])
            S.op('dve', lambda e: e.tensor_scalar(out=thr[:, :], in0=m8b[:, 7:8], scalar1=0.0, scalar2=None, op0=ALU.max), r=['m8b'], w=['thr'])
            S.op('dve', lambda e: e.tensor_scalar(out=selm[:, :], in0=score[:, :], scalar1=thr[:, 0:1], scalar2=None, op0=ALU.is_ge), r=['score', 'thr'], w=['selm'])
            S.op('dve', lambda e: e.tensor_scalar(out=selm[:, :], in0=selm[:, :], scalar1=-NEGB, scalar2=NEGB, op0=ALU.mult, op1=ALU.add), r=['selm'], w=['selm'])
            attend(qt, list(range(max(0, qt - 4), qt + 1)), kwT, 'kwT', vw, 'vw', accs[1], ACK[1], False)
            bk, bkk = banks[3], ('bk', 3)
            S.op('pe', lambda e, bk=bk: e.transpose(bk[0:64, 0:128], selm[:, :], IDF[:, :]), r=['selm', 'IDF'], w=[bkk])
            S.op('act', lambda e, bk=bk: e.copy(out=selmT[:, :, :], in_=bk[0:64, 0:128].unsqueeze(1).to_broadcast([64, 4, 128])), r=[bkk], w=['selmT'])
            attend(qt, list(range(0, qt + 1)), kslT, 'kslT', vsl, 'vsl', accs[0], ACK[0], True)
            finish_branch(accs[1], ACK[1], gts[:, qt, 8:12], False)
            finish_branch(accs[0], ACK[0], gts[:, qt, 4:8], False)
            oi = qt % 2
            S.op('act', lambda e, oi=oi: e.copy(out=ob[oi][:, :, :], in_=oacc[:, :, :]), r=['oacc'], w=[('ob', oi)])
            S.dma('sp', o_d[qt * 128:(qt + 1) * 128, :], ob[oi][:, :, :].rearrange("p h d -> p (h d)"), r=[('ob', oi)])
        S.finish()
    return nc


def nsa_consts():
    p = np.arange(128)
    c_all = np.arange(256)
    maskc = np.zeros((128, 32, 2, 128), np.float32)
    for qt in range(32):
        t = qt * 128 + p
        for cc in range(2):
            c = cc * 128 + p
            maskc[:, qt, cc, :] = ((16 * c[:, None] + 31 <= t[None, :]) & (c[:, None] < 255))
    blk = np.arange(64)
    sm = np.zeros((2, 128, 32, 64), np.float32)
    for qt in range(32):
        t = qt * 128 + p
        cur = (t // 64)[:, None]
        causal = blk[None, :] <= cur
        forced = (blk[None, :] == 0) | (causal & (blk[None, :] > cur - 2))
        sm[0, :, qt, :] = (causal & ~forced)
        sm[1, :, qt, :] = np.where(forced, 1e6, np.where(causal, 0.0, -1.0))
    ind = np.zeros((64, 32, 128), np.float32)
    for kt in range(32):
        ind[2 * kt + p // 64, kt, p] = 1.0
    cb = np.zeros((128, 2, 4, 128), np.float32)
    cb[:, 0] = np.where(p[:, None] > p[None, :], NEGB, 0.0)[:, None, :]
    cb[:, 1] = np.where(p[:, None] <= p[None, :], NEGB, 0.0)[:, None, :]
    c0 = np.arange(255) * 16
    s0 = np.arange(64) * 64
    ovm = np.clip(np.minimum(c0[:, None] + 32, s0[None, :] + 64) - np.maximum(c0[:, None], s0[None, :]), 0, None) / 32.0
    ov = np.zeros((256, 65), np.float32)
    ov[:255, :64] = ovm
    ov[:255, 64] = 1.0
    return dict(maskc=maskc.reshape(128, -1).astype(NPBF), sm=sm.reshape(2, 128, -1), ind=ind.reshape(64, -1).astype(NPBF),
                cbias=cb.reshape(128, -1).astype(NPBF), ov=ov.reshape(2, 128, 65),
                identb=np.eye(128, dtype=np.float32).astype(NPBF), identf=np.eye(128, dtype=np.float32))


def nsa_inputs(proj, qgate, cmp_w1, cmp_w2, cmp_pe):
    cst = nsa_consts()
    ims = []
    pj = proj.reshape(2, 4096, 7168)
    qg = qgate.reshape(2, 4096, 3, 16)
    peT = np.ascontiguousarray(np.transpose(cmp_pe, (0, 2, 1))).astype(np.float32)
    for c in range(NCORES):
        b, g = c // 4, c % 4
        P = pj[b]

        def sec(s):
            return P[:, s * 512 + g * 128: s * 512 + (g + 1) * 128]

        def qlay(off):
            q = P[:, off + g * 512: off + (g + 1) * 512].reshape(32, 128, 4, 128)
            return np.ascontiguousarray(np.transpose(q, (3, 0, 2, 1))).reshape(128, 32 * 512)
        im = dict(qn=qlay(3072), qr=qlay(5120),
                  kcT=np.ascontiguousarray(sec(0).T), vcT=np.ascontiguousarray(sec(1).T),
                  kslT=np.ascontiguousarray(sec(2).T), kwT=np.ascontiguousarray(sec(4).T),
                  vsl=np.ascontiguousarray(sec(3)), vw=np.ascontiguousarray(sec(5)),
                  gates=np.ascontiguousarray(qg[b][:, :, g * 4:(g + 1) * 4].reshape(4096, 12)),
                  w1=cmp_w1, w2=cmp_w2, peT=peT)
        im.update(cst)
        ims.append(im)
    return ims


def _rope_table():
    pos = np.arange(4096, dtype=np.float32)
    inv = (np.float32(10000.0) ** (-np.arange(64, dtype=np.float32) / np.float32(64))).astype(np.float32)
    ang = (pos[:, None] * inv[None, :]).astype(np.float32)
    return np.concatenate([np.cos(ang), np.sin(ang)], axis=1).astype(np.float32)


def _post1(aT_list, xres, w_out, g, b, router_w, router_bias):
    KIN = w_out.shape[0]
    nc = build_post1(KIN)
    ln_gb = np.stack([g, b]).astype(np.float32)
    ident = np.eye(128, dtype=np.float32)
    rb = router_bias.reshape(1, 32).astype(np.float32)
    ims = [{"aT": aT_list[c], "xres": np.ascontiguousarray(xres[c * 1024:(c + 1) * 1024]), "w_out": w_out, "ln_gb": ln_gb,
            "router_w": router_w, "router_b": rb, "ident": ident} for c in range(NCORES)]
    res = _run(nc, ims)
    x1 = np.concatenate([r["x1"] for r in res], axis=0)
    x1T = np.concatenate([r["x1T"] for r in res], axis=1)
    gates = np.concatenate([r["gates"] for r in res], axis=0)
    return x1, x1T, gates


def _moe(x1T, gates, wg, wu, wd):
    nc = build_moe(8192)
    x1T = np.ascontiguousarray(x1T)
    ims = [{"xT": x1T, "gates_c": np.ascontiguousarray(gates[:, 4 * c:4 * c + 4]), "wg": wg[4 * c:4 * c + 4],
            "wu": wu[4 * c:4 * c + 4], "wd": wd[4 * c:4 * c + 4]} for c in range(NCORES)]
    res = _run(nc, ims)
    return [r["y"] for r in res]


def _post2(ys, x1, g, b, proj_w=None):
    PROJ = proj_w is not None
    nc = build_post2(PROJ)
    ln_gb = np.stack([g, b]).astype(np.float32)
    ims = []
    if PROJ:
        cs = _rope_table()
        ident = np.eye(128, dtype=np.float32)
    for c in range(NCORES):
        rows = slice(c * 1024, (c + 1) * 1024)
        im = {"yp": np.stack([y[rows] for y in ys]), "x1": np.ascontiguousarray(x1[rows]), "ln_gb": ln_gb}
        if PROJ:
            p0 = (c * 1024) % 4096
            im.update({"kv_w": proj_w[0], "w_q": proj_w[1], "cs": np.ascontiguousarray(cs[p0:p0 + 1024]), "ident": ident})
        ims.append(im)
    res = _run(nc, ims)
    x2 = np.concatenate([r["x2"] for r in res], axis=0)
    if PROJ:
        return x2, np.concatenate([r["proj"] for r in res], axis=0), np.concatenate([r["qgate"] for r in res], axis=0)
    return x2


def kernel(x, a_w_in, a_conv_w, a_a_log, a_dt_bias, a_norm_w, a_w_out, kv_w, cmp_pe, cmp_w1, cmp_w2,
           b_w_q, b_w_out, router_w, router_bias, moe_w_gate, moe_w_up, moe_w_down, ln_g, ln_b):
    f = lambda a: np.asarray(a, dtype=np.float32)
    x, a_w_in, a_conv_w, a_a_log, a_dt_bias, a_norm_w, a_w_out = map(f, (x, a_w_in, a_conv_w, a_a_log, a_dt_bias, a_norm_w, a_w_out))
    kv_w, cmp_pe, cmp_w1, cmp_w2, b_w_q, b_w_out, router_w, router_bias = map(f, (kv_w, cmp_pe, cmp_w1, cmp_w2, b_w_q, b_w_out, router_w, router_bias))
    moe_w_gate, moe_w_up, moe_w_down, ln_g, ln_b = map(f, (moe_w_gate, moe_w_up, moe_w_down, ln_g, ln_b))
    xf = x.reshape(8192, D)
    res = _run(build_gdn_a(), gdn_a_inputs(x, a_w_in[0], a_conv_w[0]))
    res = _run(build_gdn_b(32), gdn_b_inputs([r["qkvT"] for r in res], [r["zT"] for r in res], [r["abT"] for r in res],
                                            a_a_log[0], a_dt_bias[0], a_norm_w[0]))
    og = np.zeros((2, 4096, 4096), dtype=NPBF)
    for c in range(NCORES):
        og[c // 4, :, (c % 4) * 1024:(c % 4 + 1) * 1024] = res[c]["og"]
    ogf = og.reshape(8192, 4096)
    aT = [np.ascontiguousarray(ogf[c * 1024:(c + 1) * 1024].T) for c in range(NCORES)]
    x1, x1T, gates = _post1(aT, xf, a_w_out[0], ln_g[0, 0], ln_b[0, 0], router_w, router_bias)
    ys = _moe(x1T, gates, moe_w_gate[0], moe_w_up[0], moe_w_down[0])
    x2, proj, qgate = _post2(ys, x1, ln_g[0, 1], ln_b[0, 1], (kv_w, b_w_q[0]))
    del ys
    res = _run(build_nsa(32), nsa_inputs(proj, qgate, cmp_w1, cmp_w2, cmp_pe))
    o = np.zeros((2, 4096, 2048), dtype=NPBF)
    for c in range(NCORES):
        o[c // 4, :, (c % 4) * 512:(c % 4 + 1) * 512] = res[c]["o"]
    of = o.reshape(8192, 2048)
    aT = [np.ascontiguousarray(of[c * 1024:(c + 1) * 1024].T) for c in range(NCORES)]
    x3, x3T, gates = _post1(aT, x2, b_w_out[0], ln_g[1, 0], ln_b[1, 0], router_w, router_bias)
    ys = _moe(x3T, gates, moe_w_gate[1], moe_w_up[1], moe_w_down[1])
    x4 = _post2(ys, x3, ln_g[1, 1], ln_b[1, 1], None)
    return x4.reshape(2, 4096, D).astype(np.float32)
```

```python
import contextlib
import os
import numpy as np
import ml_dtypes
import concourse.bass as bass
import concourse.mybir as mybir
from concourse.bass_utils import run_bass_kernel_spmd

F32 = mybir.dt.float32
BF16 = mybir.dt.bfloat16
AF = mybir.ActivationFunctionType
ALU = mybir.AluOpType
AX = mybir.AxisListType
NPBF = ml_dtypes.bfloat16

NCORES = 8
D = 2048
ALPHA = 4.0 ** 0.25
LN_EPS = 1e-5
RMS_EPS = 1e-6


class Sched:
    LIMIT = 30000
    NDMA = 16

    def __init__(self, nc, es):
        self.nc, self.es = nc, es
        self.eng = {'pe': nc.tensor, 'act': nc.scalar, 'dve': nc.vector, 'pool': nc.gpsimd, 'sp': nc.sync}
        self.sems = []
        self.cur = {}
        self.known = {e: {} for e in self.eng}
        self.lastw = {}
        self.readers = {}
        self.pe_sids = set()
        for e in ('pe', 'act', 'dve', 'pool'):
            self.cur[e] = [self._newsem(e), 0]
        self.pe_sids.add(self.cur['pe'][0])
        self.dma_slots = [[self._newsem('dma%d' % i), 0] for i in range(self.NDMA)]
        self.dma_rr = 0
        self.rec = None

    def record(self):
        self.rec = []

    def stop(self):
        l, self.rec = self.rec, None
        return l

    def play(self, items):
        for kind, a, kw in items:
            (self.op if kind == 'op' else self.dma)(*a, **kw)

    def play_interleaved(self, la, lb):
        na, nb_ = len(la), len(lb)
        ia = ib = 0
        while ia < na or ib < nb_:
            if ib >= nb_ or (ia < na and ia * nb_ <= ib * na):
                self.play([la[ia]])
                ia += 1
            else:
                self.play([lb[ib]])
                ib += 1

    def _newsem(self, name):
        s = self.es.enter_context(self.nc.semaphore('%s_%d' % (name, len(self.sems))))
        self.sems.append(s)
        return len(self.sems) - 1

    def _wait(self, e, deps):
        for sid, val in deps.items():
            if self.known[e].get(sid, 0) < val:
                self.eng[e].wait_ge(self.sems[sid], val)
                self.known[e][sid] = val

    def _deps(self, e, r, w):
        d = {}

        def add(sid, val):
            if d.get(sid, 0) < val:
                d[sid] = val
        for k in r:
            ev = self.lastw.get(k)
            if ev is not None:
                add(*ev)
        for k in w:
            ev = self.lastw.get(k)
            if ev is not None:
                add(*ev)
            for sid, val in self.readers.get(k, {}).items():
                add(sid, val)
        if e == 'pe':
            for sid in list(d):
                if sid in self.pe_sids:
                    del d[sid]
        return d

    def _record(self, ev, r, w):
        sid, val = ev
        for k in r:
            rd = self.readers.setdefault(k, {})
            if rd.get(sid, 0) < val:
                rd[sid] = val
        for k in w:
            self.lastw[k] = ev
            self.readers[k] = {}

    def op(self, e, fn, r=(), w=()):
        if self.rec is not None:
            self.rec.append(('op', (e, fn), dict(r=r, w=w)))
            return
        w = list(w) + [k for k in r if isinstance(k, tuple) and k[0] == 'bk']
        self._wait(e, self._deps(e, r, w))
        ins = fn(self.eng[e])
        c = self.cur[e]
        if c[1] >= self.LIMIT:
            c[0] = self._newsem(e)
            c[1] = 0
            if e == 'pe':
                self.pe_sids.add(c[0])
        c[1] += 1
        ins.then_inc(self.sems[c[0]], 1)
        self._record((c[0], c[1]), r, w)

    def dma(self, q, out, in_, r=(), w=(), **kw):
        if self.rec is not None:
            self.rec.append(('dma', (q, out, in_), dict(r=r, w=w, **kw)))
            return
        slot = self.dma_slots[self.dma_rr]
        self.dma_rr = (self.dma_rr + 1) % self.NDMA
        d = self._deps(q, r, w)
        if slot[1] > 0:
            d[slot[0]] = max(d.get(slot[0], 0), 16 * slot[1])
        self._wait(q, d)
        ins = self.eng[q].dma_start(out=out, in_=in_, **kw)
        slot[1] += 1
        ins.then_inc(self.sems[slot[0]], 16)
        self._record((slot[0], 16 * slot[1]), r, w)

    def _all_events(self):
        d = {slot[0]: 16 * slot[1] for slot in self.dma_slots if slot[1] > 0}
        for e, c in self.cur.items():
            if c[1] > 0:
                d[c[0]] = c[1]
        return d

    def sync_all(self):
        d = self._all_events()
        for e in self.eng:
            self._wait(e, d)

    def finish(self):
        self._wait('sp', self._all_events())


def _mk():
    return bass.Bass("TRN2", target_bir_lowering=False)


def _run(nc, in_maps):
    if os.environ.get("MK_TRACE"):
        res = run_bass_kernel_spmd(nc, in_maps, core_ids=list(range(NCORES)), trace=True)
        print("MK_TRACE exec_time_ns", res.exec_time_ns, flush=True)
        return res.results
    res = run_bass_kernel_spmd(nc, in_maps, core_ids=list(range(NCORES)))
    return res.results


def _layer_norm_tile(S, es, nc, h, tt, gbc, bbc, tmp):
    hk = ('h', tt)
    st, mv, rs = tmp['st'], tmp['mv'], tmp['rs']
    for c in range(4):
        S.op('dve', lambda e, c=c: e.bn_stats(out=st[:, c, :], in_=h[:, tt, c * 512:(c + 1) * 512]),
             r=[hk], w=['ln_st'])
    S.op('dve', lambda e: e.bn_aggr(out=mv[:, :], in_=st[:, :, :]), r=['ln_st'], w=['ln_mv'])
    S.op('act', lambda e: e.activation(out=rs[:, :], in_=mv[:, 1:2], func=AF.Sqrt, bias=tmp['eps'][:, 0:1]),
         r=['ln_mv', 'ln_eps'], w=['ln_rs'])
    S.op('dve', lambda e: e.reciprocal(out=rs[:, :], in_=rs[:, :]), r=['ln_rs'], w=['ln_rs'])
    S.op('dve', lambda e: e.tensor_scalar(out=h[:, tt, :], in0=h[:, tt, :], scalar1=mv[:, 0:1], scalar2=rs[:, 0:1],
                                          op0=ALU.subtract, op1=ALU.mult), r=[hk, 'ln_mv', 'ln_rs'], w=[hk])
    S.op('pool', lambda e: e.tensor_tensor(out=h[:, tt, :], in0=h[:, tt, :], in1=gbc[:, :], op=ALU.mult),
         r=[hk, 'gbc'], w=[hk])
    S.op('pool', lambda e: e.tensor_tensor(out=h[:, tt, :], in0=h[:, tt, :], in1=bbc[:, :], op=ALU.add),
         r=[hk, 'bbc'], w=[hk])


def build_post1(KIN):
    nc = _mk()
    KC = KIN // 128
    NT = 8
    aT = nc.dram_tensor("aT", [KIN, 1024], BF16, kind="ExternalInput").ap()
    xres = nc.dram_tensor("xres", [1024, D], F32, kind="ExternalInput").ap()
    w_out = nc.dram_tensor("w_out", [KIN, D], F32, kind="ExternalInput").ap()
    ln_gb = nc.dram_tensor("ln_gb", [2, D], F32, kind="ExternalInput").ap()
    router_w = nc.dram_tensor("router_w", [D, 32], F32, kind="ExternalInput").ap()
    router_b = nc.dram_tensor("router_b", [1, 32], F32, kind="ExternalInput").ap()
    ident_in = nc.dram_tensor("ident", [128, 128], F32, kind="ExternalInput").ap()
    x1 = nc.dram_tensor("x1", [1024, D], F32, kind="ExternalOutput").ap()
    x1T = nc.dram_tensor("x1T", [D, 1024], BF16, kind="ExternalOutput").ap()
    gates = nc.dram_tensor("gates", [1024, 32], F32, kind="ExternalOutput").ap()
    DC = 256
    with contextlib.ExitStack() as es:
        S = Sched(nc, es)
        sb = lambda name, shape, dt: es.enter_context(nc.sbuf_tensor(name, shape, dt))
        h = sb("h", [128, NT, D], F32)
        banks = [es.enter_context(nc.psum_tensor("bk%d" % i, [128, 512], F32)) for i in range(8)]
        with contextlib.ExitStack() as es1:
            a_sb = es1.enter_context(nc.sbuf_tensor("a_sb", [128, KC, 1024], BF16))
            wbuf = [es1.enter_context(nc.sbuf_tensor("wb%d" % i, [128, KC, DC], BF16)) for i in range(2)]
            half = KC // 2
            S.dma('sp', a_sb[:, 0:half, :], aT[0:half * 128, :].rearrange("(kc p) t -> p kc t", p=128), w=['a_sb'])
            S.dma('sp', a_sb[:, half:KC, :], aT[half * 128:KIN, :].rearrange("(kc p) t -> p kc t", p=128), w=['a_sb'])
            for tt in range(NT):
                S.dma('sp', h[:, tt, :], xres[tt * 128:(tt + 1) * 128, :], w=[('h', tt)])
            nb = 0
            for dc in range(D // DC):
                wb = wbuf[dc % 2]
                wk = ('wb', dc % 2)
                S.dma('pool', wb[:, :, :], w_out[:, dc * DC:(dc + 1) * DC].rearrange("(kc p) f -> p kc f", p=128), w=[wk])
                for tt in range(NT):
                    bk = banks[nb % 8]
                    bkk = ('bk', nb % 8)
                    nb += 1
                    for kc in range(KC):
                        S.op('pe', lambda e, kc=kc, bk=bk, tt=tt, wb=wb: e.matmul(
                            bk[:, 0:DC], a_sb[:, kc, tt * 128:(tt + 1) * 128], wb[:, kc, :],
                            start=(kc == 0), stop=(kc == KC - 1)), r=['a_sb', wk], w=[bkk])
                    S.op('dve', lambda e, bk=bk, tt=tt, dc=dc: e.scalar_tensor_tensor(
                        out=h[:, tt, dc * DC:(dc + 1) * DC], in0=h[:, tt, dc * DC:(dc + 1) * DC], scalar=ALPHA,
                        in1=bk[:, 0:DC], op0=ALU.mult, op1=ALU.add), r=[bkk, ('h', tt)], w=[('h', tt)])
            S.sync_all()
        gbc = sb("gbc", [128, D], F32)
        bbc = sb("bbc", [128, D], F32)
        rw = sb("rw", [128, 16, 32], F32)
        rb = sb("rb", [128, 32], F32)
        ident = sb("ident_sb", [128, 128], F32)
        tmp = dict(st=sb("ln_st", [128, 4, 6], F32), mv=sb("ln_mv", [128, 2], F32), rs=sb("ln_rs", [128, 1], F32),
                   eps=sb("ln_eps", [128, 1], F32))
        S.op('dve', lambda e: e.memset(tmp['eps'][:, :], LN_EPS), w=['ln_eps'])
        xT32 = sb("xT32", [128, 16, 128], F32)
        xTb = sb("xTb", [128, 16, 128], BF16)
        S.dma('sp', gbc[:, :], ln_gb[0:1, :].partition_broadcast(128), w=['gbc'])
        S.dma('sp', bbc[:, :], ln_gb[1:2, :].partition_broadcast(128), w=['bbc'])
        S.dma('sp', rw[:, :, :], router_w.rearrange("(kc p) e -> p kc e", p=128), w=['rw'])
        S.dma('sp', rb[:, :], router_b[0:1, :].partition_broadcast(128), w=['rb'])
        S.dma('sp', ident[:, :], ident_in[:, :], w=['ident'])
        r_aff = sb("r_aff", [128, 32], F32)
        r_bia = sb("r_bia", [128, 32], F32)
        r_ps = [sb("r_ps%d" % i, [128, 8], F32) for i in range(6)]
        r_gs = sb("r_gs", [128, 8], F32)
        r_gm = sb("r_gm", [128, 1], F32)
        r_gmask = sb("r_gmask", [128, 8], F32)
        r_m1 = sb("r_m1", [128, 8], F32)
        r_eq = sb("r_eq", [128, 32], F32)
        r_tmp = sb("r_tmp", [128, 32], F32)
        r_m2 = sb("r_m2", [128, 8], F32)
        r_sel = sb("r_sel", [128, 32], F32)
        r_den = sb("r_den", [128, 1], F32)
        r_gate = sb("r_gate", [128, 32], F32)
        RT = ['rtmp']
        for tt in range(NT):
            _layer_norm_tile(S, es, nc, h, tt, gbc, bbc, tmp)
            S.dma('sp', x1[tt * 128:(tt + 1) * 128, :], h[:, tt, :], r=[('h', tt)])
            for q4 in range(4):
                bk = banks[q4]
                bkk = ('bk', q4)
                for j in range(4):
                    kc = q4 * 4 + j
                    S.op('pe', lambda e, bk=bk, j=j, kc=kc, tt=tt: e.transpose(
                        bk[:, j * 128:(j + 1) * 128], h[:, tt, kc * 128:(kc + 1) * 128], ident[:, :]),
                        r=[('h', tt), 'ident'], w=[bkk])
                S.op('act', lambda e, bk=bk, q4=q4: e.copy(
                    out=xT32[:, q4 * 4:(q4 + 1) * 4, :], in_=bk[:, :].rearrange("p (j t) -> p j t", j=4)),
                    r=[bkk], w=['xT32'])
                S.op('dve', lambda e, bk=bk, q4=q4: e.tensor_copy(
                    out=xTb[:, q4 * 4:(q4 + 1) * 4, :], in_=bk[:, :].rearrange("p (j t) -> p j t", j=4)),
                    r=[bkk], w=['xTb'])
            S.dma('sp', x1T[:, tt * 128:(tt + 1) * 128].rearrange("(kc p) t -> p kc t", p=128), xTb[:, :, :], r=['xTb'])
            bk = banks[4]
            bkk = ('bk', 4)
            for kc in range(16):
                S.op('pe', lambda e, kc=kc, bk=bk: e.matmul(bk[:, 0:32], xT32[:, kc, :], rw[:, kc, :],
                                                           start=(kc == 0), stop=(kc == 15)),
                     r=['xT32', 'rw'], w=[bkk])
            S.op('act', lambda e, bk=bk: e.activation(out=r_aff[:, :], in_=bk[:, 0:32], func=AF.Sigmoid),
                 r=[bkk], w=['r_aff'])
            S.op('dve', lambda e: e.tensor_tensor(out=r_bia[:, :], in0=r_aff[:, :], in1=rb[:, :], op=ALU.add),
                 r=['r_aff', 'rb'], w=RT)
            b3 = r_bia[:, :].rearrange("p (g i) -> p g i", i=4)
            pairs = [(0, 1), (0, 2), (0, 3), (1, 2), (1, 3), (2, 3)]
            for pi, (i0, i1) in enumerate(pairs):
                S.op('dve', lambda e, pi=pi, i0=i0, i1=i1: e.tensor_tensor(
                    out=r_ps[pi][:, :], in0=b3[:, :, i0], in1=b3[:, :, i1], op=ALU.add), r=RT, w=RT)
            S.op('dve', lambda e: e.tensor_tensor(out=r_gs[:, :], in0=r_ps[0][:, :], in1=r_ps[1][:, :], op=ALU.max), r=RT, w=RT)
            for pi in range(2, 6):
                S.op('dve', lambda e, pi=pi: e.tensor_tensor(out=r_gs[:, :], in0=r_gs[:, :], in1=r_ps[pi][:, :], op=ALU.max), r=RT, w=RT)
            S.op('dve', lambda e: e.tensor_reduce(out=r_gm[:, :], in_=r_gs[:, :], axis=AX.X, op=ALU.max), r=RT, w=RT)
            S.op('dve', lambda e: e.tensor_scalar(out=r_gmask[:, :], in0=r_gs[:, :], scalar1=r_gm[:, 0:1], scalar2=None,
                                                  op0=ALU.is_ge), r=RT, w=RT)
            S.op('dve', lambda e: e.tensor_reduce(out=r_m1[:, :], in_=b3, axis=AX.X, op=ALU.max), r=RT, w=RT)
            S.op('dve', lambda e: e.tensor_tensor(out=r_eq[:, :].rearrange("p (g i) -> p g i", i=4), in0=b3,
                                                  in1=r_m1[:, :].unsqueeze(2).to_broadcast([128, 8, 4]), op=ALU.is_equal), r=RT, w=RT)
            S.op('dve', lambda e: e.scalar_tensor_tensor(out=r_tmp[:, :], in0=r_eq[:, :], scalar=-1e30, in1=r_bia[:, :],
                                                         op0=ALU.mult, op1=ALU.add), r=RT, w=RT)
            S.op('dve', lambda e: e.tensor_reduce(out=r_m2[:, :], in_=r_tmp[:, :].rearrange("p (g i) -> p g i", i=4),
                                                  axis=AX.X, op=ALU.max), r=RT, w=RT)
            S.op('dve', lambda e: e.tensor_tensor(out=r_sel[:, :].rearrange("p (g i) -> p g i", i=4), in0=b3,
                                                  in1=r_m2[:, :].unsqueeze(2).to_broadcast([128, 8, 4]), op=ALU.is_ge), r=RT, w=RT)
            S.op('dve', lambda e: e.tensor_tensor(out=r_sel[:, :].rearrange("p (g i) -> p g i", i=4),
                                                  in0=r_sel[:, :].rearrange("p (g i) -> p g i", i=4),
                                                  in1=r_gmask[:, :].unsqueeze(2).to_broadcast([128, 8, 4]), op=ALU.mult), r=RT, w=RT)
            S.op('dve', lambda e: e.tensor_tensor(out=r_sel[:, :], in0=r_sel[:, :], in1=r_aff[:, :], op=ALU.mult),
                 r=RT + ['r_aff'], w=RT)
            S.op('dve', lambda e: e.tensor_reduce(out=r_den[:, :], in_=r_sel[:, :], axis=AX.X, op=ALU.add), r=RT, w=RT)
            S.op('dve', lambda e: e.reciprocal(out=r_den[:, :], in_=r_den[:, :]), r=RT, w=RT)
            S.op('dve', lambda e: e.tensor_scalar(out=r_gate[:, :], in0=r_sel[:, :], scalar1=r_den[:, 0:1], scalar2=None,
                                                  op0=ALU.mult), r=RT, w=['r_gate'])
            S.dma('sp', gates[tt * 128:(tt + 1) * 128, :], r_gate[:, :], r=['r_gate'])
        S.finish()
    return nc


def build_moe(NTOK=8192):
    nc = _mk()
    NB = NTOK // 512
    xT = nc.dram_tensor("xT", [D, NTOK], BF16, kind="ExternalInput").ap()
    gates_c = nc.dram_tensor("gates_c", [NTOK, 4], F32, kind="ExternalInput").ap()
    wg = nc.dram_tensor("wg", [4, D, 512], F32, kind="ExternalInput").ap()
    wu = nc.dram_tensor("wu", [4, D, 512], F32, kind="ExternalInput").ap()
    wd = nc.dram_tensor("wd", [4, 512, D], F32, kind="ExternalInput").ap()
    y = nc.dram_tensor("y", [NTOK, D], F32, kind="ExternalOutput").ap()
    with contextlib.ExitStack() as es:
        S = Sched(nc, es)
        sb = lambda name, shape, dt: es.enter_context(nc.sbuf_tensor(name, shape, dt))
        banks = [es.enter_context(nc.psum_tensor("bk%d" % i, [128, 512], F32)) for i in range(8)]
        wg_sb = [sb("wg%d" % i, [128, 16, 512], BF16) for i in range(2)]
        wu_sb = [sb("wu%d" % i, [128, 16, 512], BF16) for i in range(2)]
        wd_sb = [sb("wd%d" % i, [128, 4, D], BF16) for i in range(2)]
        x_sb = [sb("x%d" % i, [128, 16, 512], BF16) for i in range(2)]
        hid = [sb("hid%d" % i, [128, 4, 512], BF16) for i in range(2)]
        sg = [sb("sg%d" % i, [128, 512], F32) for i in range(2)]
        ost = [sb("ost%d" % i, [128, D], F32) for i in range(3)]
        g_sb = sb("g_sb", [128, NTOK // 128, 4], F32)
        S.dma('sp', g_sb[:, :, :], gates_c.rearrange("(tt p) e -> p tt e", p=128), w=['g_sb'])

        def load_w(e):
            b = e % 2
            S.dma('pool', wg_sb[b][:, :, :], wg[e].rearrange("(kc p) f -> p kc f", p=128), w=[('wg', b)])
            S.dma('pool', wu_sb[b][:, :, :], wu[e].rearrange("(kc p) f -> p kc f", p=128), w=[('wu', b)])
            S.dma('pool', wd_sb[b][:, :, :], wd[e].rearrange("(fc p) d -> p fc d", p=128), w=[('wd', b)])

        def load_x(i):
            tb = i % NB
            b = i % 2
            S.dma('sp', x_sb[b][:, :, :], xT[:, tb * 512:(tb + 1) * 512].rearrange("(kc p) t -> p kc t", p=128),
                  w=[('x', b)])

        load_w(0)
        load_x(0)
        it = 0
        ngu = 0
        nd = 0
        nos = 0
        for e in range(4):
            if e + 1 < 4:
                load_w(e + 1)
            wb = e % 2
            for tb in range(NB):
                if it + 1 < 4 * NB:
                    load_x(it + 1)
                xb = it % 2
                hb = it % 2
                for fc in range(4):
                    gb, ub = (0, 1) if ngu % 2 == 0 else (2, 3)
                    ngu += 1
                    for kc in range(16):
                        S.op('pe', lambda en, kc=kc, fc=fc, gb=gb: en.matmul(
                            banks[gb][:, :], wg_sb[wb][:, kc, fc * 128:(fc + 1) * 128], x_sb[xb][:, kc, :],
                            start=(kc == 0), stop=(kc == 15)), r=[('wg', wb), ('x', xb)], w=[('bk', gb)])
                    for kc in range(16):
                        S.op('pe', lambda en, kc=kc, fc=fc, ub=ub: en.matmul(
                            banks[ub][:, :], wu_sb[wb][:, kc, fc * 128:(fc + 1) * 128], x_sb[xb][:, kc, :],
                            start=(kc == 0), stop=(kc == 15)), r=[('wu', wb), ('x', xb)], w=[('bk', ub)])
                    sgi = ngu % 2
                    S.op('act', lambda en, gb=gb, sgi=sgi: en.activation(out=sg[sgi][:, :], in_=banks[gb][:, :], func=AF.Silu),
                         r=[('bk', gb)], w=[('sg', sgi)])
                    S.op('dve', lambda en, ub=ub, sgi=sgi, fc=fc: en.tensor_tensor(
                        out=hid[hb][:, fc, :], in0=sg[sgi][:, :], in1=banks[ub][:, :], op=ALU.mult),
                        r=[('sg', sgi), ('bk', ub)], w=[('hid', hb)])
                for t4 in range(4):
                    tt = tb * 4 + t4
                    oi = nos % 3
                    nos += 1
                    for dc in range(4):
                        db = 4 + nd % 4
                        nd += 1
                        for fc in range(4):
                            S.op('pe', lambda en, fc=fc, dc=dc, db=db, t4=t4: en.matmul(
                                banks[db][:, :], hid[hb][:, fc, t4 * 128:(t4 + 1) * 128], wd_sb[wb][:, fc, dc * 512:(dc + 1) * 512],
                                start=(fc == 0), stop=(fc == 3)), r=[('hid', hb), ('wd', wb)], w=[('bk', db)])
                        if dc % 2 == 0:
                            S.op('act', lambda en, db=db, dc=dc, oi=oi, tt=tt: en.activation(
                                out=ost[oi][:, dc * 512:(dc + 1) * 512], in_=banks[db][:, :], func=AF.Identity,
                                scale=g_sb[:, tt, e:e + 1]), r=[('bk', db), 'g_sb'], w=[('os', oi)])
                        else:
                            S.op('dve', lambda en, db=db, dc=dc, oi=oi, tt=tt: en.tensor_scalar(
                                out=ost[oi][:, dc * 512:(dc + 1) * 512], in0=banks[db][:, :], scalar1=g_sb[:, tt, e:e + 1],
                                scalar2=None, op0=ALU.mult), r=[('bk', db), 'g_sb'], w=[('os', oi)])
                    if e == 0:
                        S.dma('sp', y[tt * 128:(tt + 1) * 128, :], ost[oi][:, :], r=[('os', oi)], w=[('y', tt)])
                    else:
                        S.dma('pool', y[tt * 128:(tt + 1) * 128, :], ost[oi][:, :], r=[('os', oi)], w=[('y', tt)],
                              accum_op=ALU.add)
                it += 1
        S.finish()
    return nc


def build_post2(PROJ):
    nc = _mk()
    NT = 8
    yp = nc.dram_tensor("yp", [8, 1024, D], F32, kind="ExternalInput").ap()
    x1 = nc.dram_tensor("x1", [1024, D], F32, kind="ExternalInput").ap()
    ln_gb = nc.dram_tensor("ln_gb", [2, D], F32, kind="ExternalInput").ap()
    x2 = nc.dram_tensor("x2", [1024, D], F32, kind="ExternalOutput").ap()
    if PROJ:
        kv_w = nc.dram_tensor("kv_w", [D, 3072], F32, kind="ExternalInput").ap()
        w_q = nc.dram_tensor("w_q", [D, 2096], F32, kind="ExternalInput").ap()
        cs = nc.dram_tensor("cs", [1024, 128], F32, kind="ExternalInput").ap()
        ident_in = nc.dram_tensor("ident", [128, 128], F32, kind="ExternalInput").ap()
        proj = nc.dram_tensor("proj", [1024, 7168], BF16, kind="ExternalOutput").ap()
        qgate = nc.dram_tensor("qgate", [1024, 48], F32, kind="ExternalOutput").ap()
    with contextlib.ExitStack() as es:
        S = Sched(nc, es)
        sb = lambda name, shape, dt: es.enter_context(nc.sbuf_tensor(name, shape, dt))
        h = sb("h", [128, NT, D], F32)
        banks = [es.enter_context(nc.psum_tensor("bk%d" % i, [128, 512], F32)) for i in range(8)]
        gbc = sb("gbc", [128, D], F32)
        bbc = sb("bbc", [128, D], F32)
        tmp = dict(st=sb("ln_st", [128, 4, 6], F32), mv=sb("ln_mv", [128, 2], F32), rs=sb("ln_rs", [128, 1], F32),
                   eps=sb("ln_eps", [128, 1], F32))
        S.op('dve', lambda e: e.memset(tmp['eps'][:, :], LN_EPS), w=['ln_eps'])
        S.dma('sp', gbc[:, :], ln_gb[0:1, :].partition_broadcast(128), w=['gbc'])
        S.dma('sp', bbc[:, :], ln_gb[1:2, :].partition_broadcast(128), w=['bbc'])
        with contextlib.ExitStack() as es1:
            stg = [es1.enter_context(nc.sbuf_tensor("stg%d" % i, [128, D], F32)) for i in range(3)]
            ns = 0
            for tt in range(NT):
                S.dma('sp', h[:, tt, :], x1[tt * 128:(tt + 1) * 128, :], w=[('h', tt)])
                for c in range(8):
                    si = ns % 3
                    ns += 1
                    S.dma('sp', stg[si][:, :], yp[c, tt * 128:(tt + 1) * 128, :], w=[('stg', si)])
                    eng = 'dve' if c % 2 == 0 else 'pool'
                    if c == 0:
                        S.op(eng, lambda e, si=si, tt=tt: e.scalar_tensor_tensor(
                            out=h[:, tt, :], in0=h[:, tt, :], scalar=ALPHA, in1=stg[si][:, :], op0=ALU.mult, op1=ALU.add),
                            r=[('stg', si), ('h', tt)], w=[('h', tt)])
                    else:
                        S.op(eng, lambda e, si=si, tt=tt: e.tensor_tensor(
                            out=h[:, tt, :], in0=h[:, tt, :], in1=stg[si][:, :], op=ALU.add),
                            r=[('stg', si), ('h', tt)], w=[('h', tt)])
                _layer_norm_tile(S, es, nc, h, tt, gbc, bbc, tmp)
                S.dma('sp', x2[tt * 128:(tt + 1) * 128, :], h[:, tt, :], r=[('h', tt)])
            S.sync_all()
        if PROJ:
            ident = sb("ident_sb", [128, 128], F32)
            xT = sb("xT", [128, 16, 1024], BF16)
            cs_sb = sb("cs_sb", [128, NT, 128], F32)
            wbuf = [sb("wb%d" % i, [128, 16, 512], BF16) for i in range(2)]
            rsb = [sb("rsb%d" % i, [128, 512], F32) for i in range(2)]
            t1 = [sb("t1_%d" % i, [128, 256], F32) for i in range(2)]
            t2 = [sb("t2_%d" % i, [128, 256], F32) for i in range(2)]
            ob = [sb("ob%d" % i, [128, 512], BF16) for i in range(4)]
            gst = [sb("gst%d" % i, [128, 48], F32) for i in range(2)]
            S.dma('sp', ident[:, :], ident_in[:, :], w=['ident'])
            S.dma('sp', cs_sb[:, :, :], cs.rearrange("(tt p) c -> p tt c", p=128), w=['cs'])
            for tt in range(NT):
                for q4 in range(4):
                    bk = banks[q4]
                    bkk = ('bk', q4)
                    for j in range(4):
                        kc = q4 * 4 + j
                        S.op('pe', lambda e, bk=bk, j=j, kc=kc, tt=tt: e.transpose(
                            bk[:, j * 128:(j + 1) * 128], h[:, tt, kc * 128:(kc + 1) * 128], ident[:, :]),
                            r=[('h', tt), 'ident'], w=[bkk])
                    eng = 'act' if q4 % 2 == 0 else 'dve'
                    if eng == 'act':
                        S.op('act', lambda e, bk=bk, q4=q4, tt=tt: e.copy(
                            out=xT[:, q4 * 4:(q4 + 1) * 4, tt * 128:(tt + 1) * 128],
                            in_=bk[:, :].rearrange("p (j t) -> p j t", j=4)), r=[bkk], w=[('xT', tt)])
                    else:
                        S.op('dve', lambda e, bk=bk, q4=q4, tt=tt: e.tensor_copy(
                            out=xT[:, q4 * 4:(q4 + 1) * 4, tt * 128:(tt + 1) * 128],
                            in_=bk[:, :].rearrange("p (j t) -> p j t", j=4)), r=[bkk], w=[('xT', tt)])
            chunks = []
            for c in range(6):
                chunks.append((kv_w[:, c * 512:(c + 1) * 512], 512, 'rope' if c in (2, 4) else 'plain', c * 512))
            for c in range(4):
                chunks.append((w_q[:, c * 512:(c + 1) * 512], 512, 'q', c * 512))
            chunks.append((w_q[:, 2048:2096], 48, 'gate', 0))
            nb = 0
            no = 0
            nr = 0
            for ci, (wsrc, ncol, kind, c0) in enumerate(chunks):
                wb = wbuf[ci % 2]
                wk = ('wb', ci % 2)
                S.dma('pool', wb[:, :, 0:ncol], wsrc.rearrange("(kc p) f -> p kc f", p=128), w=[wk])
                for tt in range(NT):
                    bi = nb % 8
                    nb += 1
                    bk, bkk = banks[bi], ('bk', bi)
                    for kc in range(16):
                        S.op('pe', lambda e, kc=kc, bk=bk, tt=tt, wb=wb, ncol=ncol: e.matmul(
                            bk[:, 0:ncol], xT[:, kc, tt * 128:(tt + 1) * 128], wb[:, kc, 0:ncol],
                            start=(kc == 0), stop=(kc == 15)), r=[('xT', tt), wk], w=[bkk])
                    rows = slice(tt * 128, (tt + 1) * 128)
                    if kind == 'gate':
                        gi = tt % 2
                        S.op('act', lambda e, bk=bk, gi=gi: e.activation(out=gst[gi][:, :], in_=bk[:, 0:48], func=AF.Sigmoid),
                             r=[bkk], w=[('gst', gi)])
                        S.dma('sp', qgate[rows, :], gst[gi][:, :], r=[('gst', gi)])
                        continue
                    if kind in ('plain', 'q'):
                        oi = no % 4
                        no += 1
                        S.op('act', lambda e, bk=bk, oi=oi: e.copy(out=ob[oi][:, :], in_=bk[:, :]), r=[bkk], w=[('ob', oi)])
                        oc = c0 if kind == 'plain' else 3072 + c0
                        S.dma('sp', proj[rows, oc:oc + 512], ob[oi][:, :], r=[('ob', oi)])
                    if kind in ('rope', 'q'):
                        ri = nr % 2
                        nr += 1
                        oi = no % 4
                        no += 1
                        S.op('dve', lambda e, bk=bk, ri=ri: e.tensor_copy(out=rsb[ri][:, :], in_=bk[:, :]), r=[bkk], w=[('rsb', ri)])
                        rv = rsb[ri][:, :].rearrange("p (h d) -> p h d", h=4)
                        ov = ob[oi][:, :].rearrange("p (h d) -> p h d", h=4)
                        cosb = cs_sb[:, tt, 0:64].unsqueeze(1).to_broadcast([128, 4, 64])
                        sinb = cs_sb[:, tt, 64:128].unsqueeze(1).to_broadcast([128, 4, 64])
                        t1v = t1[ri][:, :].rearrange("p (h d) -> p h d", h=4)
                        t2v = t2[ri][:, :].rearrange("p (h d) -> p h d", h=4)
                        S.op('dve', lambda e, rv=rv, t1v=t1v, cosb=cosb: e.tensor_tensor(out=t1v, in0=rv[:, :, 0:64], in1=cosb, op=ALU.mult),
                             r=[('rsb', ri), 'cs'], w=[('t1', ri)])
                        S.op('dve', lambda e, rv=rv, t1v=t1v, sinb=sinb: e.scalar_tensor_tensor(
                            out=ov[:, :, 0:64], in0=rv[:, :, 64:128], scalar=-1.0, in1=sinb, op0=ALU.mult, op1=ALU.mult),
                            r=[('rsb', ri), 'cs'], w=[('ob', oi)])
                        S.op('dve', lambda e, ov=ov, t1v=t1v: e.tensor_tensor(out=ov[:, :, 0:64], in0=ov[:, :, 0:64], in1=t1v, op=ALU.add),
                             r=[('t1', ri), ('ob', oi)], w=[('ob', oi)])
                        S.op('pool', lambda e, rv=rv, t2v=t2v, cosb=cosb: e.tensor_tensor(out=t2v, in0=rv[:, :, 64:128], in1=cosb, op=ALU.mult),
                             r=[('rsb', ri), 'cs'], w=[('t2', ri)])
                        S.op('pool', lambda e, rv=rv, ov=ov, sinb=sinb: e.tensor_tensor(out=ov[:, :, 64:128], in0=rv[:, :, 0:64], in1=sinb, op=ALU.mult),
                             r=[('rsb', ri), 'cs', ('ob', oi)], w=[('ob', oi)])
                        S.op('pool', lambda e, ov=ov, t2v=t2v: e.tensor_tensor(out=ov[:, :, 64:128], in0=ov[:, :, 64:128], in1=t2v, op=ALU.add),
                             r=[('t2', ri), ('ob', oi)], w=[('ob', oi)])
                        oc = c0 if kind == 'rope' else 5120 + c0
                        S.dma('sp', proj[rows, oc:oc + 512], ob[oi][:, :], r=[('ob', oi)])
        S.finish()
    return nc


def build_gdn_a():
    nc = _mk()
    T = 4096
    HT = 2048
    xT = nc.dram_tensor("xT", [D, T], F32, kind="ExternalInput").ap()
    w_sl = nc.dram_tensor("w_sl", [D, 3088], F32, kind="ExternalInput").ap()
    conv_w = nc.dram_tensor("conv_w", [2048, 4], F32, kind="ExternalInput").ap()
    qkvT = nc.dram_tensor("qkvT", [2048, T], BF16, kind="ExternalOutput").ap()
    zT = nc.dram_tensor("zT", [1024, T], BF16, kind="ExternalOutput").ap()
    abT = nc.dram_tensor("abT", [16, T], F32, kind="ExternalOutput").ap()
    with contextlib.ExitStack() as es:
        S = Sched(nc, es)
        sb = lambda name, shape, dt: es.enter_context(nc.sbuf_tensor(name, shape, dt))
        banks = [es.enter_context(nc.psum_tensor("bk%d" % i, [128, 512], F32)) for i in range(8)]
        x_sb = sb("x_sb", [128, 16, HT], BF16)
        wt = [sb("wt%d" % i, [128, 16, 128], BF16) for i in range(2)]
        P = [sb("P%d" % i, [128, HT + 3], F32) for i in range(2)]
        acc = [sb("acc%d" % i, [128, HT], F32) for i in range(2)]
        ost = [sb("ost%d" % i, [128, HT], BF16) for i in range(2)]
        abst = sb("abst", [16, HT], F32)
        carry = sb("carry", [128, 16, 3], F32)
        cw = sb("cw", [128, 16, 4], F32)
        S.dma('sp', cw[:, :, :], conv_w.rearrange("(ct p) j -> p ct j", p=128), w=['cw'])
        nb = 0
        it = 0
        for hf in range(2):
            for g4 in range(4):
                S.dma('pool', x_sb[:, g4 * 4:(g4 + 1) * 4, :],
                      xT[g4 * 512:(g4 + 1) * 512, hf * HT:(hf + 1) * HT].rearrange("(kc p) t -> p kc t", p=128),
                      w=[('x', g4)])
            xk = [('x', g4) for g4 in range(4)]
            for ct in range(25):
                nch = 128 if ct < 24 else 16
                wi = it % 2
                pi = it % 2
                it += 1
                S.dma('pool', wt[wi][:, :, 0:nch], w_sl[:, ct * 128:ct * 128 + nch].rearrange("(kc p) f -> p kc f", p=128),
                      w=[('wt', wi)])
                if ct < 16:
                    if hf == 0:
                        S.op('dve', lambda e, pi=pi: e.memset(P[pi][:, 0:3], 0.0), w=[('P', pi)])
                    else:
                        S.op('dve', lambda e, pi=pi, ct=ct: e.tensor_copy(out=P[pi][:, 0:3], in_=carry[:, ct, :]),
                             r=['carry'], w=[('P', pi)])
                for tb in range(4):
                    bi = nb % 8
                    nb += 1
                    bk, bkk = banks[bi], ('bk', bi)
                    for kc in range(16):
                        S.op('pe', lambda e, kc=kc, bk=bk, tb=tb, wi=wi, nch=nch: e.matmul(
                            bk[0:nch, :], wt[wi][:, kc, 0:nch], x_sb[:, kc, tb * 512:(tb + 1) * 512],
                            start=(kc == 0), stop=(kc == 15)), r=[('wt', wi)] + xk, w=[bkk])
                    cols = slice(tb * 512, (tb + 1) * 512)
                    if ct < 16:
                        S.op('act', lambda e, bk=bk, tb=tb, pi=pi: e.copy(out=P[pi][:, 3 + tb * 512:3 + (tb + 1) * 512], in_=bk[:, :]),
                             r=[bkk], w=[('P', pi)])
                    elif ct < 24:
                        S.op('act', lambda e, bk=bk, cols=cols, pi=pi: e.copy(out=ost[pi][:, cols], in_=bk[:, :]),
                             r=[bkk], w=[('ost', pi)])
                    else:
                        S.op('act', lambda e, bk=bk, cols=cols: e.copy(out=abst[:, cols], in_=bk[0:16, :]),
                             r=[bkk], w=['abst'])
                tsl = slice(hf * HT, (hf + 1) * HT)
                if ct < 16:
                    S.op('dve', lambda e, pi=pi, ct=ct: e.tensor_copy(out=carry[:, ct, :], in_=P[pi][:, HT:HT + 3]),
                         r=[('P', pi)], w=['carry'])
                    S.op('dve', lambda e, pi=pi, ct=ct: e.tensor_scalar(
                        out=acc[pi][:, :], in0=P[pi][:, 0:HT], scalar1=cw[:, ct, 0:1], scalar2=None, op0=ALU.mult),
                        r=[('P', pi), 'cw'], w=[('acc', pi)])
                    for j in range(1, 4):
                        S.op('dve', lambda e, pi=pi, ct=ct, j=j: e.scalar_tensor_tensor(
                            out=acc[pi][:, :], in0=P[pi][:, j:j + HT], scalar=cw[:, ct, j:j + 1], in1=acc[pi][:, :],
                            op0=ALU.mult, op1=ALU.add), r=[('P', pi), 'cw', ('acc', pi)], w=[('acc', pi)])
                    S.op('act', lambda e, pi=pi: e.activation(out=ost[pi][:, :], in_=acc[pi][:, :], func=AF.Silu),
                         r=[('acc', pi)], w=[('ost', pi)])
                    S.dma('sp', qkvT[ct * 128:(ct + 1) * 128, tsl], ost[pi][:, :], r=[('ost', pi)])
                elif ct < 24:
                    S.dma('sp', zT[(ct - 16) * 128:(ct - 15) * 128, tsl], ost[pi][:, :], r=[('ost', pi)])
                else:
                    S.dma('sp', abT[:, tsl], abst[:, :], r=['abst'])
        S.finish()
    return nc


def gdn_a_inputs(x, a_w_in, a_conv_w):
    ims = []
    for c in range(NCORES):
        b, hg = c // 4, c % 4
        w = a_w_in
        w_sl = np.concatenate([w[:, hg * 512:(hg + 1) * 512], w[:, 2048 + hg * 512:2048 + (hg + 1) * 512],
                               w[:, 4096 + hg * 1024:4096 + (hg + 1) * 1024], w[:, 8192 + hg * 1024:8192 + (hg + 1) * 1024],
                               w[:, 12288 + hg * 8:12288 + (hg + 1) * 8], w[:, 12320 + hg * 8:12320 + (hg + 1) * 8]], axis=1)
        cwf = a_conv_w
        cw = np.concatenate([cwf[:, hg * 512:(hg + 1) * 512], cwf[:, 2048 + hg * 512:2048 + (hg + 1) * 512],
                             cwf[:, 4096 + hg * 1024:4096 + (hg + 1) * 1024]], axis=1).T
        ims.append({"xT": np.ascontiguousarray(x[b].T), "w_sl": np.ascontiguousarray(w_sl),
                    "conv_w": np.ascontiguousarray(cw)})
    return ims


def build_gdn_b(NCH=32):
    nc = _mk()
    T = 4096
    qk = nc.dram_tensor("qk", [T, 8, 128], BF16, kind="ExternalInput").ap()
    v_in = nc.dram_tensor("v", [T, 8, 128], BF16, kind="ExternalInput").ap()
    z_in = nc.dram_tensor("z", [T, 8, 128], BF16, kind="ExternalInput").ap()
    ab = nc.dram_tensor("ab", [T, 16], F32, kind="ExternalInput").ap()
    hp = nc.dram_tensor("hp", [2, 8], F32, kind="ExternalInput").ap()
    nw = nc.dram_tensor("nw", [1, 128], F32, kind="ExternalInput").ap()
    cmask = nc.dram_tensor("cmask", [4, 128, 128], F32, kind="ExternalInput").ap()
    identb_in = nc.dram_tensor("identb", [128, 128], BF16, kind="ExternalInput").ap()
    og = nc.dram_tensor("og", [T, 1024], BF16, kind="ExternalOutput").ap()
    with contextlib.ExitStack() as es:
        S = Sched(nc, es)
        sb = lambda name, shape, dt: es.enter_context(nc.sbuf_tensor(name, shape, dt))
        banks = [es.enter_context(nc.psum_tensor("bk%d" % i, [128, 512], F32)) for i in range(7)]
        tpb = es.enter_context(nc.psum_tensor("tpb", [128, 1024], BF16))
        TPK = ('bk', 7)
        nbk = [0]

        def newbank():
            i = nbk[0] % 7
            nbk[0] += 1
            return banks[i], ('bk', i)

        ML = sb("ML", [128, 128], F32)
        MU = sb("MU", [128, 128], F32)
        MUI = sb("MUI", [128, 128], F32)
        IDF = sb("IDF", [128, 128], F32)
        ONES = sb("ONES", [128, 128], F32)
        IDB = sb("IDB", [128, 128], BF16)
        hpb = sb("hpb", [128, 16], F32)
        nwb = sb("nwb", [128, 128], F32)
        negexpA = sb("negexpA", [128, 8], F32)
        epsr = sb("epsr", [128, 1], F32)
        one1 = sb("one1", [128, 1], F32)
        for i, t in enumerate((ML, MU, MUI, IDF)):
            S.dma('sp', t[:, :], cmask[i], w=[t.name])
        S.dma('sp', IDB[:, :], identb_in[:, :], w=['IDB'])
        S.dma('sp', hpb[:, 0:8], hp[0:1, :].partition_broadcast(128), w=['hpb'])
        S.dma('sp', hpb[:, 8:16], hp[1:2, :].partition_broadcast(128), w=['hpb'])
        S.dma('sp', nwb[:, :], nw[0:1, :].partition_broadcast(128), w=['nwb'])
        S.op('dve', lambda e: e.memset(ONES[:, :], 1.0), w=['ONES'])
        S.op('dve', lambda e: e.memset(epsr[:, :], RMS_EPS), w=['epsr'])
        S.op('dve', lambda e: e.memset(one1[:, :], 1.0), w=['one1'])
        S.op('act', lambda e: e.activation(out=negexpA[:, :], in_=hpb[:, 0:8], func=AF.Exp), r=['hpb'], w=['negexpA'])
        S.op('dve', lambda e: e.tensor_scalar(out=negexpA[:, :], in0=negexpA[:, :], scalar1=-1.0, scalar2=None, op0=ALU.mult),
             r=['negexpA'], w=['negexpA'])

        qk_sb = [sb("qk%d" % i, [128, 8, 128], BF16) for i in range(2)]
        v_sb = [sb("v%d" % i, [128, 8, 128], BF16) for i in range(2)]
        z_sb = [sb("z%d" % i, [128, 8, 128], BF16) for i in range(2)]
        ab_sb = [sb("ab%d" % i, [128, 16], F32) for i in range(2)]
        sq = sb("sq", [128, 8, 128], F32)
        ssq = sb("ssq", [128, 8], F32)
        rn = sb("rn", [128, 8], F32)
        qkn = sb("qkn", [128, 8, 128], BF16)
        qkT = sb("qkT", [128, 8, 128], BF16)
        qg = sb("qg", [128, 8, 128], BF16)
        qgT = sb("qgT", [128, 8, 128], BF16)
        kd = sb("kd", [128, 8, 128], BF16)
        sz = sb("sz", [128, 8, 128], F32)
        nwz = sb("nwz", [128, 8, 128], F32)
        xa = sb("xa", [128, 8], F32)
        ax = sb("ax", [128, 8], F32)
        ex = sb("ex", [128, 8], F32)
        gg = sb("gg", [128, 8], F32)
        beta = sb("beta", [128, 8], F32)
        gcs = sb("gcs", [128, 16], F32)
        eg = sb("eg", [128, 8], F32)
        negeg = sb("negeg", [128, 8], F32)
        dl = sb("dl", [128, 8], F32)
        egl = sb("egl", [128, 8], F32)
        eglast = sb("eglast", [128, 8], F32)
        KKm = [sb("KKm%d" % i, [128, 128], F32) for i in range(4)]
        KQm = [sb("KQm%d" % i, [128, 128], F32) for i in range(4)]
        Gm = [sb("Gm%d" % i, [128, 128], F32) for i in range(8)]
        Ee = [sb("Ee%d" % i, [128, 128], F32) for i in range(8)]
        Wa = [sb("Wa%d" % i, [128, 128], F32) for i in range(8)]
        WTa = [sb("WTa%d" % i, [128, 128], F32) for i in range(8)]
        Wb = [sb("Wb%d" % i, [128, 128], F32) for i in range(8)]
        WTb = [sb("WTb%d" % i, [128, 128], F32) for i in range(8)]
        Qm = [sb("Qm%d" % i, [128, 128], F32) for i in range(8)]
        AT = [sb("AT%d" % i, [128, 128], BF16) for i in range(8)]
        PTb = [sb("PTb%d" % i, [128, 128], BF16) for i in range(8)]
        Xv = [sb("Xv%d" % i, [128, 128], BF16) for i in range(8)]
        vnew = [sb("vnew%d" % i, [128, 128], BF16) for i in range(8)]
        Sf = [sb("Sf%d" % i, [128, 128], F32) for i in range(8)]
        Sb = [sb("Sb%d" % i, [128, 128], BF16) for i in range(8)]
        o_sb = sb("o_sb", [128, 8, 128], F32)
        osq = sb("osq", [128, 8, 128], F32)
        ossq = sb("ossq", [128, 8], F32)
        orstd = sb("orstd", [128, 8], F32)
        ogst = [sb("ogst%d" % i, [128, 8, 128], BF16) for i in range(2)]
        for hv in range(8):
            S.op('dve', lambda e, hv=hv: e.memset(Sf[hv][:, :], 0.0), w=[Sf[hv].name])
            S.op('pool', lambda e, hv=hv: e.memset(Sb[hv][:, :], 0.0), w=[Sb[hv].name])

        def load(n):
            p = n % 2
            rows = slice(n * 128, (n + 1) * 128)
            S.dma('sp', qk_sb[p][:, :, :], qk[rows], w=[qk_sb[p].name])
            S.dma('sp', v_sb[p][:, :, :], v_in[rows], w=[v_sb[p].name])
            S.dma('sp', z_sb[p][:, :, :], z_in[rows], w=[z_sb[p].name])
            S.dma('sp', ab_sb[p][:, :], ab[rows, :], w=[ab_sb[p].name])

        def bc3(ap2, n):
            return ap2.unsqueeze(2).to_broadcast([128, n, 128])

        load(0)
        for n in range(NCH):
            p = n % 2
            if n + 1 < NCH:
                load(n + 1)
            QK, V, Z, AB = qk_sb[p], v_sb[p], z_sb[p], ab_sb[p]
            S.op('dve', lambda e: e.tensor_tensor(out=sq[:, :, :], in0=QK[:, :, :], in1=QK[:, :, :], op=ALU.mult), r=[QK.name], w=['sq'])
            S.op('dve', lambda e: e.tensor_reduce(out=ssq[:, :], in_=sq[:, :, :], axis=AX.X, op=ALU.add), r=['sq'], w=['ssq'])
            S.op('act', lambda e: e.activation(out=rn[:, :], in_=ssq[:, :], func=AF.Sqrt, bias=epsr[:, 0:1]), r=['ssq', 'epsr'], w=['rn'])
            S.op('dve', lambda e: e.reciprocal(out=rn[:, :], in_=rn[:, :]), r=['rn'], w=['rn'])
            S.op('dve', lambda e: e.tensor_scalar(out=rn[:, 0:4], in0=rn[:, 0:4], scalar1=128.0 ** -0.5, scalar2=None, op0=ALU.mult), r=['rn'], w=['rn'])
            S.op('dve', lambda e: e.tensor_tensor(out=qkn[:, :, :], in0=QK[:, :, :], in1=bc3(rn[:, :], 8), op=ALU.mult), r=[QK.name, 'rn'], w=['qkn'])
            S.op('dve', lambda e: e.tensor_tensor(out=xa[:, :], in0=AB[:, 0:8], in1=hpb[:, 8:16], op=ALU.add), r=[AB.name, 'hpb'], w=['xa'])
            S.op('dve', lambda e: e.tensor_scalar(out=ax[:, :], in0=xa[:, :], scalar1=-1.0, scalar2=None, op0=ALU.mult), r=['xa'], w=['ax'])
            S.op('dve', lambda e: e.tensor_tensor(out=ax[:, :], in0=ax[:, :], in1=xa[:, :], op=ALU.min), r=['xa', 'ax'], w=['ax'])
            S.op('act', lambda e: e.activation(out=ex[:, :], in_=ax[:, :], func=AF.Exp), r=['ax'], w=['ex'])
            S.op('act', lambda e: e.activation(out=ex[:, :], in_=ex[:, :], func=AF.Ln, bias=one1[:, 0:1]), r=['ex', 'one1'], w=['ex'])
            S.op('dve', lambda e: e.scalar_tensor_tensor(out=gg[:, :], in0=xa[:, :], scalar=0.0, in1=ex[:, :], op0=ALU.max, op1=ALU.add), r=['xa', 'ex'], w=['gg'])
            S.op('dve', lambda e: e.tensor_tensor(out=gg[:, :], in0=gg[:, :], in1=negexpA[:, :], op=ALU.mult), r=['gg', 'negexpA'], w=['gg'])
            S.op('act', lambda e: e.activation(out=beta[:, :], in_=AB[:, 8:16], func=AF.Sigmoid), r=[AB.name], w=['beta'])
            bk, bkk = newbank()
            S.op('pe', lambda e, bk=bk: e.matmul(bk[:, 0:8], MUI[:, :], gg[:, :], start=True, stop=True), r=['MUI', 'gg'], w=[bkk])
            S.op('pe', lambda e, bk=bk: e.matmul(bk[:, 8:16], ONES[:, :], gg[:, :], start=True, stop=True), r=['ONES', 'gg'], w=[bkk])
            S.op('act', lambda e, bk=bk: e.copy(out=gcs[:, :], in_=bk[:, 0:16]), r=[bkk], w=['gcs'])
            S.op('act', lambda e: e.activation(out=eg[:, :], in_=gcs[:, 0:8], func=AF.Exp), r=['gcs'], w=['eg'])
            S.op('dve', lambda e: e.tensor_scalar(out=negeg[:, :], in0=eg[:, :], scalar1=-1.0, scalar2=None, op0=ALU.mult), r=['eg'], w=['negeg'])
            S.op('dve', lambda e: e.tensor_tensor(out=dl[:, :], in0=gcs[:, 8:16], in1=gcs[:, 0:8], op=ALU.subtract), r=['gcs'], w=['dl'])
            S.op('act', lambda e: e.activation(out=egl[:, :], in_=dl[:, :], func=AF.Exp), r=['dl'], w=['egl'])
            S.op('act', lambda e: e.activation(out=eglast[:, :], in_=gcs[:, 8:16], func=AF.Exp), r=['gcs'], w=['eglast'])
            for hk in range(4):
                S.op('dve', lambda e, hk=hk: e.tensor_tensor(
                    out=qg[:, 2 * hk:2 * hk + 2, :], in0=qkn[:, hk:hk + 1, :].to_broadcast([128, 2, 128]),
                    in1=bc3(eg[:, 2 * hk:2 * hk + 2], 2), op=ALU.mult), r=['qkn', 'eg'], w=['qg'])
                S.op('pool', lambda e, hk=hk: e.tensor_tensor(
                    out=kd[:, 2 * hk:2 * hk + 2, :], in0=qkn[:, 4 + hk:5 + hk, :].to_broadcast([128, 2, 128]),
                    in1=bc3(egl[:, 2 * hk:2 * hk + 2], 2), op=ALU.mult), r=['qkn', 'egl'], w=['kd'])
            for j in range(8):
                S.op('pe', lambda e, j=j: e.transpose(tpb[:, j * 128:(j + 1) * 128], qkn[:, j, :], IDB[:, :]), r=['qkn', 'IDB'], w=[TPK])
            S.op('act', lambda e: e.copy(out=qkT[:, :, :], in_=tpb[:, :].rearrange("p (j t) -> p j t", j=8)), r=[TPK], w=['qkT'])
            for j in range(8):
                S.op('pe', lambda e, j=j: e.transpose(tpb[:, j * 128:(j + 1) * 128], qg[:, j, :], IDB[:, :]), r=['qg', 'IDB'], w=[TPK])
            S.op('dve', lambda e: e.tensor_copy(out=qgT[:, :, :], in_=tpb[:, :].rearrange("p (j t) -> p j t", j=8)), r=[TPK], w=['qgT'])
            S.op('act', lambda e: e.activation(out=sz[:, :, :], in_=Z[:, :, :], func=AF.Silu), r=[Z.name], w=['sz'])
            S.op('pool', lambda e: e.tensor_tensor(out=nwz[:, :, :], in0=sz[:, :, :], in1=nwb[:, :].unsqueeze(1).to_broadcast([128, 8, 128]), op=ALU.mult),
                 r=['sz', 'nwb'], w=['nwz'])
            for hk in range(4):
                bk, bkk = newbank()
                S.op('pe', lambda e, bk=bk, hk=hk: e.matmul(bk[:, 0:128], qkT[:, 4 + hk, :], qkT[:, 4 + hk, :], start=True, stop=True), r=['qkT'], w=[bkk])
                S.op('pe', lambda e, bk=bk, hk=hk: e.matmul(bk[:, 128:256], qkT[:, 4 + hk, :], qkT[:, hk, :], start=True, stop=True), r=['qkT'], w=[bkk])
                S.op('dve', lambda e, bk=bk, hk=hk: e.tensor_tensor(out=KKm[hk][:, :], in0=bk[:, 0:128], in1=MU[:, :], op=ALU.mult), r=[bkk, 'MU'], w=[KKm[hk].name])
                S.op('dve', lambda e, bk=bk, hk=hk: e.tensor_tensor(out=KQm[hk][:, :], in0=bk[:, 128:256], in1=MUI[:, :], op=ALU.mult), r=[bkk, 'MUI'], w=[KQm[hk].name])
            for hv in range(8):
                hk = hv // 2
                S.op('pool', lambda e, hv=hv: e.tensor_scalar(out=Gm[hv][:, :], in0=ML[:, :], scalar1=gg[:, hv:hv + 1], scalar2=None, op0=ALU.mult),
                     r=['ML', 'gg'], w=[Gm[hv].name])
                bk, bkk = newbank()
                S.op('pe', lambda e, bk=bk, hv=hv: e.matmul(bk[:, 0:128], Gm[hv][:, :], MUI[:, :], start=True, stop=True), r=[Gm[hv].name, 'MUI'], w=[bkk])
                S.op('act', lambda e, bk=bk, hv=hv: e.activation(out=Ee[hv][:, :], in_=bk[:, 0:128], func=AF.Exp), r=[bkk], w=[Ee[hv].name])
                S.op('dve', lambda e, hv=hv, hk=hk: e.scalar_tensor_tensor(out=Wa[hv][:, :], in0=KKm[hk][:, :], scalar=beta[:, hv:hv + 1], in1=Ee[hv][:, :],
                                                                         op0=ALU.mult, op1=ALU.mult), r=[KKm[hk].name, 'beta', Ee[hv].name], w=[Wa[hv].name])
                S.op('pool', lambda e, hv=hv, hk=hk: e.tensor_tensor(out=AT[hv][:, :], in0=KQm[hk][:, :], in1=Ee[hv][:, :], op=ALU.mult),
                     r=[KQm[hk].name, Ee[hv].name], w=[AT[hv].name])
            for grp in range(2):
                hvs = range(grp * 4, grp * 4 + 4)
                cur = {}
                for hv in hvs:
                    bk, bkk = newbank()
                    S.op('pe', lambda e, bk=bk, hv=hv: e.transpose(bk[:, 0:128], Wa[hv][:, :], IDF[:, :]), r=[Wa[hv].name, 'IDF'], w=[bkk])
                    S.op('act', lambda e, bk=bk, hv=hv: e.copy(out=WTa[hv][:, :], in_=bk[:, 0:128]), r=[bkk], w=[WTa[hv].name])
                    S.op('dve', lambda e, hv=hv: e.tensor_tensor(out=Qm[hv][:, :], in0=IDF[:, :], in1=Wa[hv][:, :], op=ALU.subtract),
                         r=['IDF', Wa[hv].name], w=[Qm[hv].name])
                    cur[hv] = (Wa[hv], WTa[hv], Wb[hv], WTb[hv])
                for k in range(1, 7):
                    for hv in hvs:
                        W, WT, Wn, WTn = cur[hv]
                        bk, bkk = newbank()
                        S.op('pe', lambda e, bk=bk, W=W, WT=WT: e.matmul(bk[:, 0:128], W[:, :], WT[:, :], start=True, stop=True), r=[W.name, WT.name], w=[bkk])
                        S.op('act', lambda e, bk=bk, WTn=WTn: e.copy(out=WTn[:, :], in_=bk[:, 0:128]), r=[bkk], w=[WTn.name])
                        if k < 6:
                            bk2, bkk2 = newbank()
                            S.op('pe', lambda e, bk2=bk2, W=W, WT=WT: e.matmul(bk2[:, 0:128], WT[:, :], W[:, :], start=True, stop=True), r=[W.name, WT.name], w=[bkk2])
                            S.op('dve', lambda e, bk2=bk2, Wn=Wn: e.tensor_copy(out=Wn[:, :], in_=bk2[:, 0:128]), r=[bkk2], w=[Wn.name])
                    for hv in hvs:
                        W, WT, Wn, WTn = cur[hv]
                        bk, bkk = newbank()
                        S.op('pe', lambda e, bk=bk, WTn=WTn, hv=hv: e.matmul(bk[:, 0:128], WTn[:, :], Qm[hv][:, :], start=True, stop=True),
                             r=[WTn.name, Qm[hv].name], w=[bkk])
                        S.op('dve', lambda e, bk=bk, hv=hv: e.tensor_tensor(out=Qm[hv][:, :], in0=Qm[hv][:, :], in1=bk[:, 0:128], op=ALU.add),
                             r=[bkk, Qm[hv].name], w=[Qm[hv].name])
                        cur[hv] = (Wn, WTn, W, WT)
                for hv in hvs:
                    S.op('act', lambda e, hv=hv: e.copy(out=PTb[hv][:, :], in_=Qm[hv][:, :]), r=[Qm[hv].name], w=[PTb[hv].name])
            for grp in range(2):
                hvs = range(grp * 4, grp * 4 + 4)
                bR = {}
                for hv in hvs:
                    hk = hv // 2
                    bR[hv] = newbank()
                    bk, bkk = bR[hv]
                    S.op('pe', lambda e, bk=bk, hv=hv, hk=hk: e.matmul(bk[:, 0:128], qkT[:, 4 + hk, :], Sb[hv][:, :], start=True, stop=True),
                         r=['qkT', Sb[hv].name], w=[bkk])
                for hv in hvs:
                    bk, bkk = bR[hv]
                    S.op('dve', lambda e, bk=bk, hv=hv: e.scalar_tensor_tensor(out=Xv[hv][:, :], in0=bk[:, 0:128], scalar=negeg[:, hv:hv + 1], in1=V[:, hv, :],
                                                                         op0=ALU.mult, op1=ALU.add), r=[bkk, 'negeg', V.name], w=[Xv[hv].name])
                for hv in hvs:
                    bR[hv] = newbank()
                    bk, bkk = bR[hv]
                    S.op('pe', lambda e, bk=bk, hv=hv: e.matmul(bk[:, 0:128], PTb[hv][:, :], Xv[hv][:, :], start=True, stop=True),
                         r=[PTb[hv].name, Xv[hv].name], w=[bkk])
                for hv in hvs:
                    bk, bkk = bR[hv]
                    S.op('act', lambda e, bk=bk, hv=hv: e.activation(out=vnew[hv][:, :], in_=bk[:, 0:128], func=AF.Identity, scale=beta[:, hv:hv + 1]),
                         r=[bkk, 'beta'], w=[vnew[hv].name])
                for hv in hvs:
                    bk, bkk = newbank()
                    S.op('pe', lambda e, bk=bk, hv=hv: e.matmul(bk[:, 0:128], qgT[:, hv, :], Sb[hv][:, :], start=True, stop=False),
                         r=['qgT', Sb[hv].name], w=[bkk])
                    S.op('pe', lambda e, bk=bk, hv=hv: e.matmul(bk[:, 0:128], AT[hv][:, :], vnew[hv][:, :], start=False, stop=True),
                         r=[AT[hv].name, vnew[hv].name], w=[bkk])
                    S.op('pe', lambda e, bk=bk, hv=hv: e.matmul(bk[:, 128:256], kd[:, hv, :], vnew[hv][:, :], start=True, stop=True),
                         r=['kd', vnew[hv].name], w=[bkk])
                    S.op('act', lambda e, bk=bk, hv=hv: e.copy(out=o_sb[:, hv, :], in_=bk[:, 0:128]), r=[bkk], w=['o_sb'])
                    S.op('dve', lambda e, bk=bk, hv=hv: e.scalar_tensor_tensor(out=Sf[hv][:, :], in0=Sf[hv][:, :], scalar=eglast[:, hv:hv + 1], in1=bk[:, 128:256],
                                                                         op0=ALU.mult, op1=ALU.add), r=[bkk, 'eglast', Sf[hv].name], w=[Sf[hv].name])
                    S.op('act', lambda e, hv=hv: e.copy(out=Sb[hv][:, :], in_=Sf[hv][:, :]), r=[Sf[hv].name], w=[Sb[hv].name])
            S.op('dve', lambda e: e.tensor_tensor(out=osq[:, :, :], in0=o_sb[:, :, :], in1=o_sb[:, :, :], op=ALU.mult), r=['o_sb'], w=['osq'])
            S.op('dve', lambda e: e.tensor_reduce(out=ossq[:, :], in_=osq[:, :, :], axis=AX.X, op=ALU.add), r=['osq'], w=['ossq'])
            S.op('act', lambda e: e.activation(out=orstd[:, :], in_=ossq[:, :], func=AF.Sqrt, bias=epsr[:, 0:1], scale=1.0 / 128.0), r=['ossq', 'epsr'], w=['orstd'])
            S.op('dve', lambda e: e.reciprocal(out=orstd[:, :], in_=orstd[:, :]), r=['orstd'], w=['orstd'])
            S.op('dve', lambda e: e.tensor_tensor(out=osq[:, :, :], in0=o_sb[:, :, :], in1=bc3(orstd[:, :], 8), op=ALU.mult), r=['o_sb', 'orstd'], w=['osq'])
            S.op('pool', lambda e, p=p: e.tensor_tensor(out=ogst[p][:, :, :], in0=osq[:, :, :], in1=nwz[:, :, :], op=ALU.mult), r=['osq', 'nwz'], w=[ogst[p].name])
            S.dma('sp', og[n * 128:(n + 1) * 128, :], ogst[p][:, :, :].rearrange("p h d -> p (h d)"), r=[ogst[p].name])
        S.finish()
    return nc


def gdn_b_consts():
    i = np.arange(128)
    ML = (i[:, None] > i[None, :]).astype(np.float32)
    MU = (i[None, :] > i[:, None]).astype(np.float32)
    MUI = (i[None, :] >= i[:, None]).astype(np.float32)
    return np.stack([ML, MU, MUI, np.eye(128, dtype=np.float32)]), np.eye(128, dtype=np.float32).astype(NPBF)


def gdn_b_inputs(qkvT_all, zT_all, abT_all, a_log, dt_bias, norm_w):
    cm, idb = gdn_b_consts()
    ims = []
    for c in range(NCORES):
        hg = c % 4
        qkvT, zT, abT = qkvT_all[c], zT_all[c], abT_all[c]
        qk = np.ascontiguousarray(qkvT[0:1024].T).reshape(4096, 8, 128)
        v = np.ascontiguousarray(qkvT[1024:2048].T).reshape(4096, 8, 128)
        z = np.ascontiguousarray(zT.T).reshape(4096, 8, 128)
        ab = np.ascontiguousarray(abT.T)
        hp = np.stack([a_log[hg * 8:(hg + 1) * 8], dt_bias[hg * 8:(hg + 1) * 8]]).astype(np.float32)
        ims.append({"qk": qk, "v": v, "z": z, "ab": ab, "hp": hp, "nw": norm_w.reshape(1, 128).astype(np.float32),
                    "cmask": cm, "identb": idb})
    return ims


SCL = 128.0 ** -0.5
NEGB = -30000.0


def build_nsa(NQT=32):
    nc = _mk()
    T = 4096
    qn_d = nc.dram_tensor("qn", [128, 32 * 512], BF16, kind="ExternalInput").ap()
    qr_d = nc.dram_tensor("qr", [128, 32 * 512], BF16, kind="ExternalInput").ap()
    kcT_d = nc.dram_tensor("kcT", [128, T], BF16, kind="ExternalInput").ap()
    vcT_d = nc.dram_tensor("vcT", [128, T], BF16, kind="ExternalInput").ap()
    kslT_d = nc.dram_tensor("kslT", [128, T], BF16, kind="ExternalInput").ap()
    kwT_d = nc.dram_tensor("kwT", [128, T], BF16, kind="ExternalInput").ap()
    vsl_d = nc.dram_tensor("vsl", [T, 128], BF16, kind="ExternalInput").ap()
    vw_d = nc.dram_tensor("vw", [T, 128], BF16, kind="ExternalInput").ap()
    gates_d = nc.dram_tensor("gates", [T, 12], F32, kind="ExternalInput").ap()
    w1_d = nc.dram_tensor("w1", [2, 4096, 512], F32, kind="ExternalInput").ap()
    w2_d = nc.dram_tensor("w2", [2, 512, 128], F32, kind="ExternalInput").ap()
    peT_d = nc.dram_tensor("peT", [2, 128, 32], F32, kind="ExternalInput").ap()
    ov_d = nc.dram_tensor("ov", [2, 128, 65], F32, kind="ExternalInput").ap()
    maskc_d = nc.dram_tensor("maskc", [128, 32 * 2 * 128], BF16, kind="ExternalInput").ap()
    sm_d = nc.dram_tensor("sm", [2, 128, 32 * 64], F32, kind="ExternalInput").ap()
    ind_d = nc.dram_tensor("ind", [64, 32 * 128], BF16, kind="ExternalInput").ap()
    cbias_d = nc.dram_tensor("cbias", [128, 2 * 512], BF16, kind="ExternalInput").ap()
    identb_d = nc.dram_tensor("identb", [128, 128], BF16, kind="ExternalInput").ap()
    identf_d = nc.dram_tensor("identf", [128, 128], F32, kind="ExternalInput").ap()
    o_d = nc.dram_tensor("o", [T, 512], BF16, kind="ExternalOutput").ap()
    with contextlib.ExitStack() as es:
        S = Sched(nc, es)
        sb = lambda name, shape, dt: es.enter_context(nc.sbuf_tensor(name + "_s", shape, dt))
        banks = [es.enter_context(nc.psum_tensor("bk%d" % i, [128, 512], F32)) for i in range(4)]
        accs = [es.enter_context(nc.psum_tensor("acc%d" % i, [128, 4, 256], F32)) for i in range(2)]
        ACK = [('bk', 'A'), ('bk', 'B')]
        nbk = [0]

        def newbank(n=3):
            i = nbk[0] % n
            nbk[0] += 1
            return banks[i], ('bk', i)

        kcmpT = sb("kcmpT", [128, 256], BF16)
        Rext = sb("Rext", [128, 2, 193], BF16)
        IDB = sb("IDB", [128, 128], BF16)
        IDF = sb("IDF", [128, 128], F32)
        S.dma('sp', IDB[:, :], identb_d[:, :], w=['IDB'])
        S.dma('sp', IDF[:, :], identf_d[:, :], w=['IDF'])
        S.op('dve', lambda e: e.memset(kcmpT[:, :], 0.0), w=['kcmpT'])
        for cc in range(2):
            S.dma('pool', Rext[:, cc, 128:193], ov_d[cc], w=['Rext'])
        with contextlib.ExitStack() as es0:
            sb0 = lambda name, shape, dt: es0.enter_context(nc.sbuf_tensor(name + "_s", shape, dt))
            srcT = [sb0("kcT", [128, T], BF16), sb0("vcT", [128, T], BF16)]
            S.dma('sp', srcT[0][:, :], kcT_d[:, :], w=['srcT0'])
            S.dma('sp', srcT[1][:, :], vcT_d[:, :], w=['srcT1'])
            w1b = [sb0("w1b%d" % i, [128, 32, 128], BF16) for i in range(2)]
            w2b = [sb0("w2b%d" % i, [128, 4, 128], BF16) for i in range(2)]
            peb = [sb0("peb%d" % i, [128, 32], BF16) for i in range(2)]
            hid = [sb0("hid%d" % i, [128, 4, 256], BF16) for i in range(2)]
            biasv = sb0("biasv", [128, 8], F32)
            nw1 = 0
            for kv in range(2):
                S.dma('pool', w2b[kv][:, :, :], w2_d[kv].rearrange("(hc p) d -> p hc d", p=128), w=['w2b%d' % kv])
                S.dma('pool', peb[kv][:, :], peT_d[kv], w=['peb%d' % kv])
                S.op('dve', lambda e, kv=kv: e.memset(hid[kv][:, :, :], 0.0), w=['hid%d' % kv])
                for hc in range(4):
                    wi = nw1 % 2
                    nw1 += 1
                    for half in range(2):
                        S.dma('pool', w1b[wi][:, half * 16:(half + 1) * 16, :],
                              w1_d[kv, half * 2048:(half + 1) * 2048, hc * 128:(hc + 1) * 128].rearrange("(l d) f -> d l f", d=128),
                              w=[('w1b', wi)])
                    bk, bkk = newbank()
                    for l in range(32):
                        S.op('pe', lambda e, bk=bk, l=l, wi=wi, kv=kv: e.matmul(bk[:, 0:1], w1b[wi][:, l, :], peb[kv][:, l:l + 1],
                                                                              start=(l == 0), stop=(l == 31)), r=[('w1b', wi), 'peb%d' % kv], w=[bkk])
                    S.op('act', lambda e, bk=bk, kv=kv, hc=hc: e.copy(out=biasv[:, kv * 4 + hc:kv * 4 + hc + 1], in_=bk[:, 0:1]), r=[bkk], w=['biasv'])
                    bk, bkk = newbank()
                    for l in range(32):
                        S.op('pe', lambda e, bk=bk, l=l, wi=wi, kv=kv: e.matmul(bk[:, 0:255], w1b[wi][:, l, :], srcT[kv][:, l:l + 16 * 254 + 1:16],
                                                                              start=(l == 0), stop=(l == 31)), r=[('w1b', wi), 'srcT%d' % kv], w=[bkk])
                    S.op('act', lambda e, bk=bk, kv=kv, hc=hc: e.activation(out=hid[kv][:, hc, 0:255], in_=bk[:, 0:255], func=AF.Silu,
                                                                          bias=biasv[:, kv * 4 + hc:kv * 4 + hc + 1]), r=[bkk, 'biasv'], w=['hid%d' % kv])
            bk, bkk = newbank()
            for hc in range(4):
                S.op('pe', lambda e, bk=bk, hc=hc: e.matmul(bk[:, 0:255], w2b[0][:, hc, :], hid[0][:, hc, 0:255], start=(hc == 0), stop=(hc == 3)),
                     r=['w2b0', 'hid0'], w=[bkk])
            S.op('act', lambda e, bk=bk: e.copy(out=kcmpT[:, 0:255], in_=bk[:, 0:255]), r=[bkk], w=['kcmpT'])
            for cc in range(2):
                bk, bkk = newbank()
                for hc in range(4):
                    S.op('pe', lambda e, bk=bk, hc=hc, cc=cc: e.matmul(bk[:, 0:128], hid[1][:, hc, cc * 128:(cc + 1) * 128], w2b[1][:, hc, :],
                                                                     start=(hc == 0), stop=(hc == 3)), r=['w2b1', 'hid1'], w=[bkk])
                S.op('act', lambda e, bk=bk, cc=cc: e.copy(out=Rext[:, cc, 0:128], in_=bk[:, 0:128]), r=[bkk], w=['Rext'])
            S.sync_all()
        qn = sb("qn", [128, 32, 512], BF16)
        qr = sb("qr", [128, 32, 512], BF16)
        kslT = sb("kslT", [128, T], BF16)
        kwT = sb("kwT", [128, T], BF16)
        vsl = sb("vsl", [128, 32, 129], BF16)
        vw = sb("vw", [128, 32, 129], BF16)
        gts = sb("gts", [128, 32, 12], F32)
        maskc = sb("maskc", [128, 32, 2, 128], BF16)
        sm1 = sb("sm1", [128, 32, 64], F32)
        sm2 = sb("sm2", [128, 32, 64], F32)
        ind = sb("ind", [64, 32, 128], BF16)
        cbias = sb("cbias", [128, 2, 512], BF16)
        for half in range(2):
            hs = slice(half * 16, (half + 1) * 16)
            S.dma('sp', qn[:, hs, :], qn_d[:, half * 8192:(half + 1) * 8192].rearrange("p (a b) -> p a b", b=512), w=['qn'])
            S.dma('sp', qr[:, hs, :], qr_d[:, half * 8192:(half + 1) * 8192].rearrange("p (a b) -> p a b", b=512), w=['qr'])
        S.dma('sp', kslT[:, :], kslT_d[:, :], w=['kslT'])
        S.dma('sp', kwT[:, :], kwT_d[:, :], w=['kwT'])
        S.op('dve', lambda e: e.memset(vsl[:, :, 128:129], 1.0), w=['vsl'])
        S.op('dve', lambda e: e.memset(vw[:, :, 128:129], 1.0), w=['vw'])
        S.dma('sp', vsl[:, :, 0:128], vsl_d.rearrange("(kt p) d -> p kt d", p=128), w=['vsl'])
        S.dma('sp', vw[:, :, 0:128], vw_d.rearrange("(kt p) d -> p kt d", p=128), w=['vw'])
        S.dma('sp', gts[:, :, :], gates_d.rearrange("(qt p) g -> p qt g", p=128), w=['gts'])
        S.dma('sp', maskc[:, :, :, :], maskc_d.rearrange("p (a b c) -> p a b c", a=32, b=2), w=['maskc'])
        S.dma('sp', sm1[:, :, :], sm_d[0].rearrange("p (a b) -> p a b", b=64), w=['sm1'])
        S.dma('sp', sm2[:, :, :], sm_d[1].rearrange("p (a b) -> p a b", b=64), w=['sm2'])
        S.dma('sp', ind[:, :, :], ind_d.rearrange("p (a b) -> p a b", b=128), w=['ind'])
        S.dma('sp', cbias[:, :, :], cbias_d.rearrange("p (a b) -> p a b", b=512), w=['cbias'])
        Ec = [sb("Ec%d" % i, [128, 4, 128], BF16) for i in range(2)]
        Es = [sb("Es%d" % i, [128, 512], BF16) for i in range(3)]
        den = sb("den", [128, 4], F32)
        rec = sb("rec", [128, 4], F32)
        rg = sb("rg", [128, 4], F32)
        pn = sb("pn", [128, 4, 64], F32)
        pslc = sb("pslc", [128, 64], F32)
        score = sb("score", [128, 64], F32)
        work = sb("work", [128, 64], F32)
        m8a = sb("m8a", [128, 8], F32)
        m8b = sb("m8b", [128, 8], F32)
        thr = sb("thr", [128, 1], F32)
        selm = sb("selm", [128, 64], F32)
        selmT = sb("selmT", [64, 4, 128], BF16)
        oacc = sb("oacc", [128, 4, 128], F32)
        otmp = sb("otmp", [128, 4, 128], F32)
        ob = [sb("ob%d" % i, [128, 4, 128], BF16) for i in range(2)]
        nes = [0]

        def bc(ap2):
            return ap2.unsqueeze(2).to_broadcast([128, 4, 128])

        def attend(qt, kts, kT, kTk, vext, vk, acc, ack, use_sel):
            def emit_scores(idx):
                kt = kts[idx]
                bk, bkk = newbank()
                diag = (kt == qt)
                far = (not use_sel) and (kt == qt - 4)
                more = use_sel or diag or far
                S.op('pe', lambda e, bk=bk, kt=kt: e.matmul(bk[:, :], kT[:, kt * 128:(kt + 1) * 128], qr[:, qt, :], start=True, stop=not more),
                     r=[kTk, 'qr'], w=[bkk])
                if use_sel:
                    S.op('pe', lambda e, bk=bk, kt=kt: e.matmul(bk[:, :], ind[:, kt, :], selmT[:, :, :].rearrange("p h q -> p (h q)"), start=False,
                                                              stop=not diag), r=['ind', 'selmT'], w=[bkk])
                if diag:
                    S.op('pe', lambda e, bk=bk: e.matmul(bk[:, :], IDB[:, :], cbias[:, 0, :], start=False, stop=not far), r=['IDB', 'cbias'], w=[bkk])
                if far:
                    S.op('pe', lambda e, bk=bk: e.matmul(bk[:, :], IDB[:, :], cbias[:, 1, :], start=False, stop=True), r=['IDB', 'cbias'], w=[bkk])
                ei = nes[0] % 3
                nes[0] += 1
                S.op('act', lambda e, bk=bk, ei=ei: e.activation(out=Es[ei][:, :], in_=bk[:, :], func=AF.Exp, scale=SCL), r=[bkk], w=[('Es', ei)])
                return ei

            def emit_pv(idx, ei):
                kt = kts[idx]
                for h in range(4):
                    S.op('pe', lambda e, h=h, ei=ei, kt=kt, idx=idx: e.matmul(acc[:, h, 0:129], Es[ei][:, h * 128:(h + 1) * 128], vext[:, kt, :],
                                                                            start=(idx == 0 and h % 2 == 0), stop=(idx == len(kts) - 1)),
                         r=[('Es', ei), vk], w=[ack])

            prev = None
            for idx in range(len(kts)):
                ei = emit_scores(idx)
                if prev is not None:
                    emit_pv(*prev)
                prev = (idx, ei)
            emit_pv(*prev)

        def finish_branch(acc, ack, gcol, first):
            S.op('dve', lambda e: e.reciprocal(out=rec[:, :], in_=acc[:, :, 128]), r=[ack], w=['rec'])
            S.op('dve', lambda e: e.tensor_tensor(out=rg[:, :], in0=rec[:, :], in1=gcol, op=ALU.mult), r=['rec', 'gts'], w=['rg'])
            if first:
                S.op('dve', lambda e: e.tensor_tensor(out=oacc[:, :, :], in0=acc[:, :, 0:128], in1=bc(rg[:, :]), op=ALU.mult), r=[ack, 'rg'], w=['oacc'])
            else:
                S.op('dve', lambda e: e.tensor_tensor(out=otmp[:, :, :], in0=acc[:, :, 0:128], in1=bc(rg[:, :]), op=ALU.mult), r=[ack, 'rg'], w=['otmp'])
                S.op('pool', lambda e: e.tensor_tensor(out=oacc[:, :, :], in0=oacc[:, :, :], in1=otmp[:, :, :], op=ALU.add), r=['otmp', 'oacc'], w=['oacc'])

        for qt in range(NQT):
            ccs = [0] if qt < 16 else [0, 1]
            A, AK = accs[0], ACK[0]
            for cc in ccs:
                bk, bkk = newbank()
                S.op('pe', lambda e, bk=bk, cc=cc: e.matmul(bk[:, :], kcmpT[:, cc * 128:(cc + 1) * 128], qn[:, qt, :], start=True, stop=True),
                     r=['kcmpT', 'qn'], w=[bkk])
                S.op('act', lambda e, bk=bk, cc=cc: e.activation(out=Ec[cc][:, :, :], in_=bk[:, :].rearrange("p (h q) -> p h q", h=4), func=AF.Exp, scale=SCL),
                     r=[bkk], w=[('Ec', cc)])
                S.op('dve', lambda e, cc=cc: e.tensor_tensor(out=Ec[cc][:, :, :], in0=Ec[cc][:, :, :],
                                                            in1=maskc[:, qt, cc, :].unsqueeze(1).to_broadcast([128, 4, 128]), op=ALU.mult),
                     r=[('Ec', cc), 'maskc'], w=[('Ec', cc)])
            for ci, cc in enumerate(ccs):
                for h in range(4):
                    S.op('pe', lambda e, h=h, cc=cc, ci=ci: e.matmul(A[:, h, 0:193], Ec[cc][:, h, :], Rext[:, cc, :],
                                                                   start=(ci == 0 and h % 2 == 0), stop=(ci == len(ccs) - 1)),
                         r=[('Ec', cc), 'Rext'], w=[AK])
            S.op('dve', lambda e: e.tensor_scalar(out=den[:, :], in0=A[:, :, 192], scalar1=1e-30, scalar2=None, op0=ALU.max), r=[AK], w=['den'])
            S.op('dve', lambda e: e.reciprocal(out=rec[:, :], in_=den[:, :]), r=['den'], w=['rec'])
            S.op('dve', lambda e: e.tensor_tensor(out=pn[:, :, :], in0=A[:, :, 128:192], in1=rec[:, :].unsqueeze(2).to_broadcast([128, 4, 64]), op=ALU.mult),
                 r=[AK, 'rec'], w=['pn'])
            S.op('dve', lambda e: e.tensor_reduce(out=pslc[:, :], in_=pn[:, :, :].rearrange("p h s -> p s h"), axis=AX.X, op=ALU.add), r=['pn'], w=['pslc'])
            S.op('dve', lambda e: e.tensor_tensor(out=rg[:, :], in0=rec[:, :], in1=gts[:, qt, 0:4], op=ALU.mult), r=['rec', 'gts'], w=['rg'])
            S.op('dve', lambda e: e.tensor_tensor(out=oacc[:, :, :], in0=A[:, :, 0:128], in1=bc(rg[:, :]), op=ALU.mult), r=[AK, 'rg'], w=['oacc'])
            S.op('dve', lambda e: e.tensor_tensor(out=score[:, :], in0=pslc[:, :], in1=sm1[:, qt, :], op=ALU.mult), r=['pslc', 'sm1'], w=['score'])
            S.op('dve', lambda e: e.tensor_tensor(out=score[:, :], in0=score[:, :], in1=sm2[:, qt, :], op=ALU.add), r=['score', 'sm2'], w=['score'])
            S.op('dve', lambda e: e.max(out=m8a[:, :], in_=score[:, :]), r=['score'], w=['m8a'])
            S.op('dve', lambda e: e.match_replace(out=work[:, :], in_to_replace=m8a[:, :], in_values=score[:, :], imm_value=-2.0), r=['score', 'm8a'], w=['work'])
            S.op('dve', lambda e: e.max(out=m8b[:, :], in_=work[:, :]), r=['work'], w=['m8b'])
            S.op('dve', lambda e: e.tensor_scalar(out=thr[:, :], in0=m8b[:, 7:8], scalar1=0.0, scalar2=None, op0=ALU.max), r=['m8b'], w=['thr'])
            S.op('dve', lambda e: e.tensor_scalar(out=selm[:, :], in0=score[:, :], scalar1=thr[:, 0:1], scalar2=None, op0=ALU.is_ge), r=['score', 'thr'], w=['selm'])
            S.op('dve', lambda e: e.tensor_scalar(out=selm[:, :], in0=selm[:, :], scalar1=-NEGB, scalar2=NEGB, op0=ALU.mult, op1=ALU.add), r=['selm'], w=['selm'])
            attend(qt, list(range(max(0, qt - 4), qt + 1)), kwT, 'kwT', vw, 'vw', accs[1], ACK[1], False)
            bk, bkk = banks[3], ('bk', 3)
            S.op('pe', lambda e, bk=bk: e.transpose(bk[0:64, 0:128], selm[:, :], IDF[:, :]), r=['selm', 'IDF'], w=[bkk])
            S.op('act', lambda e, bk=bk: e.copy(out=selmT[:, :, :], in_=bk[0:64, 0:128].unsqueeze(1).to_broadcast([64, 4, 128])), r=[bkk], w=['selmT'])
            attend(qt, list(range(0, qt + 1)), kslT, 'kslT', vsl, 'vsl', accs[0], ACK[0], True)
            finish_branch(accs[1], ACK[1], gts[:, qt, 8:12], False)
            finish_branch(accs[0], ACK[0], gts[:, qt, 4:8], False)
            oi = qt % 2
            S.op('act', lambda e, oi=oi: e.copy(out=ob[oi][:, :, :], in_=oacc[:, :, :]), r=['oacc'], w=[('ob', oi)])
            S.dma('sp', o_d[qt * 128:(qt + 1) * 128, :], ob[oi][:, :, :].rearrange("p h d -> p (h d)"), r=[('ob', oi)])
        S.finish()
    return nc


def nsa_consts():
    p = np.arange(128)
    c_all = np.arange(256)
    maskc = np.zeros((128, 32, 2, 128), np.float32)
    for qt in range(32):
        t = qt * 128 + p
        for cc in range(2):
            c = cc * 128 + p
            maskc[:, qt, cc, :] = ((16 * c[:, None] + 31 <= t[None, :]) & (c[:, None] < 255))
    blk = np.arange(64)
    sm = np.zeros((2, 128, 32, 64), np.float32)
    for qt in range(32):
        t = qt * 128 + p
        cur = (t // 64)[:, None]
        causal = blk[None, :] <= cur
        forced = (blk[None, :] == 0) | (causal & (blk[None, :] > cur - 2))
        sm[0, :, qt, :] = (causal & ~forced)
        sm[1, :, qt, :] = np.where(forced, 1e6, np.where(causal, 0.0, -1.0))
    ind = np.zeros((64, 32, 128), np.float32)
    for kt in range(32):
        ind[2 * kt + p // 64, kt, p] = 1.0
    cb = np.zeros((128, 2, 4, 128), np.float32)
    cb[:, 0] = np.where(p[:, None] > p[None, :], NEGB, 0.0)[:, None, :]
    cb[:, 1] = np.where(p[:, None] <= p[None, :], NEGB, 0.0)[:, None, :]
    c0 = np.arange(255) * 16
    s0 = np.arange(64) * 64
    ovm = np.clip(np.minimum(c0[:, None] + 32, s0[None, :] + 64) - np.maximum(c0[:, None], s0[None, :]), 0, None) / 32.0
    ov = np.zeros((256, 65), np.float32)
    ov[:255, :64] = ovm
    ov[:255, 64] = 1.0
    return dict(maskc=maskc.reshape(128, -1).astype(NPBF), sm=sm.reshape(2, 128, -1), ind=ind.reshape(64, -1).astype(NPBF),
                cbias=cb.reshape(128, -1).astype(NPBF), ov=ov.reshape(2, 128, 65),
                identb=np.eye(128, dtype=np.float32).astype(NPBF), identf=np.eye(128, dtype=np.float32))


def nsa_inputs(proj, qgate, cmp_w1, cmp_w2, cmp_pe):
    cst = nsa_consts()
    ims = []
    pj = proj.reshape(2, 4096, 7168)
    qg = qgate.reshape(2, 4096, 3, 16)
    peT = np.ascontiguousarray(np.transpose(cmp_pe, (0, 2, 1))).astype(np.float32)
    for c in range(NCORES):
        b, g = c // 4, c % 4
        P = pj[b]

        def sec(s):
            return P[:, s * 512 + g * 128: s * 512 + (g + 1) * 128]

        def qlay(off):
            q = P[:, off + g * 512: off + (g + 1) * 512].reshape(32, 128, 4, 128)
            return np.ascontiguousarray(np.transpose(q, (3, 0, 2, 1))).reshape(128, 32 * 512)
        im = dict(qn=qlay(3072), qr=qlay(5120),
                  kcT=np.ascontiguousarray(sec(0).T), vcT=np.ascontiguousarray(sec(1).T),
                  kslT=np.ascontiguousarray(sec(2).T), kwT=np.ascontiguousarray(sec(4).T),
                  vsl=np.ascontiguousarray(sec(3)), vw=np.ascontiguousarray(sec(5)),
                  gates=np.ascontiguousarray(qg[b][:, :, g * 4:(g + 1) * 4].reshape(4096, 12)),
                  w1=cmp_w1, w2=cmp_w2, peT=peT)
        im.update(cst)
        ims.append(im)
    return ims


def _rope_table():
    pos = np.arange(4096, dtype=np.float32)
    inv = (np.float32(10000.0) ** (-np.arange(64, dtype=np.float32) / np.float32(64))).astype(np.float32)
    ang = (pos[:, None] * inv[None, :]).astype(np.float32)
    return np.concatenate([np.cos(ang), np.sin(ang)], axis=1).astype(np.float32)


def _post1(aT_list, xres, w_out, g, b, router_w, router_bias):
    KIN = w_out.shape[0]
    nc = build_post1(KIN)
    ln_gb = np.stack([g, b]).astype(np.float32)
    ident = np.eye(128, dtype=np.float32)
    rb = router_bias.reshape(1, 32).astype(np.float32)
    ims = [{"aT": aT_list[c], "xres": np.ascontiguousarray(xres[c * 1024:(c + 1) * 1024]), "w_out": w_out, "ln_gb": ln_gb,
            "router_w": router_w, "router_b": rb, "ident": ident} for c in range(NCORES)]
    res = _run(nc, ims)
    x1 = np.concatenate([r["x1"] for r in res], axis=0)
    x1T = np.concatenate([r["x1T"] for r in res], axis=1)
    gates = np.concatenate([r["gates"] for r in res], axis=0)
    return x1, x1T, gates


def _moe(x1T, gates, wg, wu, wd):
    nc = build_moe(8192)
    x1T = np.ascontiguousarray(x1T)
    ims = [{"xT": x1T, "gates_c": np.ascontiguousarray(gates[:, 4 * c:4 * c + 4]), "wg": wg[4 * c:4 * c + 4],
            "wu": wu[4 * c:4 * c + 4], "wd": wd[4 * c:4 * c + 4]} for c in range(NCORES)]
    res = _run(nc, ims)
    return [r["y"] for r in res]


def _post2(ys, x1, g, b, proj_w=None):
    PROJ = proj_w is not None
    nc = build_post2(PROJ)
    ln_gb = np.stack([g, b]).astype(np.float32)
    ims = []
    if PROJ:
        cs = _rope_table()
        ident = np.eye(128, dtype=np.float32)
    for c in range(NCORES):
        rows = slice(c * 1024, (c + 1) * 1024)
        im = {"yp": np.stack([y[rows] for y in ys]), "x1": np.ascontiguousarray(x1[rows]), "ln_gb": ln_gb}
        if PROJ:
            p0 = (c * 1024) % 4096
            im.update({"kv_w": proj_w[0], "w_q": proj_w[1], "cs": np.ascontiguousarray(cs[p0:p0 + 1024]), "ident": ident})
        ims.append(im)
    res = _run(nc, ims)
    x2 = np.concatenate([r["x2"] for r in res], axis=0)
    if PROJ:
        return x2, np.concatenate([r["proj"] for r in res], axis=0), np.concatenate([r["qgate"] for r in res], axis=0)
    return x2


def kernel(x, a_w_in, a_conv_w, a_a_log, a_dt_bias, a_norm_w, a_w_out, kv_w, cmp_pe, cmp_w1, cmp_w2,
           b_w_q, b_w_out, router_w, router_bias, moe_w_gate, moe_w_up, moe_w_down, ln_g, ln_b):
    f = lambda a: np.asarray(a, dtype=np.float32)
    x, a_w_in, a_conv_w, a_a_log, a_dt_bias, a_norm_w, a_w_out = map(f, (x, a_w_in, a_conv_w, a_a_log, a_dt_bias, a_norm_w, a_w_out))
    kv_w, cmp_pe, cmp_w1, cmp_w2, b_w_q, b_w_out, router_w, router_bias = map(f, (kv_w, cmp_pe, cmp_w1, cmp_w2, b_w_q, b_w_out, router_w, router_bias))
    moe_w_gate, moe_w_up, moe_w_down, ln_g, ln_b = map(f, (moe_w_gate, moe_w_up, moe_w_down, ln_g, ln_b))
    xf = x.reshape(8192, D)
    res = _run(build_gdn_a(), gdn_a_inputs(x, a_w_in[0], a_conv_w[0]))
    res = _run(build_gdn_b(32), gdn_b_inputs([r["qkvT"] for r in res], [r["zT"] for r in res], [r["abT"] for r in res],
                                            a_a_log[0], a_dt_bias[0], a_norm_w[0]))
    og = np.zeros((2, 4096, 4096), dtype=NPBF)
    for c in range(NCORES):
        og[c // 4, :, (c % 4) * 1024:(c % 4 + 1) * 1024] = res[c]["og"]
    ogf = og.reshape(8192, 4096)
    aT = [np.ascontiguousarray(ogf[c * 1024:(c + 1) * 1024].T) for c in range(NCORES)]
    x1, x1T, gates = _post1(aT, xf, a_w_out[0], ln_g[0, 0], ln_b[0, 0], router_w, router_bias)
    ys = _moe(x1T, gates, moe_w_gate[0], moe_w_up[0], moe_w_down[0])
    x2, proj, qgate = _post2(ys, x1, ln_g[0, 1], ln_b[0, 1], (kv_w, b_w_q[0]))
    del ys
    res = _run(build_nsa(32), nsa_inputs(proj, qgate, cmp_w1, cmp_w2, cmp_pe))
    o = np.zeros((2, 4096, 2048), dtype=NPBF)
    for c in range(NCORES):
        o[c // 4, :, (c % 4) * 512:(c % 4 + 1) * 512] = res[c]["o"]
    of = o.reshape(8192, 2048)
    aT = [np.ascontiguousarray(of[c * 1024:(c + 1) * 1024].T) for c in range(NCORES)]
    x3, x3T, gates = _post1(aT, x2, b_w_out[0], ln_g[1, 0], ln_b[1, 0], router_w, router_bias)
    ys = _moe(x3T, gates, moe_w_gate[1], moe_w_up[1], moe_w_down[1])
    x4 = _post2(ys, x3, ln_g[1, 1], ln_b[1, 1], None)
    return x4.reshape(2, 4096, D).astype(np.float32)
```

```python
import contextlib
import os
import numpy as np
import ml_dtypes
import concourse.bass as bass
import concourse.mybir as mybir
from concourse.bass_utils import run_bass_kernel_spmd

F32 = mybir.dt.float32
BF16 = mybir.dt.bfloat16
AF = mybir.ActivationFunctionType
ALU = mybir.AluOpType
AX = mybir.AxisListType
NPBF = ml_dtypes.bfloat16

NCORES = 8
D = 2048
ALPHA = 4.0 ** 0.25
LN_EPS = 1e-5
RMS_EPS = 1e-6


class Sched:
    LIMIT = 30000
    NDMA = 16

    def __init__(self, nc, es):
        self.nc, self.es = nc, es
        self.eng = {'pe': nc.tensor, 'act': nc.scalar, 'dve': nc.vector, 'pool': nc.gpsimd, 'sp': nc.sync}
        self.sems = []
        self.cur = {}
        self.known = {e: {} for e in self.eng}
        self.lastw = {}
        self.readers = {}
        self.pe_sids = set()
        for e in ('pe', 'act', 'dve', 'pool'):
            self.cur[e] = [self._newsem(e), 0]
        self.pe_sids.add(self.cur['pe'][0])
        self.dma_slots = [[self._newsem('dma%d' % i), 0] for i in range(self.NDMA)]
        self.dma_rr = 0
        self.rec = None

    def record(self):
        self.rec = []

    def stop(self):
        l, self.rec = self.rec, None
        return l

    def play(self, items):
        for kind, a, kw in items:
            (self.op if kind == 'op' else self.dma)(*a, **kw)

    def play_interleaved(self, la, lb):
        na, nb_ = len(la), len(lb)
        ia = ib = 0
        while ia < na or ib < nb_:
            if ib >= nb_ or (ia < na and ia * nb_ <= ib * na):
                self.play([la[ia]])
                ia += 1
            else:
                self.play([lb[ib]])
                ib += 1

    def _newsem(self, name):
        s = self.es.enter_context(self.nc.semaphore('%s_%d' % (name, len(self.sems))))
        self.sems.append(s)
        return len(self.sems) - 1

    def _wait(self, e, deps):
        for sid, val in deps.items():
            if self.known[e].get(sid, 0) < val:
                self.eng[e].wait_ge(self.sems[sid], val)
                self.known[e][sid] = val

    def _deps(self, e, r, w):
        d = {}

        def add(sid, val):
            if d.get(sid, 0) < val:
                d[sid] = val
        for k in r:
            ev = self.lastw.get(k)
            if ev is not None:
                add(*ev)
        for k in w:
            ev = self.lastw.get(k)
            if ev is not None:
                add(*ev)
            for sid, val in self.readers.get(k, {}).items():
                add(sid, val)
        if e == 'pe':
            for sid in list(d):
                if sid in self.pe_sids:
                    del d[sid]
        return d

    def _record(self, ev, r, w):
        sid, val = ev
        for k in r:
            rd = self.readers.setdefault(k, {})
            if rd.get(sid, 0) < val:
                rd[sid] = val
        for k in w:
            self.lastw[k] = ev
            self.readers[k] = {}

    def op(self, e, fn, r=(), w=()):
        if self.rec is not None:
            self.rec.append(('op', (e, fn), dict(r=r, w=w)))
            return
        w = list(w) + [k for k in r if isinstance(k, tuple) and k[0] == 'bk']
        self._wait(e, self._deps(e, r, w))
        ins = fn(self.eng[e])
        c = self.cur[e]
        if c[1] >= self.LIMIT:
            c[0] = self._newsem(e)
            c[1] = 0
            if e == 'pe':
                self.pe_sids.add(c[0])
        c[1] += 1
        ins.then_inc(self.sems[c[0]], 1)
        self._record((c[0], c[1]), r, w)

    def dma(self, q, out, in_, r=(), w=(), **kw):
        if self.rec is not None:
            self.rec.append(('dma', (q, out, in_), dict(r=r, w=w, **kw)))
            return
        slot = self.dma_slots[self.dma_rr]
        self.dma_rr = (self.dma_rr + 1) % self.NDMA
        d = self._deps(q, r, w)
        if slot[1] > 0:
            d[slot[0]] = max(d.get(slot[0], 0), 16 * slot[1])
        self._wait(q, d)
        ins = self.eng[q].dma_start(out=out, in_=in_, **kw)
        slot[1] += 1
        ins.then_inc(self.sems[slot[0]], 16)
        self._record((slot[0], 16 * slot[1]), r, w)

    def _all_events(self):
        d = {slot[0]: 16 * slot[1] for slot in self.dma_slots if slot[1] > 0}
        for e, c in self.cur.items():
            if c[1] > 0:
                d[c[0]] = c[1]
        return d

    def sync_all(self):
        d = self._all_events()
        for e in self.eng:
            self._wait(e, d)

    def finish(self):
        self._wait('sp', self._all_events())


def _mk():
    return bass.Bass("TRN2", target_bir_lowering=False)


def _run(nc, in_maps):
    if os.environ.get("MK_TRACE"):
        res = run_bass_kernel_spmd(nc, in_maps, core_ids=list(range(NCORES)), trace=True)
        print("MK_TRACE exec_time_ns", res.exec_time_ns, flush=True)
        return res.results
    res = run_bass_kernel_spmd(nc, in_maps, core_ids=list(range(NCORES)))
    return res.results


def _layer_norm_tile(S, es, nc, h, tt, gbc, bbc, tmp):
    hk = ('h', tt)
    st, mv, rs = tmp['st'], tmp['mv'], tmp['rs']
    for c in range(4):
        S.op('dve', lambda e, c=c: e.bn_stats(out=st[:, c, :], in_=h[:, tt, c * 512:(c + 1) * 512]),
             r=[hk], w=['ln_st'])
    S.op('dve', lambda e: e.bn_aggr(out=mv[:, :], in_=st[:, :, :]), r=['ln_st'], w=['ln_mv'])
    S.op('act', lambda e: e.activation(out=rs[:, :], in_=mv[:, 1:2], func=AF.Sqrt, bias=tmp['eps'][:, 0:1]),
         r=['ln_mv', 'ln_eps'], w=['ln_rs'])
    S.op('dve', lambda e: e.reciprocal(out=rs[:, :], in_=rs[:, :]), r=['ln_rs'], w=['ln_rs'])
    S.op('dve', lambda e: e.tensor_scalar(out=h[:, tt, :], in0=h[:, tt, :], scalar1=mv[:, 0:1], scalar2=rs[:, 0:1],
                                          op0=ALU.subtract, op1=ALU.mult), r=[hk, 'ln_mv', 'ln_rs'], w=[hk])
    S.op('dve', lambda e: e.tensor_tensor(out=h[:, tt, :], in0=h[:, tt, :], in1=gbc[:, :], op=ALU.mult),
         r=[hk, 'gbc'], w=[hk])
    S.op('dve', lambda e: e.tensor_tensor(out=h[:, tt, :], in0=h[:, tt, :], in1=bbc[:, :], op=ALU.add),
         r=[hk, 'bbc'], w=[hk])


def build_post1(KIN):
    nc = _mk()
    KC = KIN // 128
    NT = 8
    aT = nc.dram_tensor("aT", [KIN, 1024], BF16, kind="ExternalInput").ap()
    xres = nc.dram_tensor("xres", [1024, D], F32, kind="ExternalInput").ap()
    w_out = nc.dram_tensor("w_out", [KIN, D], F32, kind="ExternalInput").ap()
    ln_gb = nc.dram_tensor("ln_gb", [2, D], F32, kind="ExternalInput").ap()
    router_w = nc.dram_tensor("router_w", [D, 32], F32, kind="ExternalInput").ap()
    router_b = nc.dram_tensor("router_b", [1, 32], F32, kind="ExternalInput").ap()
    ident_in = nc.dram_tensor("ident", [128, 128], F32, kind="ExternalInput").ap()
    x1 = nc.dram_tensor("x1", [1024, D], F32, kind="ExternalOutput").ap()
    x1T = nc.dram_tensor("x1T", [D, 1024], BF16, kind="ExternalOutput").ap()
    gates = nc.dram_tensor("gates", [1024, 32], F32, kind="ExternalOutput").ap()
    DC = 256
    with contextlib.ExitStack() as es:
        S = Sched(nc, es)
        sb = lambda name, shape, dt: es.enter_context(nc.sbuf_tensor(name, shape, dt))
        h = sb("h", [128, NT, D], F32)
        banks = [es.enter_context(nc.psum_tensor("bk%d" % i, [128, 512], F32)) for i in range(8)]
        with contextlib.ExitStack() as es1:
            a_sb = es1.enter_context(nc.sbuf_tensor("a_sb", [128, KC, 1024], BF16))
            wbuf = [es1.enter_context(nc.sbuf_tensor("wb%d" % i, [128, KC, DC], BF16)) for i in range(2)]
            half = KC // 2
            S.dma('sp', a_sb[:, 0:half, :], aT[0:half * 128, :].rearrange("(kc p) t -> p kc t", p=128), w=['a_sb'])
            S.dma('sp', a_sb[:, half:KC, :], aT[half * 128:KIN, :].rearrange("(kc p) t -> p kc t", p=128), w=['a_sb'])
            for tt in range(NT):
                S.dma('sp', h[:, tt, :], xres[tt * 128:(tt + 1) * 128, :], w=[('h', tt)])
            nb = 0
            for dc in range(D // DC):
                wb = wbuf[dc % 2]
                wk = ('wb', dc % 2)
                S.dma('pool', wb[:, :, :], w_out[:, dc * DC:(dc + 1) * DC].rearrange("(kc p) f -> p kc f", p=128), w=[wk])
                for tt in range(NT):
                    bk = banks[nb % 8]
                    bkk = ('bk', nb % 8)
                    nb += 1
                    for kc in range(KC):
                        S.op('pe', lambda e, kc=kc, bk=bk, tt=tt, wb=wb: e.matmul(
                            bk[:, 0:DC], a_sb[:, kc, tt * 128:(tt + 1) * 128], wb[:, kc, :],
                            start=(kc == 0), stop=(kc == KC - 1)), r=['a_sb', wk], w=[bkk])
                    S.op('dve', lambda e, bk=bk, tt=tt, dc=dc: e.scalar_tensor_tensor(
                        out=h[:, tt, dc * DC:(dc + 1) * DC], in0=h[:, tt, dc * DC:(dc + 1) * DC], scalar=ALPHA,
                        in1=bk[:, 0:DC], op0=ALU.mult, op1=ALU.add), r=[bkk, ('h', tt)], w=[('h', tt)])
            S.sync_all()
        gbc = sb("gbc", [128, D], F32)
        bbc = sb("bbc", [128, D], F32)
        rw = sb("rw", [128, 16, 32], F32)
        rb = sb("rb", [128, 32], F32)
        ident = sb("ident_sb", [128, 128], F32)
        tmp = dict(st=sb("ln_st", [128, 4, 6], F32), mv=sb("ln_mv", [128, 2], F32), rs=sb("ln_rs", [128, 1], F32),
                   eps=sb("ln_eps", [128, 1], F32))
        S.op('dve', lambda e: e.memset(tmp['eps'][:, :], LN_EPS), w=['ln_eps'])
        xT32 = sb("xT32", [128, 16, 128], F32)
        xTb = sb("xTb", [128, 16, 128], BF16)
        S.dma('sp', gbc[:, :], ln_gb[0:1, :].partition_broadcast(128), w=['gbc'])
        S.dma('sp', bbc[:, :], ln_gb[1:2, :].partition_broadcast(128), w=['bbc'])
        S.dma('sp', rw[:, :, :], router_w.rearrange("(kc p) e -> p kc e", p=128), w=['rw'])
        S.dma('sp', rb[:, :], router_b[0:1, :].partition_broadcast(128), w=['rb'])
        S.dma('sp', ident[:, :], ident_in[:, :], w=['ident'])
        r_aff = sb("r_aff", [128, 32], F32)
        r_bia = sb("r_bia", [128, 32], F32)
        r_ps = [sb("r_ps%d" % i, [128, 8], F32) for i in range(6)]
        r_gs = sb("r_gs", [128, 8], F32)
        r_gm = sb("r_gm", [128, 1], F32)
        r_gmask = sb("r_gmask", [128, 8], F32)
        r_m1 = sb("r_m1", [128, 8], F32)
        r_eq = sb("r_eq", [128, 32], F32)
        r_tmp = sb("r_tmp", [128, 32], F32)
        r_m2 = sb("r_m2", [128, 8], F32)
        r_sel = sb("r_sel", [128, 32], F32)
        r_den = sb("r_den", [128, 1], F32)
        r_gate = sb("r_gate", [128, 32], F32)
        RT = ['rtmp']
        for tt in range(NT):
            _layer_norm_tile(S, es, nc, h, tt, gbc, bbc, tmp)
            S.dma('sp', x1[tt * 128:(tt + 1) * 128, :], h[:, tt, :], r=[('h', tt)])
            for q4 in range(4):
                bk = banks[q4]
                bkk = ('bk', q4)
                for j in range(4):
                    kc = q4 * 4 + j
                    S.op('pe', lambda e, bk=bk, j=j, kc=kc, tt=tt: e.transpose(
                        bk[:, j * 128:(j + 1) * 128], h[:, tt, kc * 128:(kc + 1) * 128], ident[:, :]),
                        r=[('h', tt), 'ident'], w=[bkk])
                S.op('act', lambda e, bk=bk, q4=q4: e.copy(
                    out=xT32[:, q4 * 4:(q4 + 1) * 4, :], in_=bk[:, :].rearrange("p (j t) -> p j t", j=4)),
                    r=[bkk], w=['xT32'])
                S.op('dve', lambda e, bk=bk, q4=q4: e.tensor_copy(
                    out=xTb[:, q4 * 4:(q4 + 1) * 4, :], in_=bk[:, :].rearrange("p (j t) -> p j t", j=4)),
                    r=[bkk], w=['xTb'])
            S.dma('sp', x1T[:, tt * 128:(tt + 1) * 128].rearrange("(kc p) t -> p kc t", p=128), xTb[:, :, :], r=['xTb'])
            bk = banks[4]
            bkk = ('bk', 4)
            for kc in range(16):
                S.op('pe', lambda e, kc=kc, bk=bk: e.matmul(bk[:, 0:32], xT32[:, kc, :], rw[:, kc, :],
                                                           start=(kc == 0), stop=(kc == 15)),
                     r=['xT32', 'rw'], w=[bkk])
            S.op('act', lambda e, bk=bk: e.activation(out=r_aff[:, :], in_=bk[:, 0:32], func=AF.Sigmoid),
                 r=[bkk], w=['r_aff'])
            S.op('dve', lambda e: e.tensor_tensor(out=r_bia[:, :], in0=r_aff[:, :], in1=rb[:, :], op=ALU.add),
                 r=['r_aff', 'rb'], w=RT)
            b3 = r_bia[:, :].rearrange("p (g i) -> p g i", i=4)
            pairs = [(0, 1), (0, 2), (0, 3), (1, 2), (1, 3), (2, 3)]
            for pi, (i0, i1) in enumerate(pairs):
                S.op('dve', lambda e, pi=pi, i0=i0, i1=i1: e.tensor_tensor(
                    out=r_ps[pi][:, :], in0=b3[:, :, i0], in1=b3[:, :, i1], op=ALU.add), r=RT, w=RT)
            S.op('dve', lambda e: e.tensor_tensor(out=r_gs[:, :], in0=r_ps[0][:, :], in1=r_ps[1][:, :], op=ALU.max), r=RT, w=RT)
            for pi in range(2, 6):
                S.op('dve', lambda e, pi=pi: e.tensor_tensor(out=r_gs[:, :], in0=r_gs[:, :], in1=r_ps[pi][:, :], op=ALU.max), r=RT, w=RT)
            S.op('dve', lambda e: e.tensor_reduce(out=r_gm[:, :], in_=r_gs[:, :], axis=AX.X, op=ALU.max), r=RT, w=RT)
            S.op('dve', lambda e: e.tensor_scalar(out=r_gmask[:, :], in0=r_gs[:, :], scalar1=r_gm[:, 0:1], scalar2=None,
                                                  op0=ALU.is_ge), r=RT, w=RT)
            S.op('dve', lambda e: e.tensor_reduce(out=r_m1[:, :], in_=b3, axis=AX.X, op=ALU.max), r=RT, w=RT)
            S.op('dve', lambda e: e.tensor_tensor(out=r_eq[:, :].rearrange("p (g i) -> p g i", i=4), in0=b3,
                                                  in1=r_m1[:, :].unsqueeze(2).to_broadcast([128, 8, 4]), op=ALU.is_equal), r=RT, w=RT)
            S.op('dve', lambda e: e.scalar_tensor_tensor(out=r_tmp[:, :], in0=r_eq[:, :], scalar=-1e30, in1=r_bia[:, :],
                                                         op0=ALU.mult, op1=ALU.add), r=RT, w=RT)
            S.op('dve', lambda e: e.tensor_reduce(out=r_m2[:, :], in_=r_tmp[:, :].rearrange("p (g i) -> p g i", i=4),
                                                  axis=AX.X, op=ALU.max), r=RT, w=RT)
            S.op('dve', lambda e: e.tensor_tensor(out=r_sel[:, :].rearrange("p (g i) -> p g i", i=4), in0=b3,
                                                  in1=r_m2[:, :].unsqueeze(2).to_broadcast([128, 8, 4]), op=ALU.is_ge), r=RT, w=RT)
            S.op('dve', lambda e: e.tensor_tensor(out=r_sel[:, :].rearrange("p (g i) -> p g i", i=4),
                                                  in0=r_sel[:, :].rearrange("p (g i) -> p g i", i=4),
                                                  in1=r_gmask[:, :].unsqueeze(2).to_broadcast([128, 8, 4]), op=ALU.mult), r=RT, w=RT)
            S.op('dve', lambda e: e.tensor_tensor(out=r_sel[:, :], in0=r_sel[:, :], in1=r_aff[:, :], op=ALU.mult),
                 r=RT + ['r_aff'], w=RT)
            S.op('dve', lambda e: e.tensor_reduce(out=r_den[:, :], in_=r_sel[:, :], axis=AX.X, op=ALU.add), r=RT, w=RT)
            S.op('dve', lambda e: e.reciprocal(out=r_den[:, :], in_=r_den[:, :]), r=RT, w=RT)
            S.op('dve', lambda e: e.tensor_scalar(out=r_gate[:, :], in0=r_sel[:, :], scalar1=r_den[:, 0:1], scalar2=None,
                                                  op0=ALU.mult), r=RT, w=['r_gate'])
            S.dma('sp', gates[tt * 128:(tt + 1) * 128, :], r_gate[:, :], r=['r_gate'])
        S.finish()
    return nc


def build_moe(NTOK=8192):
    nc = _mk()
    NB = NTOK // 512
    xT = nc.dram_tensor("xT", [D, NTOK], BF16, kind="ExternalInput").ap()
    gates_c = nc.dram_tensor("gates_c", [NTOK, 4], F32, kind="ExternalInput").ap()
    wg = nc.dram_tensor("wg", [4, D, 512], F32, kind="ExternalInput").ap()
    wu = nc.dram_tensor("wu", [4, D, 512], F32, kind="ExternalInput").ap()
    wd = nc.dram_tensor("wd", [4, 512, D], F32, kind="ExternalInput").ap()
    y = nc.dram_tensor("y", [NTOK, D], F32, kind="ExternalOutput").ap()
    with contextlib.ExitStack() as es:
        S = Sched(nc, es)
        sb = lambda name, shape, dt: es.enter_context(nc.sbuf_tensor(name, shape, dt))
        banks = [es.enter_context(nc.psum_tensor("bk%d" % i, [128, 512], F32)) for i in range(8)]
        wg_sb = [sb("wg%d" % i, [128, 16, 512], BF16) for i in range(2)]
        wu_sb = [sb("wu%d" % i, [128, 16, 512], BF16) for i in range(2)]
        wd_sb = [sb("wd%d" % i, [128, 4, D], BF16) for i in range(2)]
        x_sb = [sb("x%d" % i, [128, 16, 512], BF16) for i in range(2)]
        hid = [sb("hid%d" % i, [128, 4, 512], BF16) for i in range(2)]
        sg = [sb("sg%d" % i, [128, 512], F32) for i in range(2)]
        ost = [sb("ost%d" % i, [128, D], F32) for i in range(3)]
        g_sb = sb("g_sb", [128, NTOK // 128, 4], F32)
        S.dma('sp', g_sb[:, :, :], gates_c.rearrange("(tt p) e -> p tt e", p=128), w=['g_sb'])

        def load_w(e):
            b = e % 2
            S.dma('pool', wg_sb[b][:, :, :], wg[e].rearrange("(kc p) f -> p kc f", p=128), w=[('wg', b)])
            S.dma('pool', wu_sb[b][:, :, :], wu[e].rearrange("(kc p) f -> p kc f", p=128), w=[('wu', b)])
            S.dma('pool', wd_sb[b][:, :, :], wd[e].rearrange("(fc p) d -> p fc d", p=128), w=[('wd', b)])

        def load_x(i):
            tb = i % NB
            b = i % 2
            S.dma('sp', x_sb[b][:, :, :], xT[:, tb * 512:(tb + 1) * 512].rearrange("(kc p) t -> p kc t", p=128),
                  w=[('x', b)])

        load_w(0)
        load_x(0)
        it = 0
        ngu = 0
        nd = 0
        nos = 0
        for e in range(4):
            if e + 1 < 4:
                load_w(e + 1)
            wb = e % 2
            for tb in range(NB):
                if it + 1 < 4 * NB:
                    load_x(it + 1)
                xb = it % 2
                hb = it % 2
                for fc in range(4):
                    gb, ub = (0, 1) if ngu % 2 == 0 else (2, 3)
                    ngu += 1
                    for kc in range(16):
                        S.op('pe', lambda en, kc=kc, fc=fc, gb=gb: en.matmul(
                            banks[gb][:, :], wg_sb[wb][:, kc, fc * 128:(fc + 1) * 128], x_sb[xb][:, kc, :],
                            start=(kc == 0), stop=(kc == 15)), r=[('wg', wb), ('x', xb)], w=[('bk', gb)])
                    for kc in range(16):
                        S.op('pe', lambda en, kc=kc, fc=fc, ub=ub: en.matmul(
                            banks[ub][:, :], wu_sb[wb][:, kc, fc * 128:(fc + 1) * 128], x_sb[xb][:, kc, :],
                            start=(kc == 0), stop=(kc == 15)), r=[('wu', wb), ('x', xb)], w=[('bk', ub)])
                    sgi = ngu % 2
                    S.op('act', lambda en, gb=gb, sgi=sgi: en.activation(out=sg[sgi][:, :], in_=banks[gb][:, :], func=AF.Silu),
                         r=[('bk', gb)], w=[('sg', sgi)])
                    S.op('dve', lambda en, ub=ub, sgi=sgi, fc=fc: en.tensor_tensor(
                        out=hid[hb][:, fc, :], in0=sg[sgi][:, :], in1=banks[ub][:, :], op=ALU.mult),
                        r=[('sg', sgi), ('bk', ub)], w=[('hid', hb)])
                for t4 in range(4):
                    tt = tb * 4 + t4
                    oi = nos % 3
                    nos += 1
                    for dc in range(4):
                        db = 4 + nd % 4
                        nd += 1
                        for fc in range(4):
                            S.op('pe', lambda en, fc=fc, dc=dc, db=db, t4=t4: en.matmul(
                                banks[db][:, :], hid[hb][:, fc, t4 * 128:(t4 + 1) * 128], wd_sb[wb][:, fc, dc * 512:(dc + 1) * 512],
                                start=(fc == 0), stop=(fc == 3)), r=[('hid', hb), ('wd', wb)], w=[('bk', db)])
                        if dc % 2 == 0:
                            S.op('act', lambda en, db=db, dc=dc, oi=oi, tt=tt: en.activation(
                                out=ost[oi][:, dc * 512:(dc + 1) * 512], in_=banks[db][:, :], func=AF.Identity,
                                scale=g_sb[:, tt, e:e + 1]), r=[('bk', db), 'g_sb'], w=[('os', oi)])
                        else:
                            S.op('dve', lambda en, db=db, dc=dc, oi=oi, tt=tt: en.tensor_scalar(
                                out=ost[oi][:, dc * 512:(dc + 1) * 512], in0=banks[db][:, :], scalar1=g_sb[:, tt, e:e + 1],
                                scalar2=None, op0=ALU.mult), r=[('bk', db), 'g_sb'], w=[('os', oi)])
                    if e == 0:
                        S.dma('sp', y[tt * 128:(tt + 1) * 128, :], ost[oi][:, :], r=[('os', oi)], w=[('y', tt)])
                    else:
                        S.dma('pool', y[tt * 128:(tt + 1) * 128, :], ost[oi][:, :], r=[('os', oi)], w=[('y', tt)],
                              accum_op=ALU.add)
                it += 1
        S.finish()
    return nc


def build_post2(PROJ):
    nc = _mk()
    NT = 8
    yp = nc.dram_tensor("yp", [8, 1024, D], F32, kind="ExternalInput").ap()
    x1 = nc.dram_tensor("x1", [1024, D], F32, kind="ExternalInput").ap()
    ln_gb = nc.dram_tensor("ln_gb", [2, D], F32, kind="ExternalInput").ap()
    x2 = nc.dram_tensor("x2", [1024, D], F32, kind="ExternalOutput").ap()
    if PROJ:
        kv_w = nc.dram_tensor("kv_w", [D, 3072], F32, kind="ExternalInput").ap()
        w_q = nc.dram_tensor("w_q", [D, 2096], F32, kind="ExternalInput").ap()
        cs = nc.dram_tensor("cs", [1024, 128], F32, kind="ExternalInput").ap()
        ident_in = nc.dram_tensor("ident", [128, 128], F32, kind="ExternalInput").ap()
        proj = nc.dram_tensor("proj", [1024, 7168], BF16, kind="ExternalOutput").ap()
        qgate = nc.dram_tensor("qgate", [1024, 48], F32, kind="ExternalOutput").ap()
    with contextlib.ExitStack() as es:
        S = Sched(nc, es)
        sb = lambda name, shape, dt: es.enter_context(nc.sbuf_tensor(name, shape, dt))
        h = sb("h", [128, NT, D], F32)
        banks = [es.enter_context(nc.psum_tensor("bk%d" % i, [128, 512], F32)) for i in range(8)]
        gbc = sb("gbc", [128, D], F32)
        bbc = sb("bbc", [128, D], F32)
        tmp = dict(st=sb("ln_st", [128, 4, 6], F32), mv=sb("ln_mv", [128, 2], F32), rs=sb("ln_rs", [128, 1], F32),
                   eps=sb("ln_eps", [128, 1], F32))
        S.op('dve', lambda e: e.memset(tmp['eps'][:, :], LN_EPS), w=['ln_eps'])
        S.dma('sp', gbc[:, :], ln_gb[0:1, :].partition_broadcast(128), w=['gbc'])
        S.dma('sp', bbc[:, :], ln_gb[1:2, :].partition_broadcast(128), w=['bbc'])
        with contextlib.ExitStack() as es1:
            stg = [es1.enter_context(nc.sbuf_tensor("stg%d" % i, [128, D], F32)) for i in range(3)]
            ns = 0
            for tt in range(NT):
                S.dma('sp', h[:, tt, :], x1[tt * 128:(tt + 1) * 128, :], w=[('h', tt)])
                for c in range(8):
                    si = ns % 3
                    ns += 1
                    S.dma('sp', stg[si][:, :], yp[c, tt * 128:(tt + 1) * 128, :], w=[('stg', si)])
                    eng = 'dve'
                    if c == 0:
                        S.op(eng, lambda e, si=si, tt=tt: e.scalar_tensor_tensor(
                            out=h[:, tt, :], in0=h[:, tt, :], scalar=ALPHA, in1=stg[si][:, :], op0=ALU.mult, op1=ALU.add),
                            r=[('stg', si), ('h', tt)], w=[('h', tt)])
                    else:
                        S.op(eng, lambda e, si=si, tt=tt: e.tensor_tensor(
                            out=h[:, tt, :], in0=h[:, tt, :], in1=stg[si][:, :], op=ALU.add),
                            r=[('stg', si), ('h', tt)], w=[('h', tt)])
                _layer_norm_tile(S, es, nc, h, tt, gbc, bbc, tmp)
                S.dma('sp', x2[tt * 128:(tt + 1) * 128, :], h[:, tt, :], r=[('h', tt)])
            S.sync_all()
        if PROJ:
            ident = sb("ident_sb", [128, 128], F32)
            xT = sb("xT", [128, 16, 1024], BF16)
            cs_sb = sb("cs_sb", [128, NT, 128], F32)
            wbuf = [sb("wb%d" % i, [128, 16, 512], BF16) for i in range(2)]
            rsb = [sb("rsb%d" % i, [128, 512], F32) for i in range(2)]
            t1 = [sb("t1_%d" % i, [128, 256], F32) for i in range(2)]
            t2 = [sb("t2_%d" % i, [128, 256], F32) for i in range(2)]
            ob = [sb("ob%d" % i, [128, 512], BF16) for i in range(4)]
            gst = [sb("gst%d" % i, [128, 48], F32) for i in range(2)]
            S.dma('sp', ident[:, :], ident_in[:, :], w=['ident'])
            S.dma('sp', cs_sb[:, :, :], cs.rearrange("(tt p) c -> p tt c", p=128), w=['cs'])
            for tt in range(NT):
                for q4 in range(4):
                    bk = banks[q4]
                    bkk = ('bk', q4)
                    for j in range(4):
                        kc = q4 * 4 + j
                        S.op('pe', lambda e, bk=bk, j=j, kc=kc, tt=tt: e.transpose(
                            bk[:, j * 128:(j + 1) * 128], h[:, tt, kc * 128:(kc + 1) * 128], ident[:, :]),
                            r=[('h', tt), 'ident'], w=[bkk])
                    eng = 'act' if q4 % 2 == 0 else 'dve'
                    if eng == 'act':
                        S.op('act', lambda e, bk=bk, q4=q4, tt=tt: e.copy(
                            out=xT[:, q4 * 4:(q4 + 1) * 4, tt * 128:(tt + 1) * 128],
                            in_=bk[:, :].rearrange("p (j t) -> p j t", j=4)), r=[bkk], w=[('xT', tt)])
                    else:
                        S.op('dve', lambda e, bk=bk, q4=q4, tt=tt: e.tensor_copy(
                            out=xT[:, q4 * 4:(q4 + 1) * 4, tt * 128:(tt + 1) * 128],
                            in_=bk[:, :].rearrange("p (j t) -> p j t", j=4)), r=[bkk], w=[('xT', tt)])
            chunks = []
            for c in range(6):
                chunks.append((kv_w[:, c * 512:(c + 1) * 512], 512, 'rope' if c in (2, 4) else 'plain', c * 512))
            for c in range(4):
                chunks.append((w_q[:, c * 512:(c + 1) * 512], 512, 'q', c * 512))
            chunks.append((w_q[:, 2048:2096], 48, 'gate', 0))
            nb = 0
            no = 0
            nr = 0
            for ci, (wsrc, ncol, kind, c0) in enumerate(chunks):
                wb = wbuf[ci % 2]
                wk = ('wb', ci % 2)
                S.dma('pool', wb[:, :, 0:ncol], wsrc.rearrange("(kc p) f -> p kc f", p=128), w=[wk])
                for tt in range(NT):
                    bi = nb % 8
                    nb += 1
                    bk, bkk = banks[bi], ('bk', bi)
                    for kc in range(16):
                        S.op('pe', lambda e, kc=kc, bk=bk, tt=tt, wb=wb, ncol=ncol: e.matmul(
                            bk[:, 0:ncol], xT[:, kc, tt * 128:(tt + 1) * 128], wb[:, kc, 0:ncol],
                            start=(kc == 0), stop=(kc == 15)), r=[('xT', tt), wk], w=[bkk])
                    rows = slice(tt * 128, (tt + 1) * 128)
                    if kind == 'gate':
                        gi = tt % 2
                        S.op('act', lambda e, bk=bk, gi=gi: e.activation(out=gst[gi][:, :], in_=bk[:, 0:48], func=AF.Sigmoid),
                             r=[bkk], w=[('gst', gi)])
                        S.dma('sp', qgate[rows, :], gst[gi][:, :], r=[('gst', gi)])
                        continue
                    if kind in ('plain', 'q'):
                        oi = no % 4
                        no += 1
                        S.op('act', lambda e, bk=bk, oi=oi: e.copy(out=ob[oi][:, :], in_=bk[:, :]), r=[bkk], w=[('ob', oi)])
                        oc = c0 if kind == 'plain' else 3072 + c0
                        S.dma('sp', proj[rows, oc:oc + 512], ob[oi][:, :], r=[('ob', oi)])
                    if kind in ('rope', 'q'):
                        ri = nr % 2
                        nr += 1
                        oi = no % 4
                        no += 1
                        S.op('dve', lambda e, bk=bk, ri=ri: e.tensor_copy(out=rsb[ri][:, :], in_=bk[:, :]), r=[bkk], w=[('rsb', ri)])
                        rv = rsb[ri][:, :].rearrange("p (h d) -> p h d", h=4)
                        ov = ob[oi][:, :].rearrange("p (h d) -> p h d", h=4)
                        cosb = cs_sb[:, tt, 0:64].unsqueeze(1).to_broadcast([128, 4, 64])
                        sinb = cs_sb[:, tt, 64:128].unsqueeze(1).to_broadcast([128, 4, 64])
                        t1v = t1[ri][:, :].rearrange("p (h d) -> p h d", h=4)
                        t2v = t2[ri][:, :].rearrange("p (h d) -> p h d", h=4)
                        S.op('dve', lambda e, rv=rv, t1v=t1v, cosb=cosb: e.tensor_tensor(out=t1v, in0=rv[:, :, 0:64], in1=cosb, op=ALU.mult),
                             r=[('rsb', ri), 'cs'], w=[('t1', ri)])
                        S.op('dve', lambda e, rv=rv, t1v=t1v, sinb=sinb: e.scalar_tensor_tensor(
                            out=ov[:, :, 0:64], in0=rv[:, :, 64:128], scalar=-1.0, in1=sinb, op0=ALU.mult, op1=ALU.mult),
                            r=[('rsb', ri), 'cs'], w=[('ob', oi)])
                        S.op('dve', lambda e, ov=ov, t1v=t1v: e.tensor_tensor(out=ov[:, :, 0:64], in0=ov[:, :, 0:64], in1=t1v, op=ALU.add),
                             r=[('t1', ri), ('ob', oi)], w=[('ob', oi)])
                        S.op('dve', lambda e, rv=rv, t2v=t2v, cosb=cosb: e.tensor_tensor(out=t2v, in0=rv[:, :, 64:128], in1=cosb, op=ALU.mult),
                             r=[('rsb', ri), 'cs'], w=[('t2', ri)])
                        S.op('dve', lambda e, rv=rv, ov=ov, sinb=sinb: e.tensor_tensor(out=ov[:, :, 64:128], in0=rv[:, :, 0:64], in1=sinb, op=ALU.mult),
                             r=[('rsb', ri), 'cs', ('ob', oi)], w=[('ob', oi)])
                        S.op('dve', lambda e, ov=ov, t2v=t2v: e.tensor_tensor(out=ov[:, :, 64:128], in0=ov[:, :, 64:128], in1=t2v, op=ALU.add),
                             r=[('t2', ri), ('ob', oi)], w=[('ob', oi)])
                        oc = c0 if kind == 'rope' else 5120 + c0
                        S.dma('sp', proj[rows, oc:oc + 512], ob[oi][:, :], r=[('ob', oi)])
        S.finish()
    return nc


def build_gdn_a():
    nc = _mk()
    T = 4096
    HT = 2048
    xT = nc.dram_tensor("xT", [D, T], F32, kind="ExternalInput").ap()
    w_sl = nc.dram_tensor("w_sl", [D, 3088], F32, kind="ExternalInput").ap()
    conv_w = nc.dram_tensor("conv_w", [2048, 4], F32, kind="ExternalInput").ap()
    qkvT = nc.dram_tensor("qkvT", [2048, T], BF16, kind="ExternalOutput").ap()
    zT = nc.dram_tensor("zT", [1024, T], BF16, kind="ExternalOutput").ap()
    abT = nc.dram_tensor("abT", [16, T], F32, kind="ExternalOutput").ap()
    with contextlib.ExitStack() as es:
        S = Sched(nc, es)
        sb = lambda name, shape, dt: es.enter_context(nc.sbuf_tensor(name, shape, dt))
        banks = [es.enter_context(nc.psum_tensor("bk%d" % i, [128, 512], F32)) for i in range(8)]
        x_sb = sb("x_sb", [128, 16, HT], BF16)
        wt = [sb("wt%d" % i, [128, 16, 128], BF16) for i in range(2)]
        P = [sb("P%d" % i, [128, HT + 3], F32) for i in range(2)]
        acc = [sb("acc%d" % i, [128, HT], F32) for i in range(2)]
        ost = [sb("ost%d" % i, [128, HT], BF16) for i in range(2)]
        abst = sb("abst", [16, HT], F32)
        carry = sb("carry", [128, 16, 3], F32)
        cw = sb("cw", [128, 16, 4], F32)
        S.dma('sp', cw[:, :, :], conv_w.rearrange("(ct p) j -> p ct j", p=128), w=['cw'])
        nb = 0
        it = 0
        for hf in range(2):
            for g4 in range(4):
                S.dma('pool', x_sb[:, g4 * 4:(g4 + 1) * 4, :],
                      xT[g4 * 512:(g4 + 1) * 512, hf * HT:(hf + 1) * HT].rearrange("(kc p) t -> p kc t", p=128),
                      w=[('x', g4)])
            xk = [('x', g4) for g4 in range(4)]
            for ct in range(25):
                nch = 128 if ct < 24 else 16
                wi = it % 2
                pi = it % 2
                it += 1
                S.dma('pool', wt[wi][:, :, 0:nch], w_sl[:, ct * 128:ct * 128 + nch].rearrange("(kc p) f -> p kc f", p=128),
                      w=[('wt', wi)])
                if ct < 16:
                    if hf == 0:
                        S.op('dve', lambda e, pi=pi: e.memset(P[pi][:, 0:3], 0.0), w=[('P', pi)])
                    else:
                        S.op('dve', lambda e, pi=pi, ct=ct: e.tensor_copy(out=P[pi][:, 0:3], in_=carry[:, ct, :]),
                             r=['carry'], w=[('P', pi)])
                for tb in range(4):
                    bi = nb % 8
                    nb += 1
                    bk, bkk = banks[bi], ('bk', bi)
                    for kc in range(16):
                        S.op('pe', lambda e, kc=kc, bk=bk, tb=tb, wi=wi, nch=nch: e.matmul(
                            bk[0:nch, :], wt[wi][:, kc, 0:nch], x_sb[:, kc, tb * 512:(tb + 1) * 512],
                            start=(kc == 0), stop=(kc == 15)), r=[('wt', wi)] + xk, w=[bkk])
                    cols = slice(tb * 512, (tb + 1) * 512)
                    if ct < 16:
                        S.op('act', lambda e, bk=bk, tb=tb, pi=pi: e.copy(out=P[pi][:, 3 + tb * 512:3 + (tb + 1) * 512], in_=bk[:, :]),
                             r=[bkk], w=[('P', pi)])
                    elif ct < 24:
                        S.op('act', lambda e, bk=bk, cols=cols, pi=pi: e.copy(out=ost[pi][:, cols], in_=bk[:, :]),
                             r=[bkk], w=[('ost', pi)])
                    else:
                        S.op('act', lambda e, bk=bk, cols=cols: e.copy(out=abst[:, cols], in_=bk[0:16, :]),
                             r=[bkk], w=['abst'])
                tsl = slice(hf * HT, (hf + 1) * HT)
                if ct < 16:
                    S.op('dve', lambda e, pi=pi, ct=ct: e.tensor_copy(out=carry[:, ct, :], in_=P[pi][:, HT:HT + 3]),
                         r=[('P', pi)], w=['carry'])
                    S.op('dve', lambda e, pi=pi, ct=ct: e.tensor_scalar(
                        out=acc[pi][:, :], in0=P[pi][:, 0:HT], scalar1=cw[:, ct, 0:1], scalar2=None, op0=ALU.mult),
                        r=[('P', pi), 'cw'], w=[('acc', pi)])
                    for j in range(1, 4):
                        S.op('dve', lambda e, pi=pi, ct=ct, j=j: e.scalar_tensor_tensor(
                            out=acc[pi][:, :], in0=P[pi][:, j:j + HT], scalar=cw[:, ct, j:j + 1], in1=acc[pi][:, :],
                            op0=ALU.mult, op1=ALU.add), r=[('P', pi), 'cw', ('acc', pi)], w=[('acc', pi)])
                    S.op('act', lambda e, pi=pi: e.activation(out=ost[pi][:, :], in_=acc[pi][:, :], func=AF.Silu),
                         r=[('acc', pi)], w=[('ost', pi)])
                    S.dma('sp', qkvT[ct * 128:(ct + 1) * 128, tsl], ost[pi][:, :], r=[('ost', pi)])
                elif ct < 24:
                    S.dma('sp', zT[(ct - 16) * 128:(ct - 15) * 128, tsl], ost[pi][:, :], r=[('ost', pi)])
                else:
                    S.dma('sp', abT[:, tsl], abst[:, :], r=['abst'])
        S.finish()
    return nc


def gdn_a_inputs(x, a_w_in, a_conv_w):
    ims = []
    for c in range(NCORES):
        b, hg = c // 4, c % 4
        w = a_w_in
        w_sl = np.concatenate([w[:, hg * 512:(hg + 1) * 512], w[:, 2048 + hg * 512:2048 + (hg + 1) * 512],
                               w[:, 4096 + hg * 1024:4096 + (hg + 1) * 1024], w[:, 8192 + hg * 1024:8192 + (hg + 1) * 1024],
                               w[:, 12288 + hg * 8:12288 + (hg + 1) * 8], w[:, 12320 + hg * 8:12320 + (hg + 1) * 8]], axis=1)
        cwf = a_conv_w
        cw = np.concatenate([cwf[:, hg * 512:(hg + 1) * 512], cwf[:, 2048 + hg * 512:2048 + (hg + 1) * 512],
                             cwf[:, 4096 + hg * 1024:4096 + (hg + 1) * 1024]], axis=1).T
        ims.append({"xT": np.ascontiguousarray(x[b].T), "w_sl": np.ascontiguousarray(w_sl),
                    "conv_w": np.ascontiguousarray(cw)})
    return ims


def build_gdn_b(NCH=32):
    nc = _mk()
    T = 4096
    qk = nc.dram_tensor("qk", [T, 8, 128], BF16, kind="ExternalInput").ap()
    v_in = nc.dram_tensor("v", [T, 8, 128], BF16, kind="ExternalInput").ap()
    z_in = nc.dram_tensor("z", [T, 8, 128], BF16, kind="ExternalInput").ap()
    ab = nc.dram_tensor("ab", [T, 16], F32, kind="ExternalInput").ap()
    hp = nc.dram_tensor("hp", [2, 8], F32, kind="ExternalInput").ap()
    nw = nc.dram_tensor("nw", [1, 128], F32, kind="ExternalInput").ap()
    cmask = nc.dram_tensor("cmask", [4, 128, 128], F32, kind="ExternalInput").ap()
    identb_in = nc.dram_tensor("identb", [128, 128], BF16, kind="ExternalInput").ap()
    og = nc.dram_tensor("og", [T, 1024], BF16, kind="ExternalOutput").ap()
    with contextlib.ExitStack() as es:
        S = Sched(nc, es)
        sb = lambda name, shape, dt: es.enter_context(nc.sbuf_tensor(name, shape, dt))
        banks = [es.enter_context(nc.psum_tensor("bk%d" % i, [128, 512], F32)) for i in range(7)]
        tpb = es.enter_context(nc.psum_tensor("tpb", [128, 1024], BF16))
        TPK = ('bk', 7)
        nbk = [0]

        def newbank():
            i = nbk[0] % 7
            nbk[0] += 1
            return banks[i], ('bk', i)

        ML = sb("ML", [128, 128], F32)
        MU = sb("MU", [128, 128], F32)
        MUI = sb("MUI", [128, 128], F32)
        IDF = sb("IDF", [128, 128], F32)
        ONES = sb("ONES", [128, 128], F32)
        IDB = sb("IDB", [128, 128], BF16)
        hpb = sb("hpb", [128, 16], F32)
        nwb = sb("nwb", [128, 128], F32)
        negexpA = sb("negexpA", [128, 8], F32)
        epsr = sb("epsr", [128, 1], F32)
        one1 = sb("one1", [128, 1], F32)
        for i, t in enumerate((ML, MU, MUI, IDF)):
            S.dma('sp', t[:, :], cmask[i], w=[t.name])
        S.dma('sp', IDB[:, :], identb_in[:, :], w=['IDB'])
        S.dma('sp', hpb[:, 0:8], hp[0:1, :].partition_broadcast(128), w=['hpb'])
        S.dma('sp', hpb[:, 8:16], hp[1:2, :].partition_broadcast(128), w=['hpb'])
        S.dma('sp', nwb[:, :], nw[0:1, :].partition_broadcast(128), w=['nwb'])
        S.op('dve', lambda e: e.memset(ONES[:, :], 1.0), w=['ONES'])
        S.op('dve', lambda e: e.memset(epsr[:, :], RMS_EPS), w=['epsr'])
        S.op('dve', lambda e: e.memset(one1[:, :], 1.0), w=['one1'])
        S.op('act', lambda e: e.activation(out=negexpA[:, :], in_=hpb[:, 0:8], func=AF.Exp), r=['hpb'], w=['negexpA'])
        S.op('dve', lambda e: e.tensor_scalar(out=negexpA[:, :], in0=negexpA[:, :], scalar1=-1.0, scalar2=None, op0=ALU.mult),
             r=['negexpA'], w=['negexpA'])

        qk_sb = [sb("qk%d" % i, [128, 8, 128], BF16) for i in range(2)]
        v_sb = [sb("v%d" % i, [128, 8, 128], BF16) for i in range(2)]
        z_sb = [sb("z%d" % i, [128, 8, 128], BF16) for i in range(2)]
        ab_sb = [sb("ab%d" % i, [128, 16], F32) for i in range(2)]
        sq = sb("sq", [128, 8, 128], F32)
        ssq = sb("ssq", [128, 8], F32)
        rn = sb("rn", [128, 8], F32)
        qkn = sb("qkn", [128, 8, 128], BF16)
        qkT = sb("qkT", [128, 8, 128], BF16)
        qg = sb("qg", [128, 8, 128], BF16)
        qgT = sb("qgT", [128, 8, 128], BF16)
        kd = sb("kd", [128, 8, 128], BF16)
        sz = sb("sz", [128, 8, 128], F32)
        nwz = sb("nwz", [128, 8, 128], F32)
        xa = sb("xa", [128, 8], F32)
        ax = sb("ax", [128, 8], F32)
        ex = sb("ex", [128, 8], F32)
        gg = sb("gg", [128, 8], F32)
        beta = sb("beta", [128, 8], F32)
        gcs = sb("gcs", [128, 16], F32)
        eg = sb("eg", [128, 8], F32)
        negeg = sb("negeg", [128, 8], F32)
        dl = sb("dl", [128, 8], F32)
        egl = sb("egl", [128, 8], F32)
        eglast = sb("eglast", [128, 8], F32)
        KKm = [sb("KKm%d" % i, [128, 128], F32) for i in range(4)]
        KQm = [sb("KQm%d" % i, [128, 128], F32) for i in range(4)]
        Gm = [sb("Gm%d" % i, [128, 128], F32) for i in range(8)]
        Ee = [sb("Ee%d" % i, [128, 128], F32) for i in range(8)]
        Wa = [sb("Wa%d" % i, [128, 128], F32) for i in range(8)]
        WTa = [sb("WTa%d" % i, [128, 128], F32) for i in range(8)]
        Wb = [sb("Wb%d" % i, [128, 128], F32) for i in range(8)]
        WTb = [sb("WTb%d" % i, [128, 128], F32) for i in range(8)]
        Qm = [sb("Qm%d" % i, [128, 128], F32) for i in range(8)]
        AT = [sb("AT%d" % i, [128, 128], BF16) for i in range(8)]
        PTb = [sb("PTb%d" % i, [128, 128], BF16) for i in range(8)]
        Xv = [sb("Xv%d" % i, [128, 128], BF16) for i in range(8)]
        vnew = [sb("vnew%d" % i, [128, 128], BF16) for i in range(8)]
        Sf = [sb("Sf%d" % i, [128, 128], F32) for i in range(8)]
        Sb = [sb("Sb%d" % i, [128, 128], BF16) for i in range(8)]
        o_sb = sb("o_sb", [128, 8, 128], F32)
        osq = sb("osq", [128, 8, 128], F32)
        ossq = sb("ossq", [128, 8], F32)
        orstd = sb("orstd", [128, 8], F32)
        ogst = [sb("ogst%d" % i, [128, 8, 128], BF16) for i in range(2)]
        for hv in range(8):
            S.op('dve', lambda e, hv=hv: e.memset(Sf[hv][:, :], 0.0), w=[Sf[hv].name])
            S.op('pool', lambda e, hv=hv: e.memset(Sb[hv][:, :], 0.0), w=[Sb[hv].name])

        def load(n):
            p = n % 2
            rows = slice(n * 128, (n + 1) * 128)
            S.dma('sp', qk_sb[p][:, :, :], qk[rows], w=[qk_sb[p].name])
            S.dma('sp', v_sb[p][:, :, :], v_in[rows], w=[v_sb[p].name])
            S.dma('sp', z_sb[p][:, :, :], z_in[rows], w=[z_sb[p].name])
            S.dma('sp', ab_sb[p][:, :], ab[rows, :], w=[ab_sb[p].name])

        def bc3(ap2, n):
            return ap2.unsqueeze(2).to_broadcast([128, n, 128])

        load(0)
        for n in range(NCH):
            p = n % 2
            if n + 1 < NCH:
                load(n + 1)
            QK, V, Z, AB = qk_sb[p], v_sb[p], z_sb[p], ab_sb[p]
            S.op('dve', lambda e: e.tensor_tensor(out=sq[:, :, :], in0=QK[:, :, :], in1=QK[:, :, :], op=ALU.mult), r=[QK.name], w=['sq'])
            S.op('dve', lambda e: e.tensor_reduce(out=ssq[:, :], in_=sq[:, :, :], axis=AX.X, op=ALU.add), r=['sq'], w=['ssq'])
            S.op('act', lambda e: e.activation(out=rn[:, :], in_=ssq[:, :], func=AF.Sqrt, bias=epsr[:, 0:1]), r=['ssq', 'epsr'], w=['rn'])
            S.op('dve', lambda e: e.reciprocal(out=rn[:, :], in_=rn[:, :]), r=['rn'], w=['rn'])
            S.op('dve', lambda e: e.tensor_scalar(out=rn[:, 0:4], in0=rn[:, 0:4], scalar1=128.0 ** -0.5, scalar2=None, op0=ALU.mult), r=['rn'], w=['rn'])
            S.op('dve', lambda e: e.tensor_tensor(out=qkn[:, :, :], in0=QK[:, :, :], in1=bc3(rn[:, :], 8), op=ALU.mult), r=[QK.name, 'rn'], w=['qkn'])
            S.op('dve', lambda e: e.tensor_tensor(out=xa[:, :], in0=AB[:, 0:8], in1=hpb[:, 8:16], op=ALU.add), r=[AB.name, 'hpb'], w=['xa'])
            S.op('dve', lambda e: e.tensor_scalar(out=ax[:, :], in0=xa[:, :], scalar1=-1.0, scalar2=None, op0=ALU.mult), r=['xa'], w=['ax'])
            S.op('dve', lambda e: e.tensor_tensor(out=ax[:, :], in0=ax[:, :], in1=xa[:, :], op=ALU.min), r=['xa', 'ax'], w=['ax'])
            S.op('act', lambda e: e.activation(out=ex[:, :], in_=ax[:, :], func=AF.Exp), r=['ax'], w=['ex'])
            S.op('act', lambda e: e.activation(out=ex[:, :], in_=ex[:, :], func=AF.Ln, bias=one1[:, 0:1]), r=['ex', 'one1'], w=['ex'])
            S.op('dve', lambda e: e.scalar_tensor_tensor(out=gg[:, :], in0=xa[:, :], scalar=0.0, in1=ex[:, :], op0=ALU.max, op1=ALU.add), r=['xa', 'ex'], w=['gg'])
            S.op('dve', lambda e: e.tensor_tensor(out=gg[:, :], in0=gg[:, :], in1=negexpA[:, :], op=ALU.mult), r=['gg', 'negexpA'], w=['gg'])
            S.op('act', lambda e: e.activation(out=beta[:, :], in_=AB[:, 8:16], func=AF.Sigmoid), r=[AB.name], w=['beta'])
            bk, bkk = newbank()
            S.op('pe', lambda e, bk=bk: e.matmul(bk[:, 0:8], MUI[:, :], gg[:, :], start=True, stop=True), r=['MUI', 'gg'], w=[bkk])
            S.op('pe', lambda e, bk=bk: e.matmul(bk[:, 8:16], ONES[:, :], gg[:, :], start=True, stop=True), r=['ONES', 'gg'], w=[bkk])
            S.op('act', lambda e, bk=bk: e.copy(out=gcs[:, :], in_=bk[:, 0:16]), r=[bkk], w=['gcs'])
            S.op('act', lambda e: e.activation(out=eg[:, :], in_=gcs[:, 0:8], func=AF.Exp), r=['gcs'], w=['eg'])
            S.op('dve', lambda e: e.tensor_scalar(out=negeg[:, :], in0=eg[:, :], scalar1=-1.0, scalar2=None, op0=ALU.mult), r=['eg'], w=['negeg'])
            S.op('dve', lambda e: e.tensor_tensor(out=dl[:, :], in0=gcs[:, 8:16], in1=gcs[:, 0:8], op=ALU.subtract), r=['gcs'], w=['dl'])
            S.op('act', lambda e: e.activation(out=egl[:, :], in_=dl[:, :], func=AF.Exp), r=['dl'], w=['egl'])
            S.op('act', lambda e: e.activation(out=eglast[:, :], in_=gcs[:, 8:16], func=AF.Exp), r=['gcs'], w=['eglast'])
            for hk in range(4):
                S.op('dve', lambda e, hk=hk: e.tensor_tensor(
                    out=qg[:, 2 * hk:2 * hk + 2, :], in0=qkn[:, hk:hk + 1, :].to_broadcast([128, 2, 128]),
                    in1=bc3(eg[:, 2 * hk:2 * hk + 2], 2), op=ALU.mult), r=['qkn', 'eg'], w=['qg'])
                S.op('dve', lambda e, hk=hk: e.tensor_tensor(
                    out=kd[:, 2 * hk:2 * hk + 2, :], in0=qkn[:, 4 + hk:5 + hk, :].to_broadcast([128, 2, 128]),
                    in1=bc3(egl[:, 2 * hk:2 * hk + 2], 2), op=ALU.mult), r=['qkn', 'egl'], w=['kd'])
            for j in range(8):
                S.op('pe', lambda e, j=j: e.transpose(tpb[:, j * 128:(j + 1) * 128], qkn[:, j, :], IDB[:, :]), r=['qkn', 'IDB'], w=[TPK])
            S.op('act', lambda e: e.copy(out=qkT[:, :, :], in_=tpb[:, :].rearrange("p (j t) -> p j t", j=8)), r=[TPK], w=['qkT'])
            for j in range(8):
                S.op('pe', lambda e, j=j: e.transpose(tpb[:, j * 128:(j + 1) * 128], qg[:, j, :], IDB[:, :]), r=['qg', 'IDB'], w=[TPK])
            S.op('dve', lambda e: e.tensor_copy(out=qgT[:, :, :], in_=tpb[:, :].rearrange("p (j t) -> p j t", j=8)), r=[TPK], w=['qgT'])
            S.op('act', lambda e: e.activation(out=sz[:, :, :], in_=Z[:, :, :], func=AF.Silu), r=[Z.name], w=['sz'])
            S.op('dve', lambda e: e.tensor_tensor(out=nwz[:, :, :], in0=sz[:, :, :], in1=nwb[:, :].unsqueeze(1).to_broadcast([128, 8, 128]), op=ALU.mult),
                 r=['sz', 'nwb'], w=['nwz'])
            for hk in range(4):
                bk, bkk = newbank()
                S.op('pe', lambda e, bk=bk, hk=hk: e.matmul(bk[:, 0:128], qkT[:, 4 + hk, :], qkT[:, 4 + hk, :], start=True, stop=True), r=['qkT'], w=[bkk])
                S.op('pe', lambda e, bk=bk, hk=hk: e.matmul(bk[:, 128:256], qkT[:, 4 + hk, :], qkT[:, hk, :], start=True, stop=True), r=['qkT'], w=[bkk])
                S.op('dve', lambda e, bk=bk, hk=hk: e.tensor_tensor(out=KKm[hk][:, :], in0=bk[:, 0:128], in1=MU[:, :], op=ALU.mult), r=[bkk, 'MU'], w=[KKm[hk].name])
                S.op('dve', lambda e, bk=bk, hk=hk: e.tensor_tensor(out=KQm[hk][:, :], in0=bk[:, 128:256], in1=MUI[:, :], op=ALU.mult), r=[bkk, 'MUI'], w=[KQm[hk].name])
            for hv in range(8):
                hk = hv // 2
                S.op('act', lambda e, hv=hv: e.activation(out=Gm[hv][:, :], in_=ML[:, :], func=AF.Identity, scale=gg[:, hv:hv + 1]),
                     r=['ML', 'gg'], w=[Gm[hv].name])
                bk, bkk = newbank()
                S.op('pe', lambda e, bk=bk, hv=hv: e.matmul(bk[:, 0:128], Gm[hv][:, :], MUI[:, :], start=True, stop=True), r=[Gm[hv].name, 'MUI'], w=[bkk])
                S.op('act', lambda e, bk=bk, hv=hv: e.activation(out=Ee[hv][:, :], in_=bk[:, 0:128], func=AF.Exp), r=[bkk], w=[Ee[hv].name])
                S.op('dve', lambda e, hv=hv, hk=hk: e.scalar_tensor_tensor(out=Wa[hv][:, :], in0=KKm[hk][:, :], scalar=beta[:, hv:hv + 1], in1=Ee[hv][:, :],
                                                                         op0=ALU.mult, op1=ALU.mult), r=[KKm[hk].name, 'beta', Ee[hv].name], w=[Wa[hv].name])
                S.op('dve', lambda e, hv=hv, hk=hk: e.tensor_tensor(out=AT[hv][:, :], in0=KQm[hk][:, :], in1=Ee[hv][:, :], op=ALU.mult),
                     r=[KQm[hk].name, Ee[hv].name], w=[AT[hv].name])
            for grp in range(1):
                hvs = range(8)
                cur = {}
                for hv in hvs:
                    bk, bkk = newbank()
                    S.op('pe', lambda e, bk=bk, hv=hv: e.transpose(bk[:, 0:128], Wa[hv][:, :], IDF[:, :]), r=[Wa[hv].name, 'IDF'], w=[bkk])
                    S.op('act', lambda e, bk=bk, hv=hv: e.copy(out=WTa[hv][:, :], in_=bk[:, 0:128]), r=[bkk], w=[WTa[hv].name])
                    S.op('dve', lambda e, hv=hv: e.tensor_tensor(out=Qm[hv][:, :], in0=IDF[:, :], in1=Wa[hv][:, :], op=ALU.subtract),
                         r=['IDF', Wa[hv].name], w=[Qm[hv].name])
                    cur[hv] = (Wa[hv], WTa[hv], Wb[hv], WTb[hv])
                for k in range(1, 7):
                    for hv in hvs:
                        W, WT, Wn, WTn = cur[hv]
                        bk, bkk = newbank()
                        S.op('pe', lambda e, bk=bk, W=W, WT=WT: e.matmul(bk[:, 0:128], W[:, :], WT[:, :], start=True, stop=True), r=[W.name, WT.name], w=[bkk])
                        S.op('act', lambda e, bk=bk, WTn=WTn: e.copy(out=WTn[:, :], in_=bk[:, 0:128]), r=[bkk], w=[WTn.name])
                        if k < 6:
                            bk2, bkk2 = newbank()
                            S.op('pe', lambda e, bk2=bk2, W=W, WT=WT: e.matmul(bk2[:, 0:128], WT[:, :], W[:, :], start=True, stop=True), r=[W.name, WT.name], w=[bkk2])
                            S.op('dve', lambda e, bk2=bk2, Wn=Wn: e.tensor_copy(out=Wn[:, :], in_=bk2[:, 0:128]), r=[bkk2], w=[Wn.name])
                    for hv in hvs:
                        W, WT, Wn, WTn = cur[hv]
                        bk, bkk = newbank()
                        S.op('pe', lambda e, bk=bk, WTn=WTn, hv=hv: e.matmul(bk[:, 0:128], WTn[:, :], Qm[hv][:, :], start=True, stop=True),
                             r=[WTn.name, Qm[hv].name], w=[bkk])
                        S.op('dve', lambda e, bk=bk, hv=hv: e.tensor_tensor(out=Qm[hv][:, :], in0=Qm[hv][:, :], in1=bk[:, 0:128], op=ALU.add),
                             r=[bkk, Qm[hv].name], w=[Qm[hv].name])
                        cur[hv] = (Wn, WTn, W, WT)
                for hv in hvs:
                    S.op('act', lambda e, hv=hv: e.copy(out=PTb[hv][:, :], in_=Qm[hv][:, :]), r=[Qm[hv].name], w=[PTb[hv].name])
            for grp in range(2):
                hvs = range(grp * 4, grp * 4 + 4)
                bR = {}
                for hv in hvs:
                    hk = hv // 2
                    bR[hv] = newbank()
                    bk, bkk = bR[hv]
                    S.op('pe', lambda e, bk=bk, hv=hv, hk=hk: e.matmul(bk[:, 0:128], qkT[:, 4 + hk, :], Sb[hv][:, :], start=True, stop=True),
                         r=['qkT', Sb[hv].name], w=[bkk])
                for hv in hvs:
                    bk, bkk = bR[hv]
                    S.op('dve', lambda e, bk=bk, hv=hv: e.scalar_tensor_tensor(out=Xv[hv][:, :], in0=bk[:, 0:128], scalar=negeg[:, hv:hv + 1], in1=V[:, hv, :],
                                                                         op0=ALU.mult, op1=ALU.add), r=[bkk, 'negeg', V.name], w=[Xv[hv].name])
                for hv in hvs:
                    bR[hv] = newbank()
                    bk, bkk = bR[hv]
                    S.op('pe', lambda e, bk=bk, hv=hv: e.matmul(bk[:, 0:128], PTb[hv][:, :], Xv[hv][:, :], start=True, stop=True),
                         r=[PTb[hv].name, Xv[hv].name], w=[bkk])
                for hv in hvs:
                    bk, bkk = bR[hv]
                    S.op('act', lambda e, bk=bk, hv=hv: e.activation(out=vnew[hv][:, :], in_=bk[:, 0:128], func=AF.Identity, scale=beta[:, hv:hv + 1]),
                         r=[bkk, 'beta'], w=[vnew[hv].name])
                for hv in hvs:
                    bk, bkk = newbank()
                    S.op('pe', lambda e, bk=bk, hv=hv: e.matmul(bk[:, 0:128], qgT[:, hv, :], Sb[hv][:, :], start=True, stop=False),
                         r=['qgT', Sb[hv].name], w=[bkk])
                    S.op('pe', lambda e, bk=bk, hv=hv: e.matmul(bk[:, 0:128], AT[hv][:, :], vnew[hv][:, :], start=False, stop=True),
                         r=[AT[hv].name, vnew[hv].name], w=[bkk])
                    S.op('pe', lambda e, bk=bk, hv=hv: e.matmul(bk[:, 128:256], kd[:, hv, :], vnew[hv][:, :], start=True, stop=True),
                         r=['kd', vnew[hv].name], w=[bkk])
                    S.op('act', lambda e, bk=bk, hv=hv: e.copy(out=o_sb[:, hv, :], in_=bk[:, 0:128]), r=[bkk], w=['o_sb'])
                    S.op('dve', lambda e, bk=bk, hv=hv: e.scalar_tensor_tensor(out=Sf[hv][:, :], in0=Sf[hv][:, :], scalar=eglast[:, hv:hv + 1], in1=bk[:, 128:256],
                                                                         op0=ALU.mult, op1=ALU.add), r=[bkk, 'eglast', Sf[hv].name], w=[Sf[hv].name])
                    S.op('act', lambda e, hv=hv: e.copy(out=Sb[hv][:, :], in_=Sf[hv][:, :]), r=[Sf[hv].name], w=[Sb[hv].name])
            S.op('dve', lambda e: e.tensor_tensor(out=osq[:, :, :], in0=o_sb[:, :, :], in1=o_sb[:, :, :], op=ALU.mult), r=['o_sb'], w=['osq'])
            S.op('dve', lambda e: e.tensor_reduce(out=ossq[:, :], in_=osq[:, :, :], axis=AX.X, op=ALU.add), r=['osq'], w=['ossq'])
            S.op('act', lambda e: e.activation(out=orstd[:, :], in_=ossq[:, :], func=AF.Sqrt, bias=epsr[:, 0:1], scale=1.0 / 128.0), r=['ossq', 'epsr'], w=['orstd'])
            S.op('dve', lambda e: e.reciprocal(out=orstd[:, :], in_=orstd[:, :]), r=['orstd'], w=['orstd'])
            S.op('dve', lambda e: e.tensor_tensor(out=osq[:, :, :], in0=o_sb[:, :, :], in1=bc3(orstd[:, :], 8), op=ALU.mult), r=['o_sb', 'orstd'], w=['osq'])
            S.op('dve', lambda e, p=p: e.tensor_tensor(out=ogst[p][:, :, :], in0=osq[:, :, :], in1=nwz[:, :, :], op=ALU.mult), r=['osq', 'nwz'], w=[ogst[p].name])
            S.dma('sp', og[n * 128:(n + 1) * 128, :], ogst[p][:, :, :].rearrange("p h d -> p (h d)"), r=[ogst[p].name])
        S.finish()
    return nc


def gdn_b_consts():
    i = np.arange(128)
    ML = (i[:, None] > i[None, :]).astype(np.float32)
    MU = (i[None, :] > i[:, None]).astype(np.float32)
    MUI = (i[None, :] >= i[:, None]).astype(np.float32)
    return np.stack([ML, MU, MUI, np.eye(128, dtype=np.float32)]), np.eye(128, dtype=np.float32).astype(NPBF)


def gdn_b_inputs(qkvT_all, zT_all, abT_all, a_log, dt_bias, norm_w):
    cm, idb = gdn_b_consts()
    ims = []
    for c in range(NCORES):
        hg = c % 4
        qkvT, zT, abT = qkvT_all[c], zT_all[c], abT_all[c]
        qk = np.ascontiguousarray(qkvT[0:1024].T).reshape(4096, 8, 128)
        v = np.ascontiguousarray(qkvT[1024:2048].T).reshape(4096, 8, 128)
        z = np.ascontiguousarray(zT.T).reshape(4096, 8, 128)
        ab = np.ascontiguousarray(abT.T)
        hp = np.stack([a_log[hg * 8:(hg + 1) * 8], dt_bias[hg * 8:(hg + 1) * 8]]).astype(np.float32)
        ims.append({"qk": qk, "v": v, "z": z, "ab": ab, "hp": hp, "nw": norm_w.reshape(1, 128).astype(np.float32),
                    "cmask": cm, "identb": idb})
    return ims


SCL = 128.0 ** -0.5
NEGB = -30000.0


def build_nsa(NQT=32):
    nc = _mk()
    T = 4096
    qn_d = nc.dram_tensor("qn", [128, 32 * 512], BF16, kind="ExternalInput").ap()
    qr_d = nc.dram_tensor("qr", [128, 32 * 512], BF16, kind="ExternalInput").ap()
    kcT_d = nc.dram_tensor("kcT", [128, T], BF16, kind="ExternalInput").ap()
    vcT_d = nc.dram_tensor("vcT", [128, T], BF16, kind="ExternalInput").ap()
    kslT_d = nc.dram_tensor("kslT", [128, T], BF16, kind="ExternalInput").ap()
    kwT_d = nc.dram_tensor("kwT", [128, T], BF16, kind="ExternalInput").ap()
    vsl_d = nc.dram_tensor("vsl", [T, 128], BF16, kind="ExternalInput").ap()
    vw_d = nc.dram_tensor("vw", [T, 128], BF16, kind="ExternalInput").ap()
    gates_d = nc.dram_tensor("gates", [T, 12], F32, kind="ExternalInput").ap()
    w1_d = nc.dram_tensor("w1", [2, 4096, 512], F32, kind="ExternalInput").ap()
    w2_d = nc.dram_tensor("w2", [2, 512, 128], F32, kind="ExternalInput").ap()
    peT_d = nc.dram_tensor("peT", [2, 128, 32], F32, kind="ExternalInput").ap()
    ov_d = nc.dram_tensor("ov", [2, 128, 65], F32, kind="ExternalInput").ap()
    maskc_d = nc.dram_tensor("maskc", [128, 32 * 2 * 128], BF16, kind="ExternalInput").ap()
    sm_d = nc.dram_tensor("sm", [2, 128, 32 * 64], F32, kind="ExternalInput").ap()
    ind_d = nc.dram_tensor("ind", [64, 32 * 128], BF16, kind="ExternalInput").ap()
    cbias_d = nc.dram_tensor("cbias", [128, 2 * 512], BF16, kind="ExternalInput").ap()
    identb_d = nc.dram_tensor("identb", [128, 128], BF16, kind="ExternalInput").ap()
    identf_d = nc.dram_tensor("identf", [128, 128], F32, kind="ExternalInput").ap()
    o_d = nc.dram_tensor("o", [T, 512], BF16, kind="ExternalOutput").ap()
    with contextlib.ExitStack() as es:
        S = Sched(nc, es)
        sb = lambda name, shape, dt: es.enter_context(nc.sbuf_tensor(name + "_s", shape, dt))
        banks = [es.enter_context(nc.psum_tensor("bk%d" % i, [128, 512], F32)) for i in range(4)]
        accs = [es.enter_context(nc.psum_tensor("acc%d" % i, [128, 4, 256], F32)) for i in range(2)]
        ACK = [('bk', 'A'), ('bk', 'B')]
        nbk = [0]

        def newbank(n=3):
            i = nbk[0] % n
            nbk[0] += 1
            return banks[i], ('bk', i)

        kcmpT = sb("kcmpT", [128, 256], BF16)
        Rext = sb("Rext", [128, 2, 193], BF16)
        IDB = sb("IDB", [128, 128], BF16)
        IDF = sb("IDF", [128, 128], F32)
        S.dma('sp', IDB[:, :], identb_d[:, :], w=['IDB'])
        S.dma('sp', IDF[:, :], identf_d[:, :], w=['IDF'])
        S.op('dve', lambda e: e.memset(kcmpT[:, :], 0.0), w=['kcmpT'])
        for cc in range(2):
            S.dma('pool', Rext[:, cc, 128:193], ov_d[cc], w=['Rext'])
        with contextlib.ExitStack() as es0:
            sb0 = lambda name, shape, dt: es0.enter_context(nc.sbuf_tensor(name + "_s", shape, dt))
            srcT = [sb0("kcT", [128, T], BF16), sb0("vcT", [128, T], BF16)]
            S.dma('sp', srcT[0][:, :], kcT_d[:, :], w=['srcT0'])
            S.dma('sp', srcT[1][:, :], vcT_d[:, :], w=['srcT1'])
            w1b = [sb0("w1b%d" % i, [128, 32, 128], BF16) for i in range(2)]
            w2b = [sb0("w2b%d" % i, [128, 4, 128], BF16) for i in range(2)]
            peb = [sb0("peb%d" % i, [128, 32], BF16) for i in range(2)]
            hid = [sb0("hid%d" % i, [128, 4, 256], BF16) for i in range(2)]
            biasv = sb0("biasv", [128, 8], F32)
            nw1 = 0
            for kv in range(2):
                S.dma('pool', w2b[kv][:, :, :], w2_d[kv].rearrange("(hc p) d -> p hc d", p=128), w=['w2b%d' % kv])
                S.dma('pool', peb[kv][:, :], peT_d[kv], w=['peb%d' % kv])
                S.op('dve', lambda e, kv=kv: e.memset(hid[kv][:, :, :], 0.0), w=['hid%d' % kv])
                for hc in range(4):
                    wi = nw1 % 2
                    nw1 += 1
                    for half in range(2):
                        S.dma('pool', w1b[wi][:, half * 16:(half + 1) * 16, :],
                              w1_d[kv, half * 2048:(half + 1) * 2048, hc * 128:(hc + 1) * 128].rearrange("(l d) f -> d l f", d=128),
                              w=[('w1b', wi)])
                    bk, bkk = newbank()
                    for l in range(32):
                        S.op('pe', lambda e, bk=bk, l=l, wi=wi, kv=kv: e.matmul(bk[:, 0:1], w1b[wi][:, l, :], peb[kv][:, l:l + 1],
                                                                              start=(l == 0), stop=(l == 31)), r=[('w1b', wi), 'peb%d' % kv], w=[bkk])
                    S.op('act', lambda e, bk=bk, kv=kv, hc=hc: e.copy(out=biasv[:, kv * 4 + hc:kv * 4 + hc + 1], in_=bk[:, 0:1]), r=[bkk], w=['biasv'])
                    bk, bkk = newbank()
                    for l in range(32):
                        S.op('pe', lambda e, bk=bk, l=l, wi=wi, kv=kv: e.matmul(bk[:, 0:255], w1b[wi][:, l, :], srcT[kv][:, l:l + 16 * 254 + 1:16],
                                                                              start=(l == 0), stop=(l == 31)), r=[('w1b', wi), 'srcT%d' % kv], w=[bkk])
                    S.op('act', lambda e, bk=bk, kv=kv, hc=hc: e.activation(out=hid[kv][:, hc, 0:255], in_=bk[:, 0:255], func=AF.Silu,
                                                                          bias=biasv[:, kv * 4 + hc:kv * 4 + hc + 1]), r=[bkk, 'biasv'], w=['hid%d' % kv])
            bk, bkk = newbank()
            for hc in range(4):
                S.op('pe', lambda e, bk=bk, hc=hc: e.matmul(bk[:, 0:255], w2b[0][:, hc, :], hid[0][:, hc, 0:255], start=(hc == 0), stop=(hc == 3)),
                     r=['w2b0', 'hid0'], w=[bkk])
            S.op('act', lambda e, bk=bk: e.copy(out=kcmpT[:, 0:255], in_=bk[:, 0:255]), r=[bkk], w=['kcmpT'])
            for cc in range(2):
                bk, bkk = newbank()
                for hc in range(4):
                    S.op('pe', lambda e, bk=bk, hc=hc, cc=cc: e.matmul(bk[:, 0:128], hid[1][:, hc, cc * 128:(cc + 1) * 128], w2b[1][:, hc, :],
                                                                     start=(hc == 0), stop=(hc == 3)), r=['w2b1', 'hid1'], w=[bkk])
                S.op('act', lambda e, bk=bk, cc=cc: e.copy(out=Rext[:, cc, 0:128], in_=bk[:, 0:128]), r=[bkk], w=['Rext'])
            S.sync_all()
        qn = sb("qn", [128, 32, 512], BF16)
        qr = sb("qr", [128, 32, 512], BF16)
        kslT = sb("kslT", [128, T], BF16)
        kwT = sb("kwT", [128, T], BF16)
        vsl = sb("vsl", [128, 32, 129], BF16)
        vw = sb("vw", [128, 32, 129], BF16)
        gts = sb("gts", [128, 32, 12], F32)
        maskc = sb("maskc", [128, 32, 2, 128], BF16)
        sm1 = sb("sm1", [128, 32, 64], F32)
        sm2 = sb("sm2", [128, 32, 64], F32)
        ind = sb("ind", [64, 32, 128], BF16)
        cbias = sb("cbias", [128, 2, 512], BF16)
        for half in range(2):
            hs = slice(half * 16, (half + 1) * 16)
            S.dma('sp', qn[:, hs, :], qn_d[:, half * 8192:(half + 1) * 8192].rearrange("p (a b) -> p a b", b=512), w=['qn'])
            S.dma('sp', qr[:, hs, :], qr_d[:, half * 8192:(half + 1) * 8192].rearrange("p (a b) -> p a b", b=512), w=['qr'])
        S.dma('sp', kslT[:, :], kslT_d[:, :], w=['kslT'])
        S.dma('sp', kwT[:, :], kwT_d[:, :], w=['kwT'])
        S.op('dve', lambda e: e.memset(vsl[:, :, 128:129], 1.0), w=['vsl'])
        S.op('dve', lambda e: e.memset(vw[:, :, 128:129], 1.0), w=['vw'])
        S.dma('sp', vsl[:, :, 0:128], vsl_d.rearrange("(kt p) d -> p kt d", p=128), w=['vsl'])
        S.dma('sp', vw[:, :, 0:128], vw_d.rearrange("(kt p) d -> p kt d", p=128), w=['vw'])
        S.dma('sp', gts[:, :, :], gates_d.rearrange("(qt p) g -> p qt g", p=128), w=['gts'])
        S.dma('sp', maskc[:, :, :, :], maskc_d.rearrange("p (a b c) -> p a b c", a=32, b=2), w=['maskc'])
        S.dma('sp', sm1[:, :, :], sm_d[0].rearrange("p (a b) -> p a b", b=64), w=['sm1'])
        S.dma('sp', sm2[:, :, :], sm_d[1].rearrange("p (a b) -> p a b", b=64), w=['sm2'])
        S.dma('sp', ind[:, :, :], ind_d.rearrange("p (a b) -> p a b", b=128), w=['ind'])
        S.dma('sp', cbias[:, :, :], cbias_d.rearrange("p (a b) -> p a b", b=512), w=['cbias'])
        Ec = [sb("Ec%d" % i, [128, 4, 128], BF16) for i in range(2)]
        Es = [sb("Es%d" % i, [128, 512], BF16) for i in range(3)]
        den = sb("den", [128, 4], F32)
        rec = sb("rec", [128, 4], F32)
        rg = sb("rg", [128, 4], F32)
        pn = sb("pn", [128, 4, 64], F32)
        pslc = sb("pslc", [128, 64], F32)
        score = sb("score", [128, 64], F32)
        work = sb("work", [128, 64], F32)
        m8a = sb("m8a", [128, 8], F32)
        m8b = sb("m8b", [128, 8], F32)
        thr = sb("thr", [128, 1], F32)
        selm = sb("selm", [128, 64], F32)
        selmT = sb("selmT", [64, 4, 128], BF16)
        oacc = sb("oacc", [128, 4, 128], F32)
        otmp = sb("otmp", [128, 4, 128], F32)
        ob = [sb("ob%d" % i, [128, 4, 128], BF16) for i in range(2)]
        nes = [0]

        def bc(ap2):
            return ap2.unsqueeze(2).to_broadcast([128, 4, 128])

        def attend(qt, kts, kT, kTk, vext, vk, acc, ack, use_sel):
            def emit_scores(idx):
                kt = kts[idx]
                bk, bkk = newbank()
                diag = (kt == qt)
                far = (not use_sel) and (kt == qt - 4)
                more = use_sel or diag or far
                S.op('pe', lambda e, bk=bk, kt=kt: e.matmul(bk[:, :], kT[:, kt * 128:(kt + 1) * 128], qr[:, qt, :], start=True, stop=not more),
                     r=[kTk, 'qr'], w=[bkk])
                if use_sel:
                    S.op('pe', lambda e, bk=bk, kt=kt: e.matmul(bk[:, :], ind[:, kt, :], selmT[:, :, :].rearrange("p h q -> p (h q)"), start=False,
                                                              stop=not diag), r=['ind', 'selmT'], w=[bkk])
                if diag:
                    S.op('pe', lambda e, bk=bk: e.matmul(bk[:, :], IDB[:, :], cbias[:, 0, :], start=False, stop=not far), r=['IDB', 'cbias'], w=[bkk])
                if far:
                    S.op('pe', lambda e, bk=bk: e.matmul(bk[:, :], IDB[:, :], cbias[:, 1, :], start=False, stop=True), r=['IDB', 'cbias'], w=[bkk])
                ei = nes[0] % 3
                nes[0] += 1
                S.op('act', lambda e, bk=bk, ei=ei: e.activation(out=Es[ei][:, :], in_=bk[:, :], func=AF.Exp, scale=SCL), r=[bkk], w=[('Es', ei)])
                return ei

            def emit_pv(idx, ei):
                kt = kts[idx]
                for h in range(4):
                    S.op('pe', lambda e, h=h, ei=ei, kt=kt, idx=idx: e.matmul(acc[:, h, 0:129], Es[ei][:, h * 128:(h + 1) * 128], vext[:, kt, :],
                                                                            start=(idx == 0 and h % 2 == 0), stop=(idx == len(kts) - 1)),
                         r=[('Es', ei), vk], w=[ack])

            prev = None
            for idx in range(len(kts)):
                ei = emit_scores(idx)
                if prev is not None:
                    emit_pv(*prev)
                prev = (idx, ei)
            emit_pv(*prev)

        def finish_branch(acc, ack, gcol, first):
            S.op('dve', lambda e: e.reciprocal(out=rec[:, :], in_=acc[:, :, 128]), r=[ack], w=['rec'])
            S.op('dve', lambda e: e.tensor_tensor(out=rg[:, :], in0=rec[:, :], in1=gcol, op=ALU.mult), r=['rec', 'gts'], w=['rg'])
            if first:
                S.op('dve', lambda e: e.tensor_tensor(out=oacc[:, :, :], in0=acc[:, :, 0:128], in1=bc(rg[:, :]), op=ALU.mult), r=[ack, 'rg'], w=['oacc'])
            else:
                S.op('dve', lambda e: e.tensor_tensor(out=otmp[:, :, :], in0=acc[:, :, 0:128], in1=bc(rg[:, :]), op=ALU.mult), r=[ack, 'rg'], w=['otmp'])
                S.op('dve', lambda e: e.tensor_tensor(out=oacc[:, :, :], in0=oacc[:, :, :], in1=otmp[:, :, :], op=ALU.add), r=['otmp', 'oacc'], w=['oacc'])

        for qt in range(NQT):
            ccs = [0] if qt < 16 else [0, 1]
            A, AK = accs[0], ACK[0]
            for cc in ccs:
                bk, bkk = newbank()
                S.op('pe', lambda e, bk=bk, cc=cc: e.matmul(bk[:, :], kcmpT[:, cc * 128:(cc + 1) * 128], qn[:, qt, :], start=True, stop=True),
                     r=['kcmpT', 'qn'], w=[bkk])
                S.op('act', lambda e, bk=bk, cc=cc: e.activation(out=Ec[cc][:, :, :], in_=bk[:, :].rearrange("p (h q) -> p h q", h=4), func=AF.Exp, scale=SCL),
                     r=[bkk], w=[('Ec', cc)])
                S.op('dve', lambda e, cc=cc: e.tensor_tensor(out=Ec[cc][:, :, :], in0=Ec[cc][:, :, :],
                                                            in1=maskc[:, qt, cc, :].unsqueeze(1).to_broadcast([128, 4, 128]), op=ALU.mult),
                     r=[('Ec', cc), 'maskc'], w=[('Ec', cc)])
            for ci, cc in enumerate(ccs):
                for h in range(4):
                    S.op('pe', lambda e, h=h, cc=cc, ci=ci: e.matmul(A[:, h, 0:193], Ec[cc][:, h, :], Rext[:, cc, :],
                                                                   start=(ci == 0 and h % 2 == 0), stop=(ci == len(ccs) - 1)),
                         r=[('Ec', cc), 'Rext'], w=[AK])
            S.op('dve', lambda e: e.tensor_scalar(out=den[:, :], in0=A[:, :, 192], scalar1=1e-30, scalar2=None, op0=ALU.max), r=[AK], w=['den'])
            S.op('dve', lambda e: e.reciprocal(out=rec[:, :], in_=den[:, :]), r=['den'], w=['rec'])
            S.op('dve', lambda e: e.tensor_tensor(out=pn[:, :, :], in0=A[:, :, 128:192], in1=rec[:, :].unsqueeze(2).to_broadcast([128, 4, 64]), op=ALU.mult),
                 r=[AK, 'rec'], w=['pn'])
            S.op('dve', lambda e: e.tensor_reduce(out=pslc[:, :], in_=pn[:, :, :].rearrange("p h s -> p s h"), axis=AX.X, op=ALU.add), r=['pn'], w=['pslc'])
            S.op('dve', lambda e: e.tensor_tensor(out=rg[:, :], in0=rec[:, :], in1=gts[:, qt, 0:4], op=ALU.mult), r=['rec', 'gts'], w=['rg'])
            S.op('dve', lambda e: e.tensor_tensor(out=oacc[:, :, :], in0=A[:, :, 0:128], in1=bc(rg[:, :]), op=ALU.mult), r=[AK, 'rg'], w=['oacc'])
            S.op('dve', lambda e: e.tensor_tensor(out=score[:, :], in0=pslc[:, :], in1=sm1[:, qt, :], op=ALU.mult), r=['pslc', 'sm1'], w=['score'])
            S.op('dve', lambda e: e.tensor_tensor(out=score[:, :], in0=score[:, :], in1=sm2[:, qt, :], op=ALU.add), r=['score', 'sm2'], w=['score'])
            S.op('dve', lambda e: e.max(out=m8a[:, :], in_=score[:, :]), r=['score'], w=['m8a'])
            S.op('dve', lambda e: e.match_replace(out=work[:, :], in_to_replace=m8a[:, :], in_values=score[:, :], imm_value=-2.0), r=['score', 'm8a'], w=['work'])
            S.op('dve', lambda e: e.max(out=m8b[:, :], in_=work[:, :]), r=['work'], w=['m8b'])
            S.op('dve', lambda e: e.tensor_scalar(out=thr[:, :], in0=m8b[:, 7:8], scalar1=0.0, scalar2=None, op0=ALU.max), r=['m8b'], w=['thr'])
            S.op('dve', lambda e: e.tensor_scalar(out=selm[:, :], in0=score[:, :], scalar1=thr[:, 0:1], scalar2=None, op0=ALU.is_ge), r=['score', 'thr'], w=['selm'])
            S.op('dve', lambda e: e.tensor_scalar(out=selm[:, :], in0=selm[:, :], scalar1=-NEGB, scalar2=NEGB, op0=ALU.mult, op1=ALU.add), r=['selm'], w=['selm'])
            attend(qt, list(range(max(0, qt - 4), qt + 1)), kwT, 'kwT', vw, 'vw', accs[1], ACK[1], False)
            bk, bkk = banks[3], ('bk', 3)
            S.op('pe', lambda e, bk=bk: e.transpose(bk[0:64, 0:128], selm[:, :], IDF[:, :]), r=['selm', 'IDF'], w=[bkk])
            S.op('act', lambda e, bk=bk: e.copy(out=selmT[:, :, :], in_=bk[0:64, 0:128].unsqueeze(1).to_broadcast([64, 4, 128])), r=[bkk], w=['selmT'])
            attend(qt, list(range(0, qt + 1)), kslT, 'kslT', vsl, 'vsl', accs[0], ACK[0], True)
            finish_branch(accs[1], ACK[1], gts[:, qt, 8:12], False)
            finish_branch(accs[0], ACK[0], gts[:, qt, 4:8], False)
            oi = qt % 2
            S.op('act', lambda e, oi=oi: e.copy(out=ob[oi][:, :, :], in_=oacc[:, :, :]), r=['oacc'], w=[('ob', oi)])
            S.dma('sp', o_d[qt * 128:(qt + 1) * 128, :], ob[oi][:, :, :].rearrange("p h d -> p (h d)"), r=[('ob', oi)])
        S.finish()
    return nc


def nsa_consts():
    p = np.arange(128)
    c_all = np.arange(256)
    maskc = np.zeros((128, 32, 2, 128), np.float32)
    for qt in range(32):
        t = qt * 128 + p
        for cc in range(2):
            c = cc * 128 + p
            maskc[:, qt, cc, :] = ((16 * c[:, None] + 31 <= t[None, :]) & (c[:, None] < 255))
    blk = np.arange(64)
    sm = np.zeros((2, 128, 32, 64), np.float32)
    for qt in range(32):
        t = qt * 128 + p
        cur = (t // 64)[:, None]
        causal = blk[None, :] <= cur
        forced = (blk[None, :] == 0) | (causal & (blk[None, :] > cur - 2))
        sm[0, :, qt, :] = (causal & ~forced)
        sm[1, :, qt, :] = np.where(forced, 1e6, np.where(causal, 0.0, -1.0))
    ind = np.zeros((64, 32, 128), np.float32)
    for kt in range(32):
        ind[2 * kt + p // 64, kt, p] = 1.0
    cb = np.zeros((128, 2, 4, 128), np.float32)
    cb[:, 0] = np.where(p[:, None] > p[None, :], NEGB, 0.0)[:, None, :]
    cb[:, 1] = np.where(p[:, None] <= p[None, :], NEGB, 0.0)[:, None, :]
    c0 = np.arange(255) * 16
    s0 = np.arange(64) * 64
    ovm = np.clip(np.minimum(c0[:, None] + 32, s0[None, :] + 64) - np.maximum(c0[:, None], s0[None, :]), 0, None) / 32.0
    ov = np.zeros((256, 65), np.float32)
    ov[:255, :64] = ovm
    ov[:255, 64] = 1.0
    return dict(maskc=maskc.reshape(128, -1).astype(NPBF), sm=sm.reshape(2, 128, -1), ind=ind.reshape(64, -1).astype(NPBF),
                cbias=cb.reshape(128, -1).astype(NPBF), ov=ov.reshape(2, 128, 65),
                identb=np.eye(128, dtype=np.float32).astype(NPBF), identf=np.eye(128, dtype=np.float32))


def nsa_inputs(proj, qgate, cmp_w1, cmp_w2, cmp_pe):
    cst = nsa_consts()
    ims = []
    pj = proj.reshape(2, 4096, 7168)
    qg = qgate.reshape(2, 4096, 3, 16)
    peT = np.ascontiguousarray(np.transpose(cmp_pe, (0, 2, 1))).astype(np.float32)
    for c in range(NCORES):
        b, g = c // 4, c % 4
        P = pj[b]

        def sec(s):
            return P[:, s * 512 + g * 128: s * 512 + (g + 1) * 128]

        def qlay(off):
            q = P[:, off + g * 512: off + (g + 1) * 512].reshape(32, 128, 4, 128)
            return np.ascontiguousarray(np.transpose(q, (3, 0, 2, 1))).reshape(128, 32 * 512)
        im = dict(qn=qlay(3072), qr=qlay(5120),
                  kcT=np.ascontiguousarray(sec(0).T), vcT=np.ascontiguousarray(sec(1).T),
                  kslT=np.ascontiguousarray(sec(2).T), kwT=np.ascontiguousarray(sec(4).T),
                  vsl=np.ascontiguousarray(sec(3)), vw=np.ascontiguousarray(sec(5)),
                  gates=np.ascontiguousarray(qg[b][:, :, g * 4:(g + 1) * 4].reshape(4096, 12)),
                  w1=cmp_w1, w2=cmp_w2, peT=peT)
        im.update(cst)
        ims.append(im)
    return ims


def _rope_table():
    pos = np.arange(4096, dtype=np.float32)
    inv = (np.float32(10000.0) ** (-np.arange(64, dtype=np.float32) / np.float32(64))).astype(np.float32)
    ang = (pos[:, None] * inv[None, :]).astype(np.float32)
    return np.concatenate([np.cos(ang), np.sin(ang)], axis=1).astype(np.float32)


def _post1(aT_list, xres, w_out, g, b, router_w, router_bias):
    KIN = w_out.shape[0]
    nc = build_post1(KIN)
    ln_gb = np.stack([g, b]).astype(np.float32)
    ident = np.eye(128, dtype=np.float32)
    rb = router_bias.reshape(1, 32).astype(np.float32)
    ims = [{"aT": aT_list[c], "xres": np.ascontiguousarray(xres[c * 1024:(c + 1) * 1024]), "w_out": w_out, "ln_gb": ln_gb,
            "router_w": router_w, "router_b": rb, "ident": ident} for c in range(NCORES)]
    res = _run(nc, ims)
    x1 = np.concatenate([r["x1"] for r in res], axis=0)
    x1T = np.concatenate([r["x1T"] for r in res], axis=1)
    gates = np.concatenate([r["gates"] for r in res], axis=0)
    return x1, x1T, gates


def _moe(x1T, gates, wg, wu, wd):
    nc = build_moe(8192)
    x1T = np.ascontiguousarray(x1T)
    ims = [{"xT": x1T, "gates_c": np.ascontiguousarray(gates[:, 4 * c:4 * c + 4]), "wg": wg[4 * c:4 * c + 4],
            "wu": wu[4 * c:4 * c + 4], "wd": wd[4 * c:4 * c + 4]} for c in range(NCORES)]
    res = _run(nc, ims)
    return [r["y"] for r in res]


def _post2(ys, x1, g, b, proj_w=None):
    PROJ = proj_w is not None
    nc = build_post2(PROJ)
    ln_gb = np.stack([g, b]).astype(np.float32)
    ims = []
    if PROJ:
        cs = _rope_table()
        ident = np.eye(128, dtype=np.float32)
    for c in range(NCORES):
        rows = slice(c * 1024, (c + 1) * 1024)
        im = {"yp": np.stack([y[rows] for y in ys]), "x1": np.ascontiguousarray(x1[rows]), "ln_gb": ln_gb}
        if PROJ:
            p0 = (c * 1024) % 4096
            im.update({"kv_w": proj_w[0], "w_q": proj_w[1], "cs": np.ascontiguousarray(cs[p0:p0 + 1024]), "ident": ident})
        ims.append(im)
    res = _run(nc, ims)
    x2 = np.concatenate([r["x2"] for r in res], axis=0)
    if PROJ:
        return x2, np.concatenate([r["proj"] for r in res], axis=0), np.concatenate([r["qgate"] for r in res], axis=0)
    return x2


def kernel(x, a_w_in, a_conv_w, a_a_log, a_dt_bias, a_norm_w, a_w_out, kv_w, cmp_pe, cmp_w1, cmp_w2,
           b_w_q, b_w_out, router_w, router_bias, moe_w_gate, moe_w_up, moe_w_down, ln_g, ln_b):
    f = lambda a: np.asarray(a, dtype=np.float32)
    x, a_w_in, a_conv_w, a_a_log, a_dt_bias, a_norm_w, a_w_out = map(f, (x, a_w_in, a_conv_w, a_a_log, a_dt_bias, a_norm_w, a_w_out))
    kv_w, cmp_pe, cmp_w1, cmp_w2, b_w_q, b_w_out, router_w, router_bias = map(f, (kv_w, cmp_pe, cmp_w1, cmp_w2, b_w_q, b_w_out, router_w, router_bias))
    moe_w_gate, moe_w_up, moe_w_down, ln_g, ln_b = map(f, (moe_w_gate, moe_w_up, moe_w_down, ln_g, ln_b))
    xf = x.reshape(8192, D)
    res = _run(build_gdn_a(), gdn_a_inputs(x, a_w_in[0], a_conv_w[0]))
    res = _run(build_gdn_b(32), gdn_b_inputs([r["qkvT"] for r in res], [r["zT"] for r in res], [r["abT"] for r in res],
                                            a_a_log[0], a_dt_bias[0], a_norm_w[0]))
    og = np.zeros((2, 4096, 4096), dtype=NPBF)
    for c in range(NCORES):
        og[c // 4, :, (c % 4) * 1024:(c % 4 + 1) * 1024] = res[c]["og"]
    ogf = og.reshape(8192, 4096)
    aT = [np.ascontiguousarray(ogf[c * 1024:(c + 1) * 1024].T) for c in range(NCORES)]
    x1, x1T, gates = _post1(aT, xf, a_w_out[0], ln_g[0, 0], ln_b[0, 0], router_w, router_bias)
    ys = _moe(x1T, gates, moe_w_gate[0], moe_w_up[0], moe_w_down[0])
    x2, proj, qgate = _post2(ys, x1, ln_g[0, 1], ln_b[0, 1], (kv_w, b_w_q[0]))
    del ys
    res = _run(build_nsa(32), nsa_inputs(proj, qgate, cmp_w1, cmp_w2, cmp_pe))
    o = np.zeros((2, 4096, 2048), dtype=NPBF)
    for c in range(NCORES):
        o[c // 4, :, (c % 4) * 512:(c % 4 + 1) * 512] = res[c]["o"]
    of = o.reshape(8192, 2048)
    aT = [np.ascontiguousarray(of[c * 1024:(c + 1) * 1024].T) for c in range(NCORES)]
    x3, x3T, gates = _post1(aT, x2, b_w_out[0], ln_g[1, 0], ln_b[1, 0], router_w, router_bias)
    ys = _moe(x3T, gates, moe_w_gate[1], moe_w_up[1], moe_w_down[1])
    x4 = _post2(ys, x3, ln_g[1, 1], ln_b[1, 1], None)
    return x4.reshape(2, 4096, D).astype(np.float32)
```
